# Optimizing a Trainium2 kernel written in Bass

```python
import math
import jax, jax.numpy as jnp
from jax import lax
import numpy as np

D_MODEL = 1024
BATCH = 16
SEQ = 2048
DEPTH = 1

D_MIX = D_MODEL
D_CONV = D_MIX // 2
CONV_GROUPS = 8
CONV_WIDTH = 3
D_DELTA = D_MIX - D_CONV
DN_HEADS = 4
DN_HEAD_DIM = D_DELTA // DN_HEADS
DN_CONV_WIDTH = 4
DN_CHUNK = 64
N_GROUPS = 4
EXPERTS_PER_GROUP = 8
N_EXPERTS = N_GROUPS * EXPERTS_PER_GROUP
TOP_K = 2
D_EXPERT = D_MODEL // 4
MOE_BLOCK = 128
LN_EPS = 1e-5
RMS_EPS = 1e-6
DEEP_ALPHA = (2 * DEPTH) ** 0.25
DEEP_BETA = (8 * DEPTH) ** -0.25

IN_SPLITS = [D_CONV, D_CONV, D_CONV, 3 * D_DELTA, D_DELTA, DN_HEADS, DN_HEADS]
D_IN = sum(IN_SPLITS)

kernel_name = "hymba_conv_gdn_hiermoe_deepnorm_adaln"


def layer_norm(x, g, b):
    xf = x.astype(jnp.float32)
    mu = xf.mean(-1, keepdims=True)
    var = jnp.square(xf - mu).mean(-1, keepdims=True)
    return ((xf - mu) * lax.rsqrt(var + LN_EPS) * g + b).astype(x.dtype)


def rms_normalize(x):
    xf = x.astype(jnp.float32)
    return xf * lax.rsqrt(jnp.square(xf).mean(-1, keepdims=True) + RMS_EPS)


def l2_normalize(x):
    return x * lax.rsqrt(jnp.square(x).sum(-1, keepdims=True) + RMS_EPS)


def causal_depthwise_conv(x, w):
    K, C = w.shape
    return lax.conv_general_dilated(
        x, w[:, None, :].astype(x.dtype), window_strides=(1,), padding=[(K - 1, 0)],
        dimension_numbers=("NWC", "WIO", "NWC"), feature_group_count=C)


def short_conv_mixer(b_gate, c_gate, u, conv_w, norm_w):
    y = b_gate * causal_depthwise_conv(c_gate * u, conv_w)
    B_, L, _ = y.shape
    y = rms_normalize(y.reshape(B_, L, CONV_GROUPS, D_CONV // CONV_GROUPS)).reshape(B_, L, D_CONV)
    return (y * norm_w).astype(u.dtype)


def gated_delta_rule_chunked(q, k, v, g, beta):
    B_, H, L, dk = q.shape
    dv = v.shape[-1]
    C = DN_CHUNK
    N = L // C
    q = q.reshape(B_, H, N, C, dk)
    k = k.reshape(B_, H, N, C, dk)
    v = v.reshape(B_, H, N, C, dv)
    beta = beta.reshape(B_, H, N, C)
    g = jnp.cumsum(g.reshape(B_, H, N, C), axis=-1)
    idx = jnp.arange(C)
    lower_strict = idx[:, None] > idx[None, :]
    lower_incl = idx[:, None] >= idx[None, :]
    gamma = jnp.exp(jnp.where(lower_incl, g[..., :, None] - g[..., None, :], -jnp.inf))
    kk = jnp.einsum("bhncd,bhnsd->bhncs", k, k)
    l_mat = jnp.where(lower_strict, beta[..., :, None] * kk * gamma, 0.0)
    eye = jnp.eye(C, dtype=jnp.float32)
    rhs = jnp.concatenate([beta[..., None] * v, (beta * jnp.exp(g))[..., None] * k], axis=-1)
    sol = lax.linalg.triangular_solve(eye + l_mat, rhs, left_side=True, lower=True, unit_diagonal=True)
    u, w = sol[..., :dv], sol[..., dv:]
    qk = jnp.where(lower_incl, jnp.einsum("bhncd,bhnsd->bhncs", q, k) * gamma, 0.0)
    q_decay = q * jnp.exp(g)[..., None]
    k_to_end = k * jnp.exp(g[..., -1:] - g)[..., None]
    chunk_decay = jnp.exp(g[..., -1])

    def step(S, xs):
        u_c, w_c, qk_c, qd_c, ke_c, dec_c = xs
        delta = u_c - jnp.einsum("bhcd,bhde->bhce", w_c, S)
        o = jnp.einsum("bhcd,bhde->bhce", qd_c, S) + jnp.einsum("bhcs,bhse->bhce", qk_c, delta)
        S = S * dec_c[..., None, None] + jnp.einsum("bhcd,bhce->bhde", ke_c, delta)
        return S, o

    xs = tuple(jnp.moveaxis(a, 2, 0) for a in (u, w, qk, q_decay, k_to_end, chunk_decay))
    S0 = jnp.zeros((B_, H, dk, dv), jnp.float32)
    _, o = lax.scan(step, S0, xs)
    return jnp.moveaxis(o, 0, 2).reshape(B_, H, L, dv)


def gated_deltanet(qkv, z, beta_logit, a_logit, conv_w, A_log, dt_bias, norm_w):
    B_, L, _ = qkv.shape
    qkv = jax.nn.silu(causal_depthwise_conv(qkv, conv_w))
    q, k, v = jnp.split(qkv, 3, axis=-1)

    def heads(t):
        return t.reshape(B_, L, DN_HEADS, DN_HEAD_DIM).transpose(0, 2, 1, 3).astype(jnp.float32)

    qh = l2_normalize(heads(q)) * (DN_HEAD_DIM ** -0.5)
    kh = l2_normalize(heads(k))
    vh = heads(v)
    beta = jax.nn.sigmoid(beta_logit.astype(jnp.float32)).transpose(0, 2, 1)
    g = (-jnp.exp(A_log.astype(jnp.float32))
         * jax.nn.softplus(a_logit.astype(jnp.float32) + dt_bias.astype(jnp.float32))).transpose(0, 2, 1)
    o = gated_delta_rule_chunked(qh, kh, vh, g, beta).transpose(0, 2, 1, 3)
    zf = z.reshape(B_, L, DN_HEADS, DN_HEAD_DIM).astype(jnp.float32)
    o = rms_normalize(o) * norm_w * jax.nn.silu(zf)
    return o.reshape(B_, L, D_DELTA).astype(qkv.dtype)


def hierarchical_moe(h, w_grp, b_grp, w_exp, b_exp, w_gate, w_up, w_down):
    B_, L, D = h.shape
    T = B_ * L
    xf = h.reshape(T, D)
    grp_prob = jax.nn.softmax((xf @ w_grp).astype(jnp.float32) + b_grp, axis=-1)
    grp_w, grp_idx = lax.top_k(grp_prob, 1)
    exp_logits = ((xf @ w_exp).astype(jnp.float32) + b_exp).reshape(T, N_GROUPS, EXPERTS_PER_GROUP)
    exp_logits = jnp.take_along_axis(exp_logits, grp_idx[:, :, None], axis=1)[:, 0]
    top_p, top_i = lax.top_k(jax.nn.softmax(exp_logits, axis=-1), TOP_K)
    gate = grp_w * (top_p / top_p.sum(-1, keepdims=True))
    expert = grp_idx * EXPERTS_PER_GROUP + top_i
    A = T * TOP_K
    e_flat = expert.reshape(A)
    tok = jnp.repeat(jnp.arange(T), TOP_K)
    order = jnp.argsort(e_flat)
    e_sorted = e_flat[order]
    tok_sorted = tok[order]
    g_sorted = gate.reshape(A)[order]
    counts = jnp.bincount(e_flat, length=N_EXPERTS)
    padded = ((counts + MOE_BLOCK - 1) // MOE_BLOCK) * MOE_BLOCK
    start = jnp.cumsum(counts) - counts
    pend = jnp.cumsum(padded)
    pstart = pend - padded
    pos = pstart[e_sorted] + jnp.arange(A) - start[e_sorted]
    n_blocks = (A + MOE_BLOCK - 1) // MOE_BLOCK + N_EXPERTS
    n_rows = n_blocks * MOE_BLOCK
    x_rows = jnp.zeros((n_rows, D), h.dtype).at[pos].set(xf[tok_sorted])
    block_expert = jnp.minimum(
        jnp.searchsorted(pend, jnp.arange(n_blocks) * MOE_BLOCK, side="right"), N_EXPERTS - 1)

    def expert_block(args):
        xb, e = args
        hid = jax.nn.silu(xb @ w_gate[e]) * (xb @ w_up[e])
        return hid @ w_down[e]

    y_rows = lax.map(expert_block, (x_rows.reshape(n_blocks, MOE_BLOCK, D), block_expert)).reshape(n_rows, D)
    y = jnp.zeros((T, D), jnp.float32).at[tok_sorted].add(y_rows[pos].astype(jnp.float32) * g_sorted[:, None])
    return y.reshape(B_, L, D).astype(h.dtype)


def setup_inputs(seed: int = 0) -> dict:
    key = jax.random.key(seed)
    ks = jax.random.split(key, 24)
    f32 = jnp.float32
    nrm = lambda k, shape, s: jax.random.normal(k, shape, f32) * s
    dt = jnp.exp(jax.random.uniform(ks[10], (DEPTH, DN_HEADS), f32, math.log(1e-3), math.log(1e-1)))
    return {
        "x": nrm(ks[0], (BATCH, SEQ, D_MODEL), 1.0),
        "c": nrm(ks[1], (BATCH, D_MODEL), 1.0),
        "w_ada": nrm(ks[2], (DEPTH, D_MODEL, 6 * D_MODEL), D_MODEL ** -0.5),
        "b_ada": nrm(ks[3], (DEPTH, 6 * D_MODEL), 0.01),
        "w_in": nrm(ks[4], (DEPTH, D_MODEL, D_IN), D_MODEL ** -0.5),
        "conv_w": nrm(ks[5], (DEPTH, CONV_WIDTH, D_CONV), CONV_WIDTH ** -0.5),
        "conv_norm_w": 1.0 + nrm(ks[6], (DEPTH, D_CONV), 0.01),
        "dn_conv_w": nrm(ks[7], (DEPTH, DN_CONV_WIDTH, 3 * D_DELTA), DN_CONV_WIDTH ** -0.5),
        "dn_A_log": jnp.log(jax.random.uniform(ks[8], (DEPTH, DN_HEADS), f32, 1.0, 16.0)),
        "dn_dt_bias": dt + jnp.log(-jnp.expm1(-dt)),
        "dn_norm_w": 1.0 + nrm(ks[9], (DEPTH, DN_HEAD_DIM), 0.01),
        "w_out": nrm(ks[11], (DEPTH, D_MIX, D_MODEL), D_MIX ** -0.5 * DEEP_BETA),
        "ln1_g": 1.0 + nrm(ks[12], (DEPTH, D_MODEL), 0.01),
        "ln1_b": nrm(ks[13], (DEPTH, D_MODEL), 0.01),
        "w_grp": nrm(ks[14], (DEPTH, D_MODEL, N_GROUPS), D_MODEL ** -0.5),
        "b_grp": nrm(ks[15], (DEPTH, N_GROUPS), 0.01),
        "w_exp": nrm(ks[16], (DEPTH, D_MODEL, N_EXPERTS), D_MODEL ** -0.5),
        "b_exp": nrm(ks[17], (DEPTH, N_EXPERTS), 0.01),
        "w_gate": nrm(ks[18], (DEPTH, N_EXPERTS, D_MODEL, D_EXPERT), D_MODEL ** -0.5),
        "w_up": nrm(ks[19], (DEPTH, N_EXPERTS, D_MODEL, D_EXPERT), D_MODEL ** -0.5),
        "w_down": nrm(ks[20], (DEPTH, N_EXPERTS, D_EXPERT, D_MODEL), D_EXPERT ** -0.5 * DEEP_BETA),
        "ln2_g": 1.0 + nrm(ks[21], (DEPTH, D_MODEL), 0.01),
        "ln2_b": nrm(ks[22], (DEPTH, D_MODEL), 0.01),
    }


def reference(x, c, w_ada, b_ada, w_in, conv_w, conv_norm_w, dn_conv_w, dn_A_log, dn_dt_bias, dn_norm_w,
              w_out, ln1_g, ln1_b, w_grp, b_grp, w_exp, b_exp, w_gate, w_up, w_down, ln2_g, ln2_b):
    split_at = [int(i) for i in np.cumsum(IN_SPLITS)[:-1]]
    c_act = jax.nn.silu(c)
    for l in range(DEPTH):
        mod = c_act @ w_ada[l] + b_ada[l]
        shift1, scale1, gate1, shift2, scale2, gate2 = [m[:, None, :] for m in jnp.split(mod, 6, axis=-1)]
        h = x * (1.0 + scale1) + shift1
        proj = h @ w_in[l]
        b_gate, c_gate, u, qkv, z, beta_logit, a_logit = jnp.split(proj, split_at, axis=-1)
        y_conv = short_conv_mixer(b_gate, c_gate, u, conv_w[l], conv_norm_w[l])
        y_dn = gated_deltanet(qkv, z, beta_logit, a_logit, dn_conv_w[l], dn_A_log[l], dn_dt_bias[l], dn_norm_w[l])
        mix = jnp.concatenate([y_conv, y_dn], axis=-1) @ w_out[l]
        x = layer_norm(DEEP_ALPHA * x + gate1 * mix, ln1_g[l], ln1_b[l])
        h = x * (1.0 + scale2) + shift2
        ffn = hierarchical_moe(h, w_grp[l], b_grp[l], w_exp[l], b_exp[l], w_gate[l], w_up[l], w_down[l])
        x = layer_norm(DEEP_ALPHA * x + gate2 * ffn, ln2_g[l], ln2_b[l])
    return x
```

```python
import contextlib
import numpy as np
import concourse.bass as bass
import concourse.mybir as mybir
from concourse.bass_utils import run_bass_kernel_spmd

F32 = mybir.dt.float32
BF16 = mybir.dt.bfloat16
AF = mybir.ActivationFunctionType
ALU = mybir.AluOpType
AX = mybir.AxisListType

ENGS = ("pe", "act", "dve", "pool", "sp")

D = 1024
SEQ = 2048
NB = 2
TOK = NB * SEQ
DIN = 3592
NEXP = 32
ALPHA = 2.0 ** 0.25
TT = 256
NT = TOK // TT
BIG = 30000.0


class Prog:
    NDMA = 24

    def __init__(self, nc, tag):
        self.nc = nc
        self.tag = tag
        self.ops = []
        self.chain_dma = False

    def op(self, eng, fn, r=(), w=()):
        self.ops.append(dict(eng=eng, fn=fn, r=tuple(r), w=tuple(w), dma=False))

    def dma(self, eng, fn, r=(), w=()):
        chain = (f"__q_{eng}",) if self.chain_dma else ()
        self.ops.append(dict(eng=eng, fn=fn, r=tuple(r), w=tuple(w) + chain, dma=True))

    def emit(self, final_wait_engine="sp"):
        nc = self.nc
        esem = {e: nc.alloc_semaphore(f"s_{e}_{self.tag}") for e in ENGS if e != "sp"}
        dsem = [nc.alloc_semaphore(f"d_{i}_{self.tag}") for i in range(self.NDMA)]
        ecount = {e: 0 for e in ENGS}
        dtotal = [0] * self.NDMA
        dnext = 0
        last_w = {}
        readers = {}
        waited = {e: {} for e in ENGS}
        per_eng = {e: [] for e in ENGS}
        tokens = []
        for i, o in enumerate(self.ops):
            E = o["eng"]
            deps = set()
            for k in o["r"]:
                if k in last_w:
                    deps.add(last_w[k])
            for k in o["w"]:
                if k in last_w:
                    deps.add(last_w[k])
                for rd in readers.get(k, ()):
                    deps.add(rd)
            waits = []
            for d in sorted(deps):
                od = self.ops[d]
                if (not od["dma"]) and od["eng"] == E and E == "pe" and not o["dma"]:
                    continue
                s, v = tokens[d]
                key = id(s)
                if waited[E].get(key, 0) >= v:
                    continue
                waited[E][key] = v
                waits.append((s, v))
            if o["dma"]:
                j = dnext
                dnext = (dnext + 1) % self.NDMA
                s = dsem[j]
                if dtotal[j] > 0 and waited[E].get(id(s), 0) < dtotal[j]:
                    waited[E][id(s)] = dtotal[j]
                    waits.append((s, dtotal[j]))
                dtotal[j] += 16
                tok = (s, dtotal[j])
                inc = 16
            else:
                ecount[E] += 1
                tok = (esem[E], ecount[E])
                inc = 1
            tokens.append(tok)
            per_eng[E].append((waits, o["fn"], tok[0], inc))
            for k in o["r"]:
                readers.setdefault(k, []).append(i)
            for k in o["w"]:
                last_w[k] = i
                readers[k] = []
        final_waits = [(esem[e], ecount[e]) for e in esem if ecount[e] > 0]
        final_waits += [(dsem[j], dtotal[j]) for j in range(self.NDMA) if dtotal[j] > 0]

        with nc.Block() as block:
            def mk(ename):
                def body(eng):
                    for waits, fn, s, inc in per_eng[ename]:
                        for (ws, wv) in waits:
                            eng.wait_ge(ws, wv)
                        ins = fn(eng)
                        ins.then_inc(s, inc)
                    if ename == final_wait_engine:
                        for (ws, wv) in final_waits:
                            eng.wait_ge(ws, wv)
                return body
            block.tensor(mk("pe"))
            block.scalar(mk("act"))
            block.vector(mk("dve"))
            block.gpsimd(mk("pool"))
            block.sync(mk("sp"))
        return dict(ecount)


class PsumRot:
    def __init__(self, items):
        self.items = list(items)
        self.i = 0

    def get(self):
        it = self.items[self.i]
        self.i = (self.i + 1) % len(self.items)
        return it


def make_consts():
    idx = np.arange(128)
    same = (idx[:, None] // 64) == (idx[None, :] // 64)
    c = {}
    c["ident"] = np.eye(128)
    c["ltb"] = (same & (idx[:, None] <= idx[None, :])) * 1.0
    c["bd"] = same * 1.0
    c["mnu"] = np.where(same & (idx[:, None] <= idx[None, :]), 0.0, -BIG)
    c["mnus"] = np.where(same & (idx[:, None] < idx[None, :]), 0.0, -BIG)
    c["mnls"] = np.where(same & (idx[:, None] > idx[None, :]), 0.0, -BIG)
    c["ones"] = np.ones((128, 128))
    c["gmat"] = same / 64.0
    c["omean"] = np.ones((128, 128)) / 128.0
    names = ["ident", "ltb", "bd", "mnu", "mnus", "mnls", "ones", "gmat", "omean"]
    cm = np.concatenate([c[n] for n in names], axis=1).astype(np.float32)
    mab = np.stack([(idx < 64) * 1.0, (idx >= 64) * 1.0], axis=1).astype(np.float32)
    return names, cm, mab


CNAMES, CMAT, MAB = make_consts()
NBLK = TOK * 2 // 128 + NEXP
NROWS = NBLK * 128


def make_consts_b():
    thr = np.broadcast_to(128.0 * np.arange(64)[None, :], (128, 64))
    e = np.arange(32)
    sl = np.broadcast_to((e[None, :] < e[:, None]).astype(np.float64).reshape(1, 1024), (128, 1024))
    blk = np.broadcast_to(np.arange(NBLK, dtype=np.float64)[None, :], (128, NBLK))
    iop = np.arange(128, dtype=np.float64)[:, None]
    idx = np.arange(128)
    su = (idx[:, None] < idx[None, :]) * 1.0
    return np.concatenate([thr, sl, blk, iop, su], axis=1).astype(np.float32)


CB = make_consts_b()


def build(stage="full", dbg=()):
    nc = bass.Bass("TRN2", target_bir_lowering=False)

    def din(name, shape, dt=F32):
        return nc.dram_tensor(name, list(shape), dt, kind="ExternalInput").ap()

    x = din("x", [TOK, D])
    c_in = din("c", [NB, D])
    w_ada = din("w_ada", [D, 6 * D])
    b_ada = din("b_ada", [1, 6 * D])
    w_in = din("w_in", [D, DIN])
    conv_w = din("conv_w", [3, 512])
    conv_norm_w = din("conv_norm_w", [1, 512])
    dn_conv_w = din("dn_conv_w", [4, 1536])
    dn_A_log = din("dn_A_log", [1, 4])
    dn_dt_bias = din("dn_dt_bias", [1, 4])
    dn_norm_w = din("dn_norm_w", [1, 128])
    w_out = din("w_out", [D, D])
    ln1_g = din("ln1_g", [1, D])
    ln1_b = din("ln1_b", [1, D])
    w_grp = din("w_grp", [D, 4])
    b_grp = din("b_grp", [1, 4])
    w_exp = din("w_exp", [D, 32])
    b_exp = din("b_exp", [1, 32])
    w_gate = din("w_gate", [NEXP, D, 256])
    w_up = din("w_up", [NEXP, D, 256])
    w_down = din("w_down", [NEXP, 256, D])
    ln2_g = din("ln2_g", [1, D])
    ln2_b = din("ln2_b", [1, D])
    cmat_d = din("cmat", list(CMAT.shape))
    mab_d = din("mab", [128, 2])
    cb_d = din("cb", list(CB.shape))
    out = nc.dram_tensor("out", [TOK, D], F32, kind="ExternalOutput").ap()
    r_s = nc.dram_tensor("r_scr", [TOK, D], F32, kind="Internal").ap()
    h2_s = nc.dram_tensor("h2_scr", [128, 8, TOK], BF16, kind="Internal").ap()
    g2_s = nc.dram_tensor("g2_scr", [128, 2 * D], F32, kind="Internal").ap()
    sh2_s = nc.dram_tensor("sh2_scr", [128, 2 * D], F32, kind="Internal").ap()
    sc2_s = nc.dram_tensor("sc2_scr", [128, 2 * D], F32, kind="Internal").ap()
    xrows_s = nc.dram_tensor("xrows_scr", [NROWS if stage != "pa1" else 128, D], F32, kind="Internal").ap()
    yrows_s = nc.dram_tensor("yrows_scr", [NROWS if stage != "pa1" else 128, D], F32, kind="Internal").ap()
    dbg_out = {}
    for (nm, shp) in dbg:
        dbg_out[nm] = nc.dram_tensor("dbg_" + nm, list(shp), F32, kind="ExternalOutput").ap()

    def sb(name, shape, dt=F32):
        return nc.alloc_sbuf_tensor("sb_" + name, list(shape), dt).ap()

    PS2 = [nc.alloc_psum_tensor(f"ps2_{i}", [128, 1024], F32).ap() for i in range(4)]
    BANKS = []
    for i in range(4):
        BANKS.append((PS2[i][:, 0:512], f"bank{2 * i}"))
        BANKS.append((PS2[i][:, 512:1024], f"bank{2 * i + 1}"))

    cm = sb("cm", [128, CMAT.shape[1]])
    C = {n: cm[:, i * 128:(i + 1) * 128] for i, n in enumerate(CNAMES)}
    mab = sb("mab", [128, 2])
    identb = sb("identb", [128, 128], BF16)
    modT = sb("modT", [128, 48, 2])
    s1p = sb("s1p", [128, 8, 2])
    A2 = sb("A2", [128, 8, 2])
    B2 = sb("B2", [128, 8, 2])
    gate_bc = {2: sb("gate1bc", [128, 2, D])}
    cw = sb("cw", [128, 4, 3])
    cnw = sb("cnw", [128, 4])
    dcw = sb("dcw", [128, 12, 4])
    dnw = sb("dnw", [128, 1])
    g1T = sb("g1T", [128, 8])
    b1T = sb("b1T", [128, 8])
    negA = sb("negA", [128, 4])
    dtb = sb("dtb", [128, 4])
    ag = sb("ag", [128, D])
    ab = sb("ab", [128, D])

    P = Prog(nc, "p0")
    P.dma("sp", lambda e: e.dma_start(out=cm, in_=cmat_d), w=["cm"])
    P.dma("sp", lambda e: e.dma_start(out=mab, in_=mab_d), w=["mab"])
    P.op("dve", lambda e: e.tensor_copy(out=identb, in_=C["ident"]), r=["cm"], w=["identb"])

    with nc.sbuf_tensor("t_cT", [128, 8, 2], F32) as cT_h, \
            nc.sbuf_tensor("t_cact", [128, 8, 2], BF16) as cact_h, \
            nc.sbuf_tensor("t_cbc", [128, 8, 2, 128], BF16) as cbc_h, \
            nc.sbuf_tensor("t_brow", [1, 6 * D], BF16) as brow_h, \
            nc.sbuf_tensor("t_onesr", [1, 128], BF16) as onesr_h, \
            nc.sbuf_tensor("t_wa0", [128, 8, D], BF16) as wa0_h, \
            nc.sbuf_tensor("t_wa1", [128, 8, D], BF16) as wa1_h, \
            nc.sbuf_tensor("t_wa2", [128, 8, D], BF16) as wa2_h, \
            nc.sbuf_tensor("t_wa3", [128, 8, D], BF16) as wa3_h, \
            nc.sbuf_tensor("t_sp2", [128, 8, 2], F32) as sp2_h, \
            nc.sbuf_tensor("t_g2bc", [128, 2, D], F32) as g2bc_h, \
            nc.sbuf_tensor("t_sh2bc", [128, 2, D], F32) as sh2bc_h, \
            nc.sbuf_tensor("t_sc2bc", [128, 2, D], F32) as sc2bc_h:
        gate_bc[5] = g2bc_h.ap()
        gate_bc[3] = sh2bc_h.ap()
        gate_bc[4] = sc2bc_h.ap()
        cT, cact, cbc, brow, onesr, sp2 = (t.ap() for t in (cT_h, cact_h, cbc_h, brow_h, onesr_h, sp2_h))
        wa = [wa0_h.ap(), wa1_h.ap(), wa2_h.ap(), wa3_h.ap()]
        for b in range(NB):
            P.dma("sp", lambda e, b=b: e.dma_start(
                out=cT[:, :, b], in_=c_in[b, :].rearrange("(kc p) -> p kc", p=128),
                allow_slow_non_contiguous=True), w=["cT"])
        P.op("act", lambda e: e.activation(out=cact, in_=cT, func=AF.Silu), r=["cT"], w=["cact"])
        P.op("dve", lambda e: e.tensor_copy(out=cbc, in_=cact.unsqueeze(3).to_broadcast([128, 8, 2, 128])),
             r=["cact"], w=["cbc"])
        P.dma("pool", lambda e: e.dma_start(out=brow, in_=b_ada), w=["brow"])
        P.op("dve", lambda e: e.memset(onesr, 1.0), w=["onesr"])
        for k in range(3):
            P.dma("sp", lambda e, k=k: e.dma_start(out=cw[:, :, k],
                                                   in_=conv_w[k, :].rearrange("(j p) -> p j", p=128),
                                                   allow_slow_non_contiguous=True), w=["cw"])
        P.dma("sp", lambda e: e.dma_start(out=cnw, in_=conv_norm_w[0, :].rearrange("(j p) -> p j", p=128),
                                          allow_slow_non_contiguous=True), w=["cnw"])
        for k in range(4):
            P.dma("sp", lambda e, k=k: e.dma_start(out=dcw[:, :, k],
                                                   in_=dn_conv_w[k, :].rearrange("(j p) -> p j", p=128),
                                                   allow_slow_non_contiguous=True), w=["dcw"])
        P.dma("sp", lambda e: e.dma_start(out=dnw, in_=dn_norm_w.rearrange("o p -> p o"),
                                          allow_slow_non_contiguous=True), w=["dnw"])
        P.dma("sp", lambda e: e.dma_start(out=g1T, in_=ln1_g[0, :].rearrange("(j p) -> p j", p=128),
                                          allow_slow_non_contiguous=True), w=["g1T"])
        P.dma("sp", lambda e: e.dma_start(out=b1T, in_=ln1_b[0, :].rearrange("(j p) -> p j", p=128),
                                          allow_slow_non_contiguous=True), w=["b1T"])
        P.dma("sp", lambda e: e.dma_start(out=negA, in_=dn_A_log[0, :].partition_broadcast(128)), w=["negA"])
        P.dma("sp", lambda e: e.dma_start(out=dtb, in_=dn_dt_bias[0, :].partition_broadcast(128)), w=["dtb"])
        P.op("act", lambda e: e.activation(out=negA, in_=negA, func=AF.Exp), r=["negA"], w=["negA"])
        P.op("dve", lambda e: e.tensor_scalar(out=negA, in0=negA, scalar1=-1.0, scalar2=None, op0=ALU.mult),
             r=["negA"], w=["negA"])

        ps0 = PsumRot(BANKS[0:1])
        psr = PsumRot(BANKS[1:3])
        modps, modk = ps0.get()
        for j in range(6):
            wj = wa[j % 4]
            wk = f"wa{j % 4}"
            for kc in range(8):
                P.dma("pool", lambda e, j=j, kc=kc, wj=wj: e.dma_start(
                    out=wj[:, kc, :], in_=w_ada[kc * 128:(kc + 1) * 128, j * D:(j + 1) * D]), w=[wk])
            for cc in range(8):
                col = (j * 8 + cc) * 2
                for kc in range(8):
                    P.op("pe", lambda e, wj=wj, kc=kc, cc=cc, col=col: e.matmul(
                        modps[:, col:col + 2], lhsT=wj[:, kc, cc * 128:(cc + 1) * 128], rhs=cact[:, kc, :],
                        start=(kc == 0), stop=False), r=[wk, "cact"], w=[modk])
                P.op("pe", lambda e, j=j, cc=cc, col=col: e.matmul(
                    modps[:, col:col + 2], lhsT=brow[0:1, j * D + cc * 128:j * D + (cc + 1) * 128],
                    rhs=onesr[0:1, 0:2], start=False, stop=True), r=["brow", "onesr"], w=[modk])
            if j in (2, 3, 4, 5):
                for b in range(NB):
                    for half in range(2):
                        pt, pk = psr.get()
                        for kc in range(8):
                            P.op("pe", lambda e, wj=wj, kc=kc, b=b, half=half, pt=pt: e.matmul(
                                pt, lhsT=cbc[:, kc, b, :], rhs=wj[:, kc, half * 512:(half + 1) * 512],
                                start=(kc == 0), stop=False), r=[wk, "cbc"], w=[pk])
                        P.op("pe", lambda e, j=j, half=half, pt=pt: e.matmul(
                            pt, lhsT=onesr[0:1, :], rhs=brow[0:1, j * D + half * 512:j * D + (half + 1) * 512],
                            start=False, stop=True), r=["brow", "onesr"], w=[pk])
                        P.op("act", lambda e, j=j, b=b, half=half, pt=pt: e.activation(
                            out=gate_bc[j][:, b, half * 512:(half + 1) * 512], in_=pt, func=AF.Copy),
                            r=[pk], w=[f"gbc{j}"])
        P.op("dve", lambda e: e.tensor_copy(out=modT.rearrange("p a b -> p (a b)"), in_=modps[:, 0:96]),
             r=[modk], w=["modT"])
        P.op("dve", lambda e: e.tensor_scalar(out=s1p, in0=modT[:, 8:16, :], scalar1=1.0, scalar2=None, op0=ALU.add),
             r=["modT"], w=["s1p"])
        P.op("dve", lambda e: e.tensor_scalar(out=sp2, in0=modT[:, 32:40, :], scalar1=1.0, scalar2=None, op0=ALU.add),
             r=["modT"], w=["sp2"])
        P.op("dve", lambda e: e.tensor_tensor(out=A2, in0=sp2, in1=g1T.unsqueeze(2).to_broadcast([128, 8, 2]),
                                              op=ALU.mult), r=["sp2", "g1T"], w=["A2"])
        P.op("dve", lambda e: e.tensor_tensor(out=B2, in0=sp2, in1=b1T.unsqueeze(2).to_broadcast([128, 8, 2]),
                                              op=ALU.mult), r=["sp2", "b1T"], w=["B2"])
        P.op("dve", lambda e: e.tensor_tensor(out=B2, in0=B2, in1=modT[:, 24:32, :], op=ALU.add),
             r=["B2", "modT"], w=["B2"])
        P.dma("sp", lambda e: e.dma_start(out=ag, in_=ln1_g[0, :].partition_broadcast(128)), w=["ag"])
        P.dma("sp", lambda e: e.dma_start(out=ab, in_=ln1_b[0, :].partition_broadcast(128)), w=["ab"])
        P.op("pool", lambda e: e.tensor_scalar(out=ag, in0=ag, scalar1=ALPHA, scalar2=None, op0=ALU.mult),
             r=["ag"], w=["ag"])
        P.op("pool", lambda e: e.tensor_scalar(out=ab, in0=ab, scalar1=ALPHA, scalar2=None, op0=ALU.mult),
             r=["ab"], w=["ab"])
        P.dma("sp", lambda e: e.dma_start(out=g2_s, in_=gate_bc[5].rearrange("p a b -> p (a b)")),
              r=["gbc5"], w=["g2s"])
        P.dma("sp", lambda e: e.dma_start(out=sh2_s, in_=gate_bc[3].rearrange("p a b -> p (a b)")),
              r=["gbc3"], w=["sh2s"])
        P.dma("sp", lambda e: e.dma_start(out=sc2_s, in_=gate_bc[4].rearrange("p a b -> p (a b)")),
              r=["gbc4"], w=["sc2s"])
        if stage == "p0":
            P.dma("sp", lambda e: e.dma_start(out=dbg_out["modT"], in_=modT.rearrange("p a b -> p (a b)")),
                  r=["modT"], w=["dbgo"])
            P.dma("sp", lambda e: e.dma_start(out=dbg_out["g1bc"], in_=gate_bc[2].rearrange("p a b -> p (a b)")),
                  r=["gbc2"], w=["dbgo2"])
        P.emit()

    if stage == "p0":
        return nc
    with contextlib.ExitStack() as es:
        def tsb(name, shape, dt=F32):
            return es.enter_context(nc.sbuf_tensor("a_" + name, list(shape), dt)).ap()

        Win = tsb("win", [128, 8, DIN], BF16)
        Wout = tsb("wout", [128, 8, D], BF16)
        P = Prog(nc, "pa")
        for kc in range(8):
            for (c0, c1) in ((0, 2048), (2048, DIN)):
                P.dma("pool", lambda e, kc=kc, c0=c0, c1=c1: e.dma_start(
                    out=Win[:, kc, c0:c1], in_=w_in[kc * 128:(kc + 1) * 128, c0:c1]), w=["Win"])
            P.dma("pool", lambda e, kc=kc: e.dma_start(
                out=Wout[:, kc, :], in_=w_out[kc * 128:(kc + 1) * 128, :]), w=["Wout"])

        xt = tsb("xt", [128, 2, D])
        hT = tsb("hT", [128, 8, TT], BF16)
        cutail = tsb("cutail", [128, 4, 2])
        qtail = tsb("qtail", [128, 12, 3])
        qkvc2 = [tsb(f"qkvc{i}", [128, 12, TT]) for i in range(2)]
        zs2 = [tsb(f"zs{i}", [128, 4, TT]) for i in range(2)]
        mixT2 = [tsb(f"mixT{i}", [128, 8, TT], BF16) for i in range(3)]
        blsb2 = [tsb(f"blsb{i}", [128, 16]) for i in range(2)]
        S = tsb("S", [128, 4, 128])
        csb = tsb("csb", [128, TT])
        cuf2 = [tsb(f"cuf{i}", [128, TT + 2]) for i in range(2)]
        acc2 = [tsb(f"acc{i}", [128, TT]) for i in range(2)]
        ybuf2 = [tsb(f"ybuf{i}", [128, TT]) for i in range(2)]
        sqb2 = [tsb(f"sqb{i}", [128, TT]) for i in range(2)]
        sgt2 = [tsb(f"sgt{i}", [128, TT]) for i in range(2)]
        halo = [tsb(f"halo{i}", [128, TT + 3]) for i in range(2)]
        sm2 = [tsb(f"sm{i}", [128, 2, 64]) for i in range(2)]
        T = [tsb(f"T{i}", [128, 512]) for i in range(13)]
        r0 = tsb("r0", [128, D])
        xh = tsb("xh", [128, D])
        h2t = tsb("h2t", [128, 8, TT], BF16)
        bst = tsb("bst", [128, 2, 6])
        mv = tsb("mv", [128, 4])
        rotA = PsumRot(BANKS[0:4])
        rotB = PsumRot(BANKS[4:8])
        rot2A = PsumRot([(PS2[i], (f"bank{2 * i}", f"bank{2 * i + 1}")) for i in (0, 1)])

        def v3(ap):
            return ap.rearrange("p (h j) -> p h j", h=4)

        def bc_h(ap128):
            return ap128.unsqueeze(1).to_broadcast([128, 4, 128])

        def bc_j(ap4):
            return ap4.unsqueeze(2).to_broadcast([128, 4, 128])

        EPS_RMS = 1e-6

        def stage1(ti):
            pp = ti % 2
            b = ti // (NT // NB)
            first = (ti % (NT // NB) == 0)
            qkvc, zs, mixT, blsb = qkvc2[pp], zs2[pp], mixT2[ti % 3], blsb2[pp]
            mp = ti % 3
            rot = rotA
            P.dma("sp", lambda e: e.dma_start(
                out=xt, in_=x[ti * TT:(ti + 1) * TT, :].rearrange("(s p) f -> p s f", p=128)), w=["xt"])
            if first:
                P.op("pool", lambda e: e.memset(cutail, 0.0), w=["cutail"])
                P.op("pool", lambda e: e.memset(qtail, 0.0), w=["qtail"])
            for kc in range(8):
                pt, pk = rot.get()
                for s_ in range(2):
                    P.op("pe", lambda e, pt=pt, s_=s_, kc=kc: e.transpose(
                        out=pt[:, s_ * 128:(s_ + 1) * 128], in_=xt[:, s_, kc * 128:(kc + 1) * 128],
                        identity=C["ident"]), r=["xt"], w=[pk])
                P.op("act", lambda e, pt=pt, kc=kc: e.activation(
                    out=hT[:, kc, :], in_=pt[:, 0:TT], func=AF.Identity,
                    scale=s1p[:, kc, b:b + 1], bias=modT[:, kc, b:b + 1]), r=[pk], w=[f"hT{kc}"])

            def proj(oc):
                pt, pk = rot.get()
                for kc in range(8):
                    P.op("pe", lambda e, pt=pt, kc=kc: e.matmul(
                        pt[:, 0:TT], lhsT=Win[:, kc, oc * 128:(oc + 1) * 128], rhs=hT[:, kc, :],
                        start=(kc == 0), stop=(kc == 7)), r=[f"hT{kc}", "Win"], w=[pk])
                return pt[:, 0:TT], pk

            deferred = []

            def flush(keep=0):
                while len(deferred) > keep:
                    deferred.pop(0)()

            def rstd_part2(srcbuf, srck, lhs, q):
                sqb = sqb2[q]
                pm, pmk = rot.get()
                P.op("pe", lambda e: e.matmul(pm[:, 0:TT], lhsT=lhs, rhs=sqb, start=True, stop=True),
                     r=[f"sqb{q}"], w=[pmk])
                P.op("act", lambda e: e.activation(out=sqb, in_=pm[:, 0:TT], func=AF.Ln, bias=EPS_RMS),
                     r=[pmk], w=[f"sqb{q}"])
                P.op("act", lambda e: e.activation(out=sqb, in_=sqb, func=AF.Exp, scale=-0.5),
                     r=[f"sqb{q}"], w=[f"sqb{q}"])

            for j in range(4):
                q = j % 2
                cuf, acc, ybuf, sqb = cuf2[q], acc2[q], ybuf2[q], sqb2[q]
                pb, pbk = proj(j)
                pc, pck = proj(4 + j)
                pu, puk = proj(8 + j)
                P.op("act", lambda e, pc=pc: e.activation(out=csb, in_=pc, func=AF.Copy), r=[pck], w=["csb"])
                P.op("dve", lambda e, pu=pu, cuf=cuf: e.tensor_tensor(out=cuf[:, 2:TT + 2], in0=pu, in1=csb, op=ALU.mult),
                     r=[puk, "csb"], w=[f"cuf{q}"])
                P.op("pool", lambda e, j=j, cuf=cuf: e.tensor_copy(out=cuf[:, 0:2], in_=cutail[:, j, :]),
                     r=["cutail"], w=[f"cufh{q}"])
                P.op("act", lambda e, j=j, cuf=cuf, acc=acc: e.activation(out=acc, in_=cuf[:, 2:TT + 2], func=AF.Copy,
                                                                          scale=cw[:, j, 2:3]), r=[f"cuf{q}"], w=[f"acc{q}"])
                P.op("dve", lambda e, j=j, cuf=cuf, acc=acc: e.scalar_tensor_tensor(
                    out=acc, in0=cuf[:, 1:TT + 1], scalar=cw[:, j, 1:2], in1=acc, op0=ALU.mult, op1=ALU.add),
                    r=[f"cuf{q}", f"cufh{q}", f"acc{q}"], w=[f"acc{q}"])
                P.op("dve", lambda e, j=j, cuf=cuf, acc=acc: e.scalar_tensor_tensor(
                    out=acc, in0=cuf[:, 0:TT], scalar=cw[:, j, 0:1], in1=acc, op0=ALU.mult, op1=ALU.add),
                    r=[f"cuf{q}", f"cufh{q}", f"acc{q}"], w=[f"acc{q}"])
                P.op("pool", lambda e, j=j, cuf=cuf: e.tensor_copy(out=cutail[:, j, :], in_=cuf[:, TT:TT + 2]),
                     r=[f"cuf{q}"], w=["cutail"])
                P.op("dve", lambda e, pb=pb, acc=acc, ybuf=ybuf: e.tensor_tensor(out=ybuf, in0=pb, in1=acc, op=ALU.mult),
                     r=[pbk, f"acc{q}"], w=[f"ybuf{q}"])
                P.op("act", lambda e, ybuf=ybuf, sqb=sqb: e.activation(out=sqb, in_=ybuf, func=AF.Square),
                     r=[f"ybuf{q}"], w=[f"sqb{q}"])

                def part2(j=j, q=q, ybuf=ybuf, sqb=sqb):
                    rstd_part2(None, None, C["gmat"], q)
                    P.op("dve", lambda e: e.scalar_tensor_tensor(
                        out=mixT[:, j, :], in0=ybuf, scalar=cnw[:, j:j + 1], in1=sqb, op0=ALU.mult, op1=ALU.mult),
                        r=[f"ybuf{q}", f"sqb{q}"], w=[f"mixT{mp}_{j}"])
                flush(0)
                deferred.append(part2)

            for j in range(12):
                q = j % 2
                sqb = sqb2[q]
                pq, pqk = proj(12 + j)
                hb = halo[q]
                hk = f"halo{q}"
                qk_ = f"qk{pp}_{j}"
                P.op("act", lambda e, pq=pq, hb=hb: e.activation(out=hb[:, 3:TT + 3], in_=pq, func=AF.Copy),
                     r=[pqk], w=[hk])
                P.op("pool", lambda e, j=j, hb=hb: e.tensor_copy(out=hb[:, 0:3], in_=qtail[:, j, :]),
                     r=["qtail"], w=[hk + "h"])
                P.op("pool", lambda e, j=j, hb=hb: e.tensor_scalar(
                    out=qkvc[:, j, :], in0=hb[:, 3:TT + 3], scalar1=dcw[:, j, 3:4], scalar2=0.0, op0=ALU.mult,
                    op1=ALU.add), r=[hk], w=[qk_])
                for k in (2, 1, 0):
                    P.op("dve", lambda e, j=j, hb=hb, k=k: e.scalar_tensor_tensor(
                        out=qkvc[:, j, :], in0=hb[:, k:TT + k], scalar=dcw[:, j, k:k + 1], in1=qkvc[:, j, :],
                        op0=ALU.mult, op1=ALU.add), r=[hk, hk + "h", qk_], w=[qk_])
                P.op("pool", lambda e, j=j, hb=hb: e.tensor_copy(out=qtail[:, j, :], in_=hb[:, TT:TT + 3]),
                     r=[hk], w=["qtail"])
                sgt = sgt2[q]
                P.op("act", lambda e, j=j, sgt=sgt: e.activation(out=sgt, in_=qkvc[:, j, :], func=AF.Exp, scale=-1.0),
                     r=[qk_], w=[f"sgt{q}"])
                P.op("act", lambda e, sgt=sgt: e.activation(out=sgt, in_=sgt, func=AF.Ln, bias=1.0),
                     r=[f"sgt{q}"], w=[f"sgt{q}"])
                P.op("act", lambda e, sgt=sgt: e.activation(out=sgt, in_=sgt, func=AF.Exp, scale=-1.0),
                     r=[f"sgt{q}"], w=[f"sgt{q}"])
                P.op("pool", lambda e, j=j, sgt=sgt: e.tensor_tensor(out=qkvc[:, j, :], in0=qkvc[:, j, :], in1=sgt,
                                                                     op=ALU.mult), r=[qk_, f"sgt{q}"], w=[qk_])
                if j < 8:
                    P.op("act", lambda e, j=j, sqb=sqb: e.activation(out=sqb, in_=qkvc[:, j, :], func=AF.Square),
                         r=[qk_], w=[f"sqb{q}"])

                    def part2(j=j, q=q, sqb=sqb, qk_=qk_):
                        rstd_part2(None, None, C["ones"], q)
                        sc = (128.0 ** -0.5) if j < 4 else 1.0
                        P.op("dve", lambda e: e.scalar_tensor_tensor(
                            out=qkvc[:, j, :], in0=qkvc[:, j, :], scalar=sc, in1=sqb, op0=ALU.mult, op1=ALU.mult),
                            r=[qk_, f"sqb{q}"], w=[qk_])
                    flush(0)
                    deferred.append(part2)
                else:
                    flush(0)
            flush(0)
            for j in range(4):
                pz, pzk = proj(24 + j)
                sgt = sgt2[j % 2]
                sk = f"sgt{j % 2}"
                P.op("act", lambda e, pz=pz, sgt=sgt: e.activation(out=sgt, in_=pz, func=AF.Exp, scale=-1.0),
                     r=[pzk], w=[sk])
                P.op("act", lambda e, sgt=sgt: e.activation(out=sgt, in_=sgt, func=AF.Ln, bias=1.0), r=[sk], w=[sk])
                P.op("act", lambda e, sgt=sgt: e.activation(out=sgt, in_=sgt, func=AF.Exp, scale=-1.0), r=[sk], w=[sk])
                P.op("dve", lambda e, pz=pz, j=j, sgt=sgt: e.tensor_tensor(out=zs[:, j, :], in0=pz, in1=sgt, op=ALU.mult),
                     r=[pzk, sk], w=[f"zs{pp}_{j}"])
            p8, p8k = rot.get()
            for s_ in range(2):
                for kc in range(8):
                    P.op("pe", lambda e, s_=s_, kc=kc: e.matmul(
                        p8[:, s_ * 8:(s_ + 1) * 8], lhsT=hT[:, kc, s_ * 128:(s_ + 1) * 128],
                        rhs=Win[:, kc, 3584:3592], start=(kc == 0), stop=(kc == 7)),
                        r=[f"hT{kc}", "Win"], w=[p8k])
            P.op("act", lambda e: e.activation(out=blsb, in_=p8[:, 0:16], func=AF.Copy), r=[p8k], w=[f"blsb{pp}"])
            for s_ in range(2):
                smx = sm2[pp][:, s_, :]
                beta, xa, g, _g, egc, bge, dl, eL, sA, sB = [smx[:, i * 4:(i + 1) * 4] for i in range(10)]
                gcs = smx[:, 40:48]
                gcum = gcs[:, 0:4]
                glast = gcs[:, 4:8]
                SK = f"smk{pp}_{s_}"
                bl = blsb[:, s_ * 8:(s_ + 1) * 8]
                blk = f"blsb{pp}"
                P.op("act", lambda e, beta=beta, bl=bl: e.activation(out=beta, in_=bl[:, 0:4], func=AF.Exp, scale=-1.0),
                     r=[blk], w=[SK])
                P.op("dve", lambda e, beta=beta: e.tensor_scalar(out=beta, in0=beta, scalar1=1.0, scalar2=None, op0=ALU.add),
                     r=[SK], w=[SK])
                P.op("dve", lambda e, beta=beta: e.reciprocal(out=beta, in_=beta), r=[SK], w=[SK])
                P.op("dve", lambda e, xa=xa, bl=bl: e.tensor_tensor(out=xa, in0=bl[:, 4:8], in1=dtb, op=ALU.add),
                     r=[blk, SK], w=[SK])
                P.op("act", lambda e, xa=xa: e.activation(out=xa, in_=xa, func=AF.Exp), r=[SK], w=[SK])
                P.op("act", lambda e, xa=xa: e.activation(out=xa, in_=xa, func=AF.Ln, bias=1.0), r=[SK], w=[SK])
                P.op("dve", lambda e, g=g, xa=xa: e.tensor_tensor(out=g, in0=xa, in1=negA, op=ALU.mult), r=[SK], w=[SK])
                pc_, pck = rot.get()
                P.op("pe", lambda e, pc_=pc_, g=g: e.matmul(pc_[:, 0:4], lhsT=C["ltb"], rhs=g, start=True, stop=True),
                     r=[SK], w=[pck])
                P.op("pe", lambda e, pc_=pc_, g=g: e.matmul(pc_[:, 4:8], lhsT=C["bd"], rhs=g, start=True, stop=True),
                     r=[SK], w=[pck])
                P.op("act", lambda e, pc_=pc_, gcs=gcs: e.activation(out=gcs, in_=pc_[:, 0:8], func=AF.Copy), r=[pck], w=[SK])
                P.op("act", lambda e, egc=egc, gcum=gcum: e.activation(out=egc, in_=gcum, func=AF.Exp), r=[SK], w=[SK])
                P.op("dve", lambda e, bge=bge, beta=beta, egc=egc: e.tensor_tensor(out=bge, in0=beta, in1=egc, op=ALU.mult),
                     r=[SK], w=[SK])
                P.op("dve", lambda e, dl=dl, glast=glast, gcum=gcum: e.tensor_tensor(out=dl, in0=glast, in1=gcum,
                                                                                     op=ALU.subtract), r=[SK], w=[SK])
                P.op("act", lambda e, eL=eL, dl=dl: e.activation(out=eL, in_=dl, func=AF.Exp), r=[SK], w=[SK])
                P.op("dve", lambda e, sA=sA, eL=eL: e.tensor_scalar(out=sA, in0=eL, scalar1=mab[:, 0:1], scalar2=None,
                                                                    op0=ALU.mult), r=[SK], w=[SK])
                P.op("dve", lambda e, sB=sB, eL=eL: e.tensor_scalar(out=sB, in0=eL, scalar1=mab[:, 1:2], scalar2=None,
                                                                    op0=ALU.mult), r=[SK], w=[SK])

        def stage2(ti):
            first = (ti % (NT // NB) == 0)
            if first:
                P.op("pool", lambda e: e.memset(S, 0.0), w=["S0", "S1", "S2", "S3"])
            for s_ in range(2):
                gdn_sub(ti, s_)

        def stage3(ti):
            b = ti // (NT // NB)
            for s_ in range(2):
                ln1_sub(ti, s_, b)
            P.dma("sp", lambda e: e.dma_start(out=h2_s[:, :, ti * TT:(ti + 1) * TT], in_=h2t),
                  r=["h2t"], w=["h2s"])

        def gdn_sub(ti, s_):
            pp = ti % 2
            rot = rotB
            qkvc, zs, mixT, blsb = qkvc2[pp], zs2[pp], mixT2[ti % 3], blsb2[pp]
            mp = ti % 3
            bl = blsb[:, s_ * 8:(s_ + 1) * 8]
            blk = f"blsb{pp}"
            cs = slice(s_ * 128, (s_ + 1) * 128)
            R1, R2, tU, tL, egr, U, L, QKm, Xa, Xb, Pb, PTb, bv = T
            kR1, kR2, ktU, ktL, kegr, kU, kL, kQKm, kXa, kXb, kPb, kPTb, kbv = [f"T{i}" for i in range(13)]
            keA, kkeA, keB, kkeB = R2, kR2, tL, ktL
            u, ku, wT, kwT, qdT, kqdT, delta, kdelta = U, kU, L, kL, Pb, kPb, PTb, kPTb
            smx = sm2[pp][:, s_, :]
            beta, xa, g, _g, egc, bge, dl, eL, sA, sB = [smx[:, i * 4:(i + 1) * 4] for i in range(10)]
            gcs = smx[:, 40:48]
            gcum = gcs[:, 0:4]
            glast = gcs[:, 4:8]
            SK = f"smk{pp}_{s_}"
            qk = lambda j: f"qk{pp}_{j}"
            P.op("dve", lambda e: e.tensor_tensor(out=v3(R1), in0=bc_h(C["ltb"]), in1=bc_j(g), op=ALU.mult),
                 r=[SK], w=[kR1])
            P.op("pool", lambda e: e.tensor_tensor(out=v3(R2), in0=bc_h(C["ident"]), in1=bc_j(beta), op=ALU.mult),
                 r=[SK], w=[kR2])
            pgr, pgrk = rot.get()
            P.op("pe", lambda e: e.matmul(pgr, lhsT=C["ones"], rhs=R1, start=True, stop=True), r=[kR1], w=[pgrk])
            pbr, pbrk = rot.get()
            P.op("pe", lambda e: e.matmul(pbr, lhsT=C["ones"], rhs=R2, start=True, stop=True), r=[kR2], w=[pbrk])
            P.op("dve", lambda e: e.tensor_tensor(out=v3(tU), in0=v3(pgr), in1=bc_j(gcum), op=ALU.subtract),
                 r=[pgrk, SK], w=[ktU])
            P.op("pool", lambda e: e.tensor_tensor(out=v3(R1), in0=v3(tU), in1=bc_h(C["mnu"]), op=ALU.add),
                 r=[ktU], w=[kR1])
            P.op("act", lambda e: e.activation(out=R1, in_=R1, func=AF.Exp), r=[kR1], w=[kR1])
            P.op("pool", lambda e: e.tensor_tensor(out=v3(R2), in0=v3(tU), in1=bc_h(C["mnus"]), op=ALU.add),
                 r=[ktU], w=[kR2])
            P.op("act", lambda e: e.activation(out=R2, in_=R2, func=AF.Exp), r=[kR2], w=[kR2])
            P.op("dve", lambda e: e.tensor_tensor(out=R2, in0=R2, in1=pbr, op=ALU.mult), r=[kR2, pbrk], w=[kR2])
            P.op("dve", lambda e: e.scalar_tensor_tensor(out=v3(tL), in0=v3(pgr), scalar=-1.0, in1=bc_j(gcum),
                                                         op0=ALU.mult, op1=ALU.add), r=[pgrk, SK], w=[ktL])
            P.op("pool", lambda e: e.tensor_tensor(out=v3(tL), in0=v3(tL), in1=bc_h(C["mnls"]), op=ALU.add),
                 r=[ktL], w=[ktL])
            P.op("act", lambda e: e.activation(out=tL, in_=tL, func=AF.Exp), r=[ktL], w=[ktL])
            P.op("pool", lambda e: e.tensor_tensor(out=v3(tL), in0=v3(tL), in1=bc_j(beta), op=ALU.mult),
                 r=[ktL, SK], w=[ktL])
            P.op("act", lambda e: e.activation(out=egr, in_=pgr, func=AF.Exp), r=[pgrk], w=[kegr])
            pkk, pkkk = rot.get()
            for h in range(4):
                P.op("pe", lambda e, h=h: e.matmul(pkk[:, h * 128:(h + 1) * 128], lhsT=qkvc[:, 4 + h, cs],
                                                   rhs=qkvc[:, 4 + h, cs], start=True, stop=True),
                     r=[qk(4 + h)], w=[pkkk])
            P.op("dve", lambda e: e.tensor_tensor(out=U, in0=pkk, in1=R2, op=ALU.mult), r=[pkkk, kR2], w=[kU])
            P.op("dve", lambda e: e.tensor_tensor(out=L, in0=pkk, in1=tL, op=ALU.mult), r=[pkkk, ktL], w=[kL])
            pqk_, pqkk = rot.get()
            for h in range(4):
                P.op("pe", lambda e, h=h: e.matmul(pqk_[:, h * 128:(h + 1) * 128], lhsT=qkvc[:, 4 + h, cs],
                                                   rhs=qkvc[:, h, cs], start=True, stop=True),
                     r=[qk(4 + h), qk(h)], w=[pqkk])
            P.op("dve", lambda e: e.tensor_tensor(out=QKm, in0=pqk_, in1=R1, op=ALU.mult), r=[pqkk, kR1], w=[kQKm])
            P.op("pool", lambda e: e.tensor_tensor(out=v3(Xa), in0=bc_h(C["ident"]), in1=v3(U), op=ALU.subtract),
                 r=[kU], w=[kXa])
            pkt, pktk = rot.get()
            for h in range(4):
                P.op("pe", lambda e, h=h: e.transpose(out=pkt[:, h * 128:(h + 1) * 128], in_=qkvc[:, 4 + h, cs],
                                                      identity=C["ident"]), r=[qk(4 + h)], w=[pktk])
            kbg, kkbg = tU, ktU
            P.op("dve", lambda e: e.tensor_tensor(out=v3(kbg), in0=v3(pkt), in1=bc_j(bge), op=ALU.mult),
                 r=[pktk, SK], w=[kkbg])
            P.op("dve", lambda e: e.tensor_tensor(out=v3(keA), in0=v3(pkt), in1=bc_j(sA), op=ALU.mult),
                 r=[pktk, SK, kU], w=[kkeA])
            P.op("dve", lambda e: e.tensor_tensor(out=v3(keB), in0=v3(pkt), in1=bc_j(sB), op=ALU.mult),
                 r=[pktk, SK, kL], w=[kkeB])
            pvt, pvtk = rot.get()
            for h in range(4):
                P.op("pe", lambda e, h=h: e.transpose(out=pvt[:, h * 128:(h + 1) * 128], in_=qkvc[:, 8 + h, cs],
                                                      identity=C["ident"]), r=[qk(8 + h)], w=[pvtk])
            P.op("dve", lambda e: e.tensor_tensor(out=v3(bv), in0=v3(pvt), in1=bc_j(beta), op=ALU.mult),
                 r=[pvtk, SK], w=[kbv])
            Pc, PTc, kPc, kPTc = U, L, kU, kL
            Pn, PTn, kPn, kPTn = Pb, PTb, kPb, kPTb
            Xc, Xn, kXc, kXn = Xa, Xb, kXa, kXb
            for k in range(1, 6):
                if k < 5:
                    pp_, ppk = rot.get()
                    for h in range(4):
                        hs = slice(h * 128, (h + 1) * 128)
                        P.op("pe", lambda e, hs=hs, PTc=PTc, Pc=Pc, pp_=pp_: e.matmul(
                            pp_[:, hs], lhsT=PTc[:, hs], rhs=Pc[:, hs], start=True, stop=True),
                            r=[kPc, kPTc], w=[ppk])
                ppt, pptk = rot.get()
                for h in range(4):
                    hs = slice(h * 128, (h + 1) * 128)
                    P.op("pe", lambda e, hs=hs, PTc=PTc, Pc=Pc, ppt=ppt: e.matmul(
                        ppt[:, hs], lhsT=Pc[:, hs], rhs=PTc[:, hs], start=True, stop=True),
                        r=[kPc, kPTc], w=[pptk])
                if k < 5:
                    P.op("act", lambda e, Pn=Pn, pp_=pp_: e.activation(out=Pn, in_=pp_, func=AF.Copy), r=[ppk], w=[kPn])
                P.op("dve", lambda e, PTn=PTn, ppt=ppt: e.tensor_copy(out=PTn, in_=ppt), r=[pptk], w=[kPTn])
                px, pxk = rot.get()
                for h in range(4):
                    hs = slice(h * 128, (h + 1) * 128)
                    P.op("pe", lambda e, hs=hs, PTn=PTn, Xc=Xc, px=px: e.matmul(
                        px[:, hs], lhsT=PTn[:, hs], rhs=Xc[:, hs], start=True, stop=True),
                        r=[kPTn, kXc], w=[pxk])
                P.op("dve", lambda e, Xn=Xn, Xc=Xc, px=px: e.tensor_tensor(out=Xn, in0=px, in1=Xc, op=ALU.add),
                     r=[pxk, kXc], w=[kXn])
                Pc, Pn, kPc, kPn = Pn, Pc, kPn, kPc
                PTc, PTn, kPTc, kPTn = PTn, PTc, kPTn, kPTc
                Xc, Xn, kXc, kXn = Xn, Xc, kXn, kXc
            TTm, kTT = Xc, kXc
            assert TTm is Xb
            pu_, puk = rot.get()
            pw_, pwk = rot.get()
            for h in range(4):
                hs = slice(h * 128, (h + 1) * 128)
                P.op("pe", lambda e, hs=hs: e.matmul(pu_[:, hs], lhsT=TTm[:, hs], rhs=bv[:, hs], start=True, stop=True),
                     r=[kTT, kbv], w=[puk])
            for h in range(4):
                hs = slice(h * 128, (h + 1) * 128)
                P.op("pe", lambda e, hs=hs: e.matmul(pw_[:, hs], lhsT=kbg[:, hs], rhs=TTm[:, hs], start=True, stop=True),
                     r=[kTT, kkbg], w=[pwk])
            P.op("act", lambda e: e.activation(out=u, in_=pu_, func=AF.Copy), r=[puk], w=[ku])
            P.op("act", lambda e: e.activation(out=wT, in_=pw_, func=AF.Copy), r=[pwk], w=[kwT])
            P.op("dve", lambda e: e.tensor_tensor(out=v3(qdT), in0=qkvc[:, 0:4, cs], in1=v3(egr), op=ALU.mult),
                 r=[qk(h) for h in range(4)] + [kegr], w=[kqdT])
            po, pok = rot.get()
            others = [it_ for it_ in rotB.items if it_[1] != pok]
            oi = 0
            for ch in range(2):
                rows = slice(ch * 64, ch * 64 + 64)
                keX, kkeX = (keA, kkeA) if ch == 0 else (keB, kkeB)
                pws, pwsk = others[oi % 3]
                oi += 1
                for h in range(4):
                    hs = slice(h * 128, (h + 1) * 128)
                    P.op("pe", lambda e, hs=hs, h=h, pws=pws: e.matmul(pws[:, hs], lhsT=wT[:, hs], rhs=S[:, h, :],
                                                                      start=True, stop=True),
                         r=[kwT, f"S{h}"], w=[pwsk])
                P.op("dve", lambda e, rows=rows, pws=pws: e.tensor_tensor(out=delta[rows, :], in0=u[rows, :],
                                                                          in1=pws[rows, :], op=ALU.subtract),
                     r=[ku, pwsk], w=[kdelta])
                for h in range(4):
                    hs = slice(h * 128, (h + 1) * 128)
                    oc_ = slice(h * 128 + ch * 64, h * 128 + ch * 64 + 64)
                    P.op("pe", lambda e, h=h, oc_=oc_: e.matmul(po[:, oc_], lhsT=S[:, h, :], rhs=qdT[:, oc_],
                                                                start=True, stop=False),
                         r=[f"S{h}", kqdT], w=[pok])
                    P.op("pe", lambda e, hs=hs, oc_=oc_: e.matmul(po[:, oc_], lhsT=delta[:, hs], rhs=QKm[:, oc_],
                                                                  start=False, stop=True),
                         r=[kdelta, kQKm], w=[pok])
                pss, pssk = others[oi % 3]
                oi += 1
                for h in range(4):
                    hs = slice(h * 128, (h + 1) * 128)
                    P.op("pe", lambda e, hs=hs, keX=keX, pss=pss: e.matmul(pss[:, hs], lhsT=keX[:, hs], rhs=delta[:, hs],
                                                                          start=True, stop=True),
                         r=[kkeX, kdelta], w=[pssk])
                for h in range(4):
                    hs = slice(h * 128, (h + 1) * 128)
                    dcol = h * 128 + ch * 64 + 63
                    P.op("dve", lambda e, h=h, hs=hs, dcol=dcol, pss=pss: e.scalar_tensor_tensor(
                        out=S[:, h, :], in0=S[:, h, :], scalar=egr[:, dcol:dcol + 1], in1=pss[:, hs],
                        op0=ALU.mult, op1=ALU.add), r=[f"S{h}", kegr, pssk], w=[f"S{h}"])
            osb, kosb = bv, kbv
            sq2, ksq2 = R1, kR1
            P.op("act", lambda e: e.activation(out=osb, in_=po, func=AF.Copy), r=[pok], w=[kosb])
            P.op("act", lambda e: e.activation(out=sq2, in_=po, func=AF.Square), r=[pok], w=[ksq2])
            pm, pmk = others[oi % 3]
            P.op("pe", lambda e: e.matmul(pm, lhsT=C["omean"], rhs=sq2, start=True, stop=True), r=[ksq2], w=[pmk])
            P.op("act", lambda e: e.activation(out=sq2, in_=pm, func=AF.Ln, bias=EPS_RMS), r=[pmk], w=[ksq2])
            P.op("act", lambda e: e.activation(out=sq2, in_=sq2, func=AF.Exp, scale=-0.5), r=[ksq2], w=[ksq2])
            P.op("dve", lambda e: e.scalar_tensor_tensor(out=osb, in0=osb, scalar=dnw[:, 0:1], in1=sq2,
                                                         op0=ALU.mult, op1=ALU.mult), r=[kosb, ksq2], w=[kosb])
            P.op("pool", lambda e: e.tensor_tensor(out=mixT[:, 4:8, cs], in0=v3(osb), in1=zs[:, :, cs], op=ALU.mult),
                 r=[kosb] + [f"zs{pp}_{j}" for j in range(4)], w=[f"mixT{mp}_{4 + j}" for j in range(4)])

        def ln1_sub(ti, s_, b):
            mp = ti % 3
            mixT = mixT2[mp]
            rot = rotA
            cs = slice(s_ * 128, (s_ + 1) * 128)
            tok0 = ti * TT + s_ * 128
            P.dma("sp", lambda e: e.dma_start(out=xh, in_=x[tok0:tok0 + 128, :]), w=["xh"])
            pm2, (k0, k1) = rot2A.get()
            for half in range(2):
                for kc in range(8):
                    P.op("pe", lambda e, half=half, kc=kc: e.matmul(
                        pm2[:, half * 512:(half + 1) * 512], lhsT=mixT[:, kc, cs],
                        rhs=Wout[:, kc, half * 512:(half + 1) * 512], start=(kc == 0), stop=(kc == 7)),
                        r=[f"mixT{mp}_{kc}", "Wout"], w=[(k0, k1)[half]])
            P.op("dve", lambda e: e.tensor_tensor(out=r0, in0=pm2, in1=gate_bc[2][:, b, :], op=ALU.mult),
                 r=[k0, k1], w=["r0"])
            P.op("dve", lambda e: e.scalar_tensor_tensor(out=r0, in0=xh, scalar=ALPHA, in1=r0,
                                                         op0=ALU.mult, op1=ALU.add), r=["xh", "r0"], w=["r0"])
            for hf in range(2):
                P.op("dve", lambda e, hf=hf: e.bn_stats(out=bst[:, hf, :], in_=r0[:, hf * 512:(hf + 1) * 512]),
                     r=["r0"], w=["bst"])
            P.op("dve", lambda e: e.bn_aggr(out=mv[:, 0:2], in_=bst.rearrange("p a b -> p (a b)")), r=["bst"], w=["mv"])
            P.op("act", lambda e: e.activation(out=mv[:, 2:3], in_=mv[:, 1:2], func=AF.Ln, bias=1e-5),
                 r=["mv"], w=["mv2"])
            P.op("act", lambda e: e.activation(out=mv[:, 3:4], in_=mv[:, 2:3], func=AF.Exp, scale=-0.5),
                 r=["mv2"], w=["mv3"])
            P.op("dve", lambda e: e.tensor_scalar(out=xh, in0=r0, scalar1=mv[:, 0:1], scalar2=mv[:, 3:4],
                                                  op0=ALU.subtract, op1=ALU.mult), r=["r0", "mv", "mv3"], w=["xh"])
            P.op("pool", lambda e: e.tensor_tensor(out=r0, in0=xh, in1=ag, op=ALU.mult), r=["xh"], w=["r0"])
            P.op("pool", lambda e: e.tensor_tensor(out=r0, in0=r0, in1=ab, op=ALU.add), r=["r0"], w=["r0"])
            P.dma("sp", lambda e: e.dma_start(out=r_s[tok0:tok0 + 128, :], in_=r0), r=["r0"], w=["rs"])
            for kc in range(8):
                pt, pk = rot.get()
                P.op("pe", lambda e, pt=pt, kc=kc: e.transpose(out=pt[:, 0:128], in_=xh[:, kc * 128:(kc + 1) * 128],
                                                               identity=C["ident"]), r=["xh"], w=[pk])
                P.op("act", lambda e, pt=pt, kc=kc: e.activation(
                    out=h2t[:, kc, cs], in_=pt[:, 0:128], func=AF.Identity,
                    scale=A2[:, kc, b:b + 1], bias=B2[:, kc, b:b + 1]), r=[pk], w=["h2t"])

        def capture(fn, *a):
            old = P.ops
            P.ops = []
            fn(*a)
            got = P.ops
            P.ops = old
            return got

        def merge(a, b_):
            out_, i, j = [], 0, 0
            na, nb = max(len(a), 1), max(len(b_), 1)
            while i < len(a) or j < len(b_):
                if j >= len(b_) or (i < len(a) and i * nb <= j * na):
                    out_.append(a[i]); i += 1
                else:
                    out_.append(b_[j]); j += 1
            return out_

        ntiles = NT if stage != "pa1" else 2

        def threadA(ti):
            if ti + 1 < ntiles:
                stage1(ti + 1)
            if ti - 1 >= 0:
                stage3(ti - 1)

        P.ops += capture(stage1, 0)
        for ti in range(ntiles):
            a = capture(threadA, ti)
            b_ = capture(stage2, ti)
            P.ops += merge(a, b_)
        P.ops += capture(stage3, ntiles - 1)
        if stage == "pa1":
            P.dma("pool", lambda e: e.dma_start(out=dbg_out["mixT"].rearrange("p (a b) -> p a b", a=8), in_=mixT2[1]),
                  r=[f"mixT1_{j}" for j in range(8)], w=["dbg1"])
            P.dma("sp", lambda e: e.dma_start(out=dbg_out["r"], in_=r_s[0:512, :]), r=["rs"], w=["dbg2"])
            P.dma("pool", lambda e: e.dma_start(out=dbg_out["h2"].rearrange("p (a b) -> p a b", a=8),
                                                in_=h2_s[:, :, 0:512]), r=["h2s"], w=["dbg3"])
            P.dma("sp", lambda e: e.dma_start(out=dbg_out["qkvc"].rearrange("p (a b) -> p a b", a=12), in_=qkvc2[1]),
                  r=[f"qk1_{j}" for j in range(12)], w=["dbg4"])
        cnt = P.emit()
        print("phaseA op counts", cnt)

    if stage in ("pa", "pa1"):
        return nc
    I32 = mybir.dt.int32
    with contextlib.ExitStack() as es:
        def tsb(name, shape, dt=F32):
            return es.enter_context(nc.sbuf_tensor("b_" + name, list(shape), dt)).ap()

        P = Prog(nc, "pb")
        NSTT = TOK // 128
        NW = 3
        cb = tsb("cb", list(CB.shape))
        thr = cb[:, 0:64]
        SLc = cb[:, 64:64 + 1024]
        blkio = cb[:, 1088:1088 + NBLK]
        iotaP = cb[:, 1088 + NBLK:1089 + NBLK]
        SUf = cb[:, 1089 + NBLK:1089 + NBLK + 128]
        SUb = tsb("sub", [128, 128], BF16)
        onesb = tsb("onesb", [128, 128], BF16)
        Wr = tsb("wr", [128, 8, 36], BF16)
        b36 = tsb("b36", [128, 36])
        g2 = tsb("ln2g", [128, D])
        b2 = tsb("ln2b", [128, D])
        gate2 = tsb("gate2", [128, 2, D])
        h2c = [tsb(f"h2c{i}", [128, 8, 512], BF16) for i in range(2)]
        OHall = tsb("ohall", [128, NSTT, 32], BF16)
        oh1all = tsb("oh1all", [128, NSTT, 32])
        oh2all = tsb("oh2all", [128, NSTT, 32])
        gAB = tsb("gab", [128, NSTT, 2])
        POSf = tsb("posf", [128, NSTT, 2])
        POSi = tsb("posi", [128, NSTT, 2], I32)
        cnt = tsb("cnt", [128, 32])
        nblk = tsb("nblk", [128, 32])
        pstb = tsb("pstb", [128, 32])
        pend = tsb("pend", [128, 32])
        base = tsb("base", [128, 32])
        big = tsb("big", [128, NBLK * 32])
        bexp = tsb("bexp", [128, NBLK])
        bskip = tsb("bskip", [128, NBLK])
        bsame = tsb("bsame", [128, NBLK])
        IDXW = tsb("idxw", [128, NBLK], I32)
        posv = tsb("posv", [128, 32])
        ptmp = tsb("ptmp", [128, 32])
        lg = tsb("lg", [128, 36])
        lgm = tsb("lgm", [128, 32])
        lgm2 = tsb("lgm2", [128, 32])
        rs_ = tsb("rsm", [128, 32])
        Wg = [tsb(f"wg{i}", [128, 2048], BF16) for i in range(NW)]
        Wu = [tsb(f"wu{i}", [128, 2048], BF16) for i in range(NW)]
        Wd = [tsb(f"wd{i}", [128, 2048], BF16) for i in range(NW)]
        xb = [tsb(f"xb{i}", [128, D]) for i in range(3)]
        sh2r = tsb("sh2r", [128, 2, D])
        sc2r = tsb("sc2r", [128, 2, D])
        xT = [tsb(f"xT{i}", [128, 8, 128], BF16) for i in range(3)]
        sg = [tsb(f"sg{i}", [128, 256]) for i in range(3)]
        hid = [tsb(f"hid{i}", [128, 256]) for i in range(3)]
        hidT = [tsb(f"hidT{i}", [128, 256], BF16) for i in range(3)]
        yb = [tsb(f"yb{i}", [128, D]) for i in range(2)]
        y1 = [tsb(f"y1_{i}", [128, D]) for i in range(2)]
        y2 = [tsb(f"y2_{i}", [128, D]) for i in range(2)]
        rr = [tsb(f"rr{i}", [128, D]) for i in range(2)]
        xh2 = tsb("xh2", [128, D])
        ob = tsb("ob", [128, D])
        bst2 = tsb("bst2", [128, 2, 6])
        mv2 = tsb("mv2", [128, 4])
        rot = PsumRot(BANKS[0:2])
        rot2 = PsumRot([(PS2[i], (f"bank{2 * i}", f"bank{2 * i + 1}")) for i in (2, 3)])

        P.dma("sp", lambda e: e.dma_start(out=cb, in_=cb_d), w=["cb"])
        P.op("dve", lambda e: e.tensor_copy(out=SUb, in_=SUf), r=["cb"], w=["SUb"])
        P.op("dve", lambda e: e.memset(onesb, 1.0), w=["onesb"])
        for kc in range(8):
            P.dma("pool", lambda e, kc=kc: e.dma_start(out=Wr[:, kc, 0:4], in_=w_grp[kc * 128:(kc + 1) * 128, :]),
                  w=["Wr"])
            P.dma("pool", lambda e, kc=kc: e.dma_start(out=Wr[:, kc, 4:36], in_=w_exp[kc * 128:(kc + 1) * 128, :]),
                  w=["Wr"])
        P.dma("sp", lambda e: e.dma_start(out=b36[:, 0:4], in_=b_grp[0, :].partition_broadcast(128)), w=["b36"])
        P.dma("sp", lambda e: e.dma_start(out=b36[:, 4:36], in_=b_exp[0, :].partition_broadcast(128)), w=["b36"])
        P.dma("sp", lambda e: e.dma_start(out=gate2.rearrange("p a b -> p (a b)"), in_=g2_s), w=["gate2"])
        P.dma("sp", lambda e: e.dma_start(out=sh2r.rearrange("p a b -> p (a b)"), in_=sh2_s), w=["sh2r"])
        P.dma("sp", lambda e: e.dma_start(out=sc2r.rearrange("p a b -> p (a b)"), in_=sc2_s), w=["sc2r"])
        P.op("pool", lambda e: e.tensor_scalar(out=sc2r, in0=sc2r, scalar1=1.0, scalar2=1.0 / ALPHA, op0=ALU.add,
                                               op1=ALU.mult), r=["sc2r"], w=["sc2r"])
        P.dma("sp", lambda e: e.dma_start(out=g2, in_=ln2_g[0, :].partition_broadcast(128)), w=["g2"])
        P.dma("sp", lambda e: e.dma_start(out=b2, in_=ln2_b[0, :].partition_broadcast(128)), w=["b2"])

        RG = 4
        lg4 = tsb("lg4", [128, RG, 36])
        lgm4 = tsb("lgm4", [128, RG, 32])
        lgm24 = tsb("lgm24", [128, RG, 32])
        eg4 = tsb("eg4", [128, RG, 4])
        ohg4 = tsb("ohg4", [128, RG, 4])
        rsc = tsb("rsc", [128, 12, RG])

        def router4(g_):
            st0 = g_ * RG
            ci = g_ % 2
            P.dma("sp", lambda e: e.dma_start(out=h2c[ci], in_=h2_s[:, :, st0 * 128:st0 * 128 + 512]), w=[f"h2c{ci}"])
            plg, plgk = rot.get()
            for i in range(RG):
                cs = slice(i * 128, (i + 1) * 128)
                for kc in range(8):
                    P.op("pe", lambda e, kc=kc, i=i, cs=cs: e.matmul(plg[:, i * 36:(i + 1) * 36], lhsT=h2c[ci][:, kc, cs],
                                                                      rhs=Wr[:, kc, :], start=(kc == 0), stop=(kc == 7)),
                         r=[f"h2c{ci}", "Wr"], w=[plgk])
            gmax, sume, grpw, m1, m2, d21, p2, den, rden = [rsc[:, i, :] for i in range(9)]
            sts = range(st0, st0 + RG)
            K1 = [f"oh1_{st}" for st in sts]
            K2 = [f"oh2_{st}" for st in sts]
            KA = [f"gA{st}" for st in sts]
            KB = [f"gB{st}" for st in sts]
            KO = [f"OH{st}" for st in sts]
            oh1 = oh1all[:, st0:st0 + RG, :]
            oh2 = oh2all[:, st0:st0 + RG, :]
            gA = gAB[:, st0:st0 + RG, 0]
            gB = gAB[:, st0:st0 + RG, 1]
            bcx = lambda ap, n: ap.unsqueeze(2).to_broadcast([128, RG, n])
            P.op("dve", lambda e: e.tensor_tensor(out=lg4, in0=plg[:, 0:RG * 36].rearrange("p (s n) -> p s n", s=RG),
                                                  in1=b36.unsqueeze(1).to_broadcast([128, RG, 36]), op=ALU.add),
                 r=[plgk, "b36"], w=["lg4"])
            P.op("dve", lambda e: e.tensor_reduce(out=gmax, in_=lg4[:, :, 0:4], axis=AX.X, op=ALU.max), r=["lg4"], w=["q_gmax"])
            P.op("dve", lambda e: e.tensor_tensor(out=eg4, in0=lg4[:, :, 0:4], in1=bcx(gmax, 4), op=ALU.subtract),
                 r=["lg4", "q_gmax"], w=["eg4"])
            P.op("act", lambda e: e.activation(out=eg4, in_=eg4, func=AF.Exp), r=["eg4"], w=["eg4"])
            P.op("dve", lambda e: e.tensor_reduce(out=sume, in_=eg4, axis=AX.X, op=ALU.add), r=["eg4"], w=["q_sume"])
            P.op("dve", lambda e: e.reciprocal(out=grpw, in_=sume), r=["q_sume"], w=["q_grpw"])
            P.op("dve", lambda e: e.tensor_tensor(out=ohg4, in0=lg4[:, :, 0:4], in1=bcx(gmax, 4), op=ALU.is_equal),
                 r=["lg4", "q_gmax"], w=["ohg4"])
            P.op("dve", lambda e: e.tensor_scalar(out=ohg4, in0=ohg4, scalar1=-1.0, scalar2=BIG, op0=ALU.add, op1=ALU.mult),
                 r=["ohg4"], w=["ohg4"])
            P.op("dve", lambda e: e.tensor_tensor(out=lgm4.rearrange("p s (g k) -> p s g k", g=4),
                                                  in0=lg4[:, :, 4:36].rearrange("p s (g k) -> p s g k", g=4),
                                                  in1=ohg4.unsqueeze(3).to_broadcast([128, RG, 4, 8]), op=ALU.add),
                 r=["lg4", "ohg4"], w=["lgm4"])
            P.op("dve", lambda e: e.tensor_reduce(out=m1, in_=lgm4, axis=AX.X, op=ALU.max), r=["lgm4"], w=["q_m1"])
            P.op("dve", lambda e: e.tensor_tensor(out=oh1, in0=lgm4, in1=bcx(m1, 32), op=ALU.is_equal),
                 r=["lgm4", "q_m1"], w=K1)
            P.op("dve", lambda e: e.scalar_tensor_tensor(out=lgm24, in0=oh1, scalar=-BIG, in1=lgm4, op0=ALU.mult,
                                                         op1=ALU.add), r=K1 + ["lgm4"], w=["lgm24"])
            P.op("dve", lambda e: e.tensor_reduce(out=m2, in_=lgm24, axis=AX.X, op=ALU.max), r=["lgm24"], w=["q_m2"])
            P.op("dve", lambda e: e.tensor_tensor(out=oh2, in0=lgm24, in1=bcx(m2, 32), op=ALU.is_equal),
                 r=["lgm24", "q_m2"], w=K2)
            P.op("dve", lambda e: e.tensor_tensor(out=d21, in0=m2, in1=m1, op=ALU.subtract), r=["q_m1", "q_m2"], w=["q_d21"])
            P.op("act", lambda e: e.activation(out=p2, in_=d21, func=AF.Exp), r=["q_d21"], w=["q_p2"])
            P.op("dve", lambda e: e.tensor_scalar(out=den, in0=p2, scalar1=1.0, scalar2=None, op0=ALU.add),
                 r=["q_p2"], w=["q_den"])
            P.op("dve", lambda e: e.reciprocal(out=rden, in_=den), r=["q_den"], w=["q_rden"])
            P.op("dve", lambda e: e.tensor_tensor(out=gA, in0=grpw, in1=rden, op=ALU.mult), r=["q_grpw", "q_rden"], w=KA)
            P.op("dve", lambda e: e.tensor_tensor(out=gB, in0=gA, in1=p2, op=ALU.mult), r=KA + ["q_p2"], w=KB)
            P.op("pool", lambda e: e.tensor_tensor(out=OHall[:, st0:st0 + RG, :], in0=oh1, in1=oh2, op=ALU.add),
                 r=K1 + K2, w=KO)

        for g_ in range(NSTT // RG):
            router4(g_)

        pcnt, pcntk = rot.get()
        for st in range(NSTT):
            P.op("pe", lambda e, st=st: e.matmul(pcnt[:, 0:32], lhsT=onesb, rhs=OHall[:, st, :],
                                                 start=(st == 0), stop=(st == NSTT - 1)),
                 r=[f"OH{st}", "onesb"], w=[pcntk])
        P.op("act", lambda e: e.activation(out=cnt, in_=pcnt[:, 0:32], func=AF.Copy), r=[pcntk], w=["cnt"])
        big3 = big[:, 0:32 * 64].rearrange("p (e k) -> p e k", e=32)
        P.op("dve", lambda e: e.tensor_tensor(out=big3, in0=cnt.unsqueeze(2).to_broadcast([128, 32, 64]),
                                              in1=thr.unsqueeze(1).to_broadcast([128, 32, 64]), op=ALU.is_gt),
             r=["cnt", "cb"], w=["big"])
        P.op("dve", lambda e: e.tensor_reduce(out=nblk, in_=big3, axis=AX.X, op=ALU.add), r=["big"], w=["nblk"])
        big3b = big[:, 0:1024].rearrange("p (e f) -> p e f", e=32)
        P.op("dve", lambda e: e.tensor_tensor(out=big3b, in0=nblk.unsqueeze(1).to_broadcast([128, 32, 32]),
                                              in1=SLc.rearrange("p (e f) -> p e f", e=32), op=ALU.mult),
             r=["nblk", "cb", "big"], w=["big"])
        P.op("dve", lambda e: e.tensor_reduce(out=pstb, in_=big3b, axis=AX.X, op=ALU.add), r=["big"], w=["pstb"])
        P.op("dve", lambda e: e.tensor_tensor(out=pend, in0=pstb, in1=nblk, op=ALU.add), r=["pstb", "nblk"], w=["pend"])
        P.op("dve", lambda e: e.tensor_scalar(out=base, in0=pstb, scalar1=128.0, scalar2=None, op0=ALU.mult),
             r=["pstb"], w=["base"])
        big3c = big.rearrange("p (b e) -> p b e", b=NBLK)
        P.op("dve", lambda e: e.tensor_tensor(out=big3c, in0=pend.unsqueeze(1).to_broadcast([128, NBLK, 32]),
                                              in1=blkio.unsqueeze(2).to_broadcast([128, NBLK, 32]), op=ALU.is_le),
             r=["pend", "cb", "big", "pstb"], w=["big"])
        P.op("dve", lambda e: e.tensor_reduce(out=bexp, in_=big3c, axis=AX.X, op=ALU.add), r=["big"], w=["bexp"])
        P.op("dve", lambda e: e.tensor_scalar(out=bskip, in0=bexp, scalar1=float(NEXP) - 0.5, scalar2=None, op0=ALU.is_ge),
             r=["bexp"], w=["bskip"])
        P.op("dve", lambda e: e.tensor_tensor(out=bsame[:, NW:NBLK], in0=bexp[:, NW:NBLK], in1=bexp[:, 0:NBLK - NW],
                                              op=ALU.is_equal), r=["bexp"], w=["bsame"])
        P.op("dve", lambda e: e.tensor_tensor(out=bskip[:, NW:NBLK], in0=bskip[:, NW:NBLK], in1=bsame[:, NW:NBLK],
                                              op=ALU.max), r=["bskip", "bsame"], w=["bskip"])
        P.op("dve", lambda e: e.tensor_scalar(out=bexp, in0=bexp, scalar1=float(NEXP - 1), scalar2=128.0,
                                              op0=ALU.min, op1=ALU.mult), r=["bexp"], w=["bexp"])
        P.op("dve", lambda e: e.tensor_scalar(out=bexp, in0=bexp, scalar1=iotaP, scalar2=None, op0=ALU.add),
             r=["bexp", "cb"], w=["bexp"])
        P.op("dve", lambda e: e.scalar_tensor_tensor(out=bexp, in0=bskip, scalar=1.0e6, in1=bexp, op0=ALU.mult,
                                                     op1=ALU.add), r=["bexp", "bskip"], w=["bexp"])
        P.op("dve", lambda e: e.tensor_copy(out=IDXW, in_=bexp), r=["bexp"], w=["IDXW"])
        for st in range(NSTT):
            prk, prkk = rot.get()
            P.op("pe", lambda e, st=st, prk=prk: e.matmul(prk[:, 0:32], lhsT=SUb, rhs=OHall[:, st, :],
                                                          start=True, stop=(st == 0)), r=[f"OH{st}", "SUb"], w=[prkk])
            for s2 in range(st):
                P.op("pe", lambda e, s2=s2, st=st, prk=prk: e.matmul(prk[:, 0:32], lhsT=onesb, rhs=OHall[:, s2, :],
                                                                     start=False, stop=(s2 == st - 1)),
                     r=[f"OH{s2}", "onesb"], w=[prkk])
            P.op("dve", lambda e, prk=prk: e.tensor_tensor(out=posv, in0=prk[:, 0:32], in1=base, op=ALU.add),
                 r=[prkk, "base"], w=["posv"])
            for k, oha in ((0, oh1all), (1, oh2all)):
                P.op("dve", lambda e, st=st, oha=oha: e.tensor_tensor(out=ptmp, in0=posv, in1=oha[:, st, :], op=ALU.mult),
                     r=["posv", f"oh1_{st}", f"oh2_{st}"], w=["ptmp"])
                P.op("dve", lambda e, st=st, k=k: e.tensor_reduce(out=POSf[:, st, k:k + 1], in_=ptmp, axis=AX.X, op=ALU.add),
                     r=["ptmp"], w=["POSf"])
        P.op("dve", lambda e: e.tensor_copy(out=POSi, in_=POSf), r=["POSf"], w=["POSi"])

        if stage == "pbdbg":
            P.dma("sp", lambda e: e.dma_start(out=dbg_out["cnt"], in_=cnt), r=["cnt"], w=["dg1"])
            P.dma("sp", lambda e: e.dma_start(out=dbg_out["nblk"], in_=nblk), r=["nblk"], w=["dg2"])
            P.dma("sp", lambda e: e.dma_start(out=dbg_out["pstb"], in_=pstb), r=["pstb"], w=["dg3"])
            P.dma("sp", lambda e: e.dma_start(out=dbg_out["bexp"], in_=bexp), r=["bexp"], w=["dg4"])
            P.dma("sp", lambda e: e.dma_start(out=dbg_out["posf"], in_=POSf.rearrange("p a b -> p (a b)")), r=["POSf"], w=["dg5"])
            P.dma("sp", lambda e: e.dma_start(out=dbg_out["oh1"], in_=oh1all.rearrange("p a b -> p (a b)")),
                  r=[f"oh1_{st}" for st in range(NSTT)], w=["dg6"])
            P.dma("sp", lambda e: e.dma_start(out=dbg_out["oh2"], in_=oh2all.rearrange("p a b -> p (a b)")),
                  r=[f"oh2_{st}" for st in range(NSTT)], w=["dg7"])
            P.dma("sp", lambda e: e.dma_start(out=dbg_out["gab"], in_=gAB.rearrange("p a b -> p (a b)")),
                  r=[f"gA{st}" for st in range(NSTT)] + [f"gB{st}" for st in range(NSTT)], w=["dg8"])
            P.emit()
            return nc
        for st in range(NSTT):
            b = st // (NSTT // NB)
            xq = xb[st % 3]
            xk = f"xb{st % 3}"
            P.dma("sp", lambda e, st=st, xq=xq: e.dma_start(out=xq, in_=r_s[st * 128:(st + 1) * 128, :]), w=[xk])
            P.op("dve", lambda e, xq=xq, b=b: e.tensor_tensor(out=xq, in0=xq, in1=sc2r[:, b, :], op=ALU.mult),
                 r=[xk, "sc2r"], w=[xk])
            P.op("dve", lambda e, xq=xq, b=b: e.tensor_tensor(out=xq, in0=xq, in1=sh2r[:, b, :], op=ALU.add),
                 r=[xk, "sh2r"], w=[xk])
            for k in range(2):
                P.dma("pool", lambda e, st=st, k=k, xq=xq: e.indirect_dma_start(
                    out=xrows_s, out_offset=bass.IndirectOffsetOnAxis(ap=POSi[:, st, k:k + 1], axis=0),
                    in_=xq, in_offset=None), r=[xk, "POSi"], w=[f"xrows{st}_{k}"])
        if stage in ("pbs1", "pbs2"):
            P.emit()
            return nc

        pgu_banks = PsumRot(BANKS[0:2])
        NQ = 3
        wgate_rows = w_gate.rearrange("e (p k) n -> (e p) (k n)", k=8)
        wup_rows = w_up.rearrange("e (p k) n -> (e p) (k n)", k=8)
        wdn_rows = w_down.rearrange("e (p k) n -> (e p) (k n)", k=2)

        def blk_loadx(bi):
            q = bi % NQ
            P.dma("sp", lambda e: e.dma_start(out=xb[q], in_=xrows_s[bi * 128:(bi + 1) * 128, :]),
                  r=[f"xrows{st}_{k}" for st in range(NSTT) for k in range(2)], w=[f"xb{q}"])

        bnd = {}

        def bnd_reg(e):
            if "r" not in bnd:
                bnd["r"] = e.alloc_register("wbound")
                e.reg_mov(bnd["r"], NEXP * 128 - 1)
            return bnd["r"]

        def blk_load(bi):
            slot = bi % NW
            ioff = bass.IndirectOffsetOnAxis(ap=IDXW[:, bi:bi + 1], axis=0)
            P.dma("pool", lambda e: e.indirect_dma_start(out=Wg[slot], out_offset=None,
                                                         in_=wgate_rows, in_offset=ioff, bounds_check=bnd_reg(e), oob_is_err=False), r=["IDXW"], w=[f"wg{slot}"])
            P.dma("pool", lambda e: e.indirect_dma_start(out=Wu[slot], out_offset=None,
                                                         in_=wup_rows, in_offset=ioff, bounds_check=bnd_reg(e), oob_is_err=False), r=["IDXW"], w=[f"wu{slot}"])
            P.dma("pool", lambda e: e.indirect_dma_start(out=Wd[slot], out_offset=None,
                                                         in_=wdn_rows, in_offset=ioff, bounds_check=bnd_reg(e), oob_is_err=False), r=["IDXW"], w=[f"wd{slot}"])

        xt_banks = [BANKS[2], BANKS[3]]
        ht_banks = PsumRot(BANKS[6:8])

        def blk_xt(bi):
            q = bi % NQ
            xv = xb[q].rearrange("r (p k) -> r k p", k=8)
            xTf = xT[q].rearrange("p a b -> p (a b)")
            for hb_ in range(2):
                pt, ptk = xt_banks[hb_]
                for k4 in range(4):
                    kc = hb_ * 4 + k4
                    P.op("pe", lambda e, kc=kc, k4=k4, pt=pt: e.transpose(out=pt[:, k4 * 128:(k4 + 1) * 128],
                                                                          in_=xv[:, kc, :], identity=C["ident"]),
                         r=[f"xb{q}"], w=[ptk])
                if hb_ == 0:
                    P.op("act", lambda e, pt=pt: e.activation(out=xTf[:, 0:512], in_=pt, func=AF.Copy),
                         r=[ptk], w=[f"xTa{q}"])
                else:
                    P.op("dve", lambda e, pt=pt: e.tensor_copy(out=xTf[:, 512:1024], in_=pt), r=[ptk], w=[f"xTb{q}"])

        def blk_gu(bi):
            slot = bi % NW
            q = bi % NQ
            pgu, pguk = pgu_banks.get()
            for (Wm, wk, c0) in ((Wg, f"wg{slot}", 0), (Wu, f"wu{slot}", 256)):
                for kc in range(8):
                    P.op("pe", lambda e, kc=kc, Wm=Wm, c0=c0: e.matmul(
                        pgu[:, c0:c0 + 256], lhsT=xT[q][:, kc, :], rhs=Wm[slot][:, kc * 256:(kc + 1) * 256],
                        start=(kc == 0), stop=(kc == 7)), r=[f"xTa{q}", f"xTb{q}", wk], w=[pguk])
            P.op("act", lambda e: e.activation(out=sg[q], in_=pgu[:, 0:256], func=AF.Exp, scale=-1.0), r=[pguk], w=[f"sg{q}"])
            P.op("act", lambda e: e.activation(out=sg[q], in_=sg[q], func=AF.Ln, bias=1.0), r=[f"sg{q}"], w=[f"sg{q}"])
            P.op("act", lambda e: e.activation(out=sg[q], in_=sg[q], func=AF.Exp, scale=-1.0), r=[f"sg{q}"], w=[f"sg{q}"])
            P.op("dve", lambda e: e.tensor_tensor(out=sg[q], in0=pgu[:, 0:256], in1=sg[q], op=ALU.mult),
                 r=[pguk, f"sg{q}"], w=[f"sg{q}"])
            P.op("dve", lambda e: e.tensor_tensor(out=hid[q], in0=pgu[:, 256:512], in1=sg[q], op=ALU.mult),
                 r=[pguk, f"sg{q}"], w=[f"hid{q}"])

        def blk_tr(bi):
            q = bi % NQ
            pht, phtk = ht_banks.get()
            hv = hid[q].rearrange("r (p k) -> r k p", k=2)
            for k2 in range(2):
                P.op("pe", lambda e, k2=k2: e.transpose(out=pht[:, k2 * 128:(k2 + 1) * 128], in_=hv[:, k2, :],
                                                        identity=C["ident"]), r=[f"hid{q}"], w=[phtk])
            P.op("act", lambda e: e.activation(out=hidT[q], in_=pht[:, 0:256], func=AF.Copy), r=[phtk], w=[f"hidT{q}"])

        def blk_dn(bi):
            slot = bi % NW
            q = bi % NQ
            yq = bi % 2
            py, (k0, k1) = PS2[2], ("bank4", "bank5")
            for half in range(2):
                for k2 in range(2):
                    P.op("pe", lambda e, half=half, k2=k2: e.matmul(
                        py[:, half * 512:(half + 1) * 512], lhsT=hidT[q][:, k2 * 128:(k2 + 1) * 128],
                        rhs=Wd[slot][:, k2 * 1024 + half * 512:k2 * 1024 + (half + 1) * 512], start=(k2 == 0), stop=(k2 == 1)),
                        r=[f"hidT{q}", f"wd{slot}"], w=[(k0, k1)[half]])
            P.op("act", lambda e: e.activation(out=yb[yq][:, 0:512], in_=py[:, 0:512], func=AF.Copy), r=[k0], w=[f"yba{yq}"])
            P.op("dve", lambda e: e.tensor_copy(out=yb[yq][:, 512:1024], in_=py[:, 512:1024]), r=[k1], w=[f"ybb{yq}"])
            P.dma("sp", lambda e: e.dma_start(out=yrows_s[bi * 128:(bi + 1) * 128, :], in_=yb[yq]),
                  r=[f"yba{yq}", f"ybb{yq}"], w=[f"yrows{bi}"])

        nb_run = NBLK if stage != 'pbs3' else 6
        pendq = []
        blk_load(0)
        for b0_ in range(min(2, nb_run)):
            blk_loadx(b0_)
        for bi in range(nb_run):
            if bi + 2 < nb_run:
                blk_loadx(bi + 2)
            blk_xt(bi)
            blk_gu(bi)
            pendq.append(bi)
            if len(pendq) >= 2:
                blk_tr(pendq[-2])
            if len(pendq) >= 3:
                blk_dn(pendq.pop(0))
            if bi + 1 < nb_run:
                blk_load(bi + 1)
        if len(pendq) == 2:
            blk_dn(pendq.pop(0))
        while pendq:
            a = pendq.pop(0)
            blk_tr(a)
            blk_dn(a)
        YK = [f"yrows{bi}" for bi in range(nb_run)]
        if stage == "pbs3":
            P.emit()
            return nc

        def comb_fetch(st):
            q = st % 2
            P.dma("sp", lambda e: e.dma_start(out=rr[q], in_=r_s[st * 128:(st + 1) * 128, :]), w=[f"rr{q}"])
            P.dma("pool", lambda e: e.indirect_dma_start(
                out=y1[q], out_offset=None, in_=yrows_s,
                in_offset=bass.IndirectOffsetOnAxis(ap=POSi[:, st, 0:1], axis=0)), r=YK + ["POSi"], w=[f"y1_{q}"])
            P.dma("pool", lambda e: e.indirect_dma_start(
                out=y2[q], out_offset=None, in_=yrows_s,
                in_offset=bass.IndirectOffsetOnAxis(ap=POSi[:, st, 1:2], axis=0)), r=YK + ["POSi"], w=[f"y2_{q}"])

        def comb_compute(st):
            q = st % 2
            b = st // (NSTT // NB)
            P.op("act", lambda e: e.activation(out=y1[q], in_=y1[q], func=AF.Copy, scale=gAB[:, st, 0:1]),
                 r=[f"y1_{q}", f"gA{st}"], w=[f"y1_{q}"])
            P.op("dve", lambda e: e.scalar_tensor_tensor(out=y1[q], in0=y2[q], scalar=gAB[:, st, 1:2], in1=y1[q],
                                                         op0=ALU.mult, op1=ALU.add),
                 r=[f"y1_{q}", f"y2_{q}", f"gB{st}"], w=[f"y1_{q}"])
            P.op("dve", lambda e: e.tensor_tensor(out=y1[q], in0=y1[q], in1=gate2[:, b, :], op=ALU.mult),
                 r=[f"y1_{q}", "gate2"], w=[f"y1_{q}"])
            P.op("dve", lambda e: e.tensor_tensor(out=rr[q], in0=rr[q], in1=y1[q], op=ALU.add),
                 r=[f"rr{q}", f"y1_{q}"], w=[f"rr{q}"])
            for hf in range(2):
                P.op("dve", lambda e, hf=hf: e.bn_stats(out=bst2[:, hf, :], in_=rr[q][:, hf * 512:(hf + 1) * 512]),
                     r=[f"rr{q}"], w=["bst2"])
            P.op("dve", lambda e: e.bn_aggr(out=mv2[:, 0:2], in_=bst2.rearrange("p a b -> p (a b)")), r=["bst2"], w=["mv2"])
            P.op("act", lambda e: e.activation(out=mv2[:, 2:3], in_=mv2[:, 1:2], func=AF.Ln, bias=1e-5),
                 r=["mv2"], w=["mv2b"])
            P.op("act", lambda e: e.activation(out=mv2[:, 3:4], in_=mv2[:, 2:3], func=AF.Exp, scale=-0.5),
                 r=["mv2b"], w=["mv2c"])
            P.op("dve", lambda e: e.tensor_scalar(out=xh2, in0=rr[q], scalar1=mv2[:, 0:1], scalar2=mv2[:, 3:4],
                                                  op0=ALU.subtract, op1=ALU.mult), r=[f"rr{q}", "mv2", "mv2c"], w=["xh2"])
            oq = ob2[q]
            P.op("pool", lambda e: e.tensor_tensor(out=oq, in0=xh2, in1=g2, op=ALU.mult), r=["xh2", "g2"], w=[f"ob{q}"])
            P.op("pool", lambda e: e.tensor_tensor(out=oq, in0=oq, in1=b2, op=ALU.add), r=[f"ob{q}", "b2"], w=[f"ob{q}"])
            P.dma("sp", lambda e: e.dma_start(out=out[st * 128:(st + 1) * 128, :], in_=oq), r=[f"ob{q}"], w=["outd"])

        ob2 = [ob, yb[0]]
        comb_fetch(0)
        for st in range(NSTT):
            if st + 1 < NSTT:
                comb_fetch(st + 1)
            comb_compute(st)
        cnt_ = P.emit()
        print("phaseB op counts", cnt_)

    return nc


_NC_CACHE = {}


def _get_nc():
    if "nc" not in _NC_CACHE:
        _NC_CACHE["nc"] = build("full")
    return _NC_CACHE["nc"]


def make_in_maps(inputs):
    f = lambda a: np.ascontiguousarray(np.asarray(a, dtype=np.float32))
    shared = {
        "w_ada": f(inputs["w_ada"][0]), "b_ada": f(inputs["b_ada"]), "w_in": f(inputs["w_in"][0]),
        "conv_w": f(inputs["conv_w"][0]), "conv_norm_w": f(inputs["conv_norm_w"]),
        "dn_conv_w": f(inputs["dn_conv_w"][0]), "dn_A_log": f(inputs["dn_A_log"]),
        "dn_dt_bias": f(inputs["dn_dt_bias"]), "dn_norm_w": f(inputs["dn_norm_w"]),
        "w_out": f(inputs["w_out"][0]), "ln1_g": f(inputs["ln1_g"]), "ln1_b": f(inputs["ln1_b"]),
        "w_grp": f(inputs["w_grp"][0]), "b_grp": f(inputs["b_grp"]), "w_exp": f(inputs["w_exp"][0]),
        "b_exp": f(inputs["b_exp"]), "w_gate": f(inputs["w_gate"][0]), "w_up": f(inputs["w_up"][0]),
        "w_down": f(inputs["w_down"][0]), "ln2_g": f(inputs["ln2_g"]), "ln2_b": f(inputs["ln2_b"]),
        "cmat": CMAT, "mab": MAB, "cb": CB,
    }
    xs = f(inputs["x"]).reshape(8, TOK, D)
    cs = f(inputs["c"]).reshape(8, NB, D)
    return [dict(shared, x=xs[i], c=cs[i]) for i in range(8)]


def kernel(**inputs):
    nc = _get_nc()
    in_maps = make_in_maps(inputs)
    res = run_bass_kernel_spmd(nc, in_maps, core_ids=list(range(8)))
    outs = [np.asarray(r["out"], dtype=np.float32).reshape(NB, SEQ, D) for r in res.results]
    return np.concatenate(outs, axis=0)
```

```python
import contextlib
import numpy as np
import concourse.bass as bass
import concourse.mybir as mybir
from concourse.bass_utils import run_bass_kernel_spmd

F32 = mybir.dt.float32
BF16 = mybir.dt.bfloat16
AF = mybir.ActivationFunctionType
ALU = mybir.AluOpType
AX = mybir.AxisListType

ENGS = ("pe", "act", "dve", "pool", "sp")

D = 1024
SEQ = 2048
NB = 2
TOK = NB * SEQ
DIN = 3592
NEXP = 32
ALPHA = 2.0 ** 0.25
TT = 256
NT = TOK // TT
BIG = 30000.0


class Prog:
    NDMA = 24

    def __init__(self, nc, tag):
        self.nc = nc
        self.tag = tag
        self.ops = []
        self.chain_dma = False

    def op(self, eng, fn, r=(), w=()):
        self.ops.append(dict(eng=eng, fn=fn, r=tuple(r), w=tuple(w), dma=False))

    def dma(self, eng, fn, r=(), w=()):
        chain = (f"__q_{eng}",) if self.chain_dma else ()
        self.ops.append(dict(eng=eng, fn=fn, r=tuple(r), w=tuple(w) + chain, dma=True))

    def emit(self, final_wait_engine="sp"):
        nc = self.nc
        esem = {e: nc.alloc_semaphore(f"s_{e}_{self.tag}") for e in ENGS if e != "sp"}
        dsem = [nc.alloc_semaphore(f"d_{i}_{self.tag}") for i in range(self.NDMA)]
        ecount = {e: 0 for e in ENGS}
        dtotal = [0] * self.NDMA
        dnext = 0
        last_w = {}
        readers = {}
        waited = {e: {} for e in ENGS}
        per_eng = {e: [] for e in ENGS}
        tokens = []
        for i, o in enumerate(self.ops):
            E = o["eng"]
            deps = set()
            for k in o["r"]:
                if k in last_w:
                    deps.add(last_w[k])
            for k in o["w"]:
                if k in last_w:
                    deps.add(last_w[k])
                for rd in readers.get(k, ()):
                    deps.add(rd)
            waits = []
            for d in sorted(deps):
                od = self.ops[d]
                if (not od["dma"]) and od["eng"] == E and E == "pe" and not o["dma"]:
                    continue
                s, v = tokens[d]
                key = id(s)
                if waited[E].get(key, 0) >= v:
                    continue
                waited[E][key] = v
                waits.append((s, v))
            if o["dma"]:
                j = dnext
                dnext = (dnext + 1) % self.NDMA
                s = dsem[j]
                if dtotal[j] > 0 and waited[E].get(id(s), 0) < dtotal[j]:
                    waited[E][id(s)] = dtotal[j]
                    waits.append((s, dtotal[j]))
                dtotal[j] += 16
                tok = (s, dtotal[j])
                inc = 16
            else:
                ecount[E] += 1
                tok = (esem[E], ecount[E])
                inc = 1
            tokens.append(tok)
            per_eng[E].append((waits, o["fn"], tok[0], inc))
            for k in o["r"]:
                readers.setdefault(k, []).append(i)
            for k in o["w"]:
                last_w[k] = i
                readers[k] = []
        final_waits = [(esem[e], ecount[e]) for e in esem if ecount[e] > 0]
        final_waits += [(dsem[j], dtotal[j]) for j in range(self.NDMA) if dtotal[j] > 0]

        with nc.Block() as block:
            def mk(ename):
                def body(eng):
                    for waits, fn, s, inc in per_eng[ename]:
                        for (ws, wv) in waits:
                            eng.wait_ge(ws, wv)
                        ins = fn(eng)
                        ins.then_inc(s, inc)
                    if ename == final_wait_engine:
                        for (ws, wv) in final_waits:
                            eng.wait_ge(ws, wv)
                return body
            block.tensor(mk("pe"))
            block.scalar(mk("act"))
            block.vector(mk("dve"))
            block.gpsimd(mk("pool"))
            block.sync(mk("sp"))
        return dict(ecount)


class PsumRot:
    def __init__(self, items):
        self.items = list(items)
        self.i = 0

    def get(self):
        it = self.items[self.i]
        self.i = (self.i + 1) % len(self.items)
        return it


def make_consts():
    idx = np.arange(128)
    same = (idx[:, None] // 64) == (idx[None, :] // 64)
    c = {}
    c["ident"] = np.eye(128)
    c["ltb"] = (same & (idx[:, None] <= idx[None, :])) * 1.0
    c["bd"] = same * 1.0
    c["mnu"] = np.where(same & (idx[:, None] <= idx[None, :]), 0.0, -BIG)
    c["mnus"] = np.where(same & (idx[:, None] < idx[None, :]), 0.0, -BIG)
    c["mnls"] = np.where(same & (idx[:, None] > idx[None, :]), 0.0, -BIG)
    c["ones"] = np.ones((128, 128))
    c["gmat"] = same / 64.0
    c["omean"] = np.ones((128, 128)) / 128.0
    names = ["ident", "ltb", "bd", "mnu", "mnus", "mnls", "ones", "gmat", "omean"]
    cm = np.concatenate([c[n] for n in names], axis=1).astype(np.float32)
    mab = np.stack([(idx < 64) * 1.0, (idx >= 64) * 1.0], axis=1).astype(np.float32)
    return names, cm, mab


CNAMES, CMAT, MAB = make_consts()
NBLK = TOK * 2 // 128 + NEXP
NROWS = NBLK * 128


def make_consts_b():
    thr = np.broadcast_to(128.0 * np.arange(64)[None, :], (128, 64))
    e = np.arange(32)
    sl = np.broadcast_to((e[None, :] < e[:, None]).astype(np.float64).reshape(1, 1024), (128, 1024))
    blk = np.broadcast_to(np.arange(NBLK, dtype=np.float64)[None, :], (128, NBLK))
    iop = np.arange(128, dtype=np.float64)[:, None]
    idx = np.arange(128)
    su = (idx[:, None] < idx[None, :]) * 1.0
    return np.concatenate([thr, sl, blk, iop, su], axis=1).astype(np.float32)


CB = make_consts_b()


def build(stage="full", dbg=()):
    nc = bass.Bass("TRN2", target_bir_lowering=False)

    def din(name, shape, dt=F32):
        return nc.dram_tensor(name, list(shape), dt, kind="ExternalInput").ap()

    x = din("x", [TOK, D])
    c_in = din("c", [NB, D])
    w_ada = din("w_ada", [D, 6 * D])
    b_ada = din("b_ada", [1, 6 * D])
    w_in = din("w_in", [D, DIN])
    conv_w = din("conv_w", [3, 512])
    conv_norm_w = din("conv_norm_w", [1, 512])
    dn_conv_w = din("dn_conv_w", [4, 1536])
    dn_A_log = din("dn_A_log", [1, 4])
    dn_dt_bias = din("dn_dt_bias", [1, 4])
    dn_norm_w = din("dn_norm_w", [1, 128])
    w_out = din("w_out", [D, D])
    ln1_g = din("ln1_g", [1, D])
    ln1_b = din("ln1_b", [1, D])
    w_grp = din("w_grp", [D, 4])
    b_grp = din("b_grp", [1, 4])
    w_exp = din("w_exp", [D, 32])
    b_exp = din("b_exp", [1, 32])
    w_gate = din("w_gate", [NEXP, D, 256])
    w_up = din("w_up", [NEXP, D, 256])
    w_down = din("w_down", [NEXP, 256, D])
    ln2_g = din("ln2_g", [1, D])
    ln2_b = din("ln2_b", [1, D])
    cmat_d = din("cmat", list(CMAT.shape))
    mab_d = din("mab", [128, 2])
    cb_d = din("cb", list(CB.shape))
    out = nc.dram_tensor("out", [TOK, D], F32, kind="ExternalOutput").ap()
    r_s = nc.dram_tensor("r_scr", [TOK, D], F32, kind="Internal").ap()
    h2_s = nc.dram_tensor("h2_scr", [128, 8, TOK], BF16, kind="Internal").ap()
    g2_s = nc.dram_tensor("g2_scr", [128, 2 * D], F32, kind="Internal").ap()
    sh2_s = nc.dram_tensor("sh2_scr", [128, 2 * D], F32, kind="Internal").ap()
    sc2_s = nc.dram_tensor("sc2_scr", [128, 2 * D], F32, kind="Internal").ap()
    xrows_s = nc.dram_tensor("xrows_scr", [NROWS if stage != "pa1" else 128, D], F32, kind="Internal").ap()
    yrows_s = nc.dram_tensor("yrows_scr", [NROWS if stage != "pa1" else 128, D], F32, kind="Internal").ap()
    dbg_out = {}
    for (nm, shp) in dbg:
        dbg_out[nm] = nc.dram_tensor("dbg_" + nm, list(shp), F32, kind="ExternalOutput").ap()

    def sb(name, shape, dt=F32):
        return nc.alloc_sbuf_tensor("sb_" + name, list(shape), dt).ap()

    PS2 = [nc.alloc_psum_tensor(f"ps2_{i}", [128, 1024], F32).ap() for i in range(4)]
    BANKS = []
    for i in range(4):
        BANKS.append((PS2[i][:, 0:512], f"bank{2 * i}"))
        BANKS.append((PS2[i][:, 512:1024], f"bank{2 * i + 1}"))

    cm = sb("cm", [128, CMAT.shape[1]])
    C = {n: cm[:, i * 128:(i + 1) * 128] for i, n in enumerate(CNAMES)}
    mab = sb("mab", [128, 2])
    identb = sb("identb", [128, 128], BF16)
    modT = sb("modT", [128, 48, 2])
    s1p = sb("s1p", [128, 8, 2])
    A2 = sb("A2", [128, 8, 2])
    B2 = sb("B2", [128, 8, 2])
    gate_bc = {2: sb("gate1bc", [128, 2, D])}
    cw = sb("cw", [128, 4, 3])
    cnw = sb("cnw", [128, 4])
    dcw = sb("dcw", [128, 12, 4])
    dnw = sb("dnw", [128, 1])
    g1T = sb("g1T", [128, 8])
    b1T = sb("b1T", [128, 8])
    negA = sb("negA", [128, 4])
    dtb = sb("dtb", [128, 4])
    ag = sb("ag", [128, D])
    ab = sb("ab", [128, D])

    esA = contextlib.ExitStack()

    def tsbA(name, shape, dt=F32):
        return esA.enter_context(nc.sbuf_tensor("a_" + name, list(shape), dt)).ap()

    Win = tsbA("win", [128, 8, DIN], BF16)
    Wout = tsbA("wout", [128, 8, D], BF16)

    P = Prog(nc, "p0")
    P.dma("sp", lambda e: e.dma_start(out=cm, in_=cmat_d), w=["cm"])
    P.dma("sp", lambda e: e.dma_start(out=mab, in_=mab_d), w=["mab"])
    P.op("dve", lambda e: e.tensor_copy(out=identb, in_=C["ident"]), r=["cm"], w=["identb"])

    with nc.sbuf_tensor("t_cT", [128, 8, 2], F32) as cT_h, \
            nc.sbuf_tensor("t_cact", [128, 8, 2], BF16) as cact_h, \
            nc.sbuf_tensor("t_cbc", [128, 8, 2, 128], BF16) as cbc_h, \
            nc.sbuf_tensor("t_brow", [1, 6 * D], BF16) as brow_h, \
            nc.sbuf_tensor("t_onesr", [1, 128], BF16) as onesr_h, \
            nc.sbuf_tensor("t_wa0", [128, 8, D], BF16) as wa0_h, \
            nc.sbuf_tensor("t_wa1", [128, 8, D], BF16) as wa1_h, \
            nc.sbuf_tensor("t_wa2", [128, 8, D], BF16) as wa2_h, \
            nc.sbuf_tensor("t_wa3", [128, 8, D], BF16) as wa3_h, \
            nc.sbuf_tensor("t_sp2", [128, 8, 2], F32) as sp2_h, \
            nc.sbuf_tensor("t_g2bc", [128, 2, D], F32) as g2bc_h, \
            nc.sbuf_tensor("t_sh2bc", [128, 2, D], F32) as sh2bc_h, \
            nc.sbuf_tensor("t_sc2bc", [128, 2, D], F32) as sc2bc_h:
        gate_bc[5] = g2bc_h.ap()
        gate_bc[3] = sh2bc_h.ap()
        gate_bc[4] = sc2bc_h.ap()
        cT, cact, cbc, brow, onesr, sp2 = (t.ap() for t in (cT_h, cact_h, cbc_h, brow_h, onesr_h, sp2_h))
        wa = [wa0_h.ap(), wa1_h.ap(), wa2_h.ap(), wa3_h.ap()]
        for b in range(NB):
            P.dma("sp", lambda e, b=b: e.dma_start(
                out=cT[:, :, b], in_=c_in[b, :].rearrange("(kc p) -> p kc", p=128),
                allow_slow_non_contiguous=True), w=["cT"])
        P.op("act", lambda e: e.activation(out=cact, in_=cT, func=AF.Silu), r=["cT"], w=["cact"])
        P.op("dve", lambda e: e.tensor_copy(out=cbc, in_=cact.unsqueeze(3).to_broadcast([128, 8, 2, 128])),
             r=["cact"], w=["cbc"])
        P.dma("pool", lambda e: e.dma_start(out=brow, in_=b_ada), w=["brow"])
        P.op("dve", lambda e: e.memset(onesr, 1.0), w=["onesr"])
        for k in range(3):
            P.dma("sp", lambda e, k=k: e.dma_start(out=cw[:, :, k],
                                                   in_=conv_w[k, :].rearrange("(j p) -> p j", p=128),
                                                   allow_slow_non_contiguous=True), w=["cw"])
        P.dma("sp", lambda e: e.dma_start(out=cnw, in_=conv_norm_w[0, :].rearrange("(j p) -> p j", p=128),
                                          allow_slow_non_contiguous=True), w=["cnw"])
        for k in range(4):
            P.dma("sp", lambda e, k=k: e.dma_start(out=dcw[:, :, k],
                                                   in_=dn_conv_w[k, :].rearrange("(j p) -> p j", p=128),
                                                   allow_slow_non_contiguous=True), w=["dcw"])
        P.dma("sp", lambda e: e.dma_start(out=dnw, in_=dn_norm_w.rearrange("o p -> p o"),
                                          allow_slow_non_contiguous=True), w=["dnw"])
        P.dma("sp", lambda e: e.dma_start(out=g1T, in_=ln1_g[0, :].rearrange("(j p) -> p j", p=128),
                                          allow_slow_non_contiguous=True), w=["g1T"])
        P.dma("sp", lambda e: e.dma_start(out=b1T, in_=ln1_b[0, :].rearrange("(j p) -> p j", p=128),
                                          allow_slow_non_contiguous=True), w=["b1T"])
        P.dma("sp", lambda e: e.dma_start(out=negA, in_=dn_A_log[0, :].partition_broadcast(128)), w=["negA"])
        P.dma("sp", lambda e: e.dma_start(out=dtb, in_=dn_dt_bias[0, :].partition_broadcast(128)), w=["dtb"])
        P.op("act", lambda e: e.activation(out=negA, in_=negA, func=AF.Exp), r=["negA"], w=["negA"])
        P.op("dve", lambda e: e.tensor_scalar(out=negA, in0=negA, scalar1=-1.0, scalar2=None, op0=ALU.mult),
             r=["negA"], w=["negA"])

        ps0 = PsumRot(BANKS[0:1])
        psr = PsumRot(BANKS[1:3])
        modps, modk = ps0.get()
        for j in range(6):
            wj = wa[j % 4]
            wk = f"wa{j % 4}"
            for kc in range(8):
                P.dma("pool", lambda e, j=j, kc=kc, wj=wj: e.dma_start(
                    out=wj[:, kc, :], in_=w_ada[kc * 128:(kc + 1) * 128, j * D:(j + 1) * D]), w=[wk])
            for cc in range(8):
                col = (j * 8 + cc) * 2
                for kc in range(8):
                    P.op("pe", lambda e, wj=wj, kc=kc, cc=cc, col=col: e.matmul(
                        modps[:, col:col + 2], lhsT=wj[:, kc, cc * 128:(cc + 1) * 128], rhs=cact[:, kc, :],
                        start=(kc == 0), stop=False), r=[wk, "cact"], w=[modk])
                P.op("pe", lambda e, j=j, cc=cc, col=col: e.matmul(
                    modps[:, col:col + 2], lhsT=brow[0:1, j * D + cc * 128:j * D + (cc + 1) * 128],
                    rhs=onesr[0:1, 0:2], start=False, stop=True), r=["brow", "onesr"], w=[modk])
            if j in (2, 3, 4, 5):
                for b in range(NB):
                    for half in range(2):
                        pt, pk = psr.get()
                        for kc in range(8):
                            P.op("pe", lambda e, wj=wj, kc=kc, b=b, half=half, pt=pt: e.matmul(
                                pt, lhsT=cbc[:, kc, b, :], rhs=wj[:, kc, half * 512:(half + 1) * 512],
                                start=(kc == 0), stop=False), r=[wk, "cbc"], w=[pk])
                        P.op("pe", lambda e, j=j, half=half, pt=pt: e.matmul(
                            pt, lhsT=onesr[0:1, :], rhs=brow[0:1, j * D + half * 512:j * D + (half + 1) * 512],
                            start=False, stop=True), r=["brow", "onesr"], w=[pk])
                        P.op("act", lambda e, j=j, b=b, half=half, pt=pt: e.activation(
                            out=gate_bc[j][:, b, half * 512:(half + 1) * 512], in_=pt, func=AF.Copy),
                            r=[pk], w=[f"gbc{j}"])
        P.op("dve", lambda e: e.tensor_copy(out=modT.rearrange("p a b -> p (a b)"), in_=modps[:, 0:96]),
             r=[modk], w=["modT"])
        P.op("dve", lambda e: e.tensor_scalar(out=s1p, in0=modT[:, 8:16, :], scalar1=1.0, scalar2=None, op0=ALU.add),
             r=["modT"], w=["s1p"])
        P.op("dve", lambda e: e.tensor_scalar(out=sp2, in0=modT[:, 32:40, :], scalar1=1.0, scalar2=None, op0=ALU.add),
             r=["modT"], w=["sp2"])
        P.op("dve", lambda e: e.tensor_tensor(out=A2, in0=sp2, in1=g1T.unsqueeze(2).to_broadcast([128, 8, 2]),
                                              op=ALU.mult), r=["sp2", "g1T"], w=["A2"])
        P.op("dve", lambda e: e.tensor_tensor(out=B2, in0=sp2, in1=b1T.unsqueeze(2).to_broadcast([128, 8, 2]),
                                              op=ALU.mult), r=["sp2", "b1T"], w=["B2"])
        P.op("dve", lambda e: e.tensor_tensor(out=B2, in0=B2, in1=modT[:, 24:32, :], op=ALU.add),
             r=["B2", "modT"], w=["B2"])
        P.dma("sp", lambda e: e.dma_start(out=ag, in_=ln1_g[0, :].partition_broadcast(128)), w=["ag"])
        P.dma("sp", lambda e: e.dma_start(out=ab, in_=ln1_b[0, :].partition_broadcast(128)), w=["ab"])
        P.op("pool", lambda e: e.tensor_scalar(out=ag, in0=ag, scalar1=ALPHA, scalar2=None, op0=ALU.mult),
             r=["ag"], w=["ag"])
        P.op("pool", lambda e: e.tensor_scalar(out=ab, in0=ab, scalar1=ALPHA, scalar2=None, op0=ALU.mult),
             r=["ab"], w=["ab"])
        for kc in range(8):
            for (c0, c1) in ((0, 2048), (2048, DIN)):
                P.dma("pool", lambda e, kc=kc, c0=c0, c1=c1: e.dma_start(
                    out=Win[:, kc, c0:c1], in_=w_in[kc * 128:(kc + 1) * 128, c0:c1]), w=[f"Win{kc}_{c0}"])
            P.dma("pool", lambda e, kc=kc: e.dma_start(
                out=Wout[:, kc, :], in_=w_out[kc * 128:(kc + 1) * 128, :]), w=[f"Wout{kc}"])
        P.dma("sp", lambda e: e.dma_start(out=g2_s, in_=gate_bc[5].rearrange("p a b -> p (a b)")),
              r=["gbc5"], w=["g2s"])
        P.dma("sp", lambda e: e.dma_start(out=sh2_s, in_=gate_bc[3].rearrange("p a b -> p (a b)")),
              r=["gbc3"], w=["sh2s"])
        P.dma("sp", lambda e: e.dma_start(out=sc2_s, in_=gate_bc[4].rearrange("p a b -> p (a b)")),
              r=["gbc4"], w=["sc2s"])
        if stage == "p0":
            P.dma("sp", lambda e: e.dma_start(out=dbg_out["modT"], in_=modT.rearrange("p a b -> p (a b)")),
                  r=["modT"], w=["dbgo"])
            P.dma("sp", lambda e: e.dma_start(out=dbg_out["g1bc"], in_=gate_bc[2].rearrange("p a b -> p (a b)")),
                  r=["gbc2"], w=["dbgo2"])
        P.emit()

    if stage == "p0":
        return nc
    with esA as es:
        tsb = tsbA
        P = Prog(nc, "pa")

        xt = tsb("xt", [128, 2, D])
        hT = tsb("hT", [128, 8, TT], BF16)
        cutail = tsb("cutail", [128, 4, 2])
        qtail = tsb("qtail", [128, 12, 3])
        qkvc2 = [tsb(f"qkvc{i}", [128, 12, TT]) for i in range(2)]
        zs2 = [tsb(f"zs{i}", [128, 4, TT]) for i in range(2)]
        mixT2 = [tsb(f"mixT{i}", [128, 8, TT], BF16) for i in range(3)]
        blsb2 = [tsb(f"blsb{i}", [128, 16]) for i in range(2)]
        S = tsb("S", [128, 4, 128])
        csb = tsb("csb", [128, TT])
        cuf2 = [tsb(f"cuf{i}", [128, TT + 2]) for i in range(2)]
        acc2 = [tsb(f"acc{i}", [128, TT]) for i in range(2)]
        ybuf2 = [tsb(f"ybuf{i}", [128, TT]) for i in range(2)]
        sqb2 = [tsb(f"sqb{i}", [128, TT]) for i in range(2)]
        sgt2 = [tsb(f"sgt{i}", [128, TT]) for i in range(2)]
        halo = [tsb(f"halo{i}", [128, TT + 3]) for i in range(2)]
        sm2 = [tsb(f"sm{i}", [128, 2, 64]) for i in range(2)]
        T = [tsb(f"T{i}", [128, 512]) for i in range(13)]
        r0 = tsb("r0", [128, D])
        xh = tsb("xh", [128, D])
        h2t = tsb("h2t", [128, 8, TT], BF16)
        bst = tsb("bst", [128, 2, 6])
        mv = tsb("mv", [128, 4])
        rotA = PsumRot(BANKS[0:4])
        rotB = PsumRot(BANKS[4:8])
        rot2A = PsumRot([(PS2[i], (f"bank{2 * i}", f"bank{2 * i + 1}")) for i in (0, 1)])

        def v3(ap):
            return ap.rearrange("p (h j) -> p h j", h=4)

        def bc_h(ap128):
            return ap128.unsqueeze(1).to_broadcast([128, 4, 128])

        def bc_j(ap4):
            return ap4.unsqueeze(2).to_broadcast([128, 4, 128])

        EPS_RMS = 1e-6

        def stage1(ti):
            pp = ti % 2
            b = ti // (NT // NB)
            first = (ti % (NT // NB) == 0)
            qkvc, zs, mixT, blsb = qkvc2[pp], zs2[pp], mixT2[ti % 3], blsb2[pp]
            mp = ti % 3
            rot = rotA
            P.dma("sp", lambda e: e.dma_start(
                out=xt, in_=x[ti * TT:(ti + 1) * TT, :].rearrange("(s p) f -> p s f", p=128)), w=["xt"])
            if first:
                P.op("pool", lambda e: e.memset(cutail, 0.0), w=["cutail"])
                P.op("pool", lambda e: e.memset(qtail, 0.0), w=["qtail"])
            for kc in range(8):
                pt, pk = rot.get()
                for s_ in range(2):
                    P.op("pe", lambda e, pt=pt, s_=s_, kc=kc: e.transpose(
                        out=pt[:, s_ * 128:(s_ + 1) * 128], in_=xt[:, s_, kc * 128:(kc + 1) * 128],
                        identity=C["ident"]), r=["xt"], w=[pk])
                P.op("act", lambda e, pt=pt, kc=kc: e.activation(
                    out=hT[:, kc, :], in_=pt[:, 0:TT], func=AF.Identity,
                    scale=s1p[:, kc, b:b + 1], bias=modT[:, kc, b:b + 1]), r=[pk], w=[f"hT{kc}"])

            def proj(oc):
                pt, pk = rot.get()
                for kc in range(8):
                    P.op("pe", lambda e, pt=pt, kc=kc: e.matmul(
                        pt[:, 0:TT], lhsT=Win[:, kc, oc * 128:(oc + 1) * 128], rhs=hT[:, kc, :],
                        start=(kc == 0), stop=(kc == 7)), r=[f"hT{kc}", "Win"], w=[pk])
                return pt[:, 0:TT], pk

            deferred = []

            def flush(keep=0):
                while len(deferred) > keep:
                    deferred.pop(0)()

            def rstd_part2(srcbuf, srck, lhs, q):
                sqb = sqb2[q]
                pm, pmk = rot.get()
                P.op("pe", lambda e: e.matmul(pm[:, 0:TT], lhsT=lhs, rhs=sqb, start=True, stop=True),
                     r=[f"sqb{q}"], w=[pmk])
                P.op("act", lambda e: e.activation(out=sqb, in_=pm[:, 0:TT], func=AF.Ln, bias=EPS_RMS),
                     r=[pmk], w=[f"sqb{q}"])
                P.op("act", lambda e: e.activation(out=sqb, in_=sqb, func=AF.Exp, scale=-0.5),
                     r=[f"sqb{q}"], w=[f"sqb{q}"])

            for j in range(4):
                q = j % 2
                cuf, acc, ybuf, sqb = cuf2[q], acc2[q], ybuf2[q], sqb2[q]
                pb, pbk = proj(j)
                pc, pck = proj(4 + j)
                pu, puk = proj(8 + j)
                P.op("act", lambda e, pc=pc: e.activation(out=csb, in_=pc, func=AF.Copy), r=[pck], w=["csb"])
                P.op("dve", lambda e, pu=pu, cuf=cuf: e.tensor_tensor(out=cuf[:, 2:TT + 2], in0=pu, in1=csb, op=ALU.mult),
                     r=[puk, "csb"], w=[f"cuf{q}"])
                P.op("pool", lambda e, j=j, cuf=cuf: e.tensor_copy(out=cuf[:, 0:2], in_=cutail[:, j, :]),
                     r=["cutail"], w=[f"cufh{q}"])
                P.op("act", lambda e, j=j, cuf=cuf, acc=acc: e.activation(out=acc, in_=cuf[:, 2:TT + 2], func=AF.Copy,
                                                                          scale=cw[:, j, 2:3]), r=[f"cuf{q}"], w=[f"acc{q}"])
                P.op("dve", lambda e, j=j, cuf=cuf, acc=acc: e.scalar_tensor_tensor(
                    out=acc, in0=cuf[:, 1:TT + 1], scalar=cw[:, j, 1:2], in1=acc, op0=ALU.mult, op1=ALU.add),
                    r=[f"cuf{q}", f"cufh{q}", f"acc{q}"], w=[f"acc{q}"])
                P.op("dve", lambda e, j=j, cuf=cuf, acc=acc: e.scalar_tensor_tensor(
                    out=acc, in0=cuf[:, 0:TT], scalar=cw[:, j, 0:1], in1=acc, op0=ALU.mult, op1=ALU.add),
                    r=[f"cuf{q}", f"cufh{q}", f"acc{q}"], w=[f"acc{q}"])
                P.op("pool", lambda e, j=j, cuf=cuf: e.tensor_copy(out=cutail[:, j, :], in_=cuf[:, TT:TT + 2]),
                     r=[f"cuf{q}"], w=["cutail"])
                P.op("dve", lambda e, pb=pb, acc=acc, ybuf=ybuf: e.tensor_tensor(out=ybuf, in0=pb, in1=acc, op=ALU.mult),
                     r=[pbk, f"acc{q}"], w=[f"ybuf{q}"])
                P.op("act", lambda e, ybuf=ybuf, sqb=sqb: e.activation(out=sqb, in_=ybuf, func=AF.Square),
                     r=[f"ybuf{q}"], w=[f"sqb{q}"])

                def part2(j=j, q=q, ybuf=ybuf, sqb=sqb):
                    rstd_part2(None, None, C["gmat"], q)
                    P.op("dve", lambda e: e.scalar_tensor_tensor(
                        out=mixT[:, j, :], in0=ybuf, scalar=cnw[:, j:j + 1], in1=sqb, op0=ALU.mult, op1=ALU.mult),
                        r=[f"ybuf{q}", f"sqb{q}"], w=[f"mixT{mp}_{j}"])
                flush(0)
                deferred.append(part2)

            for j in range(12):
                q = j % 2
                sqb = sqb2[q]
                pq, pqk = proj(12 + j)
                hb = halo[q]
                hk = f"halo{q}"
                qk_ = f"qk{pp}_{j}"
                P.op("act", lambda e, pq=pq, hb=hb: e.activation(out=hb[:, 3:TT + 3], in_=pq, func=AF.Copy),
                     r=[pqk], w=[hk])
                P.op("pool", lambda e, j=j, hb=hb: e.tensor_copy(out=hb[:, 0:3], in_=qtail[:, j, :]),
                     r=["qtail"], w=[hk + "h"])
                P.op("pool", lambda e, j=j, hb=hb: e.tensor_scalar(
                    out=qkvc[:, j, :], in0=hb[:, 3:TT + 3], scalar1=dcw[:, j, 3:4], scalar2=0.0, op0=ALU.mult,
                    op1=ALU.add), r=[hk], w=[qk_])
                for k in (2, 1, 0):
                    P.op("dve", lambda e, j=j, hb=hb, k=k: e.scalar_tensor_tensor(
                        out=qkvc[:, j, :], in0=hb[:, k:TT + k], scalar=dcw[:, j, k:k + 1], in1=qkvc[:, j, :],
                        op0=ALU.mult, op1=ALU.add), r=[hk, hk + "h", qk_], w=[qk_])
                P.op("pool", lambda e, j=j, hb=hb: e.tensor_copy(out=qtail[:, j, :], in_=hb[:, TT:TT + 3]),
                     r=[hk], w=["qtail"])
                sgt = sgt2[q]
                P.op("act", lambda e, j=j, sgt=sgt: e.activation(out=sgt, in_=qkvc[:, j, :], func=AF.Exp, scale=-1.0),
                     r=[qk_], w=[f"sgt{q}"])
                P.op("act", lambda e, sgt=sgt: e.activation(out=sgt, in_=sgt, func=AF.Ln, bias=1.0),
                     r=[f"sgt{q}"], w=[f"sgt{q}"])
                P.op("act", lambda e, sgt=sgt: e.activation(out=sgt, in_=sgt, func=AF.Exp, scale=-1.0),
                     r=[f"sgt{q}"], w=[f"sgt{q}"])
                P.op("pool", lambda e, j=j, sgt=sgt: e.tensor_tensor(out=qkvc[:, j, :], in0=qkvc[:, j, :], in1=sgt,
                                                                     op=ALU.mult), r=[qk_, f"sgt{q}"], w=[qk_])
                if j < 8:
                    P.op("act", lambda e, j=j, sqb=sqb: e.activation(out=sqb, in_=qkvc[:, j, :], func=AF.Square),
                         r=[qk_], w=[f"sqb{q}"])

                    def part2(j=j, q=q, sqb=sqb, qk_=qk_):
                        rstd_part2(None, None, C["ones"], q)
                        sc = (128.0 ** -0.5) if j < 4 else 1.0
                        P.op("dve", lambda e: e.scalar_tensor_tensor(
                            out=qkvc[:, j, :], in0=qkvc[:, j, :], scalar=sc, in1=sqb, op0=ALU.mult, op1=ALU.mult),
                            r=[qk_, f"sqb{q}"], w=[qk_])
                    flush(0)
                    deferred.append(part2)
                else:
                    flush(0)
            flush(0)
            for j in range(4):
                pz, pzk = proj(24 + j)
                sgt = sgt2[j % 2]
                sk = f"sgt{j % 2}"
                P.op("act", lambda e, pz=pz, sgt=sgt: e.activation(out=sgt, in_=pz, func=AF.Exp, scale=-1.0),
                     r=[pzk], w=[sk])
                P.op("act", lambda e, sgt=sgt: e.activation(out=sgt, in_=sgt, func=AF.Ln, bias=1.0), r=[sk], w=[sk])
                P.op("act", lambda e, sgt=sgt: e.activation(out=sgt, in_=sgt, func=AF.Exp, scale=-1.0), r=[sk], w=[sk])
                P.op("dve", lambda e, pz=pz, j=j, sgt=sgt: e.tensor_tensor(out=zs[:, j, :], in0=pz, in1=sgt, op=ALU.mult),
                     r=[pzk, sk], w=[f"zs{pp}_{j}"])
            p8, p8k = rot.get()
            for s_ in range(2):
                for kc in range(8):
                    P.op("pe", lambda e, s_=s_, kc=kc: e.matmul(
                        p8[:, s_ * 8:(s_ + 1) * 8], lhsT=hT[:, kc, s_ * 128:(s_ + 1) * 128],
                        rhs=Win[:, kc, 3584:3592], start=(kc == 0), stop=(kc == 7)),
                        r=[f"hT{kc}", "Win"], w=[p8k])
            P.op("act", lambda e: e.activation(out=blsb, in_=p8[:, 0:16], func=AF.Copy), r=[p8k], w=[f"blsb{pp}"])
            for s_ in range(2):
                smx = sm2[pp][:, s_, :]
                beta, xa, g, _g, egc, bge, dl, eL, sA, sB = [smx[:, i * 4:(i + 1) * 4] for i in range(10)]
                gcs = smx[:, 40:48]
                gcum = gcs[:, 0:4]
                glast = gcs[:, 4:8]
                SK = f"smk{pp}_{s_}"
                bl = blsb[:, s_ * 8:(s_ + 1) * 8]
                blk = f"blsb{pp}"
                P.op("act", lambda e, beta=beta, bl=bl: e.activation(out=beta, in_=bl[:, 0:4], func=AF.Exp, scale=-1.0),
                     r=[blk], w=[SK])
                P.op("dve", lambda e, beta=beta: e.tensor_scalar(out=beta, in0=beta, scalar1=1.0, scalar2=None, op0=ALU.add),
                     r=[SK], w=[SK])
                P.op("dve", lambda e, beta=beta: e.reciprocal(out=beta, in_=beta), r=[SK], w=[SK])
                P.op("dve", lambda e, xa=xa, bl=bl: e.tensor_tensor(out=xa, in0=bl[:, 4:8], in1=dtb, op=ALU.add),
                     r=[blk, SK], w=[SK])
                P.op("act", lambda e, xa=xa: e.activation(out=xa, in_=xa, func=AF.Exp), r=[SK], w=[SK])
                P.op("act", lambda e, xa=xa: e.activation(out=xa, in_=xa, func=AF.Ln, bias=1.0), r=[SK], w=[SK])
                P.op("dve", lambda e, g=g, xa=xa: e.tensor_tensor(out=g, in0=xa, in1=negA, op=ALU.mult), r=[SK], w=[SK])
                pc_, pck = rot.get()
                P.op("pe", lambda e, pc_=pc_, g=g: e.matmul(pc_[:, 0:4], lhsT=C["ltb"], rhs=g, start=True, stop=True),
                     r=[SK], w=[pck])
                P.op("pe", lambda e, pc_=pc_, g=g: e.matmul(pc_[:, 4:8], lhsT=C["bd"], rhs=g, start=True, stop=True),
                     r=[SK], w=[pck])
                P.op("act", lambda e, pc_=pc_, gcs=gcs: e.activation(out=gcs, in_=pc_[:, 0:8], func=AF.Copy), r=[pck], w=[SK])
                P.op("act", lambda e, egc=egc, gcum=gcum: e.activation(out=egc, in_=gcum, func=AF.Exp), r=[SK], w=[SK])
                P.op("dve", lambda e, bge=bge, beta=beta, egc=egc: e.tensor_tensor(out=bge, in0=beta, in1=egc, op=ALU.mult),
                     r=[SK], w=[SK])
                P.op("dve", lambda e, dl=dl, glast=glast, gcum=gcum: e.tensor_tensor(out=dl, in0=glast, in1=gcum,
                                                                                     op=ALU.subtract), r=[SK], w=[SK])
                P.op("act", lambda e, eL=eL, dl=dl: e.activation(out=eL, in_=dl, func=AF.Exp), r=[SK], w=[SK])
                P.op("dve", lambda e, sA=sA, eL=eL: e.tensor_scalar(out=sA, in0=eL, scalar1=mab[:, 0:1], scalar2=None,
                                                                    op0=ALU.mult), r=[SK], w=[SK])
                P.op("dve", lambda e, sB=sB, eL=eL: e.tensor_scalar(out=sB, in0=eL, scalar1=mab[:, 1:2], scalar2=None,
                                                                    op0=ALU.mult), r=[SK], w=[SK])

        def stage2(ti):
            first = (ti % (NT // NB) == 0)
            if first:
                P.op("pool", lambda e: e.memset(S, 0.0), w=["S0", "S1", "S2", "S3"])
            for s_ in range(2):
                gdn_sub(ti, s_)

        def stage3(ti):
            b = ti // (NT // NB)
            for s_ in range(2):
                ln1_sub(ti, s_, b)
            P.dma("sp", lambda e: e.dma_start(out=h2_s[:, :, ti * TT:(ti + 1) * TT], in_=h2t),
                  r=["h2t"], w=["h2s"])

        def gdn_sub(ti, s_):
            pp = ti % 2
            rot = rotB
            qkvc, zs, mixT, blsb = qkvc2[pp], zs2[pp], mixT2[ti % 3], blsb2[pp]
            mp = ti % 3
            bl = blsb[:, s_ * 8:(s_ + 1) * 8]
            blk = f"blsb{pp}"
            cs = slice(s_ * 128, (s_ + 1) * 128)
            R1, R2, tU, tL, egr, U, L, QKm, Xa, Xb, Pb, PTb, bv = T
            kR1, kR2, ktU, ktL, kegr, kU, kL, kQKm, kXa, kXb, kPb, kPTb, kbv = [f"T{i}" for i in range(13)]
            keA, kkeA, keB, kkeB = R2, kR2, tL, ktL
            u, ku, wT, kwT, qdT, kqdT, delta, kdelta = U, kU, L, kL, Pb, kPb, PTb, kPTb
            smx = sm2[pp][:, s_, :]
            beta, xa, g, _g, egc, bge, dl, eL, sA, sB = [smx[:, i * 4:(i + 1) * 4] for i in range(10)]
            gcs = smx[:, 40:48]
            gcum = gcs[:, 0:4]
            glast = gcs[:, 4:8]
            SK = f"smk{pp}_{s_}"
            qk = lambda j: f"qk{pp}_{j}"
            P.op("dve", lambda e: e.tensor_tensor(out=v3(R1), in0=bc_h(C["ltb"]), in1=bc_j(g), op=ALU.mult),
                 r=[SK], w=[kR1])
            P.op("pool", lambda e: e.tensor_tensor(out=v3(R2), in0=bc_h(C["ident"]), in1=bc_j(beta), op=ALU.mult),
                 r=[SK], w=[kR2])
            pgr, pgrk = rot.get()
            P.op("pe", lambda e: e.matmul(pgr, lhsT=C["ones"], rhs=R1, start=True, stop=True), r=[kR1], w=[pgrk])
            pbr, pbrk = rot.get()
            P.op("pe", lambda e: e.matmul(pbr, lhsT=C["ones"], rhs=R2, start=True, stop=True), r=[kR2], w=[pbrk])
            P.op("dve", lambda e: e.tensor_tensor(out=v3(tU), in0=v3(pgr), in1=bc_j(gcum), op=ALU.subtract),
                 r=[pgrk, SK], w=[ktU])
            P.op("pool", lambda e: e.tensor_tensor(out=v3(R1), in0=v3(tU), in1=bc_h(C["mnu"]), op=ALU.add),
                 r=[ktU], w=[kR1])
            P.op("act", lambda e: e.activation(out=R1, in_=R1, func=AF.Exp), r=[kR1], w=[kR1])
            P.op("pool", lambda e: e.tensor_tensor(out=v3(R2), in0=v3(tU), in1=bc_h(C["mnus"]), op=ALU.add),
                 r=[ktU], w=[kR2])
            P.op("act", lambda e: e.activation(out=R2, in_=R2, func=AF.Exp), r=[kR2], w=[kR2])
            P.op("dve", lambda e: e.tensor_tensor(out=R2, in0=R2, in1=pbr, op=ALU.mult), r=[kR2, pbrk], w=[kR2])
            P.op("dve", lambda e: e.scalar_tensor_tensor(out=v3(tL), in0=v3(pgr), scalar=-1.0, in1=bc_j(gcum),
                                                         op0=ALU.mult, op1=ALU.add), r=[pgrk, SK], w=[ktL])
            P.op("pool", lambda e: e.tensor_tensor(out=v3(tL), in0=v3(tL), in1=bc_h(C["mnls"]), op=ALU.add),
                 r=[ktL], w=[ktL])
            P.op("act", lambda e: e.activation(out=tL, in_=tL, func=AF.Exp), r=[ktL], w=[ktL])
            P.op("pool", lambda e: e.tensor_tensor(out=v3(tL), in0=v3(tL), in1=bc_j(beta), op=ALU.mult),
                 r=[ktL, SK], w=[ktL])
            P.op("act", lambda e: e.activation(out=egr, in_=pgr, func=AF.Exp), r=[pgrk], w=[kegr])
            pkk, pkkk = rot.get()
            for h in range(4):
                P.op("pe", lambda e, h=h: e.matmul(pkk[:, h * 128:(h + 1) * 128], lhsT=qkvc[:, 4 + h, cs],
                                                   rhs=qkvc[:, 4 + h, cs], start=True, stop=True),
                     r=[qk(4 + h)], w=[pkkk])
            P.op("dve", lambda e: e.tensor_tensor(out=U, in0=pkk, in1=R2, op=ALU.mult), r=[pkkk, kR2], w=[kU])
            P.op("dve", lambda e: e.tensor_tensor(out=L, in0=pkk, in1=tL, op=ALU.mult), r=[pkkk, ktL], w=[kL])
            pqk_, pqkk = rot.get()
            for h in range(4):
                P.op("pe", lambda e, h=h: e.matmul(pqk_[:, h * 128:(h + 1) * 128], lhsT=qkvc[:, 4 + h, cs],
                                                   rhs=qkvc[:, h, cs], start=True, stop=True),
                     r=[qk(4 + h), qk(h)], w=[pqkk])
            P.op("dve", lambda e: e.tensor_tensor(out=QKm, in0=pqk_, in1=R1, op=ALU.mult), r=[pqkk, kR1], w=[kQKm])
            P.op("pool", lambda e: e.tensor_tensor(out=v3(Xa), in0=bc_h(C["ident"]), in1=v3(U), op=ALU.subtract),
                 r=[kU], w=[kXa])
            pkt, pktk = rot.get()
            for h in range(4):
                P.op("pe", lambda e, h=h: e.transpose(out=pkt[:, h * 128:(h + 1) * 128], in_=qkvc[:, 4 + h, cs],
                                                      identity=C["ident"]), r=[qk(4 + h)], w=[pktk])
            kbg, kkbg = tU, ktU
            P.op("dve", lambda e: e.tensor_tensor(out=v3(kbg), in0=v3(pkt), in1=bc_j(bge), op=ALU.mult),
                 r=[pktk, SK], w=[kkbg])
            P.op("dve", lambda e: e.tensor_tensor(out=v3(keA), in0=v3(pkt), in1=bc_j(sA), op=ALU.mult),
                 r=[pktk, SK, kU], w=[kkeA])
            P.op("dve", lambda e: e.tensor_tensor(out=v3(keB), in0=v3(pkt), in1=bc_j(sB), op=ALU.mult),
                 r=[pktk, SK, kL], w=[kkeB])
            pvt, pvtk = rot.get()
            for h in range(4):
                P.op("pe", lambda e, h=h: e.transpose(out=pvt[:, h * 128:(h + 1) * 128], in_=qkvc[:, 8 + h, cs],
                                                      identity=C["ident"]), r=[qk(8 + h)], w=[pvtk])
            P.op("dve", lambda e: e.tensor_tensor(out=v3(bv), in0=v3(pvt), in1=bc_j(beta), op=ALU.mult),
                 r=[pvtk, SK], w=[kbv])
            Pc, PTc, kPc, kPTc = U, L, kU, kL
            Pn, PTn, kPn, kPTn = Pb, PTb, kPb, kPTb
            Xc, Xn, kXc, kXn = Xa, Xb, kXa, kXb
            for k in range(1, 6):
                if k < 5:
                    pp_, ppk = rot.get()
                    for h in range(4):
                        hs = slice(h * 128, (h + 1) * 128)
                        P.op("pe", lambda e, hs=hs, PTc=PTc, Pc=Pc, pp_=pp_: e.matmul(
                            pp_[:, hs], lhsT=PTc[:, hs], rhs=Pc[:, hs], start=True, stop=True),
                            r=[kPc, kPTc], w=[ppk])
                ppt, pptk = rot.get()
                for h in range(4):
                    hs = slice(h * 128, (h + 1) * 128)
                    P.op("pe", lambda e, hs=hs, PTc=PTc, Pc=Pc, ppt=ppt: e.matmul(
                        ppt[:, hs], lhsT=Pc[:, hs], rhs=PTc[:, hs], start=True, stop=True),
                        r=[kPc, kPTc], w=[pptk])
                if k < 5:
                    P.op("act", lambda e, Pn=Pn, pp_=pp_: e.activation(out=Pn, in_=pp_, func=AF.Copy), r=[ppk], w=[kPn])
                P.op("dve", lambda e, PTn=PTn, ppt=ppt: e.tensor_copy(out=PTn, in_=ppt), r=[pptk], w=[kPTn])
                px, pxk = rot.get()
                for h in range(4):
                    hs = slice(h * 128, (h + 1) * 128)
                    P.op("pe", lambda e, hs=hs, PTn=PTn, Xc=Xc, px=px: e.matmul(
                        px[:, hs], lhsT=PTn[:, hs], rhs=Xc[:, hs], start=True, stop=True),
                        r=[kPTn, kXc], w=[pxk])
                P.op("dve", lambda e, Xn=Xn, Xc=Xc, px=px: e.tensor_tensor(out=Xn, in0=px, in1=Xc, op=ALU.add),
                     r=[pxk, kXc], w=[kXn])
                Pc, Pn, kPc, kPn = Pn, Pc, kPn, kPc
                PTc, PTn, kPTc, kPTn = PTn, PTc, kPTn, kPTc
                Xc, Xn, kXc, kXn = Xn, Xc, kXn, kXc
            TTm, kTT = Xc, kXc
            assert TTm is Xb
            pu_, puk = rot.get()
            pw_, pwk = rot.get()
            for h in range(4):
                hs = slice(h * 128, (h + 1) * 128)
                P.op("pe", lambda e, hs=hs: e.matmul(pu_[:, hs], lhsT=TTm[:, hs], rhs=bv[:, hs], start=True, stop=True),
                     r=[kTT, kbv], w=[puk])
            for h in range(4):
                hs = slice(h * 128, (h + 1) * 128)
                P.op("pe", lambda e, hs=hs: e.matmul(pw_[:, hs], lhsT=kbg[:, hs], rhs=TTm[:, hs], start=True, stop=True),
                     r=[kTT, kkbg], w=[pwk])
            P.op("act", lambda e: e.activation(out=u, in_=pu_, func=AF.Copy), r=[puk], w=[ku])
            P.op("act", lambda e: e.activation(out=wT, in_=pw_, func=AF.Copy), r=[pwk], w=[kwT])
            P.op("dve", lambda e: e.tensor_tensor(out=v3(qdT), in0=qkvc[:, 0:4, cs], in1=v3(egr), op=ALU.mult),
                 r=[qk(h) for h in range(4)] + [kegr], w=[kqdT])
            po, pok = rot.get()
            others = [it_ for it_ in rotB.items if it_[1] != pok]
            oi = 0
            for ch in range(2):
                rows = slice(ch * 64, ch * 64 + 64)
                keX, kkeX = (keA, kkeA) if ch == 0 else (keB, kkeB)
                pws, pwsk = others[oi % 3]
                oi += 1
                for h in range(4):
                    hs = slice(h * 128, (h + 1) * 128)
                    P.op("pe", lambda e, hs=hs, h=h, pws=pws: e.matmul(pws[:, hs], lhsT=wT[:, hs], rhs=S[:, h, :],
                                                                      start=True, stop=True),
                         r=[kwT, f"S{h}"], w=[pwsk])
                P.op("dve", lambda e, rows=rows, pws=pws: e.tensor_tensor(out=delta[rows, :], in0=u[rows, :],
                                                                          in1=pws[rows, :], op=ALU.subtract),
                     r=[ku, pwsk], w=[kdelta])
                for h in range(4):
                    hs = slice(h * 128, (h + 1) * 128)
                    oc_ = slice(h * 128 + ch * 64, h * 128 + ch * 64 + 64)
                    P.op("pe", lambda e, h=h, oc_=oc_: e.matmul(po[:, oc_], lhsT=S[:, h, :], rhs=qdT[:, oc_],
                                                                start=True, stop=False),
                         r=[f"S{h}", kqdT], w=[pok])
                    P.op("pe", lambda e, hs=hs, oc_=oc_: e.matmul(po[:, oc_], lhsT=delta[:, hs], rhs=QKm[:, oc_],
                                                                  start=False, stop=True),
                         r=[kdelta, kQKm], w=[pok])
                pss, pssk = others[oi % 3]
                oi += 1
                for h in range(4):
                    hs = slice(h * 128, (h + 1) * 128)
                    P.op("pe", lambda e, hs=hs, keX=keX, pss=pss: e.matmul(pss[:, hs], lhsT=keX[:, hs], rhs=delta[:, hs],
                                                                          start=True, stop=True),
                         r=[kkeX, kdelta], w=[pssk])
                for h in range(4):
                    hs = slice(h * 128, (h + 1) * 128)
                    dcol = h * 128 + ch * 64 + 63
                    P.op("dve", lambda e, h=h, hs=hs, dcol=dcol, pss=pss: e.scalar_tensor_tensor(
                        out=S[:, h, :], in0=S[:, h, :], scalar=egr[:, dcol:dcol + 1], in1=pss[:, hs],
                        op0=ALU.mult, op1=ALU.add), r=[f"S{h}", kegr, pssk], w=[f"S{h}"])
            osb, kosb = bv, kbv
            sq2, ksq2 = R1, kR1
            P.op("act", lambda e: e.activation(out=osb, in_=po, func=AF.Copy), r=[pok], w=[kosb])
            P.op("act", lambda e: e.activation(out=sq2, in_=po, func=AF.Square), r=[pok], w=[ksq2])
            pm, pmk = others[oi % 3]
            P.op("pe", lambda e: e.matmul(pm, lhsT=C["omean"], rhs=sq2, start=True, stop=True), r=[ksq2], w=[pmk])
            P.op("act", lambda e: e.activation(out=sq2, in_=pm, func=AF.Ln, bias=EPS_RMS), r=[pmk], w=[ksq2])
            P.op("act", lambda e: e.activation(out=sq2, in_=sq2, func=AF.Exp, scale=-0.5), r=[ksq2], w=[ksq2])
            P.op("dve", lambda e: e.scalar_tensor_tensor(out=osb, in0=osb, scalar=dnw[:, 0:1], in1=sq2,
                                                         op0=ALU.mult, op1=ALU.mult), r=[kosb, ksq2], w=[kosb])
            P.op("pool", lambda e: e.tensor_tensor(out=mixT[:, 4:8, cs], in0=v3(osb), in1=zs[:, :, cs], op=ALU.mult),
                 r=[kosb] + [f"zs{pp}_{j}" for j in range(4)], w=[f"mixT{mp}_{4 + j}" for j in range(4)])

        def ln1_sub(ti, s_, b):
            mp = ti % 3
            mixT = mixT2[mp]
            rot = rotA
            cs = slice(s_ * 128, (s_ + 1) * 128)
            tok0 = ti * TT + s_ * 128
            P.dma("sp", lambda e: e.dma_start(out=xh, in_=x[tok0:tok0 + 128, :]), w=["xh"])
            pm2, (k0, k1) = rot2A.get()
            for half in range(2):
                for kc in range(8):
                    P.op("pe", lambda e, half=half, kc=kc: e.matmul(
                        pm2[:, half * 512:(half + 1) * 512], lhsT=mixT[:, kc, cs],
                        rhs=Wout[:, kc, half * 512:(half + 1) * 512], start=(kc == 0), stop=(kc == 7)),
                        r=[f"mixT{mp}_{kc}", "Wout"], w=[(k0, k1)[half]])
            P.op("dve", lambda e: e.tensor_tensor(out=r0, in0=pm2, in1=gate_bc[2][:, b, :], op=ALU.mult),
                 r=[k0, k1], w=["r0"])
            P.op("dve", lambda e: e.scalar_tensor_tensor(out=r0, in0=xh, scalar=ALPHA, in1=r0,
                                                         op0=ALU.mult, op1=ALU.add), r=["xh", "r0"], w=["r0"])
            for hf in range(2):
                P.op("dve", lambda e, hf=hf: e.bn_stats(out=bst[:, hf, :], in_=r0[:, hf * 512:(hf + 1) * 512]),
                     r=["r0"], w=["bst"])
            P.op("dve", lambda e: e.bn_aggr(out=mv[:, 0:2], in_=bst.rearrange("p a b -> p (a b)")), r=["bst"], w=["mv"])
            P.op("act", lambda e: e.activation(out=mv[:, 2:3], in_=mv[:, 1:2], func=AF.Ln, bias=1e-5),
                 r=["mv"], w=["mv2"])
            P.op("act", lambda e: e.activation(out=mv[:, 3:4], in_=mv[:, 2:3], func=AF.Exp, scale=-0.5),
                 r=["mv2"], w=["mv3"])
            P.op("dve", lambda e: e.tensor_scalar(out=xh, in0=r0, scalar1=mv[:, 0:1], scalar2=mv[:, 3:4],
                                                  op0=ALU.subtract, op1=ALU.mult), r=["r0", "mv", "mv3"], w=["xh"])
            P.op("pool", lambda e: e.tensor_tensor(out=r0, in0=xh, in1=ag, op=ALU.mult), r=["xh"], w=["r0"])
            P.op("pool", lambda e: e.tensor_tensor(out=r0, in0=r0, in1=ab, op=ALU.add), r=["r0"], w=["r0"])
            P.dma("sp", lambda e: e.dma_start(out=r_s[tok0:tok0 + 128, :], in_=r0), r=["r0"], w=["rs"])
            for kc in range(8):
                pt, pk = rot.get()
                P.op("pe", lambda e, pt=pt, kc=kc: e.transpose(out=pt[:, 0:128], in_=xh[:, kc * 128:(kc + 1) * 128],
                                                               identity=C["ident"]), r=["xh"], w=[pk])
                P.op("act", lambda e, pt=pt, kc=kc: e.activation(
                    out=h2t[:, kc, cs], in_=pt[:, 0:128], func=AF.Identity,
                    scale=A2[:, kc, b:b + 1], bias=B2[:, kc, b:b + 1]), r=[pk], w=["h2t"])

        def capture(fn, *a):
            old = P.ops
            P.ops = []
            fn(*a)
            got = P.ops
            P.ops = old
            return got

        def merge(a, b_):
            out_, i, j = [], 0, 0
            na, nb = max(len(a), 1), max(len(b_), 1)
            while i < len(a) or j < len(b_):
                if j >= len(b_) or (i < len(a) and i * nb <= j * na):
                    out_.append(a[i]); i += 1
                else:
                    out_.append(b_[j]); j += 1
            return out_

        ntiles = NT if stage != "pa1" else 2

        def threadA(ti):
            if ti + 1 < ntiles:
                stage1(ti + 1)
            if ti - 1 >= 0:
                stage3(ti - 1)

        P.ops += capture(stage1, 0)
        for ti in range(ntiles):
            a = capture(threadA, ti)
            b_ = capture(stage2, ti)
            P.ops += merge(a, b_)
        P.ops += capture(stage3, ntiles - 1)
        if stage == "pa1":
            P.dma("pool", lambda e: e.dma_start(out=dbg_out["mixT"].rearrange("p (a b) -> p a b", a=8), in_=mixT2[1]),
                  r=[f"mixT1_{j}" for j in range(8)], w=["dbg1"])
            P.dma("sp", lambda e: e.dma_start(out=dbg_out["r"], in_=r_s[0:512, :]), r=["rs"], w=["dbg2"])
            P.dma("pool", lambda e: e.dma_start(out=dbg_out["h2"].rearrange("p (a b) -> p a b", a=8),
                                                in_=h2_s[:, :, 0:512]), r=["h2s"], w=["dbg3"])
            P.dma("sp", lambda e: e.dma_start(out=dbg_out["qkvc"].rearrange("p (a b) -> p a b", a=12), in_=qkvc2[1]),
                  r=[f"qk1_{j}" for j in range(12)], w=["dbg4"])
        cnt = P.emit()
        print("phaseA op counts", cnt)

    if stage in ("pa", "pa1"):
        return nc
    I32 = mybir.dt.int32
    with contextlib.ExitStack() as es:
        def tsb(name, shape, dt=F32):
            return es.enter_context(nc.sbuf_tensor("b_" + name, list(shape), dt)).ap()

        P = Prog(nc, "pb")
        NSTT = TOK // 128
        NW = 3
        cb = tsb("cb", list(CB.shape))
        thr = cb[:, 0:64]
        SLc = cb[:, 64:64 + 1024]
        blkio = cb[:, 1088:1088 + NBLK]
        iotaP = cb[:, 1088 + NBLK:1089 + NBLK]
        SUf = cb[:, 1089 + NBLK:1089 + NBLK + 128]
        SUb = tsb("sub", [128, 128], BF16)
        onesb = tsb("onesb", [128, 128], BF16)
        Wr = tsb("wr", [128, 8, 36], BF16)
        b36 = tsb("b36", [128, 36])
        g2 = tsb("ln2g", [128, D])
        b2 = tsb("ln2b", [128, D])
        gate2 = tsb("gate2", [128, 2, D])
        h2c = [tsb(f"h2c{i}", [128, 8, 512], BF16) for i in range(2)]
        OHall = tsb("ohall", [128, NSTT, 32], BF16)
        oh1all = tsb("oh1all", [128, NSTT, 32])
        oh2all = tsb("oh2all", [128, NSTT, 32])
        gAB = tsb("gab", [128, NSTT, 2])
        POSf = tsb("posf", [128, NSTT, 2])
        POSi = tsb("posi", [128, NSTT, 2], I32)
        cnt = tsb("cnt", [128, 32])
        nblk = tsb("nblk", [128, 32])
        pstb = tsb("pstb", [128, 32])
        pend = tsb("pend", [128, 32])
        base = tsb("base", [128, 32])
        big = tsb("big", [128, NBLK * 32])
        bexp = tsb("bexp", [128, NBLK])
        bskip = tsb("bskip", [128, NBLK])
        bsame = tsb("bsame", [128, NBLK])
        IDXW = tsb("idxw", [128, NBLK], I32)
        posv = tsb("posv", [128, 32])
        ptmp = tsb("ptmp", [128, 32])
        lg = tsb("lg", [128, 36])
        lgm = tsb("lgm", [128, 32])
        lgm2 = tsb("lgm2", [128, 32])
        rs_ = tsb("rsm", [128, 32])
        Wg = [tsb(f"wg{i}", [128, 2048], BF16) for i in range(NW)]
        Wu = [tsb(f"wu{i}", [128, 2048], BF16) for i in range(NW)]
        Wd = [tsb(f"wd{i}", [128, 2048], BF16) for i in range(NW)]
        xb = [tsb(f"xb{i}", [128, D]) for i in range(3)]
        sh2r = tsb("sh2r", [128, 2, D])
        sc2r = tsb("sc2r", [128, 2, D])
        xT = [tsb(f"xT{i}", [128, 8, 128], BF16) for i in range(3)]
        sg = [tsb(f"sg{i}", [128, 256]) for i in range(3)]
        hid = [tsb(f"hid{i}", [128, 256]) for i in range(3)]
        hidT = [tsb(f"hidT{i}", [128, 256], BF16) for i in range(3)]
        yb = [tsb(f"yb{i}", [128, D]) for i in range(2)]
        y1 = [tsb(f"y1_{i}", [128, D]) for i in range(2)]
        y2 = [tsb(f"y2_{i}", [128, D]) for i in range(2)]
        rr = [tsb(f"rr{i}", [128, D]) for i in range(2)]
        xh2 = tsb("xh2", [128, D])
        ob = tsb("ob", [128, D])
        bst2 = tsb("bst2", [128, 2, 6])
        mv2 = tsb("mv2", [128, 4])
        rot = PsumRot(BANKS[0:2])
        rot2 = PsumRot([(PS2[i], (f"bank{2 * i}", f"bank{2 * i + 1}")) for i in (2, 3)])

        P.dma("sp", lambda e: e.dma_start(out=cb, in_=cb_d), w=["cb"])
        P.op("dve", lambda e: e.tensor_copy(out=SUb, in_=SUf), r=["cb"], w=["SUb"])
        P.op("dve", lambda e: e.memset(onesb, 1.0), w=["onesb"])
        for kc in range(8):
            P.dma("pool", lambda e, kc=kc: e.dma_start(out=Wr[:, kc, 0:4], in_=w_grp[kc * 128:(kc + 1) * 128, :]),
                  w=["Wr"])
            P.dma("pool", lambda e, kc=kc: e.dma_start(out=Wr[:, kc, 4:36], in_=w_exp[kc * 128:(kc + 1) * 128, :]),
                  w=["Wr"])
        P.dma("sp", lambda e: e.dma_start(out=b36[:, 0:4], in_=b_grp[0, :].partition_broadcast(128)), w=["b36"])
        P.dma("sp", lambda e: e.dma_start(out=b36[:, 4:36], in_=b_exp[0, :].partition_broadcast(128)), w=["b36"])
        P.dma("sp", lambda e: e.dma_start(out=gate2.rearrange("p a b -> p (a b)"), in_=g2_s), w=["gate2"])
        P.dma("sp", lambda e: e.dma_start(out=sh2r.rearrange("p a b -> p (a b)"), in_=sh2_s), w=["sh2r"])
        P.dma("sp", lambda e: e.dma_start(out=sc2r.rearrange("p a b -> p (a b)"), in_=sc2_s), w=["sc2r"])
        P.op("pool", lambda e: e.tensor_scalar(out=sc2r, in0=sc2r, scalar1=1.0, scalar2=1.0 / ALPHA, op0=ALU.add,
                                               op1=ALU.mult), r=["sc2r"], w=["sc2r"])
        P.dma("sp", lambda e: e.dma_start(out=g2, in_=ln2_g[0, :].partition_broadcast(128)), w=["g2"])
        P.dma("sp", lambda e: e.dma_start(out=b2, in_=ln2_b[0, :].partition_broadcast(128)), w=["b2"])

        RG = 4
        lg4 = tsb("lg4", [128, RG, 36])
        lgm4 = tsb("lgm4", [128, RG, 32])
        lgm24 = tsb("lgm24", [128, RG, 32])
        eg4 = tsb("eg4", [128, RG, 4])
        ohg4 = tsb("ohg4", [128, RG, 4])
        rsc = tsb("rsc", [128, 12, RG])

        def router4(g_):
            st0 = g_ * RG
            ci = g_ % 2
            P.dma("sp", lambda e: e.dma_start(out=h2c[ci], in_=h2_s[:, :, st0 * 128:st0 * 128 + 512]), w=[f"h2c{ci}"])
            plg, plgk = rot.get()
            for i in range(RG):
                cs = slice(i * 128, (i + 1) * 128)
                for kc in range(8):
                    P.op("pe", lambda e, kc=kc, i=i, cs=cs: e.matmul(plg[:, i * 36:(i + 1) * 36], lhsT=h2c[ci][:, kc, cs],
                                                                      rhs=Wr[:, kc, :], start=(kc == 0), stop=(kc == 7)),
                         r=[f"h2c{ci}", "Wr"], w=[plgk])
            gmax, sume, grpw, m1, m2, d21, p2, den, rden = [rsc[:, i, :] for i in range(9)]
            sts = range(st0, st0 + RG)
            K1 = [f"oh1_{st}" for st in sts]
            K2 = [f"oh2_{st}" for st in sts]
            KA = [f"gA{st}" for st in sts]
            KB = [f"gB{st}" for st in sts]
            KO = [f"OH{st}" for st in sts]
            oh1 = oh1all[:, st0:st0 + RG, :]
            oh2 = oh2all[:, st0:st0 + RG, :]
            gA = gAB[:, st0:st0 + RG, 0]
            gB = gAB[:, st0:st0 + RG, 1]
            bcx = lambda ap, n: ap.unsqueeze(2).to_broadcast([128, RG, n])
            P.op("dve", lambda e: e.tensor_tensor(out=lg4, in0=plg[:, 0:RG * 36].rearrange("p (s n) -> p s n", s=RG),
                                                  in1=b36.unsqueeze(1).to_broadcast([128, RG, 36]), op=ALU.add),
                 r=[plgk, "b36"], w=["lg4"])
            P.op("dve", lambda e: e.tensor_reduce(out=gmax, in_=lg4[:, :, 0:4], axis=AX.X, op=ALU.max), r=["lg4"], w=["q_gmax"])
            P.op("dve", lambda e: e.tensor_tensor(out=eg4, in0=lg4[:, :, 0:4], in1=bcx(gmax, 4), op=ALU.subtract),
                 r=["lg4", "q_gmax"], w=["eg4"])
            P.op("act", lambda e: e.activation(out=eg4, in_=eg4, func=AF.Exp), r=["eg4"], w=["eg4"])
            P.op("dve", lambda e: e.tensor_reduce(out=sume, in_=eg4, axis=AX.X, op=ALU.add), r=["eg4"], w=["q_sume"])
            P.op("dve", lambda e: e.reciprocal(out=grpw, in_=sume), r=["q_sume"], w=["q_grpw"])
            P.op("dve", lambda e: e.tensor_tensor(out=ohg4, in0=lg4[:, :, 0:4], in1=bcx(gmax, 4), op=ALU.is_equal),
                 r=["lg4", "q_gmax"], w=["ohg4"])
            P.op("dve", lambda e: e.tensor_scalar(out=ohg4, in0=ohg4, scalar1=-1.0, scalar2=BIG, op0=ALU.add, op1=ALU.mult),
                 r=["ohg4"], w=["ohg4"])
            P.op("dve", lambda e: e.tensor_tensor(out=lgm4.rearrange("p s (g k) -> p s g k", g=4),
                                                  in0=lg4[:, :, 4:36].rearrange("p s (g k) -> p s g k", g=4),
                                                  in1=ohg4.unsqueeze(3).to_broadcast([128, RG, 4, 8]), op=ALU.add),
                 r=["lg4", "ohg4"], w=["lgm4"])
            P.op("dve", lambda e: e.tensor_reduce(out=m1, in_=lgm4, axis=AX.X, op=ALU.max), r=["lgm4"], w=["q_m1"])
            P.op("dve", lambda e: e.tensor_tensor(out=oh1, in0=lgm4, in1=bcx(m1, 32), op=ALU.is_equal),
                 r=["lgm4", "q_m1"], w=K1)
            P.op("dve", lambda e: e.scalar_tensor_tensor(out=lgm24, in0=oh1, scalar=-BIG, in1=lgm4, op0=ALU.mult,
                                                         op1=ALU.add), r=K1 + ["lgm4"], w=["lgm24"])
            P.op("dve", lambda e: e.tensor_reduce(out=m2, in_=lgm24, axis=AX.X, op=ALU.max), r=["lgm24"], w=["q_m2"])
            P.op("dve", lambda e: e.tensor_tensor(out=oh2, in0=lgm24, in1=bcx(m2, 32), op=ALU.is_equal),
                 r=["lgm24", "q_m2"], w=K2)
            P.op("dve", lambda e: e.tensor_tensor(out=d21, in0=m2, in1=m1, op=ALU.subtract), r=["q_m1", "q_m2"], w=["q_d21"])
            P.op("act", lambda e: e.activation(out=p2, in_=d21, func=AF.Exp), r=["q_d21"], w=["q_p2"])
            P.op("dve", lambda e: e.tensor_scalar(out=den, in0=p2, scalar1=1.0, scalar2=None, op0=ALU.add),
                 r=["q_p2"], w=["q_den"])
            P.op("dve", lambda e: e.reciprocal(out=rden, in_=den), r=["q_den"], w=["q_rden"])
            P.op("dve", lambda e: e.tensor_tensor(out=gA, in0=grpw, in1=rden, op=ALU.mult), r=["q_grpw", "q_rden"], w=KA)
            P.op("dve", lambda e: e.tensor_tensor(out=gB, in0=gA, in1=p2, op=ALU.mult), r=KA + ["q_p2"], w=KB)
            P.op("pool", lambda e: e.tensor_tensor(out=OHall[:, st0:st0 + RG, :], in0=oh1, in1=oh2, op=ALU.add),
                 r=K1 + K2, w=KO)

        for g_ in range(NSTT // RG):
            router4(g_)

        pcnt, pcntk = rot.get()
        for st in range(NSTT):
            P.op("pe", lambda e, st=st: e.matmul(pcnt[:, 0:32], lhsT=onesb, rhs=OHall[:, st, :],
                                                 start=(st == 0), stop=(st == NSTT - 1)),
                 r=[f"OH{st}", "onesb"], w=[pcntk])
        P.op("act", lambda e: e.activation(out=cnt, in_=pcnt[:, 0:32], func=AF.Copy), r=[pcntk], w=["cnt"])
        big3 = big[:, 0:32 * 64].rearrange("p (e k) -> p e k", e=32)
        P.op("dve", lambda e: e.tensor_tensor(out=big3, in0=cnt.unsqueeze(2).to_broadcast([128, 32, 64]),
                                              in1=thr.unsqueeze(1).to_broadcast([128, 32, 64]), op=ALU.is_gt),
             r=["cnt", "cb"], w=["big"])
        P.op("dve", lambda e: e.tensor_reduce(out=nblk, in_=big3, axis=AX.X, op=ALU.add), r=["big"], w=["nblk"])
        big3b = big[:, 0:1024].rearrange("p (e f) -> p e f", e=32)
        P.op("dve", lambda e: e.tensor_tensor(out=big3b, in0=nblk.unsqueeze(1).to_broadcast([128, 32, 32]),
                                              in1=SLc.rearrange("p (e f) -> p e f", e=32), op=ALU.mult),
             r=["nblk", "cb", "big"], w=["big"])
        P.op("dve", lambda e: e.tensor_reduce(out=pstb, in_=big3b, axis=AX.X, op=ALU.add), r=["big"], w=["pstb"])
        P.op("dve", lambda e: e.tensor_tensor(out=pend, in0=pstb, in1=nblk, op=ALU.add), r=["pstb", "nblk"], w=["pend"])
        P.op("dve", lambda e: e.tensor_scalar(out=base, in0=pstb, scalar1=128.0, scalar2=None, op0=ALU.mult),
             r=["pstb"], w=["base"])
        big3c = big.rearrange("p (b e) -> p b e", b=NBLK)
        P.op("dve", lambda e: e.tensor_tensor(out=big3c, in0=pend.unsqueeze(1).to_broadcast([128, NBLK, 32]),
                                              in1=blkio.unsqueeze(2).to_broadcast([128, NBLK, 32]), op=ALU.is_le),
             r=["pend", "cb", "big", "pstb"], w=["big"])
        P.op("dve", lambda e: e.tensor_reduce(out=bexp, in_=big3c, axis=AX.X, op=ALU.add), r=["big"], w=["bexp"])
        P.op("dve", lambda e: e.tensor_scalar(out=bskip, in0=bexp, scalar1=float(NEXP) - 0.5, scalar2=None, op0=ALU.is_ge),
             r=["bexp"], w=["bskip"])
        P.op("dve", lambda e: e.tensor_tensor(out=bsame[:, NW:NBLK], in0=bexp[:, NW:NBLK], in1=bexp[:, 0:NBLK - NW],
                                              op=ALU.is_equal), r=["bexp"], w=["bsame"])
        P.op("dve", lambda e: e.tensor_tensor(out=bskip[:, NW:NBLK], in0=bskip[:, NW:NBLK], in1=bsame[:, NW:NBLK],
                                              op=ALU.max), r=["bskip", "bsame"], w=["bskip"])
        P.op("dve", lambda e: e.tensor_scalar(out=bexp, in0=bexp, scalar1=float(NEXP - 1), scalar2=128.0,
                                              op0=ALU.min, op1=ALU.mult), r=["bexp"], w=["bexp"])
        P.op("dve", lambda e: e.tensor_scalar(out=bexp, in0=bexp, scalar1=iotaP, scalar2=None, op0=ALU.add),
             r=["bexp", "cb"], w=["bexp"])
        P.op("dve", lambda e: e.scalar_tensor_tensor(out=bexp, in0=bskip, scalar=1.0e6, in1=bexp, op0=ALU.mult,
                                                     op1=ALU.add), r=["bexp", "bskip"], w=["bexp"])
        P.op("dve", lambda e: e.tensor_copy(out=IDXW, in_=bexp), r=["bexp"], w=["IDXW"])
        for st in range(NSTT):
            prk, prkk = rot.get()
            P.op("pe", lambda e, st=st, prk=prk: e.matmul(prk[:, 0:32], lhsT=SUb, rhs=OHall[:, st, :],
                                                          start=True, stop=(st == 0)), r=[f"OH{st}", "SUb"], w=[prkk])
            for s2 in range(st):
                P.op("pe", lambda e, s2=s2, st=st, prk=prk: e.matmul(prk[:, 0:32], lhsT=onesb, rhs=OHall[:, s2, :],
                                                                     start=False, stop=(s2 == st - 1)),
                     r=[f"OH{s2}", "onesb"], w=[prkk])
            P.op("dve", lambda e, prk=prk: e.tensor_tensor(out=posv, in0=prk[:, 0:32], in1=base, op=ALU.add),
                 r=[prkk, "base"], w=["posv"])
            for k, oha in ((0, oh1all), (1, oh2all)):
                P.op("dve", lambda e, st=st, oha=oha: e.tensor_tensor(out=ptmp, in0=posv, in1=oha[:, st, :], op=ALU.mult),
                     r=["posv", f"oh1_{st}", f"oh2_{st}"], w=["ptmp"])
                P.op("dve", lambda e, st=st, k=k: e.tensor_reduce(out=POSf[:, st, k:k + 1], in_=ptmp, axis=AX.X, op=ALU.add),
                     r=["ptmp"], w=["POSf"])
        P.op("dve", lambda e: e.tensor_copy(out=POSi, in_=POSf), r=["POSf"], w=["POSi"])

        if stage == "pbdbg":
            P.dma("sp", lambda e: e.dma_start(out=dbg_out["cnt"], in_=cnt), r=["cnt"], w=["dg1"])
            P.dma("sp", lambda e: e.dma_start(out=dbg_out["nblk"], in_=nblk), r=["nblk"], w=["dg2"])
            P.dma("sp", lambda e: e.dma_start(out=dbg_out["pstb"], in_=pstb), r=["pstb"], w=["dg3"])
            P.dma("sp", lambda e: e.dma_start(out=dbg_out["bexp"], in_=bexp), r=["bexp"], w=["dg4"])
            P.dma("sp", lambda e: e.dma_start(out=dbg_out["posf"], in_=POSf.rearrange("p a b -> p (a b)")), r=["POSf"], w=["dg5"])
            P.dma("sp", lambda e: e.dma_start(out=dbg_out["oh1"], in_=oh1all.rearrange("p a b -> p (a b)")),
                  r=[f"oh1_{st}" for st in range(NSTT)], w=["dg6"])
            P.dma("sp", lambda e: e.dma_start(out=dbg_out["oh2"], in_=oh2all.rearrange("p a b -> p (a b)")),
                  r=[f"oh2_{st}" for st in range(NSTT)], w=["dg7"])
            P.dma("sp", lambda e: e.dma_start(out=dbg_out["gab"], in_=gAB.rearrange("p a b -> p (a b)")),
                  r=[f"gA{st}" for st in range(NSTT)] + [f"gB{st}" for st in range(NSTT)], w=["dg8"])
            P.emit()
            return nc
        for st in range(NSTT):
            b = st // (NSTT // NB)
            xq = xb[st % 3]
            xk = f"xb{st % 3}"
            P.dma("sp", lambda e, st=st, xq=xq: e.dma_start(out=xq, in_=r_s[st * 128:(st + 1) * 128, :]), w=[xk])
            P.op("dve", lambda e, xq=xq, b=b: e.tensor_tensor(out=xq, in0=xq, in1=sc2r[:, b, :], op=ALU.mult),
                 r=[xk, "sc2r"], w=[xk])
            P.op("dve", lambda e, xq=xq, b=b: e.tensor_tensor(out=xq, in0=xq, in1=sh2r[:, b, :], op=ALU.add),
                 r=[xk, "sh2r"], w=[xk])
            for k in range(2):
                P.dma("pool", lambda e, st=st, k=k, xq=xq: e.indirect_dma_start(
                    out=xrows_s, out_offset=bass.IndirectOffsetOnAxis(ap=POSi[:, st, k:k + 1], axis=0),
                    in_=xq, in_offset=None), r=[xk, "POSi"], w=[f"xrows{st}_{k}"])
        if stage in ("pbs1", "pbs2"):
            P.emit()
            return nc

        pgu_banks = PsumRot(BANKS[0:2])
        NQ = 3
        wgate_rows = w_gate.rearrange("e (p k) n -> (e p) (k n)", k=8)
        wup_rows = w_up.rearrange("e (p k) n -> (e p) (k n)", k=8)
        wdn_rows = w_down.rearrange("e (p k) n -> (e p) (k n)", k=2)

        def blk_loadx(bi):
            q = bi % NQ
            P.dma("sp", lambda e: e.dma_start(out=xb[q], in_=xrows_s[bi * 128:(bi + 1) * 128, :]),
                  r=[f"xrows{st}_{k}" for st in range(NSTT) for k in range(2)], w=[f"xb{q}"])

        bnd = {}

        def bnd_reg(e):
            if "r" not in bnd:
                bnd["r"] = e.alloc_register("wbound")
                e.reg_mov(bnd["r"], NEXP * 128 - 1)
            return bnd["r"]

        def blk_load(bi):
            slot = bi % NW
            ioff = bass.IndirectOffsetOnAxis(ap=IDXW[:, bi:bi + 1], axis=0)
            P.dma("pool", lambda e: e.indirect_dma_start(out=Wg[slot], out_offset=None,
                                                         in_=wgate_rows, in_offset=ioff, bounds_check=bnd_reg(e), oob_is_err=False), r=["IDXW"], w=[f"wg{slot}"])
            P.dma("pool", lambda e: e.indirect_dma_start(out=Wu[slot], out_offset=None,
                                                         in_=wup_rows, in_offset=ioff, bounds_check=bnd_reg(e), oob_is_err=False), r=["IDXW"], w=[f"wu{slot}"])
            P.dma("pool", lambda e: e.indirect_dma_start(out=Wd[slot], out_offset=None,
                                                         in_=wdn_rows, in_offset=ioff, bounds_check=bnd_reg(e), oob_is_err=False), r=["IDXW"], w=[f"wd{slot}"])

        xt_banks = [BANKS[2], BANKS[3]]
        ht_banks = PsumRot(BANKS[6:8])

        def blk_xt(bi):
            q = bi % NQ
            xv = xb[q].rearrange("r (p k) -> r k p", k=8)
            xTf = xT[q].rearrange("p a b -> p (a b)")
            for hb_ in range(2):
                pt, ptk = xt_banks[hb_]
                for k4 in range(4):
                    kc = hb_ * 4 + k4
                    P.op("pe", lambda e, kc=kc, k4=k4, pt=pt: e.transpose(out=pt[:, k4 * 128:(k4 + 1) * 128],
                                                                          in_=xv[:, kc, :], identity=C["ident"]),
                         r=[f"xb{q}"], w=[ptk])
                if hb_ == 0:
                    P.op("act", lambda e, pt=pt: e.activation(out=xTf[:, 0:512], in_=pt, func=AF.Copy),
                         r=[ptk], w=[f"xTa{q}"])
                else:
                    P.op("dve", lambda e, pt=pt: e.tensor_copy(out=xTf[:, 512:1024], in_=pt), r=[ptk], w=[f"xTb{q}"])

        def blk_gu(bi):
            slot = bi % NW
            q = bi % NQ
            pgu, pguk = pgu_banks.get()
            for (Wm, wk, c0) in ((Wg, f"wg{slot}", 0), (Wu, f"wu{slot}", 256)):
                for kc in range(8):
                    P.op("pe", lambda e, kc=kc, Wm=Wm, c0=c0: e.matmul(
                        pgu[:, c0:c0 + 256], lhsT=xT[q][:, kc, :], rhs=Wm[slot][:, kc * 256:(kc + 1) * 256],
                        start=(kc == 0), stop=(kc == 7)), r=[f"xTa{q}", f"xTb{q}", wk], w=[pguk])
            P.op("act", lambda e: e.activation(out=sg[q], in_=pgu[:, 0:256], func=AF.Exp, scale=-1.0), r=[pguk], w=[f"sg{q}"])
            P.op("act", lambda e: e.activation(out=sg[q], in_=sg[q], func=AF.Ln, bias=1.0), r=[f"sg{q}"], w=[f"sg{q}"])
            P.op("act", lambda e: e.activation(out=sg[q], in_=sg[q], func=AF.Exp, scale=-1.0), r=[f"sg{q}"], w=[f"sg{q}"])
            P.op("dve", lambda e: e.tensor_tensor(out=sg[q], in0=pgu[:, 0:256], in1=sg[q], op=ALU.mult),
                 r=[pguk, f"sg{q}"], w=[f"sg{q}"])
            P.op("dve", lambda e: e.tensor_tensor(out=hid[q], in0=pgu[:, 256:512], in1=sg[q], op=ALU.mult),
                 r=[pguk, f"sg{q}"], w=[f"hid{q}"])

        def blk_tr(bi):
            q = bi % NQ
            pht, phtk = ht_banks.get()
            hv = hid[q].rearrange("r (p k) -> r k p", k=2)
            for k2 in range(2):
                P.op("pe", lambda e, k2=k2: e.transpose(out=pht[:, k2 * 128:(k2 + 1) * 128], in_=hv[:, k2, :],
                                                        identity=C["ident"]), r=[f"hid{q}"], w=[phtk])
            P.op("act", lambda e: e.activation(out=hidT[q], in_=pht[:, 0:256], func=AF.Copy), r=[phtk], w=[f"hidT{q}"])

        def blk_dn(bi):
            slot = bi % NW
            q = bi % NQ
            yq = bi % 2
            py, (k0, k1) = PS2[2], ("bank4", "bank5")
            for half in range(2):
                for k2 in range(2):
                    P.op("pe", lambda e, half=half, k2=k2: e.matmul(
                        py[:, half * 512:(half + 1) * 512], lhsT=hidT[q][:, k2 * 128:(k2 + 1) * 128],
                        rhs=Wd[slot][:, k2 * 1024 + half * 512:k2 * 1024 + (half + 1) * 512], start=(k2 == 0), stop=(k2 == 1)),
                        r=[f"hidT{q}", f"wd{slot}"], w=[(k0, k1)[half]])
            P.op("act", lambda e: e.activation(out=yb[yq][:, 0:512], in_=py[:, 0:512], func=AF.Copy), r=[k0], w=[f"yba{yq}"])
            P.op("dve", lambda e: e.tensor_copy(out=yb[yq][:, 512:1024], in_=py[:, 512:1024]), r=[k1], w=[f"ybb{yq}"])
            P.dma("sp", lambda e: e.dma_start(out=yrows_s[bi * 128:(bi + 1) * 128, :], in_=yb[yq]),
                  r=[f"yba{yq}", f"ybb{yq}"], w=[f"yrows{bi}"])

        nb_run = NBLK if stage != 'pbs3' else 6
        pendq = []
        blk_load(0)
        for b0_ in range(min(2, nb_run)):
            blk_loadx(b0_)
        for bi in range(nb_run):
            if bi + 2 < nb_run:
                blk_loadx(bi + 2)
            blk_xt(bi)
            blk_gu(bi)
            pendq.append(bi)
            if len(pendq) >= 2:
                blk_tr(pendq[-2])
            if len(pendq) >= 3:
                blk_dn(pendq.pop(0))
            if bi + 1 < nb_run:
                blk_load(bi + 1)
        if len(pendq) == 2:
            blk_dn(pendq.pop(0))
        while pendq:
            a = pendq.pop(0)
            blk_tr(a)
            blk_dn(a)
        YK = [f"yrows{bi}" for bi in range(nb_run)]
        if stage == "pbs3":
            P.emit()
            return nc

        def comb_fetch(st):
            q = st % 2
            P.dma("sp", lambda e: e.dma_start(out=rr[q], in_=r_s[st * 128:(st + 1) * 128, :]), w=[f"rr{q}"])
            P.dma("pool", lambda e: e.indirect_dma_start(
                out=y1[q], out_offset=None, in_=yrows_s,
                in_offset=bass.IndirectOffsetOnAxis(ap=POSi[:, st, 0:1], axis=0)), r=YK + ["POSi"], w=[f"y1_{q}"])
            P.dma("pool", lambda e: e.indirect_dma_start(
                out=y2[q], out_offset=None, in_=yrows_s,
                in_offset=bass.IndirectOffsetOnAxis(ap=POSi[:, st, 1:2], axis=0)), r=YK + ["POSi"], w=[f"y2_{q}"])

        def comb_compute(st):
            q = st % 2
            b = st // (NSTT // NB)
            P.op("act", lambda e: e.activation(out=y1[q], in_=y1[q], func=AF.Copy, scale=gAB[:, st, 0:1]),
                 r=[f"y1_{q}", f"gA{st}"], w=[f"y1_{q}"])
            P.op("dve", lambda e: e.scalar_tensor_tensor(out=y1[q], in0=y2[q], scalar=gAB[:, st, 1:2], in1=y1[q],
                                                         op0=ALU.mult, op1=ALU.add),
                 r=[f"y1_{q}", f"y2_{q}", f"gB{st}"], w=[f"y1_{q}"])
            P.op("dve", lambda e: e.tensor_tensor(out=y1[q], in0=y1[q], in1=gate2[:, b, :], op=ALU.mult),
                 r=[f"y1_{q}", "gate2"], w=[f"y1_{q}"])
            P.op("dve", lambda e: e.tensor_tensor(out=rr[q], in0=rr[q], in1=y1[q], op=ALU.add),
                 r=[f"rr{q}", f"y1_{q}"], w=[f"rr{q}"])
            for hf in range(2):
                P.op("dve", lambda e, hf=hf: e.bn_stats(out=bst2[:, hf, :], in_=rr[q][:, hf * 512:(hf + 1) * 512]),
                     r=[f"rr{q}"], w=["bst2"])
            P.op("dve", lambda e: e.bn_aggr(out=mv2[:, 0:2], in_=bst2.rearrange("p a b -> p (a b)")), r=["bst2"], w=["mv2"])
            P.op("act", lambda e: e.activation(out=mv2[:, 2:3], in_=mv2[:, 1:2], func=AF.Ln, bias=1e-5),
                 r=["mv2"], w=["mv2b"])
            P.op("act", lambda e: e.activation(out=mv2[:, 3:4], in_=mv2[:, 2:3], func=AF.Exp, scale=-0.5),
                 r=["mv2b"], w=["mv2c"])
            P.op("dve", lambda e: e.scalar_tensor_tensor(out=mv2[:, 2:3], in0=mv2[:, 0:1], scalar=-1.0, in1=mv2[:, 3:4],
                                                         op0=ALU.mult, op1=ALU.mult), r=["mv2", "mv2b", "mv2c"], w=["mv2b", "mv2d"])
            P.op("act", lambda e: e.activation(out=xh2, in_=rr[q], func=AF.Identity, scale=mv2[:, 3:4], bias=mv2[:, 2:3]),
                 r=[f"rr{q}", "mv2c", "mv2d"], w=["xh2"])
            oq = ob2[q]
            P.op("pool", lambda e: e.tensor_tensor(out=oq, in0=xh2, in1=g2, op=ALU.mult), r=["xh2", "g2"], w=[f"ob{q}"])
            P.op("pool", lambda e: e.tensor_tensor(out=oq, in0=oq, in1=b2, op=ALU.add), r=[f"ob{q}", "b2"], w=[f"ob{q}"])
            P.dma("sp", lambda e: e.dma_start(out=out[st * 128:(st + 1) * 128, :], in_=oq), r=[f"ob{q}"], w=["outd"])

        ob2 = [ob, yb[0]]
        comb_fetch(0)
        for st in range(NSTT):
            if st + 1 < NSTT:
                comb_fetch(st + 1)
            comb_compute(st)
        cnt_ = P.emit()
        print("phaseB op counts", cnt_)

    return nc


_NC_CACHE = {}


def _get_nc():
    if "nc" not in _NC_CACHE:
        _NC_CACHE["nc"] = build("full")
    return _NC_CACHE["nc"]


def make_in_maps(inputs):
    f = lambda a: np.ascontiguousarray(np.asarray(a, dtype=np.float32))
    shared = {
        "w_ada": f(inputs["w_ada"][0]), "b_ada": f(inputs["b_ada"]), "w_in": f(inputs["w_in"][0]),
        "conv_w": f(inputs["conv_w"][0]), "conv_norm_w": f(inputs["conv_norm_w"]),
        "dn_conv_w": f(inputs["dn_conv_w"][0]), "dn_A_log": f(inputs["dn_A_log"]),
        "dn_dt_bias": f(inputs["dn_dt_bias"]), "dn_norm_w": f(inputs["dn_norm_w"]),
        "w_out": f(inputs["w_out"][0]), "ln1_g": f(inputs["ln1_g"]), "ln1_b": f(inputs["ln1_b"]),
        "w_grp": f(inputs["w_grp"][0]), "b_grp": f(inputs["b_grp"]), "w_exp": f(inputs["w_exp"][0]),
        "b_exp": f(inputs["b_exp"]), "w_gate": f(inputs["w_gate"][0]), "w_up": f(inputs["w_up"][0]),
        "w_down": f(inputs["w_down"][0]), "ln2_g": f(inputs["ln2_g"]), "ln2_b": f(inputs["ln2_b"]),
        "cmat": CMAT, "mab": MAB, "cb": CB,
    }
    xs = f(inputs["x"]).reshape(8, TOK, D)
    cs = f(inputs["c"]).reshape(8, NB, D)
    return [dict(shared, x=xs[i], c=cs[i]) for i in range(8)]


def kernel(**inputs):
    nc = _get_nc()
    in_maps = make_in_maps(inputs)
    res = run_bass_kernel_spmd(nc, in_maps, core_ids=list(range(8)))
    outs = [np.asarray(r["out"], dtype=np.float32).reshape(NB, SEQ, D) for r in res.results]
    return np.concatenate(outs, axis=0)
```

```python
import contextlib
import numpy as np
import concourse.bass as bass
import concourse.mybir as mybir
from concourse.bass_utils import run_bass_kernel_spmd

F32 = mybir.dt.float32
BF16 = mybir.dt.bfloat16
AF = mybir.ActivationFunctionType
ALU = mybir.AluOpType
AX = mybir.AxisListType

ENGS = ("pe", "act", "dve", "pool", "sp")

D = 1024
SEQ = 2048
NB = 2
TOK = NB * SEQ
DIN = 3592
NEXP = 32
ALPHA = 2.0 ** 0.25
TT = 256
NT = TOK // TT
BIG = 30000.0


class Prog:
    NDMA = 24

    def __init__(self, nc, tag):
        self.nc = nc
        self.tag = tag
        self.ops = []
        self.chain_dma = False

    def op(self, eng, fn, r=(), w=()):
        self.ops.append(dict(eng=eng, fn=fn, r=tuple(r), w=tuple(w), dma=False))

    def dma(self, eng, fn, r=(), w=()):
        chain = (f"__q_{eng}",) if self.chain_dma else ()
        self.ops.append(dict(eng=eng, fn=fn, r=tuple(r), w=tuple(w) + chain, dma=True))

    def emit(self, final_wait_engine="sp"):
        nc = self.nc
        esem = {e: nc.alloc_semaphore(f"s_{e}_{self.tag}") for e in ENGS if e != "sp"}
        dsem = [nc.alloc_semaphore(f"d_{i}_{self.tag}") for i in range(self.NDMA)]
        ecount = {e: 0 for e in ENGS}
        dtotal = [0] * self.NDMA
        dnext = 0
        last_w = {}
        readers = {}
        waited = {e: {} for e in ENGS}
        per_eng = {e: [] for e in ENGS}
        tokens = []
        for i, o in enumerate(self.ops):
            E = o["eng"]
            deps = set()
            for k in o["r"]:
                if k in last_w:
                    deps.add(last_w[k])
            for k in o["w"]:
                if k in last_w:
                    deps.add(last_w[k])
                for rd in readers.get(k, ()):
                    deps.add(rd)
            waits = []
            for d in sorted(deps):
                od = self.ops[d]
                if (not od["dma"]) and od["eng"] == E and E == "pe" and not o["dma"]:
                    continue
                s, v = tokens[d]
                key = id(s)
                if waited[E].get(key, 0) >= v:
                    continue
                waited[E][key] = v
                waits.append((s, v))
            if o["dma"]:
                j = dnext
                dnext = (dnext + 1) % self.NDMA
                s = dsem[j]
                if dtotal[j] > 0 and waited[E].get(id(s), 0) < dtotal[j]:
                    waited[E][id(s)] = dtotal[j]
                    waits.append((s, dtotal[j]))
                dtotal[j] += 16
                tok = (s, dtotal[j])
                inc = 16
            else:
                ecount[E] += 1
                tok = (esem[E], ecount[E])
                inc = 1
            tokens.append(tok)
            per_eng[E].append((waits, o["fn"], tok[0], inc))
            for k in o["r"]:
                readers.setdefault(k, []).append(i)
            for k in o["w"]:
                last_w[k] = i
                readers[k] = []
        final_waits = [(esem[e], ecount[e]) for e in esem if ecount[e] > 0]
        final_waits += [(dsem[j], dtotal[j]) for j in range(self.NDMA) if dtotal[j] > 0]

        with nc.Block() as block:
            def mk(ename):
                def body(eng):
                    for waits, fn, s, inc in per_eng[ename]:
                        for (ws, wv) in waits:
                            eng.wait_ge(ws, wv)
                        ins = fn(eng)
                        ins.then_inc(s, inc)
                    if ename == final_wait_engine:
                        for (ws, wv) in final_waits:
                            eng.wait_ge(ws, wv)
                return body
            block.tensor(mk("pe"))
            block.scalar(mk("act"))
            block.vector(mk("dve"))
            block.gpsimd(mk("pool"))
            block.sync(mk("sp"))
        return dict(ecount)


class PsumRot:
    def __init__(self, items):
        self.items = list(items)
        self.i = 0

    def get(self):
        it = self.items[self.i]
        self.i = (self.i + 1) % len(self.items)
        return it


def make_consts():
    idx = np.arange(128)
    same = (idx[:, None] // 64) == (idx[None, :] // 64)
    c = {}
    c["ident"] = np.eye(128)
    c["ltb"] = (same & (idx[:, None] <= idx[None, :])) * 1.0
    c["bd"] = same * 1.0
    c["mnu"] = np.where(same & (idx[:, None] <= idx[None, :]), 0.0, -BIG)
    c["mnus"] = np.where(same & (idx[:, None] < idx[None, :]), 0.0, -BIG)
    c["mnls"] = np.where(same & (idx[:, None] > idx[None, :]), 0.0, -BIG)
    c["ones"] = np.ones((128, 128))
    c["gmat"] = same / 64.0
    c["omean"] = np.ones((128, 128)) / 128.0
    names = ["ident", "ltb", "bd", "mnu", "mnus", "mnls", "ones", "gmat", "omean"]
    cm = np.concatenate([c[n] for n in names], axis=1).astype(np.float32)
    mab = np.stack([(idx < 64) * 1.0, (idx >= 64) * 1.0], axis=1).astype(np.float32)
    return names, cm, mab


CNAMES, CMAT, MAB = make_consts()
NBLK = TOK * 2 // 128 + NEXP
NROWS = NBLK * 128


def make_consts_b():
    thr = np.broadcast_to(128.0 * np.arange(64)[None, :], (128, 64))
    e = np.arange(32)
    sl = np.broadcast_to((e[None, :] < e[:, None]).astype(np.float64).reshape(1, 1024), (128, 1024))
    blk = np.broadcast_to(np.arange(NBLK, dtype=np.float64)[None, :], (128, NBLK))
    iop = np.arange(128, dtype=np.float64)[:, None]
    idx = np.arange(128)
    su = (idx[:, None] < idx[None, :]) * 1.0
    return np.concatenate([thr, sl, blk, iop, su], axis=1).astype(np.float32)


CB = make_consts_b()


def build(stage="full", dbg=()):
    nc = bass.Bass("TRN2", target_bir_lowering=False)

    def din(name, shape, dt=F32):
        return nc.dram_tensor(name, list(shape), dt, kind="ExternalInput").ap()

    x = din("x", [TOK, D])
    c_in = din("c", [NB, D])
    w_ada = din("w_ada", [D, 6 * D])
    b_ada = din("b_ada", [1, 6 * D])
    w_in = din("w_in", [D, DIN])
    conv_w = din("conv_w", [3, 512])
    conv_norm_w = din("conv_norm_w", [1, 512])
    dn_conv_w = din("dn_conv_w", [4, 1536])
    dn_A_log = din("dn_A_log", [1, 4])
    dn_dt_bias = din("dn_dt_bias", [1, 4])
    dn_norm_w = din("dn_norm_w", [1, 128])
    w_out = din("w_out", [D, D])
    ln1_g = din("ln1_g", [1, D])
    ln1_b = din("ln1_b", [1, D])
    w_grp = din("w_grp", [D, 4])
    b_grp = din("b_grp", [1, 4])
    w_exp = din("w_exp", [D, 32])
    b_exp = din("b_exp", [1, 32])
    w_gate = din("w_gate", [NEXP, D, 256])
    w_up = din("w_up", [NEXP, D, 256])
    w_down = din("w_down", [NEXP, 256, D])
    ln2_g = din("ln2_g", [1, D])
    ln2_b = din("ln2_b", [1, D])
    cmat_d = din("cmat", list(CMAT.shape))
    mab_d = din("mab", [128, 2])
    cb_d = din("cb", list(CB.shape))
    out = nc.dram_tensor("out", [TOK, D], F32, kind="ExternalOutput").ap()
    r_s = nc.dram_tensor("r_scr", [TOK, D], F32, kind="Internal").ap()
    h2_s = nc.dram_tensor("h2_scr", [128, 8, TOK], BF16, kind="Internal").ap()
    g2_s = nc.dram_tensor("g2_scr", [128, 2 * D], F32, kind="Internal").ap()
    sh2_s = nc.dram_tensor("sh2_scr", [128, 2 * D], F32, kind="Internal").ap()
    sc2_s = nc.dram_tensor("sc2_scr", [128, 2 * D], F32, kind="Internal").ap()
    xrows_s = nc.dram_tensor("xrows_scr", [NROWS if stage != "pa1" else 128, D], F32, kind="Internal").ap()
    yrows_s = nc.dram_tensor("yrows_scr", [NROWS if stage != "pa1" else 128, D], F32, kind="Internal").ap()
    dbg_out = {}
    for (nm, shp) in dbg:
        dbg_out[nm] = nc.dram_tensor("dbg_" + nm, list(shp), F32, kind="ExternalOutput").ap()

    def sb(name, shape, dt=F32):
        return nc.alloc_sbuf_tensor("sb_" + name, list(shape), dt).ap()

    PS2 = [nc.alloc_psum_tensor(f"ps2_{i}", [128, 1024], F32).ap() for i in range(4)]
    BANKS = []
    for i in range(4):
        BANKS.append((PS2[i][:, 0:512], f"bank{2 * i}"))
        BANKS.append((PS2[i][:, 512:1024], f"bank{2 * i + 1}"))

    cm = sb("cm", [128, CMAT.shape[1]])
    C = {n: cm[:, i * 128:(i + 1) * 128] for i, n in enumerate(CNAMES)}
    mab = sb("mab", [128, 2])
    identb = sb("identb", [128, 128], BF16)
    modT = sb("modT", [128, 48, 2])
    s1p = sb("s1p", [128, 8, 2])
    A2 = sb("A2", [128, 8, 2])
    B2 = sb("B2", [128, 8, 2])
    gate_bc = {2: sb("gate1bc", [128, 2, D])}
    cw = sb("cw", [128, 4, 3])
    cnw = sb("cnw", [128, 4])
    dcw = sb("dcw", [128, 12, 4])
    dnw = sb("dnw", [128, 1])
    g1T = sb("g1T", [128, 8])
    b1T = sb("b1T", [128, 8])
    negA = sb("negA", [128, 4])
    dtb = sb("dtb", [128, 4])
    ag = sb("ag", [128, D])
    ab = sb("ab", [128, D])

    esA = contextlib.ExitStack()

    def tsbA(name, shape, dt=F32):
        return esA.enter_context(nc.sbuf_tensor("a_" + name, list(shape), dt)).ap()

    Win = tsbA("win", [128, 8, DIN], BF16)
    Wout = tsbA("wout", [128, 8, D], BF16)

    P = Prog(nc, "p0")
    P.dma("sp", lambda e: e.dma_start(out=cm, in_=cmat_d), w=["cm"])
    P.dma("sp", lambda e: e.dma_start(out=mab, in_=mab_d), w=["mab"])
    P.op("dve", lambda e: e.tensor_copy(out=identb, in_=C["ident"]), r=["cm"], w=["identb"])

    with nc.sbuf_tensor("t_cT", [128, 8, 2], F32) as cT_h, \
            nc.sbuf_tensor("t_cact", [128, 8, 2], BF16) as cact_h, \
            nc.sbuf_tensor("t_cbc", [128, 8, 2, 128], BF16) as cbc_h, \
            nc.sbuf_tensor("t_brow", [1, 6 * D], BF16) as brow_h, \
            nc.sbuf_tensor("t_onesr", [1, 128], BF16) as onesr_h, \
            nc.sbuf_tensor("t_wa0", [128, 8, D], BF16) as wa0_h, \
            nc.sbuf_tensor("t_wa1", [128, 8, D], BF16) as wa1_h, \
            nc.sbuf_tensor("t_ws0", [128, 8, 512], F32) as ws0_h, \
            nc.sbuf_tensor("t_ws1", [128, 8, 512], F32) as ws1_h, \
            nc.sbuf_tensor("t_sp2", [128, 8, 2], F32) as sp2_h, \
            nc.sbuf_tensor("t_g2bc", [128, 2, D], F32) as g2bc_h, \
            nc.sbuf_tensor("t_sh2bc", [128, 2, D], F32) as sh2bc_h, \
            nc.sbuf_tensor("t_sc2bc", [128, 2, D], F32) as sc2bc_h:
        gate_bc[5] = g2bc_h.ap()
        gate_bc[3] = sh2bc_h.ap()
        gate_bc[4] = sc2bc_h.ap()
        cT, cact, cbc, brow, onesr, sp2 = (t.ap() for t in (cT_h, cact_h, cbc_h, brow_h, onesr_h, sp2_h))
        wa = [wa0_h.ap(), wa1_h.ap()]
        wstg = [ws0_h.ap(), ws1_h.ap()]
        for b in range(NB):
            P.dma("sp", lambda e, b=b: e.dma_start(
                out=cT[:, :, b], in_=c_in[b, :].rearrange("(kc p) -> p kc", p=128),
                allow_slow_non_contiguous=True), w=["cT"])
        P.op("act", lambda e: e.activation(out=cact, in_=cT, func=AF.Silu), r=["cT"], w=["cact"])
        P.op("dve", lambda e: e.tensor_copy(out=cbc, in_=cact.unsqueeze(3).to_broadcast([128, 8, 2, 128])),
             r=["cact"], w=["cbc"])
        P.dma("pool", lambda e: e.dma_start(out=brow, in_=b_ada), w=["brow"])
        for kc in range(8):
            for (c0, c1) in ((0, 2048), (2048, DIN)):
                P.dma("pool", lambda e, kc=kc, c0=c0, c1=c1: e.dma_start(
                    out=Win[:, kc, c0:c1], in_=w_in[kc * 128:(kc + 1) * 128, c0:c1]), w=[f"Win{kc}_{c0}"])
            P.dma("pool", lambda e, kc=kc: e.dma_start(
                out=Wout[:, kc, :], in_=w_out[kc * 128:(kc + 1) * 128, :]), w=[f"Wout{kc}"])
        P.op("dve", lambda e: e.memset(onesr, 1.0), w=["onesr"])
        for k in range(3):
            P.dma("sp", lambda e, k=k: e.dma_start(out=cw[:, :, k],
                                                   in_=conv_w[k, :].rearrange("(j p) -> p j", p=128),
                                                   allow_slow_non_contiguous=True), w=["cw"])
        P.dma("sp", lambda e: e.dma_start(out=cnw, in_=conv_norm_w[0, :].rearrange("(j p) -> p j", p=128),
                                          allow_slow_non_contiguous=True), w=["cnw"])
        for k in range(4):
            P.dma("sp", lambda e, k=k: e.dma_start(out=dcw[:, :, k],
                                                   in_=dn_conv_w[k, :].rearrange("(j p) -> p j", p=128),
                                                   allow_slow_non_contiguous=True), w=["dcw"])
        P.dma("sp", lambda e: e.dma_start(out=dnw, in_=dn_norm_w.rearrange("o p -> p o"),
                                          allow_slow_non_contiguous=True), w=["dnw"])
        P.dma("sp", lambda e: e.dma_start(out=g1T, in_=ln1_g[0, :].rearrange("(j p) -> p j", p=128),
                                          allow_slow_non_contiguous=True), w=["g1T"])
        P.dma("sp", lambda e: e.dma_start(out=b1T, in_=ln1_b[0, :].rearrange("(j p) -> p j", p=128),
                                          allow_slow_non_contiguous=True), w=["b1T"])
        P.dma("sp", lambda e: e.dma_start(out=negA, in_=dn_A_log[0, :].partition_broadcast(128)), w=["negA"])
        P.dma("sp", lambda e: e.dma_start(out=dtb, in_=dn_dt_bias[0, :].partition_broadcast(128)), w=["dtb"])
        P.op("act", lambda e: e.activation(out=negA, in_=negA, func=AF.Exp), r=["negA"], w=["negA"])
        P.op("dve", lambda e: e.tensor_scalar(out=negA, in0=negA, scalar1=-1.0, scalar2=None, op0=ALU.mult),
             r=["negA"], w=["negA"])

        ps0 = PsumRot(BANKS[0:1])
        psr = PsumRot(BANKS[1:3])
        modps, modk = ps0.get()
        for j in range(6):
            wj = wa[j % 2]
            wk = f"wa{j % 2}"
            for hf in range(2):
                si = (2 * j + hf) % 2
                stg = wstg[si]
                P.dma("sp", lambda e, j=j, hf=hf, stg=stg: e.dma_start(
                    out=stg, in_=w_ada[:, j * D + hf * 512:j * D + (hf + 1) * 512].rearrange("(kc p) n -> p kc n", p=128)),
                    w=[f"wstg{si}"])
                eng_ = "dve" if hf == 0 else "act"
                if eng_ == "dve":
                    P.op("dve", lambda e, wj=wj, hf=hf, stg=stg: e.tensor_copy(out=wj[:, :, hf * 512:(hf + 1) * 512], in_=stg),
                         r=[f"wstg{si}"], w=[wk + f"_{hf}"])
                else:
                    P.op("act", lambda e, wj=wj, hf=hf, stg=stg: e.activation(out=wj[:, :, hf * 512:(hf + 1) * 512], in_=stg,
                                                                              func=AF.Copy), r=[f"wstg{si}"], w=[wk + f"_{hf}"])
            for cc in range(8):
                col = (j * 8 + cc) * 2
                for kc in range(8):
                    P.op("pe", lambda e, wj=wj, kc=kc, cc=cc, col=col: e.matmul(
                        modps[:, col:col + 2], lhsT=wj[:, kc, cc * 128:(cc + 1) * 128], rhs=cact[:, kc, :],
                        start=(kc == 0), stop=False), r=[wk + "_0", wk + "_1", "cact"], w=[modk])
                P.op("pe", lambda e, j=j, cc=cc, col=col: e.matmul(
                    modps[:, col:col + 2], lhsT=brow[0:1, j * D + cc * 128:j * D + (cc + 1) * 128],
                    rhs=onesr[0:1, 0:2], start=False, stop=True), r=["brow", "onesr"], w=[modk])
            if j in (2, 3, 4, 5):
                for b in range(NB):
                    for half in range(2):
                        pt, pk = psr.get()
                        for kc in range(8):
                            P.op("pe", lambda e, wj=wj, kc=kc, b=b, half=half, pt=pt: e.matmul(
                                pt, lhsT=cbc[:, kc, b, :], rhs=wj[:, kc, half * 512:(half + 1) * 512],
                                start=(kc == 0), stop=False), r=[wk + "_0", wk + "_1", "cbc"], w=[pk])
                        P.op("pe", lambda e, j=j, half=half, pt=pt: e.matmul(
                            pt, lhsT=onesr[0:1, :], rhs=brow[0:1, j * D + half * 512:j * D + (half + 1) * 512],
                            start=False, stop=True), r=["brow", "onesr"], w=[pk])
                        P.op("act", lambda e, j=j, b=b, half=half, pt=pt: e.activation(
                            out=gate_bc[j][:, b, half * 512:(half + 1) * 512], in_=pt, func=AF.Copy),
                            r=[pk], w=[f"gbc{j}"])
        P.op("dve", lambda e: e.tensor_copy(out=modT.rearrange("p a b -> p (a b)"), in_=modps[:, 0:96]),
             r=[modk], w=["modT"])
        P.op("dve", lambda e: e.tensor_scalar(out=s1p, in0=modT[:, 8:16, :], scalar1=1.0, scalar2=None, op0=ALU.add),
             r=["modT"], w=["s1p"])
        P.op("dve", lambda e: e.tensor_scalar(out=sp2, in0=modT[:, 32:40, :], scalar1=1.0, scalar2=None, op0=ALU.add),
             r=["modT"], w=["sp2"])
        P.op("dve", lambda e: e.tensor_tensor(out=A2, in0=sp2, in1=g1T.unsqueeze(2).to_broadcast([128, 8, 2]),
                                              op=ALU.mult), r=["sp2", "g1T"], w=["A2"])
        P.op("dve", lambda e: e.tensor_tensor(out=B2, in0=sp2, in1=b1T.unsqueeze(2).to_broadcast([128, 8, 2]),
                                              op=ALU.mult), r=["sp2", "b1T"], w=["B2"])
        P.op("dve", lambda e: e.tensor_tensor(out=B2, in0=B2, in1=modT[:, 24:32, :], op=ALU.add),
             r=["B2", "modT"], w=["B2"])
        P.dma("sp", lambda e: e.dma_start(out=ag, in_=ln1_g[0, :].partition_broadcast(128)), w=["ag"])
        P.dma("sp", lambda e: e.dma_start(out=ab, in_=ln1_b[0, :].partition_broadcast(128)), w=["ab"])
        P.op("pool", lambda e: e.tensor_scalar(out=ag, in0=ag, scalar1=ALPHA, scalar2=None, op0=ALU.mult),
             r=["ag"], w=["ag"])
        P.op("pool", lambda e: e.tensor_scalar(out=ab, in0=ab, scalar1=ALPHA, scalar2=None, op0=ALU.mult),
             r=["ab"], w=["ab"])
        P.dma("sp", lambda e: e.dma_start(out=g2_s, in_=gate_bc[5].rearrange("p a b -> p (a b)")),
              r=["gbc5"], w=["g2s"])
        P.dma("sp", lambda e: e.dma_start(out=sh2_s, in_=gate_bc[3].rearrange("p a b -> p (a b)")),
              r=["gbc3"], w=["sh2s"])
        P.dma("sp", lambda e: e.dma_start(out=sc2_s, in_=gate_bc[4].rearrange("p a b -> p (a b)")),
              r=["gbc4"], w=["sc2s"])
        if stage == "p0":
            P.dma("sp", lambda e: e.dma_start(out=dbg_out["modT"], in_=modT.rearrange("p a b -> p (a b)")),
                  r=["modT"], w=["dbgo"])
            P.dma("sp", lambda e: e.dma_start(out=dbg_out["g1bc"], in_=gate_bc[2].rearrange("p a b -> p (a b)")),
                  r=["gbc2"], w=["dbgo2"])
        P.emit()

    if stage == "p0":
        return nc
    with esA as es:
        tsb = tsbA
        P = Prog(nc, "pa")

        xt = tsb("xt", [128, 2, D])
        hT = tsb("hT", [128, 8, TT], BF16)
        cutail = tsb("cutail", [128, 4, 2])
        qtail = tsb("qtail", [128, 12, 3])
        qkvc2 = [tsb(f"qkvc{i}", [128, 12, TT]) for i in range(2)]
        zs2 = [tsb(f"zs{i}", [128, 4, TT]) for i in range(2)]
        mixT2 = [tsb(f"mixT{i}", [128, 8, TT], BF16) for i in range(3)]
        blsb2 = [tsb(f"blsb{i}", [128, 16]) for i in range(2)]
        S = tsb("S", [128, 4, 128])
        csb = tsb("csb", [128, TT])
        cuf2 = [tsb(f"cuf{i}", [128, TT + 2]) for i in range(2)]
        acc2 = [tsb(f"acc{i}", [128, TT]) for i in range(2)]
        ybuf2 = [tsb(f"ybuf{i}", [128, TT]) for i in range(2)]
        sqb2 = [tsb(f"sqb{i}", [128, TT]) for i in range(2)]
        sgt2 = [tsb(f"sgt{i}", [128, TT]) for i in range(2)]
        halo = [tsb(f"halo{i}", [128, TT + 3]) for i in range(2)]
        sm2 = [tsb(f"sm{i}", [128, 2, 64]) for i in range(2)]
        T = [tsb(f"T{i}", [128, 512]) for i in range(13)]
        r0 = tsb("r0", [128, D])
        xh = tsb("xh", [128, D])
        h2t = tsb("h2t", [128, 8, TT], BF16)
        bst = tsb("bst", [128, 2, 6])
        mv = tsb("mv", [128, 4])
        rotA = PsumRot(BANKS[0:4])
        rotB = PsumRot(BANKS[4:8])
        rot2A = PsumRot([(PS2[i], (f"bank{2 * i}", f"bank{2 * i + 1}")) for i in (0, 1)])

        def v3(ap):
            return ap.rearrange("p (h j) -> p h j", h=4)

        def bc_h(ap128):
            return ap128.unsqueeze(1).to_broadcast([128, 4, 128])

        def bc_j(ap4):
            return ap4.unsqueeze(2).to_broadcast([128, 4, 128])

        EPS_RMS = 1e-6

        def stage1(ti):
            pp = ti % 2
            b = ti // (NT // NB)
            first = (ti % (NT // NB) == 0)
            qkvc, zs, mixT, blsb = qkvc2[pp], zs2[pp], mixT2[ti % 3], blsb2[pp]
            mp = ti % 3
            rot = rotA
            P.dma("sp", lambda e: e.dma_start(
                out=xt, in_=x[ti * TT:(ti + 1) * TT, :].rearrange("(s p) f -> p s f", p=128)), w=["xt"])
            if first:
                P.op("pool", lambda e: e.memset(cutail, 0.0), w=["cutail"])
                P.op("pool", lambda e: e.memset(qtail, 0.0), w=["qtail"])
            for kc in range(8):
                pt, pk = rot.get()
                for s_ in range(2):
                    P.op("pe", lambda e, pt=pt, s_=s_, kc=kc: e.transpose(
                        out=pt[:, s_ * 128:(s_ + 1) * 128], in_=xt[:, s_, kc * 128:(kc + 1) * 128],
                        identity=C["ident"]), r=["xt"], w=[pk])
                P.op("act", lambda e, pt=pt, kc=kc: e.activation(
                    out=hT[:, kc, :], in_=pt[:, 0:TT], func=AF.Identity,
                    scale=s1p[:, kc, b:b + 1], bias=modT[:, kc, b:b + 1]), r=[pk], w=[f"hT{kc}"])

            def proj(oc):
                pt, pk = rot.get()
                for kc in range(8):
                    P.op("pe", lambda e, pt=pt, kc=kc: e.matmul(
                        pt[:, 0:TT], lhsT=Win[:, kc, oc * 128:(oc + 1) * 128], rhs=hT[:, kc, :],
                        start=(kc == 0), stop=(kc == 7)), r=[f"hT{kc}", "Win"], w=[pk])
                return pt[:, 0:TT], pk

            deferred = []

            def flush(keep=0):
                while len(deferred) > keep:
                    deferred.pop(0)()

            def rstd_part2(srcbuf, srck, lhs, q):
                sqb = sqb2[q]
                pm, pmk = rot.get()
                P.op("pe", lambda e: e.matmul(pm[:, 0:TT], lhsT=lhs, rhs=sqb, start=True, stop=True),
                     r=[f"sqb{q}"], w=[pmk])
                P.op("act", lambda e: e.activation(out=sqb, in_=pm[:, 0:TT], func=AF.Ln, bias=EPS_RMS),
                     r=[pmk], w=[f"sqb{q}"])
                P.op("act", lambda e: e.activation(out=sqb, in_=sqb, func=AF.Exp, scale=-0.5),
                     r=[f"sqb{q}"], w=[f"sqb{q}"])

            for j in range(4):
                q = j % 2
                cuf, acc, ybuf, sqb = cuf2[q], acc2[q], ybuf2[q], sqb2[q]
                pb, pbk = proj(j)
                pc, pck = proj(4 + j)
                pu, puk = proj(8 + j)
                P.op("act", lambda e, pc=pc: e.activation(out=csb, in_=pc, func=AF.Copy), r=[pck], w=["csb"])
                P.op("dve", lambda e, pu=pu, cuf=cuf: e.tensor_tensor(out=cuf[:, 2:TT + 2], in0=pu, in1=csb, op=ALU.mult),
                     r=[puk, "csb"], w=[f"cuf{q}"])
                P.op("pool", lambda e, j=j, cuf=cuf: e.tensor_copy(out=cuf[:, 0:2], in_=cutail[:, j, :]),
                     r=["cutail"], w=[f"cufh{q}"])
                P.op("act", lambda e, j=j, cuf=cuf, acc=acc: e.activation(out=acc, in_=cuf[:, 2:TT + 2], func=AF.Copy,
                                                                          scale=cw[:, j, 2:3]), r=[f"cuf{q}"], w=[f"acc{q}"])
                P.op("dve", lambda e, j=j, cuf=cuf, acc=acc: e.scalar_tensor_tensor(
                    out=acc, in0=cuf[:, 1:TT + 1], scalar=cw[:, j, 1:2], in1=acc, op0=ALU.mult, op1=ALU.add),
                    r=[f"cuf{q}", f"cufh{q}", f"acc{q}"], w=[f"acc{q}"])
                P.op("dve", lambda e, j=j, cuf=cuf, acc=acc: e.scalar_tensor_tensor(
                    out=acc, in0=cuf[:, 0:TT], scalar=cw[:, j, 0:1], in1=acc, op0=ALU.mult, op1=ALU.add),
                    r=[f"cuf{q}", f"cufh{q}", f"acc{q}"], w=[f"acc{q}"])
                P.op("pool", lambda e, j=j, cuf=cuf: e.tensor_copy(out=cutail[:, j, :], in_=cuf[:, TT:TT + 2]),
                     r=[f"cuf{q}"], w=["cutail"])
                P.op("dve", lambda e, pb=pb, acc=acc, ybuf=ybuf: e.tensor_tensor(out=ybuf, in0=pb, in1=acc, op=ALU.mult),
                     r=[pbk, f"acc{q}"], w=[f"ybuf{q}"])
                P.op("act", lambda e, ybuf=ybuf, sqb=sqb: e.activation(out=sqb, in_=ybuf, func=AF.Square),
                     r=[f"ybuf{q}"], w=[f"sqb{q}"])

                def part2(j=j, q=q, ybuf=ybuf, sqb=sqb):
                    rstd_part2(None, None, C["gmat"], q)
                    P.op("dve", lambda e: e.scalar_tensor_tensor(
                        out=mixT[:, j, :], in0=ybuf, scalar=cnw[:, j:j + 1], in1=sqb, op0=ALU.mult, op1=ALU.mult),
                        r=[f"ybuf{q}", f"sqb{q}"], w=[f"mixT{mp}_{j}"])
                flush(0)
                deferred.append(part2)

            for j in range(12):
                q = j % 2
                sqb = sqb2[q]
                pq, pqk = proj(12 + j)
                hb = halo[q]
                hk = f"halo{q}"
                qk_ = f"qk{pp}_{j}"
                P.op("act", lambda e, pq=pq, hb=hb: e.activation(out=hb[:, 3:TT + 3], in_=pq, func=AF.Copy),
                     r=[pqk], w=[hk])
                P.op("pool", lambda e, j=j, hb=hb: e.tensor_copy(out=hb[:, 0:3], in_=qtail[:, j, :]),
                     r=["qtail"], w=[hk + "h"])
                P.op("pool", lambda e, j=j, hb=hb: e.tensor_scalar(
                    out=qkvc[:, j, :], in0=hb[:, 3:TT + 3], scalar1=dcw[:, j, 3:4], scalar2=0.0, op0=ALU.mult,
                    op1=ALU.add), r=[hk], w=[qk_])
                for k in (2, 1, 0):
                    P.op("dve", lambda e, j=j, hb=hb, k=k: e.scalar_tensor_tensor(
                        out=qkvc[:, j, :], in0=hb[:, k:TT + k], scalar=dcw[:, j, k:k + 1], in1=qkvc[:, j, :],
                        op0=ALU.mult, op1=ALU.add), r=[hk, hk + "h", qk_], w=[qk_])
                P.op("pool", lambda e, j=j, hb=hb: e.tensor_copy(out=qtail[:, j, :], in_=hb[:, TT:TT + 3]),
                     r=[hk], w=["qtail"])
                sgt = sgt2[q]
                P.op("act", lambda e, j=j, sgt=sgt: e.activation(out=sgt, in_=qkvc[:, j, :], func=AF.Exp, scale=-1.0),
                     r=[qk_], w=[f"sgt{q}"])
                P.op("act", lambda e, sgt=sgt: e.activation(out=sgt, in_=sgt, func=AF.Ln, bias=1.0),
                     r=[f"sgt{q}"], w=[f"sgt{q}"])
                P.op("act", lambda e, sgt=sgt: e.activation(out=sgt, in_=sgt, func=AF.Exp, scale=-1.0),
                     r=[f"sgt{q}"], w=[f"sgt{q}"])
                P.op("pool", lambda e, j=j, sgt=sgt: e.tensor_tensor(out=qkvc[:, j, :], in0=qkvc[:, j, :], in1=sgt,
                                                                     op=ALU.mult), r=[qk_, f"sgt{q}"], w=[qk_])
                if j < 8:
                    P.op("act", lambda e, j=j, sqb=sqb: e.activation(out=sqb, in_=qkvc[:, j, :], func=AF.Square),
                         r=[qk_], w=[f"sqb{q}"])

                    def part2(j=j, q=q, sqb=sqb, qk_=qk_):
                        rstd_part2(None, None, C["ones"], q)
                        sc = (128.0 ** -0.5) if j < 4 else 1.0
                        P.op("dve", lambda e: e.scalar_tensor_tensor(
                            out=qkvc[:, j, :], in0=qkvc[:, j, :], scalar=sc, in1=sqb, op0=ALU.mult, op1=ALU.mult),
                            r=[qk_, f"sqb{q}"], w=[qk_])
                    flush(0)
                    deferred.append(part2)
                else:
                    flush(0)
            flush(0)
            for j in range(4):
                pz, pzk = proj(24 + j)
                sgt = sgt2[j % 2]
                sk = f"sgt{j % 2}"
                P.op("act", lambda e, pz=pz, sgt=sgt: e.activation(out=sgt, in_=pz, func=AF.Exp, scale=-1.0),
                     r=[pzk], w=[sk])
                P.op("act", lambda e, sgt=sgt: e.activation(out=sgt, in_=sgt, func=AF.Ln, bias=1.0), r=[sk], w=[sk])
                P.op("act", lambda e, sgt=sgt: e.activation(out=sgt, in_=sgt, func=AF.Exp, scale=-1.0), r=[sk], w=[sk])
                P.op("dve", lambda e, pz=pz, j=j, sgt=sgt: e.tensor_tensor(out=zs[:, j, :], in0=pz, in1=sgt, op=ALU.mult),
                     r=[pzk, sk], w=[f"zs{pp}_{j}"])
            p8, p8k = rot.get()
            for s_ in range(2):
                for kc in range(8):
                    P.op("pe", lambda e, s_=s_, kc=kc: e.matmul(
                        p8[:, s_ * 8:(s_ + 1) * 8], lhsT=hT[:, kc, s_ * 128:(s_ + 1) * 128],
                        rhs=Win[:, kc, 3584:3592], start=(kc == 0), stop=(kc == 7)),
                        r=[f"hT{kc}", "Win"], w=[p8k])
            P.op("act", lambda e: e.activation(out=blsb, in_=p8[:, 0:16], func=AF.Copy), r=[p8k], w=[f"blsb{pp}"])
            for s_ in range(2):
                smx = sm2[pp][:, s_, :]
                beta, xa, g, _g, egc, bge, dl, eL, sA, sB = [smx[:, i * 4:(i + 1) * 4] for i in range(10)]
                gcs = smx[:, 40:48]
                gcum = gcs[:, 0:4]
                glast = gcs[:, 4:8]
                SK = f"smk{pp}_{s_}"
                bl = blsb[:, s_ * 8:(s_ + 1) * 8]
                blk = f"blsb{pp}"
                P.op("act", lambda e, beta=beta, bl=bl: e.activation(out=beta, in_=bl[:, 0:4], func=AF.Exp, scale=-1.0),
                     r=[blk], w=[SK])
                P.op("dve", lambda e, beta=beta: e.tensor_scalar(out=beta, in0=beta, scalar1=1.0, scalar2=None, op0=ALU.add),
                     r=[SK], w=[SK])
                P.op("dve", lambda e, beta=beta: e.reciprocal(out=beta, in_=beta), r=[SK], w=[SK])
                P.op("dve", lambda e, xa=xa, bl=bl: e.tensor_tensor(out=xa, in0=bl[:, 4:8], in1=dtb, op=ALU.add),
                     r=[blk, SK], w=[SK])
                P.op("act", lambda e, xa=xa: e.activation(out=xa, in_=xa, func=AF.Exp), r=[SK], w=[SK])
                P.op("act", lambda e, xa=xa: e.activation(out=xa, in_=xa, func=AF.Ln, bias=1.0), r=[SK], w=[SK])
                P.op("dve", lambda e, g=g, xa=xa: e.tensor_tensor(out=g, in0=xa, in1=negA, op=ALU.mult), r=[SK], w=[SK])
                pc_, pck = rot.get()
                P.op("pe", lambda e, pc_=pc_, g=g: e.matmul(pc_[:, 0:4], lhsT=C["ltb"], rhs=g, start=True, stop=True),
                     r=[SK], w=[pck])
                P.op("pe", lambda e, pc_=pc_, g=g: e.matmul(pc_[:, 4:8], lhsT=C["bd"], rhs=g, start=True, stop=True),
                     r=[SK], w=[pck])
                P.op("act", lambda e, pc_=pc_, gcs=gcs: e.activation(out=gcs, in_=pc_[:, 0:8], func=AF.Copy), r=[pck], w=[SK])
                P.op("act", lambda e, egc=egc, gcum=gcum: e.activation(out=egc, in_=gcum, func=AF.Exp), r=[SK], w=[SK])
                P.op("dve", lambda e, bge=bge, beta=beta, egc=egc: e.tensor_tensor(out=bge, in0=beta, in1=egc, op=ALU.mult),
                     r=[SK], w=[SK])
                P.op("dve", lambda e, dl=dl, glast=glast, gcum=gcum: e.tensor_tensor(out=dl, in0=glast, in1=gcum,
                                                                                     op=ALU.subtract), r=[SK], w=[SK])
                P.op("act", lambda e, eL=eL, dl=dl: e.activation(out=eL, in_=dl, func=AF.Exp), r=[SK], w=[SK])
                P.op("dve", lambda e, sA=sA, eL=eL: e.tensor_scalar(out=sA, in0=eL, scalar1=mab[:, 0:1], scalar2=None,
                                                                    op0=ALU.mult), r=[SK], w=[SK])
                P.op("dve", lambda e, sB=sB, eL=eL: e.tensor_scalar(out=sB, in0=eL, scalar1=mab[:, 1:2], scalar2=None,
                                                                    op0=ALU.mult), r=[SK], w=[SK])

        def stage2(ti):
            first = (ti % (NT // NB) == 0)
            if first:
                P.op("pool", lambda e: e.memset(S, 0.0), w=["S0", "S1", "S2", "S3"])
            for s_ in range(2):
                gdn_sub(ti, s_)

        def stage3(ti):
            b = ti // (NT // NB)
            for s_ in range(2):
                ln1_sub(ti, s_, b)
            P.dma("sp", lambda e: e.dma_start(out=h2_s[:, :, ti * TT:(ti + 1) * TT], in_=h2t),
                  r=["h2t"], w=["h2s"])

        def gdn_sub(ti, s_):
            pp = ti % 2
            rot = rotB
            qkvc, zs, mixT, blsb = qkvc2[pp], zs2[pp], mixT2[ti % 3], blsb2[pp]
            mp = ti % 3
            bl = blsb[:, s_ * 8:(s_ + 1) * 8]
            blk = f"blsb{pp}"
            cs = slice(s_ * 128, (s_ + 1) * 128)
            R1, R2, tU, tL, egr, U, L, QKm, Xa, Xb, Pb, PTb, bv = T
            kR1, kR2, ktU, ktL, kegr, kU, kL, kQKm, kXa, kXb, kPb, kPTb, kbv = [f"T{i}" for i in range(13)]
            keA, kkeA, keB, kkeB = R2, kR2, tL, ktL
            u, ku, wT, kwT, qdT, kqdT, delta, kdelta = U, kU, L, kL, Pb, kPb, PTb, kPTb
            smx = sm2[pp][:, s_, :]
            beta, xa, g, _g, egc, bge, dl, eL, sA, sB = [smx[:, i * 4:(i + 1) * 4] for i in range(10)]
            gcs = smx[:, 40:48]
            gcum = gcs[:, 0:4]
            glast = gcs[:, 4:8]
            SK = f"smk{pp}_{s_}"
            qk = lambda j: f"qk{pp}_{j}"
            P.op("dve", lambda e: e.tensor_tensor(out=v3(R1), in0=bc_h(C["ltb"]), in1=bc_j(g), op=ALU.mult),
                 r=[SK], w=[kR1])
            P.op("pool", lambda e: e.tensor_tensor(out=v3(R2), in0=bc_h(C["ident"]), in1=bc_j(beta), op=ALU.mult),
                 r=[SK], w=[kR2])
            pgr, pgrk = rot.get()
            P.op("pe", lambda e: e.matmul(pgr, lhsT=C["ones"], rhs=R1, start=True, stop=True), r=[kR1], w=[pgrk])
            pbr, pbrk = rot.get()
            P.op("pe", lambda e: e.matmul(pbr, lhsT=C["ones"], rhs=R2, start=True, stop=True), r=[kR2], w=[pbrk])
            P.op("dve", lambda e: e.tensor_tensor(out=v3(tU), in0=v3(pgr), in1=bc_j(gcum), op=ALU.subtract),
                 r=[pgrk, SK], w=[ktU])
            P.op("pool", lambda e: e.tensor_tensor(out=v3(R1), in0=v3(tU), in1=bc_h(C["mnu"]), op=ALU.add),
                 r=[ktU], w=[kR1])
            P.op("act", lambda e: e.activation(out=R1, in_=R1, func=AF.Exp), r=[kR1], w=[kR1])
            P.op("pool", lambda e: e.tensor_tensor(out=v3(R2), in0=v3(tU), in1=bc_h(C["mnus"]), op=ALU.add),
                 r=[ktU], w=[kR2])
            P.op("act", lambda e: e.activation(out=R2, in_=R2, func=AF.Exp), r=[kR2], w=[kR2])
            P.op("dve", lambda e: e.tensor_tensor(out=R2, in0=R2, in1=pbr, op=ALU.mult), r=[kR2, pbrk], w=[kR2])
            P.op("dve", lambda e: e.scalar_tensor_tensor(out=v3(tL), in0=v3(pgr), scalar=-1.0, in1=bc_j(gcum),
                                                         op0=ALU.mult, op1=ALU.add), r=[pgrk, SK], w=[ktL])
            P.op("pool", lambda e: e.tensor_tensor(out=v3(tL), in0=v3(tL), in1=bc_h(C["mnls"]), op=ALU.add),
                 r=[ktL], w=[ktL])
            P.op("act", lambda e: e.activation(out=tL, in_=tL, func=AF.Exp), r=[ktL], w=[ktL])
            P.op("pool", lambda e: e.tensor_tensor(out=v3(tL), in0=v3(tL), in1=bc_j(beta), op=ALU.mult),
                 r=[ktL, SK], w=[ktL])
            P.op("act", lambda e: e.activation(out=egr, in_=pgr, func=AF.Exp), r=[pgrk], w=[kegr])
            pkk, pkkk = rot.get()
            for h in range(4):
                P.op("pe", lambda e, h=h: e.matmul(pkk[:, h * 128:(h + 1) * 128], lhsT=qkvc[:, 4 + h, cs],
                                                   rhs=qkvc[:, 4 + h, cs], start=True, stop=True),
                     r=[qk(4 + h)], w=[pkkk])
            P.op("dve", lambda e: e.tensor_tensor(out=U, in0=pkk, in1=R2, op=ALU.mult), r=[pkkk, kR2], w=[kU])
            P.op("dve", lambda e: e.tensor_tensor(out=L, in0=pkk, in1=tL, op=ALU.mult), r=[pkkk, ktL], w=[kL])
            pqk_, pqkk = rot.get()
            for h in range(4):
                P.op("pe", lambda e, h=h: e.matmul(pqk_[:, h * 128:(h + 1) * 128], lhsT=qkvc[:, 4 + h, cs],
                                                   rhs=qkvc[:, h, cs], start=True, stop=True),
                     r=[qk(4 + h), qk(h)], w=[pqkk])
            P.op("dve", lambda e: e.tensor_tensor(out=QKm, in0=pqk_, in1=R1, op=ALU.mult), r=[pqkk, kR1], w=[kQKm])
            P.op("pool", lambda e: e.tensor_tensor(out=v3(Xa), in0=bc_h(C["ident"]), in1=v3(U), op=ALU.subtract),
                 r=[kU], w=[kXa])
            pkt, pktk = rot.get()
            for h in range(4):
                P.op("pe", lambda e, h=h: e.transpose(out=pkt[:, h * 128:(h + 1) * 128], in_=qkvc[:, 4 + h, cs],
                                                      identity=C["ident"]), r=[qk(4 + h)], w=[pktk])
            kbg, kkbg = tU, ktU
            P.op("dve", lambda e: e.tensor_tensor(out=v3(kbg), in0=v3(pkt), in1=bc_j(bge), op=ALU.mult),
                 r=[pktk, SK], w=[kkbg])
            P.op("dve", lambda e: e.tensor_tensor(out=v3(keA), in0=v3(pkt), in1=bc_j(sA), op=ALU.mult),
                 r=[pktk, SK, kU], w=[kkeA])
            P.op("dve", lambda e: e.tensor_tensor(out=v3(keB), in0=v3(pkt), in1=bc_j(sB), op=ALU.mult),
                 r=[pktk, SK, kL], w=[kkeB])
            pvt, pvtk = rot.get()
            for h in range(4):
                P.op("pe", lambda e, h=h: e.transpose(out=pvt[:, h * 128:(h + 1) * 128], in_=qkvc[:, 8 + h, cs],
                                                      identity=C["ident"]), r=[qk(8 + h)], w=[pvtk])
            P.op("dve", lambda e: e.tensor_tensor(out=v3(bv), in0=v3(pvt), in1=bc_j(beta), op=ALU.mult),
                 r=[pvtk, SK], w=[kbv])
            Pc, PTc, kPc, kPTc = U, L, kU, kL
            Pn, PTn, kPn, kPTn = Pb, PTb, kPb, kPTb
            Xc, Xn, kXc, kXn = Xa, Xb, kXa, kXb
            for k in range(1, 6):
                if k < 5:
                    pp_, ppk = rot.get()
                    for h in range(4):
                        hs = slice(h * 128, (h + 1) * 128)
                        P.op("pe", lambda e, hs=hs, PTc=PTc, Pc=Pc, pp_=pp_: e.matmul(
                            pp_[:, hs], lhsT=PTc[:, hs], rhs=Pc[:, hs], start=True, stop=True),
                            r=[kPc, kPTc], w=[ppk])
                ppt, pptk = rot.get()
                for h in range(4):
                    hs = slice(h * 128, (h + 1) * 128)
                    P.op("pe", lambda e, hs=hs, PTc=PTc, Pc=Pc, ppt=ppt: e.matmul(
                        ppt[:, hs], lhsT=Pc[:, hs], rhs=PTc[:, hs], start=True, stop=True),
                        r=[kPc, kPTc], w=[pptk])
                if k < 5:
                    P.op("act", lambda e, Pn=Pn, pp_=pp_: e.activation(out=Pn, in_=pp_, func=AF.Copy), r=[ppk], w=[kPn])
                P.op("dve", lambda e, PTn=PTn, ppt=ppt: e.tensor_copy(out=PTn, in_=ppt), r=[pptk], w=[kPTn])
                px, pxk = rot.get()
                for h in range(4):
                    hs = slice(h * 128, (h + 1) * 128)
                    P.op("pe", lambda e, hs=hs, PTn=PTn, Xc=Xc, px=px: e.matmul(
                        px[:, hs], lhsT=PTn[:, hs], rhs=Xc[:, hs], start=True, stop=True),
                        r=[kPTn, kXc], w=[pxk])
                P.op("dve", lambda e, Xn=Xn, Xc=Xc, px=px: e.tensor_tensor(out=Xn, in0=px, in1=Xc, op=ALU.add),
                     r=[pxk, kXc], w=[kXn])
                Pc, Pn, kPc, kPn = Pn, Pc, kPn, kPc
                PTc, PTn, kPTc, kPTn = PTn, PTc, kPTn, kPTc
                Xc, Xn, kXc, kXn = Xn, Xc, kXn, kXc
            TTm, kTT = Xc, kXc
            assert TTm is Xb
            pu_, puk = rot.get()
            pw_, pwk = rot.get()
            for h in range(4):
                hs = slice(h * 128, (h + 1) * 128)
                P.op("pe", lambda e, hs=hs: e.matmul(pu_[:, hs], lhsT=TTm[:, hs], rhs=bv[:, hs], start=True, stop=True),
                     r=[kTT, kbv], w=[puk])
            for h in range(4):
                hs = slice(h * 128, (h + 1) * 128)
                P.op("pe", lambda e, hs=hs: e.matmul(pw_[:, hs], lhsT=kbg[:, hs], rhs=TTm[:, hs], start=True, stop=True),
                     r=[kTT, kkbg], w=[pwk])
            P.op("act", lambda e: e.activation(out=u, in_=pu_, func=AF.Copy), r=[puk], w=[ku])
            P.op("act", lambda e: e.activation(out=wT, in_=pw_, func=AF.Copy), r=[pwk], w=[kwT])
            P.op("dve", lambda e: e.tensor_tensor(out=v3(qdT), in0=qkvc[:, 0:4, cs], in1=v3(egr), op=ALU.mult),
                 r=[qk(h) for h in range(4)] + [kegr], w=[kqdT])
            po, pok = rot.get()
            others = [it_ for it_ in rotB.items if it_[1] != pok]
            oi = 0
            for ch in range(2):
                rows = slice(ch * 64, ch * 64 + 64)
                keX, kkeX = (keA, kkeA) if ch == 0 else (keB, kkeB)
                pws, pwsk = others[oi % 3]
                oi += 1
                for h in range(4):
                    hs = slice(h * 128, (h + 1) * 128)
                    P.op("pe", lambda e, hs=hs, h=h, pws=pws: e.matmul(pws[:, hs], lhsT=wT[:, hs], rhs=S[:, h, :],
                                                                      start=True, stop=True),
                         r=[kwT, f"S{h}"], w=[pwsk])
                P.op("dve", lambda e, rows=rows, pws=pws: e.tensor_tensor(out=delta[rows, :], in0=u[rows, :],
                                                                          in1=pws[rows, :], op=ALU.subtract),
                     r=[ku, pwsk], w=[kdelta])
                for h in range(4):
                    hs = slice(h * 128, (h + 1) * 128)
                    oc_ = slice(h * 128 + ch * 64, h * 128 + ch * 64 + 64)
                    P.op("pe", lambda e, h=h, oc_=oc_: e.matmul(po[:, oc_], lhsT=S[:, h, :], rhs=qdT[:, oc_],
                                                                start=True, stop=False),
                         r=[f"S{h}", kqdT], w=[pok])
                    P.op("pe", lambda e, hs=hs, oc_=oc_: e.matmul(po[:, oc_], lhsT=delta[:, hs], rhs=QKm[:, oc_],
                                                                  start=False, stop=True),
                         r=[kdelta, kQKm], w=[pok])
                pss, pssk = others[oi % 3]
                oi += 1
                for h in range(4):
                    hs = slice(h * 128, (h + 1) * 128)
                    P.op("pe", lambda e, hs=hs, keX=keX, pss=pss: e.matmul(pss[:, hs], lhsT=keX[:, hs], rhs=delta[:, hs],
                                                                          start=True, stop=True),
                         r=[kkeX, kdelta], w=[pssk])
                for h in range(4):
                    hs = slice(h * 128, (h + 1) * 128)
                    dcol = h * 128 + ch * 64 + 63
                    P.op("dve", lambda e, h=h, hs=hs, dcol=dcol, pss=pss: e.scalar_tensor_tensor(
                        out=S[:, h, :], in0=S[:, h, :], scalar=egr[:, dcol:dcol + 1], in1=pss[:, hs],
                        op0=ALU.mult, op1=ALU.add), r=[f"S{h}", kegr, pssk], w=[f"S{h}"])
            osb, kosb = bv, kbv
            sq2, ksq2 = R1, kR1
            P.op("act", lambda e: e.activation(out=osb, in_=po, func=AF.Copy), r=[pok], w=[kosb])
            P.op("act", lambda e: e.activation(out=sq2, in_=po, func=AF.Square), r=[pok], w=[ksq2])
            pm, pmk = others[oi % 3]
            P.op("pe", lambda e: e.matmul(pm, lhsT=C["omean"], rhs=sq2, start=True, stop=True), r=[ksq2], w=[pmk])
            P.op("act", lambda e: e.activation(out=sq2, in_=pm, func=AF.Ln, bias=EPS_RMS), r=[pmk], w=[ksq2])
            P.op("act", lambda e: e.activation(out=sq2, in_=sq2, func=AF.Exp, scale=-0.5), r=[ksq2], w=[ksq2])
            P.op("dve", lambda e: e.scalar_tensor_tensor(out=osb, in0=osb, scalar=dnw[:, 0:1], in1=sq2,
                                                         op0=ALU.mult, op1=ALU.mult), r=[kosb, ksq2], w=[kosb])
            P.op("pool", lambda e: e.tensor_tensor(out=mixT[:, 4:8, cs], in0=v3(osb), in1=zs[:, :, cs], op=ALU.mult),
                 r=[kosb] + [f"zs{pp}_{j}" for j in range(4)], w=[f"mixT{mp}_{4 + j}" for j in range(4)])

        def ln1_sub(ti, s_, b):
            mp = ti % 3
            mixT = mixT2[mp]
            rot = rotA
            cs = slice(s_ * 128, (s_ + 1) * 128)
            tok0 = ti * TT + s_ * 128
            P.dma("sp", lambda e: e.dma_start(out=xh, in_=x[tok0:tok0 + 128, :]), w=["xh"])
            pm2, (k0, k1) = rot2A.get()
            for half in range(2):
                for kc in range(8):
                    P.op("pe", lambda e, half=half, kc=kc: e.matmul(
                        pm2[:, half * 512:(half + 1) * 512], lhsT=mixT[:, kc, cs],
                        rhs=Wout[:, kc, half * 512:(half + 1) * 512], start=(kc == 0), stop=(kc == 7)),
                        r=[f"mixT{mp}_{kc}", "Wout"], w=[(k0, k1)[half]])
            P.op("dve", lambda e: e.tensor_tensor(out=r0, in0=pm2, in1=gate_bc[2][:, b, :], op=ALU.mult),
                 r=[k0, k1], w=["r0"])
            P.op("dve", lambda e: e.scalar_tensor_tensor(out=r0, in0=xh, scalar=ALPHA, in1=r0,
                                                         op0=ALU.mult, op1=ALU.add), r=["xh", "r0"], w=["r0"])
            for hf in range(2):
                P.op("dve", lambda e, hf=hf: e.bn_stats(out=bst[:, hf, :], in_=r0[:, hf * 512:(hf + 1) * 512]),
                     r=["r0"], w=["bst"])
            P.op("dve", lambda e: e.bn_aggr(out=mv[:, 0:2], in_=bst.rearrange("p a b -> p (a b)")), r=["bst"], w=["mv"])
            P.op("act", lambda e: e.activation(out=mv[:, 2:3], in_=mv[:, 1:2], func=AF.Ln, bias=1e-5),
                 r=["mv"], w=["mv2"])
            P.op("act", lambda e: e.activation(out=mv[:, 3:4], in_=mv[:, 2:3], func=AF.Exp, scale=-0.5),
                 r=["mv2"], w=["mv3"])
            P.op("dve", lambda e: e.tensor_scalar(out=xh, in0=r0, scalar1=mv[:, 0:1], scalar2=mv[:, 3:4],
                                                  op0=ALU.subtract, op1=ALU.mult), r=["r0", "mv", "mv3"], w=["xh"])
            P.op("pool", lambda e: e.tensor_tensor(out=r0, in0=xh, in1=ag, op=ALU.mult), r=["xh"], w=["r0"])
            P.op("pool", lambda e: e.tensor_tensor(out=r0, in0=r0, in1=ab, op=ALU.add), r=["r0"], w=["r0"])
            P.dma("sp", lambda e: e.dma_start(out=r_s[tok0:tok0 + 128, :], in_=r0), r=["r0"], w=["rs"])
            for kc in range(8):
                pt, pk = rot.get()
                P.op("pe", lambda e, pt=pt, kc=kc: e.transpose(out=pt[:, 0:128], in_=xh[:, kc * 128:(kc + 1) * 128],
                                                               identity=C["ident"]), r=["xh"], w=[pk])
                P.op("act", lambda e, pt=pt, kc=kc: e.activation(
                    out=h2t[:, kc, cs], in_=pt[:, 0:128], func=AF.Identity,
                    scale=A2[:, kc, b:b + 1], bias=B2[:, kc, b:b + 1]), r=[pk], w=["h2t"])

        def capture(fn, *a):
            old = P.ops
            P.ops = []
            fn(*a)
            got = P.ops
            P.ops = old
            return got

        def merge(a, b_):
            out_, i, j = [], 0, 0
            na, nb = max(len(a), 1), max(len(b_), 1)
            while i < len(a) or j < len(b_):
                if j >= len(b_) or (i < len(a) and i * nb <= j * na):
                    out_.append(a[i]); i += 1
                else:
                    out_.append(b_[j]); j += 1
            return out_

        ntiles = NT if stage != "pa1" else 2

        def threadA(ti):
            if ti + 1 < ntiles:
                stage1(ti + 1)
            if ti - 1 >= 0:
                stage3(ti - 1)

        P.ops += capture(stage1, 0)
        for ti in range(ntiles):
            a = capture(threadA, ti)
            b_ = capture(stage2, ti)
            P.ops += merge(a, b_)
        P.ops += capture(stage3, ntiles - 1)
        if stage == "pa1":
            P.dma("pool", lambda e: e.dma_start(out=dbg_out["mixT"].rearrange("p (a b) -> p a b", a=8), in_=mixT2[1]),
                  r=[f"mixT1_{j}" for j in range(8)], w=["dbg1"])
            P.dma("sp", lambda e: e.dma_start(out=dbg_out["r"], in_=r_s[0:512, :]), r=["rs"], w=["dbg2"])
            P.dma("pool", lambda e: e.dma_start(out=dbg_out["h2"].rearrange("p (a b) -> p a b", a=8),
                                                in_=h2_s[:, :, 0:512]), r=["h2s"], w=["dbg3"])
            P.dma("sp", lambda e: e.dma_start(out=dbg_out["qkvc"].rearrange("p (a b) -> p a b", a=12), in_=qkvc2[1]),
                  r=[f"qk1_{j}" for j in range(12)], w=["dbg4"])
        cnt = P.emit()
        print("phaseA op counts", cnt)

    if stage in ("pa", "pa1"):
        return nc
    I32 = mybir.dt.int32
    with contextlib.ExitStack() as es:
        def tsb(name, shape, dt=F32):
            return es.enter_context(nc.sbuf_tensor("b_" + name, list(shape), dt)).ap()

        P = Prog(nc, "pb")
        NSTT = TOK // 128
        NW = 3
        cb = tsb("cb", list(CB.shape))
        thr = cb[:, 0:64]
        SLc = cb[:, 64:64 + 1024]
        blkio = cb[:, 1088:1088 + NBLK]
        iotaP = cb[:, 1088 + NBLK:1089 + NBLK]
        SUf = cb[:, 1089 + NBLK:1089 + NBLK + 128]
        SUb = tsb("sub", [128, 128], BF16)
        onesb = tsb("onesb", [128, 128], BF16)
        Wr = tsb("wr", [128, 8, 36], BF16)
        b36 = tsb("b36", [128, 36])
        g2 = tsb("ln2g", [128, D])
        b2 = tsb("ln2b", [128, D])
        gate2 = tsb("gate2", [128, 2, D])
        h2c = [tsb(f"h2c{i}", [128, 8, 512], BF16) for i in range(2)]
        OHall = tsb("ohall", [128, NSTT, 32], BF16)
        oh1all = tsb("oh1all", [128, NSTT, 32])
        oh2all = tsb("oh2all", [128, NSTT, 32])
        gAB = tsb("gab", [128, NSTT, 2])
        POSf = tsb("posf", [128, NSTT, 2])
        POSi = tsb("posi", [128, NSTT, 2], I32)
        cnt = tsb("cnt", [128, 32])
        nblk = tsb("nblk", [128, 32])
        pstb = tsb("pstb", [128, 32])
        pend = tsb("pend", [128, 32])
        base = tsb("base", [128, 32])
        big = tsb("big", [128, NBLK * 32])
        bexp = tsb("bexp", [128, NBLK])
        bskip = tsb("bskip", [128, NBLK])
        bsame = tsb("bsame", [128, NBLK])
        IDXW = tsb("idxw", [128, NBLK], I32)
        posv = tsb("posv", [128, 32])
        ptmp = tsb("ptmp", [128, 32])
        lg = tsb("lg", [128, 36])
        lgm = tsb("lgm", [128, 32])
        lgm2 = tsb("lgm2", [128, 32])
        rs_ = tsb("rsm", [128, 32])
        Wg = [tsb(f"wg{i}", [128, 2048], BF16) for i in range(NW)]
        Wu = [tsb(f"wu{i}", [128, 2048], BF16) for i in range(NW)]
        Wd = [tsb(f"wd{i}", [128, 2048], BF16) for i in range(NW)]
        xb = [tsb(f"xb{i}", [128, D]) for i in range(3)]
        sh2r = tsb("sh2r", [128, 2, D])
        sc2r = tsb("sc2r", [128, 2, D])
        xT = [tsb(f"xT{i}", [128, 8, 128], BF16) for i in range(3)]
        sg = [tsb(f"sg{i}", [128, 256]) for i in range(3)]
        hid = [tsb(f"hid{i}", [128, 256]) for i in range(3)]
        hidT = [tsb(f"hidT{i}", [128, 256], BF16) for i in range(3)]
        yb = [tsb(f"yb{i}", [128, D]) for i in range(2)]
        y1 = [tsb(f"y1_{i}", [128, D]) for i in range(2)]
        y2 = [tsb(f"y2_{i}", [128, D]) for i in range(2)]
        rr = [tsb(f"rr{i}", [128, D]) for i in range(2)]
        xh2 = tsb("xh2", [128, D])
        ob = tsb("ob", [128, D])
        bst2 = tsb("bst2", [128, 2, 6])
        mv2 = tsb("mv2", [128, 4])
        rot = PsumRot(BANKS[0:2])
        rot2 = PsumRot([(PS2[i], (f"bank{2 * i}", f"bank{2 * i + 1}")) for i in (2, 3)])

        P.dma("sp", lambda e: e.dma_start(out=cb, in_=cb_d), w=["cb"])
        P.op("dve", lambda e: e.tensor_copy(out=SUb, in_=SUf), r=["cb"], w=["SUb"])
        P.op("dve", lambda e: e.memset(onesb, 1.0), w=["onesb"])
        for kc in range(8):
            P.dma("pool", lambda e, kc=kc: e.dma_start(out=Wr[:, kc, 0:4], in_=w_grp[kc * 128:(kc + 1) * 128, :]),
                  w=["Wr"])
            P.dma("pool", lambda e, kc=kc: e.dma_start(out=Wr[:, kc, 4:36], in_=w_exp[kc * 128:(kc + 1) * 128, :]),
                  w=["Wr"])
        P.dma("sp", lambda e: e.dma_start(out=b36[:, 0:4], in_=b_grp[0, :].partition_broadcast(128)), w=["b36"])
        P.dma("sp", lambda e: e.dma_start(out=b36[:, 4:36], in_=b_exp[0, :].partition_broadcast(128)), w=["b36"])
        P.dma("sp", lambda e: e.dma_start(out=gate2.rearrange("p a b -> p (a b)"), in_=g2_s), w=["gate2"])
        P.dma("sp", lambda e: e.dma_start(out=sh2r.rearrange("p a b -> p (a b)"), in_=sh2_s), w=["sh2r"])
        P.dma("sp", lambda e: e.dma_start(out=sc2r.rearrange("p a b -> p (a b)"), in_=sc2_s), w=["sc2r"])
        P.op("pool", lambda e: e.tensor_scalar(out=sc2r, in0=sc2r, scalar1=1.0, scalar2=1.0 / ALPHA, op0=ALU.add,
                                               op1=ALU.mult), r=["sc2r"], w=["sc2r"])
        P.dma("sp", lambda e: e.dma_start(out=g2, in_=ln2_g[0, :].partition_broadcast(128)), w=["g2"])
        P.dma("sp", lambda e: e.dma_start(out=b2, in_=ln2_b[0, :].partition_broadcast(128)), w=["b2"])

        RG = 4
        lg4 = tsb("lg4", [128, RG, 36])
        lgm4 = tsb("lgm4", [128, RG, 32])
        lgm24 = tsb("lgm24", [128, RG, 32])
        eg4 = tsb("eg4", [128, RG, 4])
        ohg4 = tsb("ohg4", [128, RG, 4])
        rsc = tsb("rsc", [128, 12, RG])

        def router4(g_):
            st0 = g_ * RG
            ci = g_ % 2
            P.dma("sp", lambda e: e.dma_start(out=h2c[ci], in_=h2_s[:, :, st0 * 128:st0 * 128 + 512]), w=[f"h2c{ci}"])
            plg, plgk = rot.get()
            for i in range(RG):
                cs = slice(i * 128, (i + 1) * 128)
                for kc in range(8):
                    P.op("pe", lambda e, kc=kc, i=i, cs=cs: e.matmul(plg[:, i * 36:(i + 1) * 36], lhsT=h2c[ci][:, kc, cs],
                                                                      rhs=Wr[:, kc, :], start=(kc == 0), stop=(kc == 7)),
                         r=[f"h2c{ci}", "Wr"], w=[plgk])
            gmax, sume, grpw, m1, m2, d21, p2, den, rden = [rsc[:, i, :] for i in range(9)]
            sts = range(st0, st0 + RG)
            K1 = [f"oh1_{st}" for st in sts]
            K2 = [f"oh2_{st}" for st in sts]
            KA = [f"gA{st}" for st in sts]
            KB = [f"gB{st}" for st in sts]
            KO = [f"OH{st}" for st in sts]
            oh1 = oh1all[:, st0:st0 + RG, :]
            oh2 = oh2all[:, st0:st0 + RG, :]
            gA = gAB[:, st0:st0 + RG, 0]
            gB = gAB[:, st0:st0 + RG, 1]
            bcx = lambda ap, n: ap.unsqueeze(2).to_broadcast([128, RG, n])
            P.op("dve", lambda e: e.tensor_tensor(out=lg4, in0=plg[:, 0:RG * 36].rearrange("p (s n) -> p s n", s=RG),
                                                  in1=b36.unsqueeze(1).to_broadcast([128, RG, 36]), op=ALU.add),
                 r=[plgk, "b36"], w=["lg4"])
            P.op("dve", lambda e: e.tensor_reduce(out=gmax, in_=lg4[:, :, 0:4], axis=AX.X, op=ALU.max), r=["lg4"], w=["q_gmax"])
            P.op("dve", lambda e: e.tensor_tensor(out=eg4, in0=lg4[:, :, 0:4], in1=bcx(gmax, 4), op=ALU.subtract),
                 r=["lg4", "q_gmax"], w=["eg4"])
            P.op("act", lambda e: e.activation(out=eg4, in_=eg4, func=AF.Exp), r=["eg4"], w=["eg4"])
            P.op("dve", lambda e: e.tensor_reduce(out=sume, in_=eg4, axis=AX.X, op=ALU.add), r=["eg4"], w=["q_sume"])
            P.op("dve", lambda e: e.reciprocal(out=grpw, in_=sume), r=["q_sume"], w=["q_grpw"])
            P.op("dve", lambda e: e.tensor_tensor(out=ohg4, in0=lg4[:, :, 0:4], in1=bcx(gmax, 4), op=ALU.is_equal),
                 r=["lg4", "q_gmax"], w=["ohg4"])
            P.op("dve", lambda e: e.tensor_scalar(out=ohg4, in0=ohg4, scalar1=-1.0, scalar2=BIG, op0=ALU.add, op1=ALU.mult),
                 r=["ohg4"], w=["ohg4"])
            P.op("dve", lambda e: e.tensor_tensor(out=lgm4.rearrange("p s (g k) -> p s g k", g=4),
                                                  in0=lg4[:, :, 4:36].rearrange("p s (g k) -> p s g k", g=4),
                                                  in1=ohg4.unsqueeze(3).to_broadcast([128, RG, 4, 8]), op=ALU.add),
                 r=["lg4", "ohg4"], w=["lgm4"])
            P.op("dve", lambda e: e.tensor_reduce(out=m1, in_=lgm4, axis=AX.X, op=ALU.max), r=["lgm4"], w=["q_m1"])
            P.op("dve", lambda e: e.tensor_tensor(out=oh1, in0=lgm4, in1=bcx(m1, 32), op=ALU.is_equal),
                 r=["lgm4", "q_m1"], w=K1)
            P.op("dve", lambda e: e.scalar_tensor_tensor(out=lgm24, in0=oh1, scalar=-BIG, in1=lgm4, op0=ALU.mult,
                                                         op1=ALU.add), r=K1 + ["lgm4"], w=["lgm24"])
            P.op("dve", lambda e: e.tensor_reduce(out=m2, in_=lgm24, axis=AX.X, op=ALU.max), r=["lgm24"], w=["q_m2"])
            P.op("dve", lambda e: e.tensor_tensor(out=oh2, in0=lgm24, in1=bcx(m2, 32), op=ALU.is_equal),
                 r=["lgm24", "q_m2"], w=K2)
            P.op("dve", lambda e: e.tensor_tensor(out=d21, in0=m2, in1=m1, op=ALU.subtract), r=["q_m1", "q_m2"], w=["q_d21"])
            P.op("act", lambda e: e.activation(out=p2, in_=d21, func=AF.Exp), r=["q_d21"], w=["q_p2"])
            P.op("dve", lambda e: e.tensor_scalar(out=den, in0=p2, scalar1=1.0, scalar2=None, op0=ALU.add),
                 r=["q_p2"], w=["q_den"])
            P.op("dve", lambda e: e.reciprocal(out=rden, in_=den), r=["q_den"], w=["q_rden"])
            P.op("dve", lambda e: e.tensor_tensor(out=gA, in0=grpw, in1=rden, op=ALU.mult), r=["q_grpw", "q_rden"], w=KA)
            P.op("dve", lambda e: e.tensor_tensor(out=gB, in0=gA, in1=p2, op=ALU.mult), r=KA + ["q_p2"], w=KB)
            P.op("pool", lambda e: e.tensor_tensor(out=OHall[:, st0:st0 + RG, :], in0=oh1, in1=oh2, op=ALU.add),
                 r=K1 + K2, w=KO)

        for g_ in range(NSTT // RG):
            router4(g_)

        pcnt, pcntk = rot.get()
        for st in range(NSTT):
            P.op("pe", lambda e, st=st: e.matmul(pcnt[:, 0:32], lhsT=onesb, rhs=OHall[:, st, :],
                                                 start=(st == 0), stop=(st == NSTT - 1)),
                 r=[f"OH{st}", "onesb"], w=[pcntk])
        P.op("act", lambda e: e.activation(out=cnt, in_=pcnt[:, 0:32], func=AF.Copy), r=[pcntk], w=["cnt"])
        big3 = big[:, 0:32 * 64].rearrange("p (e k) -> p e k", e=32)
        P.op("dve", lambda e: e.tensor_tensor(out=big3, in0=cnt.unsqueeze(2).to_broadcast([128, 32, 64]),
                                              in1=thr.unsqueeze(1).to_broadcast([128, 32, 64]), op=ALU.is_gt),
             r=["cnt", "cb"], w=["big"])
        P.op("dve", lambda e: e.tensor_reduce(out=nblk, in_=big3, axis=AX.X, op=ALU.add), r=["big"], w=["nblk"])
        big3b = big[:, 0:1024].rearrange("p (e f) -> p e f", e=32)
        P.op("dve", lambda e: e.tensor_tensor(out=big3b, in0=nblk.unsqueeze(1).to_broadcast([128, 32, 32]),
                                              in1=SLc.rearrange("p (e f) -> p e f", e=32), op=ALU.mult),
             r=["nblk", "cb", "big"], w=["big"])
        P.op("dve", lambda e: e.tensor_reduce(out=pstb, in_=big3b, axis=AX.X, op=ALU.add), r=["big"], w=["pstb"])
        P.op("dve", lambda e: e.tensor_tensor(out=pend, in0=pstb, in1=nblk, op=ALU.add), r=["pstb", "nblk"], w=["pend"])
        P.op("dve", lambda e: e.tensor_scalar(out=base, in0=pstb, scalar1=128.0, scalar2=None, op0=ALU.mult),
             r=["pstb"], w=["base"])
        big3c = big.rearrange("p (b e) -> p b e", b=NBLK)
        P.op("dve", lambda e: e.tensor_tensor(out=big3c, in0=pend.unsqueeze(1).to_broadcast([128, NBLK, 32]),
                                              in1=blkio.unsqueeze(2).to_broadcast([128, NBLK, 32]), op=ALU.is_le),
             r=["pend", "cb", "big", "pstb"], w=["big"])
        P.op("dve", lambda e: e.tensor_reduce(out=bexp, in_=big3c, axis=AX.X, op=ALU.add), r=["big"], w=["bexp"])
        P.op("dve", lambda e: e.tensor_scalar(out=bskip, in0=bexp, scalar1=float(NEXP) - 0.5, scalar2=None, op0=ALU.is_ge),
             r=["bexp"], w=["bskip"])
        P.op("dve", lambda e: e.tensor_tensor(out=bsame[:, NW:NBLK], in0=bexp[:, NW:NBLK], in1=bexp[:, 0:NBLK - NW],
                                              op=ALU.is_equal), r=["bexp"], w=["bsame"])
        P.op("dve", lambda e: e.tensor_tensor(out=bskip[:, NW:NBLK], in0=bskip[:, NW:NBLK], in1=bsame[:, NW:NBLK],
                                              op=ALU.max), r=["bskip", "bsame"], w=["bskip"])
        P.op("dve", lambda e: e.tensor_scalar(out=bexp, in0=bexp, scalar1=float(NEXP - 1), scalar2=128.0,
                                              op0=ALU.min, op1=ALU.mult), r=["bexp"], w=["bexp"])
        P.op("dve", lambda e: e.tensor_scalar(out=bexp, in0=bexp, scalar1=iotaP, scalar2=None, op0=ALU.add),
             r=["bexp", "cb"], w=["bexp"])
        P.op("dve", lambda e: e.scalar_tensor_tensor(out=bexp, in0=bskip, scalar=1.0e6, in1=bexp, op0=ALU.mult,
                                                     op1=ALU.add), r=["bexp", "bskip"], w=["bexp"])
        P.op("dve", lambda e: e.tensor_copy(out=IDXW, in_=bexp), r=["bexp"], w=["IDXW"])
        for st in range(NSTT):
            prk, prkk = rot.get()
            P.op("pe", lambda e, st=st, prk=prk: e.matmul(prk[:, 0:32], lhsT=SUb, rhs=OHall[:, st, :],
                                                          start=True, stop=(st == 0)), r=[f"OH{st}", "SUb"], w=[prkk])
            for s2 in range(st):
                P.op("pe", lambda e, s2=s2, st=st, prk=prk: e.matmul(prk[:, 0:32], lhsT=onesb, rhs=OHall[:, s2, :],
                                                                     start=False, stop=(s2 == st - 1)),
                     r=[f"OH{s2}", "onesb"], w=[prkk])
            P.op("dve", lambda e, prk=prk: e.tensor_tensor(out=posv, in0=prk[:, 0:32], in1=base, op=ALU.add),
                 r=[prkk, "base"], w=["posv"])
            for k, oha in ((0, oh1all), (1, oh2all)):
                P.op("dve", lambda e, st=st, oha=oha: e.tensor_tensor(out=ptmp, in0=posv, in1=oha[:, st, :], op=ALU.mult),
                     r=["posv", f"oh1_{st}", f"oh2_{st}"], w=["ptmp"])
                P.op("dve", lambda e, st=st, k=k: e.tensor_reduce(out=POSf[:, st, k:k + 1], in_=ptmp, axis=AX.X, op=ALU.add),
                     r=["ptmp"], w=["POSf"])
        P.op("dve", lambda e: e.tensor_copy(out=POSi, in_=POSf), r=["POSf"], w=["POSi"])

        if stage == "pbdbg":
            P.dma("sp", lambda e: e.dma_start(out=dbg_out["cnt"], in_=cnt), r=["cnt"], w=["dg1"])
            P.dma("sp", lambda e: e.dma_start(out=dbg_out["nblk"], in_=nblk), r=["nblk"], w=["dg2"])
            P.dma("sp", lambda e: e.dma_start(out=dbg_out["pstb"], in_=pstb), r=["pstb"], w=["dg3"])
            P.dma("sp", lambda e: e.dma_start(out=dbg_out["bexp"], in_=bexp), r=["bexp"], w=["dg4"])
            P.dma("sp", lambda e: e.dma_start(out=dbg_out["posf"], in_=POSf.rearrange("p a b -> p (a b)")), r=["POSf"], w=["dg5"])
            P.dma("sp", lambda e: e.dma_start(out=dbg_out["oh1"], in_=oh1all.rearrange("p a b -> p (a b)")),
                  r=[f"oh1_{st}" for st in range(NSTT)], w=["dg6"])
            P.dma("sp", lambda e: e.dma_start(out=dbg_out["oh2"], in_=oh2all.rearrange("p a b -> p (a b)")),
                  r=[f"oh2_{st}" for st in range(NSTT)], w=["dg7"])
            P.dma("sp", lambda e: e.dma_start(out=dbg_out["gab"], in_=gAB.rearrange("p a b -> p (a b)")),
                  r=[f"gA{st}" for st in range(NSTT)] + [f"gB{st}" for st in range(NSTT)], w=["dg8"])
            P.emit()
            return nc
        for st in range(NSTT):
            b = st // (NSTT // NB)
            xq = xb[st % 3]
            xk = f"xb{st % 3}"
            P.dma("sp", lambda e, st=st, xq=xq: e.dma_start(out=xq, in_=r_s[st * 128:(st + 1) * 128, :]), w=[xk])
            P.op("dve", lambda e, xq=xq, b=b: e.tensor_tensor(out=xq, in0=xq, in1=sc2r[:, b, :], op=ALU.mult),
                 r=[xk, "sc2r"], w=[xk])
            P.op("dve", lambda e, xq=xq, b=b: e.tensor_tensor(out=xq, in0=xq, in1=sh2r[:, b, :], op=ALU.add),
                 r=[xk, "sh2r"], w=[xk])
            for k in range(2):
                P.dma("pool", lambda e, st=st, k=k, xq=xq: e.indirect_dma_start(
                    out=xrows_s, out_offset=bass.IndirectOffsetOnAxis(ap=POSi[:, st, k:k + 1], axis=0),
                    in_=xq, in_offset=None), r=[xk, "POSi"], w=[f"xrows{st}_{k}"])
        if stage in ("pbs1", "pbs2"):
            P.emit()
            return nc

        pgu_banks = PsumRot(BANKS[0:2])
        NQ = 3
        wgate_rows = w_gate.rearrange("e (p k) n -> (e p) (k n)", k=8)
        wup_rows = w_up.rearrange("e (p k) n -> (e p) (k n)", k=8)
        wdn_rows = w_down.rearrange("e (p k) n -> (e p) (k n)", k=2)

        def blk_loadx(bi):
            q = bi % NQ
            P.dma("sp", lambda e: e.dma_start(out=xb[q], in_=xrows_s[bi * 128:(bi + 1) * 128, :]),
                  r=[f"xrows{st}_{k}" for st in range(NSTT) for k in range(2)], w=[f"xb{q}"])

        bnd = {}

        def bnd_reg(e):
            if "r" not in bnd:
                bnd["r"] = e.alloc_register("wbound")
                e.reg_mov(bnd["r"], NEXP * 128 - 1)
            return bnd["r"]

        def blk_load(bi):
            slot = bi % NW
            ioff = bass.IndirectOffsetOnAxis(ap=IDXW[:, bi:bi + 1], axis=0)
            P.dma("pool", lambda e: e.indirect_dma_start(out=Wg[slot], out_offset=None,
                                                         in_=wgate_rows, in_offset=ioff, bounds_check=bnd_reg(e), oob_is_err=False), r=["IDXW"], w=[f"wg{slot}"])
            P.dma("pool", lambda e: e.indirect_dma_start(out=Wu[slot], out_offset=None,
                                                         in_=wup_rows, in_offset=ioff, bounds_check=bnd_reg(e), oob_is_err=False), r=["IDXW"], w=[f"wu{slot}"])
            P.dma("pool", lambda e: e.indirect_dma_start(out=Wd[slot], out_offset=None,
                                                         in_=wdn_rows, in_offset=ioff, bounds_check=bnd_reg(e), oob_is_err=False), r=["IDXW"], w=[f"wd{slot}"])

        xt_banks = [BANKS[2], BANKS[3]]
        ht_banks = PsumRot(BANKS[6:8])

        def blk_xt(bi):
            q = bi % NQ
            xv = xb[q].rearrange("r (p k) -> r k p", k=8)
            xTf = xT[q].rearrange("p a b -> p (a b)")
            for hb_ in range(2):
                pt, ptk = xt_banks[hb_]
                for k4 in range(4):
                    kc = hb_ * 4 + k4
                    P.op("pe", lambda e, kc=kc, k4=k4, pt=pt: e.transpose(out=pt[:, k4 * 128:(k4 + 1) * 128],
                                                                          in_=xv[:, kc, :], identity=C["ident"]),
                         r=[f"xb{q}"], w=[ptk])
                if hb_ == 0:
                    P.op("act", lambda e, pt=pt: e.activation(out=xTf[:, 0:512], in_=pt, func=AF.Copy),
                         r=[ptk], w=[f"xTa{q}"])
                else:
                    P.op("dve", lambda e, pt=pt: e.tensor_copy(out=xTf[:, 512:1024], in_=pt), r=[ptk], w=[f"xTb{q}"])

        def blk_gu(bi):
            slot = bi % NW
            q = bi % NQ
            pgu, pguk = pgu_banks.get()
            for (Wm, wk, c0) in ((Wg, f"wg{slot}", 0), (Wu, f"wu{slot}", 256)):
                for kc in range(8):
                    P.op("pe", lambda e, kc=kc, Wm=Wm, c0=c0: e.matmul(
                        pgu[:, c0:c0 + 256], lhsT=xT[q][:, kc, :], rhs=Wm[slot][:, kc * 256:(kc + 1) * 256],
                        start=(kc == 0), stop=(kc == 7)), r=[f"xTa{q}", f"xTb{q}", wk], w=[pguk])
            P.op("act", lambda e: e.activation(out=sg[q], in_=pgu[:, 0:256], func=AF.Exp, scale=-1.0), r=[pguk], w=[f"sg{q}"])
            P.op("act", lambda e: e.activation(out=sg[q], in_=sg[q], func=AF.Ln, bias=1.0), r=[f"sg{q}"], w=[f"sg{q}"])
            P.op("act", lambda e: e.activation(out=sg[q], in_=sg[q], func=AF.Exp, scale=-1.0), r=[f"sg{q}"], w=[f"sg{q}"])
            P.op("dve", lambda e: e.tensor_tensor(out=sg[q], in0=pgu[:, 0:256], in1=sg[q], op=ALU.mult),
                 r=[pguk, f"sg{q}"], w=[f"sg{q}"])
            P.op("dve", lambda e: e.tensor_tensor(out=hid[q], in0=pgu[:, 256:512], in1=sg[q], op=ALU.mult),
                 r=[pguk, f"sg{q}"], w=[f"hid{q}"])

        def blk_tr(bi):
            q = bi % NQ
            pht, phtk = ht_banks.get()
            hv = hid[q].rearrange("r (p k) -> r k p", k=2)
            for k2 in range(2):
                P.op("pe", lambda e, k2=k2: e.transpose(out=pht[:, k2 * 128:(k2 + 1) * 128], in_=hv[:, k2, :],
                                                        identity=C["ident"]), r=[f"hid{q}"], w=[phtk])
            P.op("act", lambda e: e.activation(out=hidT[q], in_=pht[:, 0:256], func=AF.Copy), r=[phtk], w=[f"hidT{q}"])

        def blk_dn(bi):
            slot = bi % NW
            q = bi % NQ
            yq = bi % 2
            py, (k0, k1) = PS2[2], ("bank4", "bank5")
            for half in range(2):
                for k2 in range(2):
                    P.op("pe", lambda e, half=half, k2=k2: e.matmul(
                        py[:, half * 512:(half + 1) * 512], lhsT=hidT[q][:, k2 * 128:(k2 + 1) * 128],
                        rhs=Wd[slot][:, k2 * 1024 + half * 512:k2 * 1024 + (half + 1) * 512], start=(k2 == 0), stop=(k2 == 1)),
                        r=[f"hidT{q}", f"wd{slot}"], w=[(k0, k1)[half]])
            P.op("act", lambda e: e.activation(out=yb[yq][:, 0:512], in_=py[:, 0:512], func=AF.Copy), r=[k0], w=[f"yba{yq}"])
            P.op("dve", lambda e: e.tensor_copy(out=yb[yq][:, 512:1024], in_=py[:, 512:1024]), r=[k1], w=[f"ybb{yq}"])
            P.dma("sp", lambda e: e.dma_start(out=yrows_s[bi * 128:(bi + 1) * 128, :], in_=yb[yq]),
                  r=[f"yba{yq}", f"ybb{yq}"], w=[f"yrows{bi}"])

        nb_run = NBLK if stage != 'pbs3' else 6
        pendq = []
        blk_load(0)
        for b0_ in range(min(2, nb_run)):
            blk_loadx(b0_)
        for bi in range(nb_run):
            if bi + 2 < nb_run:
                blk_loadx(bi + 2)
            blk_xt(bi)
            blk_gu(bi)
            pendq.append(bi)
            if len(pendq) >= 2:
                blk_tr(pendq[-2])
            if len(pendq) >= 3:
                blk_dn(pendq.pop(0))
            if bi + 1 < nb_run:
                blk_load(bi + 1)
        if len(pendq) == 2:
            blk_dn(pendq.pop(0))
        while pendq:
            a = pendq.pop(0)
            blk_tr(a)
            blk_dn(a)
        YK = [f"yrows{bi}" for bi in range(nb_run)]
        if stage == "pbs3":
            P.emit()
            return nc

        def comb_fetch(st):
            q = st % 2
            P.dma("sp", lambda e: e.dma_start(out=rr[q], in_=r_s[st * 128:(st + 1) * 128, :]), w=[f"rr{q}"])
            P.dma("pool", lambda e: e.indirect_dma_start(
                out=y1[q], out_offset=None, in_=yrows_s,
                in_offset=bass.IndirectOffsetOnAxis(ap=POSi[:, st, 0:1], axis=0)), r=YK + ["POSi"], w=[f"y1_{q}"])
            P.dma("pool", lambda e: e.indirect_dma_start(
                out=y2[q], out_offset=None, in_=yrows_s,
                in_offset=bass.IndirectOffsetOnAxis(ap=POSi[:, st, 1:2], axis=0)), r=YK + ["POSi"], w=[f"y2_{q}"])

        def comb_compute(st):
            q = st % 2
            b = st // (NSTT // NB)
            P.op("act", lambda e: e.activation(out=y1[q], in_=y1[q], func=AF.Copy, scale=gAB[:, st, 0:1]),
                 r=[f"y1_{q}", f"gA{st}"], w=[f"y1_{q}"])
            P.op("dve", lambda e: e.scalar_tensor_tensor(out=y1[q], in0=y2[q], scalar=gAB[:, st, 1:2], in1=y1[q],
                                                         op0=ALU.mult, op1=ALU.add),
                 r=[f"y1_{q}", f"y2_{q}", f"gB{st}"], w=[f"y1_{q}"])
            P.op("dve", lambda e: e.tensor_tensor(out=y1[q], in0=y1[q], in1=gate2[:, b, :], op=ALU.mult),
                 r=[f"y1_{q}", "gate2"], w=[f"y1_{q}"])
            P.op("dve", lambda e: e.tensor_tensor(out=rr[q], in0=rr[q], in1=y1[q], op=ALU.add),
                 r=[f"rr{q}", f"y1_{q}"], w=[f"rr{q}"])
            for hf in range(2):
                P.op("dve", lambda e, hf=hf: e.bn_stats(out=bst2[:, hf, :], in_=rr[q][:, hf * 512:(hf + 1) * 512]),
                     r=[f"rr{q}"], w=["bst2"])
            P.op("dve", lambda e: e.bn_aggr(out=mv2[:, 0:2], in_=bst2.rearrange("p a b -> p (a b)")), r=["bst2"], w=["mv2"])
            P.op("act", lambda e: e.activation(out=mv2[:, 2:3], in_=mv2[:, 1:2], func=AF.Ln, bias=1e-5),
                 r=["mv2"], w=["mv2b"])
            P.op("act", lambda e: e.activation(out=mv2[:, 3:4], in_=mv2[:, 2:3], func=AF.Exp, scale=-0.5),
                 r=["mv2b"], w=["mv2c"])
            P.op("dve", lambda e: e.scalar_tensor_tensor(out=mv2[:, 2:3], in0=mv2[:, 0:1], scalar=-1.0, in1=mv2[:, 3:4],
                                                         op0=ALU.mult, op1=ALU.mult), r=["mv2", "mv2b", "mv2c"], w=["mv2b", "mv2d"])
            P.op("act", lambda e: e.activation(out=xh2, in_=rr[q], func=AF.Identity, scale=mv2[:, 3:4], bias=mv2[:, 2:3]),
                 r=[f"rr{q}", "mv2c", "mv2d"], w=["xh2"])
            oq = ob2[q]
            P.op("pool", lambda e: e.tensor_tensor(out=oq, in0=xh2, in1=g2, op=ALU.mult), r=["xh2", "g2"], w=[f"ob{q}"])
            P.op("pool", lambda e: e.tensor_tensor(out=oq, in0=oq, in1=b2, op=ALU.add), r=[f"ob{q}", "b2"], w=[f"ob{q}"])
            P.dma("sp", lambda e: e.dma_start(out=out[st * 128:(st + 1) * 128, :], in_=oq), r=[f"ob{q}"], w=["outd"])

        ob2 = [ob, yb[0]]
        comb_fetch(0)
        for st in range(NSTT):
            if st + 1 < NSTT:
                comb_fetch(st + 1)
            comb_compute(st)
        cnt_ = P.emit()
        print("phaseB op counts", cnt_)

    return nc


_NC_CACHE = {}


def _get_nc():
    if "nc" not in _NC_CACHE:
        _NC_CACHE["nc"] = build("full")
    return _NC_CACHE["nc"]


def make_in_maps(inputs):
    f = lambda a: np.ascontiguousarray(np.asarray(a, dtype=np.float32))
    shared = {
        "w_ada": f(inputs["w_ada"][0]), "b_ada": f(inputs["b_ada"]), "w_in": f(inputs["w_in"][0]),
        "conv_w": f(inputs["conv_w"][0]), "conv_norm_w": f(inputs["conv_norm_w"]),
        "dn_conv_w": f(inputs["dn_conv_w"][0]), "dn_A_log": f(inputs["dn_A_log"]),
        "dn_dt_bias": f(inputs["dn_dt_bias"]), "dn_norm_w": f(inputs["dn_norm_w"]),
        "w_out": f(inputs["w_out"][0]), "ln1_g": f(inputs["ln1_g"]), "ln1_b": f(inputs["ln1_b"]),
        "w_grp": f(inputs["w_grp"][0]), "b_grp": f(inputs["b_grp"]), "w_exp": f(inputs["w_exp"][0]),
        "b_exp": f(inputs["b_exp"]), "w_gate": f(inputs["w_gate"][0]), "w_up": f(inputs["w_up"][0]),
        "w_down": f(inputs["w_down"][0]), "ln2_g": f(inputs["ln2_g"]), "ln2_b": f(inputs["ln2_b"]),
        "cmat": CMAT, "mab": MAB, "cb": CB,
    }
    xs = f(inputs["x"]).reshape(8, TOK, D)
    cs = f(inputs["c"]).reshape(8, NB, D)
    return [dict(shared, x=xs[i], c=cs[i]) for i in range(8)]


def kernel(**inputs):
    nc = _get_nc()
    in_maps = make_in_maps(inputs)
    res = run_bass_kernel_spmd(nc, in_maps, core_ids=list(range(8)))
    outs = [np.asarray(r["out"], dtype=np.float32).reshape(NB, SEQ, D) for r in res.results]
    return np.concatenate(outs, axis=0)
```

```python
import contextlib
import numpy as np
import concourse.bass as bass
import concourse.mybir as mybir
from concourse.bass_utils import run_bass_kernel_spmd

F32 = mybir.dt.float32
BF16 = mybir.dt.bfloat16
AF = mybir.ActivationFunctionType
ALU = mybir.AluOpType
AX = mybir.AxisListType

ENGS = ("pe", "act", "dve", "pool", "sp")

D = 1024
SEQ = 2048
NB = 2
TOK = NB * SEQ
DIN = 3592
NEXP = 32
ALPHA = 2.0 ** 0.25
TT = 256
NT = TOK // TT
BIG = 30000.0


class Prog:
    NDMA = 24

    def __init__(self, nc, tag):
        self.nc = nc
        self.tag = tag
        self.ops = []
        self.chain_dma = False

    def op(self, eng, fn, r=(), w=()):
        self.ops.append(dict(eng=eng, fn=fn, r=tuple(r), w=tuple(w), dma=False))

    def dma(self, eng, fn, r=(), w=()):
        chain = (f"__q_{eng}",) if self.chain_dma else ()
        self.ops.append(dict(eng=eng, fn=fn, r=tuple(r), w=tuple(w) + chain, dma=True))

    def emit(self, final_wait_engine="sp"):
        nc = self.nc
        esem = {e: nc.alloc_semaphore(f"s_{e}_{self.tag}") for e in ENGS if e != "sp"}
        dsem = [nc.alloc_semaphore(f"d_{i}_{self.tag}") for i in range(self.NDMA)]
        ecount = {e: 0 for e in ENGS}
        dtotal = [0] * self.NDMA
        dnext = 0
        last_w = {}
        readers = {}
        waited = {e: {} for e in ENGS}
        per_eng = {e: [] for e in ENGS}
        tokens = []
        for i, o in enumerate(self.ops):
            E = o["eng"]
            deps = set()
            for k in o["r"]:
                if k in last_w:
                    deps.add(last_w[k])
            for k in o["w"]:
                if k in last_w:
                    deps.add(last_w[k])
                for rd in readers.get(k, ()):
                    deps.add(rd)
            waits = []
            for d in sorted(deps):
                od = self.ops[d]
                if (not od["dma"]) and od["eng"] == E and E == "pe" and not o["dma"]:
                    continue
                s, v = tokens[d]
                key = id(s)
                if waited[E].get(key, 0) >= v:
                    continue
                waited[E][key] = v
                waits.append((s, v))
            if o["dma"]:
                j = dnext
                dnext = (dnext + 1) % self.NDMA
                s = dsem[j]
                if dtotal[j] > 0 and waited[E].get(id(s), 0) < dtotal[j]:
                    waited[E][id(s)] = dtotal[j]
                    waits.append((s, dtotal[j]))
                dtotal[j] += 16
                tok = (s, dtotal[j])
                inc = 16
            else:
                ecount[E] += 1
                tok = (esem[E], ecount[E])
                inc = 1
            tokens.append(tok)
            per_eng[E].append((waits, o["fn"], tok[0], inc))
            for k in o["r"]:
                readers.setdefault(k, []).append(i)
            for k in o["w"]:
                last_w[k] = i
                readers[k] = []
        final_waits = [(esem[e], ecount[e]) for e in esem if ecount[e] > 0]
        final_waits += [(dsem[j], dtotal[j]) for j in range(self.NDMA) if dtotal[j] > 0]

        with nc.Block() as block:
            def mk(ename):
                def body(eng):
                    for waits, fn, s, inc in per_eng[ename]:
                        for (ws, wv) in waits:
                            eng.wait_ge(ws, wv)
                        ins = fn(eng)
                        ins.then_inc(s, inc)
                    if ename == final_wait_engine:
                        for (ws, wv) in final_waits:
                            eng.wait_ge(ws, wv)
                return body
            block.tensor(mk("pe"))
            block.scalar(mk("act"))
            block.vector(mk("dve"))
            block.gpsimd(mk("pool"))
            block.sync(mk("sp"))
        return dict(ecount)


class PsumRot:
    def __init__(self, items):
        self.items = list(items)
        self.i = 0

    def get(self):
        it = self.items[self.i]
        self.i = (self.i + 1) % len(self.items)
        return it


def make_consts():
    idx = np.arange(128)
    same = (idx[:, None] // 64) == (idx[None, :] // 64)
    c = {}
    c["ident"] = np.eye(128)
    c["ltb"] = (same & (idx[:, None] <= idx[None, :])) * 1.0
    c["bd"] = same * 1.0
    c["mnu"] = np.where(same & (idx[:, None] <= idx[None, :]), 0.0, -BIG)
    c["mnus"] = np.where(same & (idx[:, None] < idx[None, :]), 0.0, -BIG)
    c["mnls"] = np.where(same & (idx[:, None] > idx[None, :]), 0.0, -BIG)
    c["ones"] = np.ones((128, 128))
    c["gmat"] = same / 64.0
    c["omean"] = np.ones((128, 128)) / 128.0
    names = ["ident", "ltb", "bd", "mnu", "mnus", "mnls", "ones", "gmat", "omean"]
    cm = np.concatenate([c[n] for n in names], axis=1).astype(np.float32)
    mab = np.stack([(idx < 64) * 1.0, (idx >= 64) * 1.0], axis=1).astype(np.float32)
    return names, cm, mab


CNAMES, CMAT, MAB = make_consts()
NBLK = TOK * 2 // 128 + NEXP
NROWS = NBLK * 128


def make_consts_b():
    thr = np.broadcast_to(128.0 * np.arange(64)[None, :], (128, 64))
    e = np.arange(32)
    sl = np.broadcast_to((e[None, :] < e[:, None]).astype(np.float64).reshape(1, 1024), (128, 1024))
    blk = np.broadcast_to(np.arange(NBLK, dtype=np.float64)[None, :], (128, NBLK))
    iop = np.arange(128, dtype=np.float64)[:, None]
    idx = np.arange(128)
    su = (idx[:, None] < idx[None, :]) * 1.0
    return np.concatenate([thr, sl, blk, iop, su], axis=1).astype(np.float32)


CB = make_consts_b()


def build(stage="full", dbg=()):
    nc = bass.Bass("TRN2", target_bir_lowering=False)

    def din(name, shape, dt=F32):
        return nc.dram_tensor(name, list(shape), dt, kind="ExternalInput").ap()

    x = din("x", [TOK, D])
    c_in = din("c", [NB, D])
    w_ada = din("w_ada", [D, 6 * D])
    b_ada = din("b_ada", [1, 6 * D])
    w_in = din("w_in", [D, DIN])
    conv_w = din("conv_w", [3, 512])
    conv_norm_w = din("conv_norm_w", [1, 512])
    dn_conv_w = din("dn_conv_w", [4, 1536])
    dn_A_log = din("dn_A_log", [1, 4])
    dn_dt_bias = din("dn_dt_bias", [1, 4])
    dn_norm_w = din("dn_norm_w", [1, 128])
    w_out = din("w_out", [D, D])
    ln1_g = din("ln1_g", [1, D])
    ln1_b = din("ln1_b", [1, D])
    w_grp = din("w_grp", [D, 4])
    b_grp = din("b_grp", [1, 4])
    w_exp = din("w_exp", [D, 32])
    b_exp = din("b_exp", [1, 32])
    w_gate = din("w_gate", [NEXP, D, 256])
    w_up = din("w_up", [NEXP, D, 256])
    w_down = din("w_down", [NEXP, 256, D])
    ln2_g = din("ln2_g", [1, D])
    ln2_b = din("ln2_b", [1, D])
    cmat_d = din("cmat", list(CMAT.shape))
    mab_d = din("mab", [128, 2])
    cb_d = din("cb", list(CB.shape))
    out = nc.dram_tensor("out", [TOK, D], F32, kind="ExternalOutput").ap()
    r_s = nc.dram_tensor("r_scr", [TOK, D], F32, kind="Internal").ap()
    h2_s = nc.dram_tensor("h2_scr", [128, 8, TOK], BF16, kind="Internal").ap()
    g2_s = nc.dram_tensor("g2_scr", [128, 2 * D], F32, kind="Internal").ap()
    sh2_s = nc.dram_tensor("sh2_scr", [128, 2 * D], F32, kind="Internal").ap()
    sc2_s = nc.dram_tensor("sc2_scr", [128, 2 * D], F32, kind="Internal").ap()
    xrows_s = nc.dram_tensor("xrows_scr", [NROWS if stage != "pa1" else 128, D], F32, kind="Internal").ap()
    yrows_s = nc.dram_tensor("yrows_scr", [NROWS if stage != "pa1" else 128, D], F32, kind="Internal").ap()
    dbg_out = {}
    for (nm, shp) in dbg:
        dbg_out[nm] = nc.dram_tensor("dbg_" + nm, list(shp), F32, kind="ExternalOutput").ap()

    def sb(name, shape, dt=F32):
        return nc.alloc_sbuf_tensor("sb_" + name, list(shape), dt).ap()

    PS2 = [nc.alloc_psum_tensor(f"ps2_{i}", [128, 1024], F32).ap() for i in range(4)]
    BANKS = []
    for i in range(4):
        BANKS.append((PS2[i][:, 0:512], f"bank{2 * i}"))
        BANKS.append((PS2[i][:, 512:1024], f"bank{2 * i + 1}"))

    cm = sb("cm", [128, CMAT.shape[1]])
    C = {n: cm[:, i * 128:(i + 1) * 128] for i, n in enumerate(CNAMES)}
    mab = sb("mab", [128, 2])
    identb = sb("identb", [128, 128], BF16)
    modT = sb("modT", [128, 48, 2])
    s1p = sb("s1p", [128, 8, 2])
    A2 = sb("A2", [128, 8, 2])
    B2 = sb("B2", [128, 8, 2])
    gate_bc = {2: sb("gate1bc", [128, 2, D])}
    cw = sb("cw", [128, 4, 3])
    cnw = sb("cnw", [128, 4])
    dcw = sb("dcw", [128, 12, 4])
    dnw = sb("dnw", [128, 1])
    g1T = sb("g1T", [128, 8])
    b1T = sb("b1T", [128, 8])
    negA = sb("negA", [128, 4])
    dtb = sb("dtb", [128, 4])
    ag = sb("ag", [128, D])
    ab = sb("ab", [128, D])

    esA = contextlib.ExitStack()

    def tsbA(name, shape, dt=F32):
        return esA.enter_context(nc.sbuf_tensor("a_" + name, list(shape), dt)).ap()

    Win = tsbA("win", [128, 8, DIN], BF16)
    Wout = tsbA("wout", [128, 8, D], BF16)

    P = Prog(nc, "p0")
    P.dma("sp", lambda e: e.dma_start(out=cm, in_=cmat_d), w=["cm"])
    P.dma("sp", lambda e: e.dma_start(out=mab, in_=mab_d), w=["mab"])
    P.op("dve", lambda e: e.tensor_copy(out=identb, in_=C["ident"]), r=["cm"], w=["identb"])

    with nc.sbuf_tensor("t_cT", [128, 8, 2], F32) as cT_h, \
            nc.sbuf_tensor("t_cact", [128, 8, 2], BF16) as cact_h, \
            nc.sbuf_tensor("t_cbc", [128, 8, 2, 128], BF16) as cbc_h, \
            nc.sbuf_tensor("t_brow", [1, 6 * D], BF16) as brow_h, \
            nc.sbuf_tensor("t_onesr", [1, 128], BF16) as onesr_h, \
            nc.sbuf_tensor("t_wa0", [128, 8, D], BF16) as wa0_h, \
            nc.sbuf_tensor("t_wa1", [128, 8, D], BF16) as wa1_h, \
            nc.sbuf_tensor("t_ws0", [128, 8, 512], F32) as ws0_h, \
            nc.sbuf_tensor("t_ws1", [128, 8, 512], F32) as ws1_h, \
            nc.sbuf_tensor("t_sp2", [128, 8, 2], F32) as sp2_h, \
            nc.sbuf_tensor("t_g2bc", [128, 2, D], F32) as g2bc_h, \
            nc.sbuf_tensor("t_sh2bc", [128, 2, D], F32) as sh2bc_h, \
            nc.sbuf_tensor("t_sc2bc", [128, 2, D], F32) as sc2bc_h:
        gate_bc[5] = g2bc_h.ap()
        gate_bc[3] = sh2bc_h.ap()
        gate_bc[4] = sc2bc_h.ap()
        cT, cact, cbc, brow, onesr, sp2 = (t.ap() for t in (cT_h, cact_h, cbc_h, brow_h, onesr_h, sp2_h))
        wa = [wa0_h.ap(), wa1_h.ap()]
        wstg = [ws0_h.ap(), ws1_h.ap()]
        for b in range(NB):
            P.dma("sp", lambda e, b=b: e.dma_start(
                out=cT[:, :, b], in_=c_in[b, :].rearrange("(kc p) -> p kc", p=128),
                allow_slow_non_contiguous=True), w=["cT"])
        P.op("act", lambda e: e.activation(out=cact, in_=cT, func=AF.Silu), r=["cT"], w=["cact"])
        P.op("dve", lambda e: e.tensor_copy(out=cbc, in_=cact.unsqueeze(3).to_broadcast([128, 8, 2, 128])),
             r=["cact"], w=["cbc"])
        P.dma("pool", lambda e: e.dma_start(out=brow, in_=b_ada), w=["brow"])
        for kc in range(8):
            for (c0, c1) in ((0, 2048), (2048, DIN)):
                P.dma("pool", lambda e, kc=kc, c0=c0, c1=c1: e.dma_start(
                    out=Win[:, kc, c0:c1], in_=w_in[kc * 128:(kc + 1) * 128, c0:c1]), w=[f"Win{kc}_{c0}"])
            P.dma("pool", lambda e, kc=kc: e.dma_start(
                out=Wout[:, kc, :], in_=w_out[kc * 128:(kc + 1) * 128, :]), w=[f"Wout{kc}"])
        P.op("dve", lambda e: e.memset(onesr, 1.0), w=["onesr"])
        for k in range(3):
            P.dma("sp", lambda e, k=k: e.dma_start(out=cw[:, :, k],
                                                   in_=conv_w[k, :].rearrange("(j p) -> p j", p=128),
                                                   allow_slow_non_contiguous=True), w=["cw"])
        P.dma("sp", lambda e: e.dma_start(out=cnw, in_=conv_norm_w[0, :].rearrange("(j p) -> p j", p=128),
                                          allow_slow_non_contiguous=True), w=["cnw"])
        for k in range(4):
            P.dma("sp", lambda e, k=k: e.dma_start(out=dcw[:, :, k],
                                                   in_=dn_conv_w[k, :].rearrange("(j p) -> p j", p=128),
                                                   allow_slow_non_contiguous=True), w=["dcw"])
        P.dma("sp", lambda e: e.dma_start(out=dnw, in_=dn_norm_w.rearrange("o p -> p o"),
                                          allow_slow_non_contiguous=True), w=["dnw"])
        P.dma("sp", lambda e: e.dma_start(out=g1T, in_=ln1_g[0, :].rearrange("(j p) -> p j", p=128),
                                          allow_slow_non_contiguous=True), w=["g1T"])
        P.dma("sp", lambda e: e.dma_start(out=b1T, in_=ln1_b[0, :].rearrange("(j p) -> p j", p=128),
                                          allow_slow_non_contiguous=True), w=["b1T"])
        P.dma("sp", lambda e: e.dma_start(out=negA, in_=dn_A_log[0, :].partition_broadcast(128)), w=["negA"])
        P.dma("sp", lambda e: e.dma_start(out=dtb, in_=dn_dt_bias[0, :].partition_broadcast(128)), w=["dtb"])
        P.op("act", lambda e: e.activation(out=negA, in_=negA, func=AF.Exp), r=["negA"], w=["negA"])
        P.op("dve", lambda e: e.tensor_scalar(out=negA, in0=negA, scalar1=-1.0, scalar2=None, op0=ALU.mult),
             r=["negA"], w=["negA"])

        ps0 = PsumRot(BANKS[0:1])
        psr = PsumRot(BANKS[1:3])
        modps, modk = ps0.get()
        for j in range(6):
            wj = wa[j % 2]
            wk = f"wa{j % 2}"
            for hf in range(2):
                si = (2 * j + hf) % 2
                stg = wstg[si]
                P.dma("sp", lambda e, j=j, hf=hf, stg=stg: e.dma_start(
                    out=stg, in_=w_ada[:, j * D + hf * 512:j * D + (hf + 1) * 512].rearrange("(kc p) n -> p kc n", p=128)),
                    w=[f"wstg{si}"])
                eng_ = "dve" if hf == 0 else "act"
                if eng_ == "dve":
                    P.op("dve", lambda e, wj=wj, hf=hf, stg=stg: e.tensor_copy(out=wj[:, :, hf * 512:(hf + 1) * 512], in_=stg),
                         r=[f"wstg{si}"], w=[wk + f"_{hf}"])
                else:
                    P.op("act", lambda e, wj=wj, hf=hf, stg=stg: e.activation(out=wj[:, :, hf * 512:(hf + 1) * 512], in_=stg,
                                                                              func=AF.Copy), r=[f"wstg{si}"], w=[wk + f"_{hf}"])
            for cc in range(8):
                col = (j * 8 + cc) * 2
                for kc in range(8):
                    P.op("pe", lambda e, wj=wj, kc=kc, cc=cc, col=col: e.matmul(
                        modps[:, col:col + 2], lhsT=wj[:, kc, cc * 128:(cc + 1) * 128], rhs=cact[:, kc, :],
                        start=(kc == 0), stop=False), r=[wk + "_0", wk + "_1", "cact"], w=[modk])
                P.op("pe", lambda e, j=j, cc=cc, col=col: e.matmul(
                    modps[:, col:col + 2], lhsT=brow[0:1, j * D + cc * 128:j * D + (cc + 1) * 128],
                    rhs=onesr[0:1, 0:2], start=False, stop=True), r=["brow", "onesr"], w=[modk])
            if j in (2, 3, 4, 5):
                for b in range(NB):
                    for half in range(2):
                        pt, pk = psr.get()
                        for kc in range(8):
                            P.op("pe", lambda e, wj=wj, kc=kc, b=b, half=half, pt=pt: e.matmul(
                                pt, lhsT=cbc[:, kc, b, :], rhs=wj[:, kc, half * 512:(half + 1) * 512],
                                start=(kc == 0), stop=False), r=[wk + "_0", wk + "_1", "cbc"], w=[pk])
                        P.op("pe", lambda e, j=j, half=half, pt=pt: e.matmul(
                            pt, lhsT=onesr[0:1, :], rhs=brow[0:1, j * D + half * 512:j * D + (half + 1) * 512],
                            start=False, stop=True), r=["brow", "onesr"], w=[pk])
                        P.op("act", lambda e, j=j, b=b, half=half, pt=pt: e.activation(
                            out=gate_bc[j][:, b, half * 512:(half + 1) * 512], in_=pt, func=AF.Copy),
                            r=[pk], w=[f"gbc{j}"])
        P.op("dve", lambda e: e.tensor_copy(out=modT.rearrange("p a b -> p (a b)"), in_=modps[:, 0:96]),
             r=[modk], w=["modT"])
        P.op("dve", lambda e: e.tensor_scalar(out=s1p, in0=modT[:, 8:16, :], scalar1=1.0, scalar2=None, op0=ALU.add),
             r=["modT"], w=["s1p"])
        P.op("dve", lambda e: e.tensor_scalar(out=sp2, in0=modT[:, 32:40, :], scalar1=1.0, scalar2=None, op0=ALU.add),
             r=["modT"], w=["sp2"])
        P.op("dve", lambda e: e.tensor_tensor(out=A2, in0=sp2, in1=g1T.unsqueeze(2).to_broadcast([128, 8, 2]),
                                              op=ALU.mult), r=["sp2", "g1T"], w=["A2"])
        P.op("dve", lambda e: e.tensor_tensor(out=B2, in0=sp2, in1=b1T.unsqueeze(2).to_broadcast([128, 8, 2]),
                                              op=ALU.mult), r=["sp2", "b1T"], w=["B2"])
        P.op("dve", lambda e: e.tensor_tensor(out=B2, in0=B2, in1=modT[:, 24:32, :], op=ALU.add),
             r=["B2", "modT"], w=["B2"])
        P.dma("sp", lambda e: e.dma_start(out=ag, in_=ln1_g[0, :].partition_broadcast(128)), w=["ag"])
        P.dma("sp", lambda e: e.dma_start(out=ab, in_=ln1_b[0, :].partition_broadcast(128)), w=["ab"])
        P.op("dve", lambda e: e.tensor_scalar(out=ag, in0=ag, scalar1=ALPHA, scalar2=None, op0=ALU.mult),
             r=["ag"], w=["ag"])
        P.op("dve", lambda e: e.tensor_scalar(out=ab, in0=ab, scalar1=ALPHA, scalar2=None, op0=ALU.mult),
             r=["ab"], w=["ab"])
        P.dma("sp", lambda e: e.dma_start(out=g2_s, in_=gate_bc[5].rearrange("p a b -> p (a b)")),
              r=["gbc5"], w=["g2s"])
        P.dma("sp", lambda e: e.dma_start(out=sh2_s, in_=gate_bc[3].rearrange("p a b -> p (a b)")),
              r=["gbc3"], w=["sh2s"])
        P.dma("sp", lambda e: e.dma_start(out=sc2_s, in_=gate_bc[4].rearrange("p a b -> p (a b)")),
              r=["gbc4"], w=["sc2s"])
        if stage == "p0":
            P.dma("sp", lambda e: e.dma_start(out=dbg_out["modT"], in_=modT.rearrange("p a b -> p (a b)")),
                  r=["modT"], w=["dbgo"])
            P.dma("sp", lambda e: e.dma_start(out=dbg_out["g1bc"], in_=gate_bc[2].rearrange("p a b -> p (a b)")),
                  r=["gbc2"], w=["dbgo2"])
        P.emit()

    if stage == "p0":
        return nc
    with esA as es:
        tsb = tsbA
        P = Prog(nc, "pa")

        xt = tsb("xt", [128, 2, D])
        hT = tsb("hT", [128, 8, TT], BF16)
        cutail = tsb("cutail", [128, 4, 2])
        qtail = tsb("qtail", [128, 12, 3])
        qkvc2 = [tsb(f"qkvc{i}", [128, 12, TT]) for i in range(2)]
        zs2 = [tsb(f"zs{i}", [128, 4, TT]) for i in range(2)]
        mixT2 = [tsb(f"mixT{i}", [128, 8, TT], BF16) for i in range(3)]
        blsb2 = [tsb(f"blsb{i}", [128, 16]) for i in range(2)]
        S = tsb("S", [128, 4, 128])
        csb = tsb("csb", [128, TT])
        cuf2 = [tsb(f"cuf{i}", [128, TT + 2]) for i in range(2)]
        acc2 = [tsb(f"acc{i}", [128, TT]) for i in range(2)]
        ybuf2 = [tsb(f"ybuf{i}", [128, TT]) for i in range(2)]
        sqb2 = [tsb(f"sqb{i}", [128, TT]) for i in range(2)]
        sgt2 = [tsb(f"sgt{i}", [128, TT]) for i in range(2)]
        halo = [tsb(f"halo{i}", [128, TT + 3]) for i in range(2)]
        sm2 = [tsb(f"sm{i}", [128, 2, 64]) for i in range(2)]
        T = [tsb(f"T{i}", [128, 512]) for i in range(13)]
        r0 = tsb("r0", [128, D])
        xh = tsb("xh", [128, D])
        h2t = tsb("h2t", [128, 8, TT], BF16)
        bst = tsb("bst", [128, 2, 6])
        mv = tsb("mv", [128, 4])
        rotA = PsumRot(BANKS[0:4])
        rotB = PsumRot(BANKS[4:8])
        rot2A = PsumRot([(PS2[i], (f"bank{2 * i}", f"bank{2 * i + 1}")) for i in (0, 1)])

        def v3(ap):
            return ap.rearrange("p (h j) -> p h j", h=4)

        def bc_h(ap128):
            return ap128.unsqueeze(1).to_broadcast([128, 4, 128])

        def bc_j(ap4):
            return ap4.unsqueeze(2).to_broadcast([128, 4, 128])

        EPS_RMS = 1e-6

        def stage1(ti):
            pp = ti % 2
            b = ti // (NT // NB)
            first = (ti % (NT // NB) == 0)
            qkvc, zs, mixT, blsb = qkvc2[pp], zs2[pp], mixT2[ti % 3], blsb2[pp]
            mp = ti % 3
            rot = rotA
            P.dma("sp", lambda e: e.dma_start(
                out=xt, in_=x[ti * TT:(ti + 1) * TT, :].rearrange("(s p) f -> p s f", p=128)), w=["xt"])
            if first:
                P.op("pool", lambda e: e.memset(cutail, 0.0), w=["cutail"])
                P.op("pool", lambda e: e.memset(qtail, 0.0), w=["qtail"])
            for kc in range(8):
                pt, pk = rot.get()
                for s_ in range(2):
                    P.op("pe", lambda e, pt=pt, s_=s_, kc=kc: e.transpose(
                        out=pt[:, s_ * 128:(s_ + 1) * 128], in_=xt[:, s_, kc * 128:(kc + 1) * 128],
                        identity=C["ident"]), r=["xt"], w=[pk])
                P.op("act", lambda e, pt=pt, kc=kc: e.activation(
                    out=hT[:, kc, :], in_=pt[:, 0:TT], func=AF.Identity,
                    scale=s1p[:, kc, b:b + 1], bias=modT[:, kc, b:b + 1]), r=[pk], w=[f"hT{kc}"])

            def proj(oc):
                pt, pk = rot.get()
                for kc in range(8):
                    P.op("pe", lambda e, pt=pt, kc=kc: e.matmul(
                        pt[:, 0:TT], lhsT=Win[:, kc, oc * 128:(oc + 1) * 128], rhs=hT[:, kc, :],
                        start=(kc == 0), stop=(kc == 7)), r=[f"hT{kc}", "Win"], w=[pk])
                return pt[:, 0:TT], pk

            deferred = []

            def flush(keep=0):
                while len(deferred) > keep:
                    deferred.pop(0)()

            def rstd_part2(srcbuf, srck, lhs, q):
                sqb = sqb2[q]
                pm, pmk = rot.get()
                P.op("pe", lambda e: e.matmul(pm[:, 0:TT], lhsT=lhs, rhs=sqb, start=True, stop=True),
                     r=[f"sqb{q}"], w=[pmk])
                P.op("act", lambda e: e.activation(out=sqb, in_=pm[:, 0:TT], func=AF.Ln, bias=EPS_RMS),
                     r=[pmk], w=[f"sqb{q}"])
                P.op("act", lambda e: e.activation(out=sqb, in_=sqb, func=AF.Exp, scale=-0.5),
                     r=[f"sqb{q}"], w=[f"sqb{q}"])

            for j in range(4):
                q = j % 2
                cuf, acc, ybuf, sqb = cuf2[q], acc2[q], ybuf2[q], sqb2[q]
                pb, pbk = proj(j)
                pc, pck = proj(4 + j)
                pu, puk = proj(8 + j)
                P.op("act", lambda e, pc=pc: e.activation(out=csb, in_=pc, func=AF.Copy), r=[pck], w=["csb"])
                P.op("dve", lambda e, pu=pu, cuf=cuf: e.tensor_tensor(out=cuf[:, 2:TT + 2], in0=pu, in1=csb, op=ALU.mult),
                     r=[puk, "csb"], w=[f"cuf{q}"])
                P.op("pool", lambda e, j=j, cuf=cuf: e.tensor_copy(out=cuf[:, 0:2], in_=cutail[:, j, :]),
                     r=["cutail"], w=[f"cufh{q}"])
                P.op("act", lambda e, j=j, cuf=cuf, acc=acc: e.activation(out=acc, in_=cuf[:, 2:TT + 2], func=AF.Copy,
                                                                          scale=cw[:, j, 2:3]), r=[f"cuf{q}"], w=[f"acc{q}"])
                P.op("dve", lambda e, j=j, cuf=cuf, acc=acc: e.scalar_tensor_tensor(
                    out=acc, in0=cuf[:, 1:TT + 1], scalar=cw[:, j, 1:2], in1=acc, op0=ALU.mult, op1=ALU.add),
                    r=[f"cuf{q}", f"cufh{q}", f"acc{q}"], w=[f"acc{q}"])
                P.op("dve", lambda e, j=j, cuf=cuf, acc=acc: e.scalar_tensor_tensor(
                    out=acc, in0=cuf[:, 0:TT], scalar=cw[:, j, 0:1], in1=acc, op0=ALU.mult, op1=ALU.add),
                    r=[f"cuf{q}", f"cufh{q}", f"acc{q}"], w=[f"acc{q}"])
                P.op("pool", lambda e, j=j, cuf=cuf: e.tensor_copy(out=cutail[:, j, :], in_=cuf[:, TT:TT + 2]),
                     r=[f"cuf{q}"], w=["cutail"])
                P.op("dve", lambda e, pb=pb, acc=acc, ybuf=ybuf: e.tensor_tensor(out=ybuf, in0=pb, in1=acc, op=ALU.mult),
                     r=[pbk, f"acc{q}"], w=[f"ybuf{q}"])
                P.op("act", lambda e, ybuf=ybuf, sqb=sqb: e.activation(out=sqb, in_=ybuf, func=AF.Square),
                     r=[f"ybuf{q}"], w=[f"sqb{q}"])

                def part2(j=j, q=q, ybuf=ybuf, sqb=sqb):
                    rstd_part2(None, None, C["gmat"], q)
                    P.op("dve", lambda e: e.scalar_tensor_tensor(
                        out=mixT[:, j, :], in0=ybuf, scalar=cnw[:, j:j + 1], in1=sqb, op0=ALU.mult, op1=ALU.mult),
                        r=[f"ybuf{q}", f"sqb{q}"], w=[f"mixT{mp}_{j}"])
                flush(0)
                deferred.append(part2)

            for j in range(12):
                q = j % 2
                sqb = sqb2[q]
                pq, pqk = proj(12 + j)
                hb = halo[q]
                hk = f"halo{q}"
                qk_ = f"qk{pp}_{j}"
                P.op("act", lambda e, pq=pq, hb=hb: e.activation(out=hb[:, 3:TT + 3], in_=pq, func=AF.Copy),
                     r=[pqk], w=[hk])
                P.op("pool", lambda e, j=j, hb=hb: e.tensor_copy(out=hb[:, 0:3], in_=qtail[:, j, :]),
                     r=["qtail"], w=[hk + "h"])
                P.op("pool", lambda e, j=j, hb=hb: e.tensor_scalar(
                    out=qkvc[:, j, :], in0=hb[:, 3:TT + 3], scalar1=dcw[:, j, 3:4], scalar2=0.0, op0=ALU.mult,
                    op1=ALU.add), r=[hk], w=[qk_])
                for k in (2, 1, 0):
                    P.op("dve", lambda e, j=j, hb=hb, k=k: e.scalar_tensor_tensor(
                        out=qkvc[:, j, :], in0=hb[:, k:TT + k], scalar=dcw[:, j, k:k + 1], in1=qkvc[:, j, :],
                        op0=ALU.mult, op1=ALU.add), r=[hk, hk + "h", qk_], w=[qk_])
                P.op("pool", lambda e, j=j, hb=hb: e.tensor_copy(out=qtail[:, j, :], in_=hb[:, TT:TT + 3]),
                     r=[hk], w=["qtail"])
                sgt = sgt2[q]
                P.op("act", lambda e, j=j, sgt=sgt: e.activation(out=sgt, in_=qkvc[:, j, :], func=AF.Exp, scale=-1.0),
                     r=[qk_], w=[f"sgt{q}"])
                P.op("act", lambda e, sgt=sgt: e.activation(out=sgt, in_=sgt, func=AF.Ln, bias=1.0),
                     r=[f"sgt{q}"], w=[f"sgt{q}"])
                P.op("act", lambda e, sgt=sgt: e.activation(out=sgt, in_=sgt, func=AF.Exp, scale=-1.0),
                     r=[f"sgt{q}"], w=[f"sgt{q}"])
                P.op("pool", lambda e, j=j, sgt=sgt: e.tensor_tensor(out=qkvc[:, j, :], in0=qkvc[:, j, :], in1=sgt,
                                                                     op=ALU.mult), r=[qk_, f"sgt{q}"], w=[qk_])
                if j < 8:
                    P.op("act", lambda e, j=j, sqb=sqb: e.activation(out=sqb, in_=qkvc[:, j, :], func=AF.Square),
                         r=[qk_], w=[f"sqb{q}"])

                    def part2(j=j, q=q, sqb=sqb, qk_=qk_):
                        rstd_part2(None, None, C["ones"], q)
                        sc = (128.0 ** -0.5) if j < 4 else 1.0
                        P.op("dve", lambda e: e.scalar_tensor_tensor(
                            out=qkvc[:, j, :], in0=qkvc[:, j, :], scalar=sc, in1=sqb, op0=ALU.mult, op1=ALU.mult),
                            r=[qk_, f"sqb{q}"], w=[qk_])
                    flush(0)
                    deferred.append(part2)
                else:
                    flush(0)
            flush(0)
            for j in range(4):
                pz, pzk = proj(24 + j)
                sgt = sgt2[j % 2]
                sk = f"sgt{j % 2}"
                P.op("act", lambda e, pz=pz, sgt=sgt: e.activation(out=sgt, in_=pz, func=AF.Exp, scale=-1.0),
                     r=[pzk], w=[sk])
                P.op("act", lambda e, sgt=sgt: e.activation(out=sgt, in_=sgt, func=AF.Ln, bias=1.0), r=[sk], w=[sk])
                P.op("act", lambda e, sgt=sgt: e.activation(out=sgt, in_=sgt, func=AF.Exp, scale=-1.0), r=[sk], w=[sk])
                P.op("dve", lambda e, pz=pz, j=j, sgt=sgt: e.tensor_tensor(out=zs[:, j, :], in0=pz, in1=sgt, op=ALU.mult),
                     r=[pzk, sk], w=[f"zs{pp}_{j}"])
            p8, p8k = rot.get()
            for s_ in range(2):
                for kc in range(8):
                    P.op("pe", lambda e, s_=s_, kc=kc: e.matmul(
                        p8[:, s_ * 8:(s_ + 1) * 8], lhsT=hT[:, kc, s_ * 128:(s_ + 1) * 128],
                        rhs=Win[:, kc, 3584:3592], start=(kc == 0), stop=(kc == 7)),
                        r=[f"hT{kc}", "Win"], w=[p8k])
            P.op("act", lambda e: e.activation(out=blsb, in_=p8[:, 0:16], func=AF.Copy), r=[p8k], w=[f"blsb{pp}"])
            for s_ in range(2):
                smx = sm2[pp][:, s_, :]
                beta, xa, g, _g, egc, bge, dl, eL, sA, sB = [smx[:, i * 4:(i + 1) * 4] for i in range(10)]
                gcs = smx[:, 40:48]
                gcum = gcs[:, 0:4]
                glast = gcs[:, 4:8]
                SK = f"smk{pp}_{s_}"
                bl = blsb[:, s_ * 8:(s_ + 1) * 8]
                blk = f"blsb{pp}"
                P.op("act", lambda e, beta=beta, bl=bl: e.activation(out=beta, in_=bl[:, 0:4], func=AF.Exp, scale=-1.0),
                     r=[blk], w=[SK])
                P.op("dve", lambda e, beta=beta: e.tensor_scalar(out=beta, in0=beta, scalar1=1.0, scalar2=None, op0=ALU.add),
                     r=[SK], w=[SK])
                P.op("dve", lambda e, beta=beta: e.reciprocal(out=beta, in_=beta), r=[SK], w=[SK])
                P.op("dve", lambda e, xa=xa, bl=bl: e.tensor_tensor(out=xa, in0=bl[:, 4:8], in1=dtb, op=ALU.add),
                     r=[blk, SK], w=[SK])
                P.op("act", lambda e, xa=xa: e.activation(out=xa, in_=xa, func=AF.Exp), r=[SK], w=[SK])
                P.op("act", lambda e, xa=xa: e.activation(out=xa, in_=xa, func=AF.Ln, bias=1.0), r=[SK], w=[SK])
                P.op("dve", lambda e, g=g, xa=xa: e.tensor_tensor(out=g, in0=xa, in1=negA, op=ALU.mult), r=[SK], w=[SK])
                pc_, pck = rot.get()
                P.op("pe", lambda e, pc_=pc_, g=g: e.matmul(pc_[:, 0:4], lhsT=C["ltb"], rhs=g, start=True, stop=True),
                     r=[SK], w=[pck])
                P.op("pe", lambda e, pc_=pc_, g=g: e.matmul(pc_[:, 4:8], lhsT=C["bd"], rhs=g, start=True, stop=True),
                     r=[SK], w=[pck])
                P.op("act", lambda e, pc_=pc_, gcs=gcs: e.activation(out=gcs, in_=pc_[:, 0:8], func=AF.Copy), r=[pck], w=[SK])
                P.op("act", lambda e, egc=egc, gcum=gcum: e.activation(out=egc, in_=gcum, func=AF.Exp), r=[SK], w=[SK])
                P.op("dve", lambda e, bge=bge, beta=beta, egc=egc: e.tensor_tensor(out=bge, in0=beta, in1=egc, op=ALU.mult),
                     r=[SK], w=[SK])
                P.op("dve", lambda e, dl=dl, glast=glast, gcum=gcum: e.tensor_tensor(out=dl, in0=glast, in1=gcum,
                                                                                     op=ALU.subtract), r=[SK], w=[SK])
                P.op("act", lambda e, eL=eL, dl=dl: e.activation(out=eL, in_=dl, func=AF.Exp), r=[SK], w=[SK])
                P.op("dve", lambda e, sA=sA, eL=eL: e.tensor_scalar(out=sA, in0=eL, scalar1=mab[:, 0:1], scalar2=None,
                                                                    op0=ALU.mult), r=[SK], w=[SK])
                P.op("dve", lambda e, sB=sB, eL=eL: e.tensor_scalar(out=sB, in0=eL, scalar1=mab[:, 1:2], scalar2=None,
                                                                    op0=ALU.mult), r=[SK], w=[SK])

        def stage2(ti):
            first = (ti % (NT // NB) == 0)
            if first:
                P.op("pool", lambda e: e.memset(S, 0.0), w=["S0", "S1", "S2", "S3"])
            for s_ in range(2):
                gdn_sub(ti, s_)

        def stage3(ti):
            b = ti // (NT // NB)
            for s_ in range(2):
                ln1_sub(ti, s_, b)
            P.dma("sp", lambda e: e.dma_start(out=h2_s[:, :, ti * TT:(ti + 1) * TT], in_=h2t),
                  r=["h2t"], w=["h2s"])

        def gdn_sub(ti, s_):
            pp = ti % 2
            rot = rotB
            qkvc, zs, mixT, blsb = qkvc2[pp], zs2[pp], mixT2[ti % 3], blsb2[pp]
            mp = ti % 3
            bl = blsb[:, s_ * 8:(s_ + 1) * 8]
            blk = f"blsb{pp}"
            cs = slice(s_ * 128, (s_ + 1) * 128)
            R1, R2, tU, tL, egr, U, L, QKm, Xa, Xb, Pb, PTb, bv = T
            kR1, kR2, ktU, ktL, kegr, kU, kL, kQKm, kXa, kXb, kPb, kPTb, kbv = [f"T{i}" for i in range(13)]
            keA, kkeA, keB, kkeB = R2, kR2, tL, ktL
            u, ku, wT, kwT, qdT, kqdT, delta, kdelta = U, kU, L, kL, Pb, kPb, PTb, kPTb
            smx = sm2[pp][:, s_, :]
            beta, xa, g, _g, egc, bge, dl, eL, sA, sB = [smx[:, i * 4:(i + 1) * 4] for i in range(10)]
            gcs = smx[:, 40:48]
            gcum = gcs[:, 0:4]
            glast = gcs[:, 4:8]
            SK = f"smk{pp}_{s_}"
            qk = lambda j: f"qk{pp}_{j}"
            P.op("dve", lambda e: e.tensor_tensor(out=v3(R1), in0=bc_h(C["ltb"]), in1=bc_j(g), op=ALU.mult),
                 r=[SK], w=[kR1])
            P.op("dve", lambda e: e.tensor_tensor(out=v3(R2), in0=bc_h(C["ident"]), in1=bc_j(beta), op=ALU.mult),
                 r=[SK], w=[kR2])
            pgr, pgrk = rot.get()
            P.op("pe", lambda e: e.matmul(pgr, lhsT=C["ones"], rhs=R1, start=True, stop=True), r=[kR1], w=[pgrk])
            pbr, pbrk = rot.get()
            P.op("pe", lambda e: e.matmul(pbr, lhsT=C["ones"], rhs=R2, start=True, stop=True), r=[kR2], w=[pbrk])
            P.op("dve", lambda e: e.tensor_tensor(out=v3(tU), in0=v3(pgr), in1=bc_j(gcum), op=ALU.subtract),
                 r=[pgrk, SK], w=[ktU])
            P.op("dve", lambda e: e.tensor_tensor(out=v3(R1), in0=v3(tU), in1=bc_h(C["mnu"]), op=ALU.add),
                 r=[ktU], w=[kR1])
            P.op("act", lambda e: e.activation(out=R1, in_=R1, func=AF.Exp), r=[kR1], w=[kR1])
            P.op("dve", lambda e: e.tensor_tensor(out=v3(R2), in0=v3(tU), in1=bc_h(C["mnus"]), op=ALU.add),
                 r=[ktU], w=[kR2])
            P.op("act", lambda e: e.activation(out=R2, in_=R2, func=AF.Exp), r=[kR2], w=[kR2])
            P.op("dve", lambda e: e.tensor_tensor(out=R2, in0=R2, in1=pbr, op=ALU.mult), r=[kR2, pbrk], w=[kR2])
            P.op("dve", lambda e: e.scalar_tensor_tensor(out=v3(tL), in0=v3(pgr), scalar=-1.0, in1=bc_j(gcum),
                                                         op0=ALU.mult, op1=ALU.add), r=[pgrk, SK], w=[ktL])
            P.op("dve", lambda e: e.tensor_tensor(out=v3(tL), in0=v3(tL), in1=bc_h(C["mnls"]), op=ALU.add),
                 r=[ktL], w=[ktL])
            P.op("act", lambda e: e.activation(out=tL, in_=tL, func=AF.Exp), r=[ktL], w=[ktL])
            P.op("dve", lambda e: e.tensor_tensor(out=v3(tL), in0=v3(tL), in1=bc_j(beta), op=ALU.mult),
                 r=[ktL, SK], w=[ktL])
            P.op("act", lambda e: e.activation(out=egr, in_=pgr, func=AF.Exp), r=[pgrk], w=[kegr])
            pkk, pkkk = rot.get()
            for h in range(4):
                P.op("pe", lambda e, h=h: e.matmul(pkk[:, h * 128:(h + 1) * 128], lhsT=qkvc[:, 4 + h, cs],
                                                   rhs=qkvc[:, 4 + h, cs], start=True, stop=True),
                     r=[qk(4 + h)], w=[pkkk])
            P.op("dve", lambda e: e.tensor_tensor(out=U, in0=pkk, in1=R2, op=ALU.mult), r=[pkkk, kR2], w=[kU])
            P.op("dve", lambda e: e.tensor_tensor(out=L, in0=pkk, in1=tL, op=ALU.mult), r=[pkkk, ktL], w=[kL])
            pqk_, pqkk = rot.get()
            for h in range(4):
                P.op("pe", lambda e, h=h: e.matmul(pqk_[:, h * 128:(h + 1) * 128], lhsT=qkvc[:, 4 + h, cs],
                                                   rhs=qkvc[:, h, cs], start=True, stop=True),
                     r=[qk(4 + h), qk(h)], w=[pqkk])
            P.op("dve", lambda e: e.tensor_tensor(out=QKm, in0=pqk_, in1=R1, op=ALU.mult), r=[pqkk, kR1], w=[kQKm])
            P.op("dve", lambda e: e.tensor_tensor(out=v3(Xa), in0=bc_h(C["ident"]), in1=v3(U), op=ALU.subtract),
                 r=[kU], w=[kXa])
            pkt, pktk = rot.get()
            for h in range(4):
                P.op("pe", lambda e, h=h: e.transpose(out=pkt[:, h * 128:(h + 1) * 128], in_=qkvc[:, 4 + h, cs],
                                                      identity=C["ident"]), r=[qk(4 + h)], w=[pktk])
            kbg, kkbg = tU, ktU
            P.op("dve", lambda e: e.tensor_tensor(out=v3(kbg), in0=v3(pkt), in1=bc_j(bge), op=ALU.mult),
                 r=[pktk, SK], w=[kkbg])
            P.op("dve", lambda e: e.tensor_tensor(out=v3(keA), in0=v3(pkt), in1=bc_j(sA), op=ALU.mult),
                 r=[pktk, SK, kU], w=[kkeA])
            P.op("dve", lambda e: e.tensor_tensor(out=v3(keB), in0=v3(pkt), in1=bc_j(sB), op=ALU.mult),
                 r=[pktk, SK, kL], w=[kkeB])
            pvt, pvtk = rot.get()
            for h in range(4):
                P.op("pe", lambda e, h=h: e.transpose(out=pvt[:, h * 128:(h + 1) * 128], in_=qkvc[:, 8 + h, cs],
                                                      identity=C["ident"]), r=[qk(8 + h)], w=[pvtk])
            P.op("dve", lambda e: e.tensor_tensor(out=v3(bv), in0=v3(pvt), in1=bc_j(beta), op=ALU.mult),
                 r=[pvtk, SK], w=[kbv])
            Pc, PTc, kPc, kPTc = U, L, kU, kL
            Pn, PTn, kPn, kPTn = Pb, PTb, kPb, kPTb
            Xc, Xn, kXc, kXn = Xa, Xb, kXa, kXb
            for k in range(1, 6):
                if k < 5:
                    pp_, ppk = rot.get()
                    for h in range(4):
                        hs = slice(h * 128, (h + 1) * 128)
                        P.op("pe", lambda e, hs=hs, PTc=PTc, Pc=Pc, pp_=pp_: e.matmul(
                            pp_[:, hs], lhsT=PTc[:, hs], rhs=Pc[:, hs], start=True, stop=True),
                            r=[kPc, kPTc], w=[ppk])
                ppt, pptk = rot.get()
                for h in range(4):
                    hs = slice(h * 128, (h + 1) * 128)
                    P.op("pe", lambda e, hs=hs, PTc=PTc, Pc=Pc, ppt=ppt: e.matmul(
                        ppt[:, hs], lhsT=Pc[:, hs], rhs=PTc[:, hs], start=True, stop=True),
                        r=[kPc, kPTc], w=[pptk])
                if k < 5:
                    P.op("act", lambda e, Pn=Pn, pp_=pp_: e.activation(out=Pn, in_=pp_, func=AF.Copy), r=[ppk], w=[kPn])
                P.op("dve", lambda e, PTn=PTn, ppt=ppt: e.tensor_copy(out=PTn, in_=ppt), r=[pptk], w=[kPTn])
                px, pxk = rot.get()
                for h in range(4):
                    hs = slice(h * 128, (h + 1) * 128)
                    P.op("pe", lambda e, hs=hs, PTn=PTn, Xc=Xc, px=px: e.matmul(
                        px[:, hs], lhsT=PTn[:, hs], rhs=Xc[:, hs], start=True, stop=True),
                        r=[kPTn, kXc], w=[pxk])
                P.op("dve", lambda e, Xn=Xn, Xc=Xc, px=px: e.tensor_tensor(out=Xn, in0=px, in1=Xc, op=ALU.add),
                     r=[pxk, kXc], w=[kXn])
                Pc, Pn, kPc, kPn = Pn, Pc, kPn, kPc
                PTc, PTn, kPTc, kPTn = PTn, PTc, kPTn, kPTc
                Xc, Xn, kXc, kXn = Xn, Xc, kXn, kXc
            TTm, kTT = Xc, kXc
            assert TTm is Xb
            pu_, puk = rot.get()
            pw_, pwk = rot.get()
            for h in range(4):
                hs = slice(h * 128, (h + 1) * 128)
                P.op("pe", lambda e, hs=hs: e.matmul(pu_[:, hs], lhsT=TTm[:, hs], rhs=bv[:, hs], start=True, stop=True),
                     r=[kTT, kbv], w=[puk])
            for h in range(4):
                hs = slice(h * 128, (h + 1) * 128)
                P.op("pe", lambda e, hs=hs: e.matmul(pw_[:, hs], lhsT=kbg[:, hs], rhs=TTm[:, hs], start=True, stop=True),
                     r=[kTT, kkbg], w=[pwk])
            P.op("act", lambda e: e.activation(out=u, in_=pu_, func=AF.Copy), r=[puk], w=[ku])
            P.op("act", lambda e: e.activation(out=wT, in_=pw_, func=AF.Copy), r=[pwk], w=[kwT])
            P.op("dve", lambda e: e.tensor_tensor(out=v3(qdT), in0=qkvc[:, 0:4, cs], in1=v3(egr), op=ALU.mult),
                 r=[qk(h) for h in range(4)] + [kegr], w=[kqdT])
            po, pok = rot.get()
            others = [it_ for it_ in rotB.items if it_[1] != pok]
            oi = 0
            for ch in range(2):
                rows = slice(ch * 64, ch * 64 + 64)
                keX, kkeX = (keA, kkeA) if ch == 0 else (keB, kkeB)
                pws, pwsk = others[oi % 3]
                oi += 1
                for h in range(4):
                    hs = slice(h * 128, (h + 1) * 128)
                    P.op("pe", lambda e, hs=hs, h=h, pws=pws: e.matmul(pws[:, hs], lhsT=wT[:, hs], rhs=S[:, h, :],
                                                                      start=True, stop=True),
                         r=[kwT, f"S{h}"], w=[pwsk])
                P.op("dve", lambda e, rows=rows, pws=pws: e.tensor_tensor(out=delta[rows, :], in0=u[rows, :],
                                                                          in1=pws[rows, :], op=ALU.subtract),
                     r=[ku, pwsk], w=[kdelta])
                for h in range(4):
                    hs = slice(h * 128, (h + 1) * 128)
                    oc_ = slice(h * 128 + ch * 64, h * 128 + ch * 64 + 64)
                    P.op("pe", lambda e, h=h, oc_=oc_: e.matmul(po[:, oc_], lhsT=S[:, h, :], rhs=qdT[:, oc_],
                                                                start=True, stop=False),
                         r=[f"S{h}", kqdT], w=[pok])
                    P.op("pe", lambda e, hs=hs, oc_=oc_: e.matmul(po[:, oc_], lhsT=delta[:, hs], rhs=QKm[:, oc_],
                                                                  start=False, stop=True),
                         r=[kdelta, kQKm], w=[pok])
                pss, pssk = others[oi % 3]
                oi += 1
                for h in range(4):
                    hs = slice(h * 128, (h + 1) * 128)
                    P.op("pe", lambda e, hs=hs, keX=keX, pss=pss: e.matmul(pss[:, hs], lhsT=keX[:, hs], rhs=delta[:, hs],
                                                                          start=True, stop=True),
                         r=[kkeX, kdelta], w=[pssk])
                for h in range(4):
                    hs = slice(h * 128, (h + 1) * 128)
                    dcol = h * 128 + ch * 64 + 63
                    P.op("dve", lambda e, h=h, hs=hs, dcol=dcol, pss=pss: e.scalar_tensor_tensor(
                        out=S[:, h, :], in0=S[:, h, :], scalar=egr[:, dcol:dcol + 1], in1=pss[:, hs],
                        op0=ALU.mult, op1=ALU.add), r=[f"S{h}", kegr, pssk], w=[f"S{h}"])
            osb, kosb = bv, kbv
            sq2, ksq2 = R1, kR1
            P.op("act", lambda e: e.activation(out=osb, in_=po, func=AF.Copy), r=[pok], w=[kosb])
            P.op("act", lambda e: e.activation(out=sq2, in_=po, func=AF.Square), r=[pok], w=[ksq2])
            pm, pmk = others[oi % 3]
            P.op("pe", lambda e: e.matmul(pm, lhsT=C["omean"], rhs=sq2, start=True, stop=True), r=[ksq2], w=[pmk])
            P.op("act", lambda e: e.activation(out=sq2, in_=pm, func=AF.Ln, bias=EPS_RMS), r=[pmk], w=[ksq2])
            P.op("act", lambda e: e.activation(out=sq2, in_=sq2, func=AF.Exp, scale=-0.5), r=[ksq2], w=[ksq2])
            P.op("dve", lambda e: e.scalar_tensor_tensor(out=osb, in0=osb, scalar=dnw[:, 0:1], in1=sq2,
                                                         op0=ALU.mult, op1=ALU.mult), r=[kosb, ksq2], w=[kosb])
            P.op("pool", lambda e: e.tensor_tensor(out=mixT[:, 4:8, cs], in0=v3(osb), in1=zs[:, :, cs], op=ALU.mult),
                 r=[kosb] + [f"zs{pp}_{j}" for j in range(4)], w=[f"mixT{mp}_{4 + j}" for j in range(4)])

        def ln1_sub(ti, s_, b):
            mp = ti % 3
            mixT = mixT2[mp]
            rot = rotA
            cs = slice(s_ * 128, (s_ + 1) * 128)
            tok0 = ti * TT + s_ * 128
            P.dma("sp", lambda e: e.dma_start(out=xh, in_=x[tok0:tok0 + 128, :]), w=["xh"])
            pm2, (k0, k1) = rot2A.get()
            for half in range(2):
                for kc in range(8):
                    P.op("pe", lambda e, half=half, kc=kc: e.matmul(
                        pm2[:, half * 512:(half + 1) * 512], lhsT=mixT[:, kc, cs],
                        rhs=Wout[:, kc, half * 512:(half + 1) * 512], start=(kc == 0), stop=(kc == 7)),
                        r=[f"mixT{mp}_{kc}", "Wout"], w=[(k0, k1)[half]])
            P.op("dve", lambda e: e.tensor_tensor(out=r0, in0=pm2, in1=gate_bc[2][:, b, :], op=ALU.mult),
                 r=[k0, k1], w=["r0"])
            P.op("dve", lambda e: e.scalar_tensor_tensor(out=r0, in0=xh, scalar=ALPHA, in1=r0,
                                                         op0=ALU.mult, op1=ALU.add), r=["xh", "r0"], w=["r0"])
            for hf in range(2):
                P.op("dve", lambda e, hf=hf: e.bn_stats(out=bst[:, hf, :], in_=r0[:, hf * 512:(hf + 1) * 512]),
                     r=["r0"], w=["bst"])
            P.op("dve", lambda e: e.bn_aggr(out=mv[:, 0:2], in_=bst.rearrange("p a b -> p (a b)")), r=["bst"], w=["mv"])
            P.op("act", lambda e: e.activation(out=mv[:, 2:3], in_=mv[:, 1:2], func=AF.Ln, bias=1e-5),
                 r=["mv"], w=["mv2"])
            P.op("act", lambda e: e.activation(out=mv[:, 3:4], in_=mv[:, 2:3], func=AF.Exp, scale=-0.5),
                 r=["mv2"], w=["mv3"])
            P.op("dve", lambda e: e.tensor_scalar(out=xh, in0=r0, scalar1=mv[:, 0:1], scalar2=mv[:, 3:4],
                                                  op0=ALU.subtract, op1=ALU.mult), r=["r0", "mv", "mv3"], w=["xh"])
            P.op("pool", lambda e: e.tensor_tensor(out=r0, in0=xh, in1=ag, op=ALU.mult), r=["xh"], w=["r0"])
            P.op("pool", lambda e: e.tensor_tensor(out=r0, in0=r0, in1=ab, op=ALU.add), r=["r0"], w=["r0"])
            P.dma("sp", lambda e: e.dma_start(out=r_s[tok0:tok0 + 128, :], in_=r0), r=["r0"], w=["rs"])
            for kc in range(8):
                pt, pk = rot.get()
                P.op("pe", lambda e, pt=pt, kc=kc: e.transpose(out=pt[:, 0:128], in_=xh[:, kc * 128:(kc + 1) * 128],
                                                               identity=C["ident"]), r=["xh"], w=[pk])
                P.op("act", lambda e, pt=pt, kc=kc: e.activation(
                    out=h2t[:, kc, cs], in_=pt[:, 0:128], func=AF.Identity,
                    scale=A2[:, kc, b:b + 1], bias=B2[:, kc, b:b + 1]), r=[pk], w=["h2t"])

        def capture(fn, *a):
            old = P.ops
            P.ops = []
            fn(*a)
            got = P.ops
            P.ops = old
            return got

        def merge(a, b_):
            out_, i, j = [], 0, 0
            na, nb = max(len(a), 1), max(len(b_), 1)
            while i < len(a) or j < len(b_):
                if j >= len(b_) or (i < len(a) and i * nb <= j * na):
                    out_.append(a[i]); i += 1
                else:
                    out_.append(b_[j]); j += 1
            return out_

        ntiles = NT if stage != "pa1" else 2

        def threadA(ti):
            if ti + 1 < ntiles:
                stage1(ti + 1)
            if ti - 1 >= 0:
                stage3(ti - 1)

        P.ops += capture(stage1, 0)
        for ti in range(ntiles):
            a = capture(threadA, ti)
            b_ = capture(stage2, ti)
            P.ops += merge(a, b_)
        P.ops += capture(stage3, ntiles - 1)
        if stage == "pa1":
            P.dma("pool", lambda e: e.dma_start(out=dbg_out["mixT"].rearrange("p (a b) -> p a b", a=8), in_=mixT2[1]),
                  r=[f"mixT1_{j}" for j in range(8)], w=["dbg1"])
            P.dma("sp", lambda e: e.dma_start(out=dbg_out["r"], in_=r_s[0:512, :]), r=["rs"], w=["dbg2"])
            P.dma("pool", lambda e: e.dma_start(out=dbg_out["h2"].rearrange("p (a b) -> p a b", a=8),
                                                in_=h2_s[:, :, 0:512]), r=["h2s"], w=["dbg3"])
            P.dma("sp", lambda e: e.dma_start(out=dbg_out["qkvc"].rearrange("p (a b) -> p a b", a=12), in_=qkvc2[1]),
                  r=[f"qk1_{j}" for j in range(12)], w=["dbg4"])
        cnt = P.emit()
        print("phaseA op counts", cnt)

    if stage in ("pa", "pa1"):
        return nc
    I32 = mybir.dt.int32
    with contextlib.ExitStack() as es:
        def tsb(name, shape, dt=F32):
            return es.enter_context(nc.sbuf_tensor("b_" + name, list(shape), dt)).ap()

        P = Prog(nc, "pb")
        NSTT = TOK // 128
        NW = 3
        cb = tsb("cb", list(CB.shape))
        thr = cb[:, 0:64]
        SLc = cb[:, 64:64 + 1024]
        blkio = cb[:, 1088:1088 + NBLK]
        iotaP = cb[:, 1088 + NBLK:1089 + NBLK]
        SUf = cb[:, 1089 + NBLK:1089 + NBLK + 128]
        SUb = tsb("sub", [128, 128], BF16)
        onesb = tsb("onesb", [128, 128], BF16)
        Wr = tsb("wr", [128, 8, 36], BF16)
        b36 = tsb("b36", [128, 36])
        g2 = tsb("ln2g", [128, D])
        b2 = tsb("ln2b", [128, D])
        gate2 = tsb("gate2", [128, 2, D])
        h2c = [tsb(f"h2c{i}", [128, 8, 512], BF16) for i in range(2)]
        OHall = tsb("ohall", [128, NSTT, 32], BF16)
        oh1all = tsb("oh1all", [128, NSTT, 32])
        oh2all = tsb("oh2all", [128, NSTT, 32])
        gAB = tsb("gab", [128, NSTT, 2])
        POSf = tsb("posf", [128, NSTT, 2])
        POSi = tsb("posi", [128, NSTT, 2], I32)
        cnt = tsb("cnt", [128, 32])
        nblk = tsb("nblk", [128, 32])
        pstb = tsb("pstb", [128, 32])
        pend = tsb("pend", [128, 32])
        base = tsb("base", [128, 32])
        big = tsb("big", [128, NBLK * 32])
        bexp = tsb("bexp", [128, NBLK])
        bskip = tsb("bskip", [128, NBLK])
        bsame = tsb("bsame", [128, NBLK])
        IDXW = tsb("idxw", [128, NBLK], I32)
        posv = tsb("posv", [128, 32])
        ptmp = tsb("ptmp", [128, 32])
        lg = tsb("lg", [128, 36])
        lgm = tsb("lgm", [128, 32])
        lgm2 = tsb("lgm2", [128, 32])
        rs_ = tsb("rsm", [128, 32])
        Wg = [tsb(f"wg{i}", [128, 2048], BF16) for i in range(NW)]
        Wu = [tsb(f"wu{i}", [128, 2048], BF16) for i in range(NW)]
        Wd = [tsb(f"wd{i}", [128, 2048], BF16) for i in range(NW)]
        xb = [tsb(f"xb{i}", [128, D]) for i in range(3)]
        sh2r = tsb("sh2r", [128, 2, D])
        sc2r = tsb("sc2r", [128, 2, D])
        xT = [tsb(f"xT{i}", [128, 8, 128], BF16) for i in range(3)]
        sg = [tsb(f"sg{i}", [128, 256]) for i in range(3)]
        hid = [tsb(f"hid{i}", [128, 256]) for i in range(3)]
        hidT = [tsb(f"hidT{i}", [128, 256], BF16) for i in range(3)]
        yb = [tsb(f"yb{i}", [128, D]) for i in range(2)]
        y1 = [tsb(f"y1_{i}", [128, D]) for i in range(2)]
        y2 = [tsb(f"y2_{i}", [128, D]) for i in range(2)]
        rr = [tsb(f"rr{i}", [128, D]) for i in range(2)]
        xh2 = tsb("xh2", [128, D])
        ob = tsb("ob", [128, D])
        bst2 = tsb("bst2", [128, 2, 6])
        mv2 = tsb("mv2", [128, 4])
        rot = PsumRot(BANKS[0:2])
        rot2 = PsumRot([(PS2[i], (f"bank{2 * i}", f"bank{2 * i + 1}")) for i in (2, 3)])

        P.dma("sp", lambda e: e.dma_start(out=cb, in_=cb_d), w=["cb"])
        P.op("dve", lambda e: e.tensor_copy(out=SUb, in_=SUf), r=["cb"], w=["SUb"])
        P.op("dve", lambda e: e.memset(onesb, 1.0), w=["onesb"])
        for kc in range(8):
            P.dma("pool", lambda e, kc=kc: e.dma_start(out=Wr[:, kc, 0:4], in_=w_grp[kc * 128:(kc + 1) * 128, :]),
                  w=["Wr"])
            P.dma("pool", lambda e, kc=kc: e.dma_start(out=Wr[:, kc, 4:36], in_=w_exp[kc * 128:(kc + 1) * 128, :]),
                  w=["Wr"])
        P.dma("sp", lambda e: e.dma_start(out=b36[:, 0:4], in_=b_grp[0, :].partition_broadcast(128)), w=["b36"])
        P.dma("sp", lambda e: e.dma_start(out=b36[:, 4:36], in_=b_exp[0, :].partition_broadcast(128)), w=["b36"])
        P.dma("sp", lambda e: e.dma_start(out=gate2.rearrange("p a b -> p (a b)"), in_=g2_s), w=["gate2"])
        P.dma("sp", lambda e: e.dma_start(out=sh2r.rearrange("p a b -> p (a b)"), in_=sh2_s), w=["sh2r"])
        P.dma("sp", lambda e: e.dma_start(out=sc2r.rearrange("p a b -> p (a b)"), in_=sc2_s), w=["sc2r"])
        P.op("pool", lambda e: e.tensor_scalar(out=sc2r, in0=sc2r, scalar1=1.0, scalar2=1.0 / ALPHA, op0=ALU.add,
                                               op1=ALU.mult), r=["sc2r"], w=["sc2r"])
        P.dma("sp", lambda e: e.dma_start(out=g2, in_=ln2_g[0, :].partition_broadcast(128)), w=["g2"])
        P.dma("sp", lambda e: e.dma_start(out=b2, in_=ln2_b[0, :].partition_broadcast(128)), w=["b2"])

        RG = 4
        lg4 = tsb("lg4", [128, RG, 36])
        lgm4 = tsb("lgm4", [128, RG, 32])
        lgm24 = tsb("lgm24", [128, RG, 32])
        eg4 = tsb("eg4", [128, RG, 4])
        ohg4 = tsb("ohg4", [128, RG, 4])
        rsc = tsb("rsc", [128, 12, RG])

        def router4(g_):
            st0 = g_ * RG
            ci = g_ % 2
            P.dma("sp", lambda e: e.dma_start(out=h2c[ci], in_=h2_s[:, :, st0 * 128:st0 * 128 + 512]), w=[f"h2c{ci}"])
            plg, plgk = rot.get()
            for i in range(RG):
                cs = slice(i * 128, (i + 1) * 128)
                for kc in range(8):
                    P.op("pe", lambda e, kc=kc, i=i, cs=cs: e.matmul(plg[:, i * 36:(i + 1) * 36], lhsT=h2c[ci][:, kc, cs],
                                                                      rhs=Wr[:, kc, :], start=(kc == 0), stop=(kc == 7)),
                         r=[f"h2c{ci}", "Wr"], w=[plgk])
            gmax, sume, grpw, m1, m2, d21, p2, den, rden = [rsc[:, i, :] for i in range(9)]
            sts = range(st0, st0 + RG)
            K1 = [f"oh1_{st}" for st in sts]
            K2 = [f"oh2_{st}" for st in sts]
            KA = [f"gA{st}" for st in sts]
            KB = [f"gB{st}" for st in sts]
            KO = [f"OH{st}" for st in sts]
            oh1 = oh1all[:, st0:st0 + RG, :]
            oh2 = oh2all[:, st0:st0 + RG, :]
            gA = gAB[:, st0:st0 + RG, 0]
            gB = gAB[:, st0:st0 + RG, 1]
            bcx = lambda ap, n: ap.unsqueeze(2).to_broadcast([128, RG, n])
            P.op("dve", lambda e: e.tensor_tensor(out=lg4, in0=plg[:, 0:RG * 36].rearrange("p (s n) -> p s n", s=RG),
                                                  in1=b36.unsqueeze(1).to_broadcast([128, RG, 36]), op=ALU.add),
                 r=[plgk, "b36"], w=["lg4"])
            P.op("dve", lambda e: e.tensor_reduce(out=gmax, in_=lg4[:, :, 0:4], axis=AX.X, op=ALU.max), r=["lg4"], w=["q_gmax"])
            P.op("dve", lambda e: e.tensor_tensor(out=eg4, in0=lg4[:, :, 0:4], in1=bcx(gmax, 4), op=ALU.subtract),
                 r=["lg4", "q_gmax"], w=["eg4"])
            P.op("act", lambda e: e.activation(out=eg4, in_=eg4, func=AF.Exp), r=["eg4"], w=["eg4"])
            P.op("dve", lambda e: e.tensor_reduce(out=sume, in_=eg4, axis=AX.X, op=ALU.add), r=["eg4"], w=["q_sume"])
            P.op("dve", lambda e: e.reciprocal(out=grpw, in_=sume), r=["q_sume"], w=["q_grpw"])
            P.op("dve", lambda e: e.tensor_tensor(out=ohg4, in0=lg4[:, :, 0:4], in1=bcx(gmax, 4), op=ALU.is_equal),
                 r=["lg4", "q_gmax"], w=["ohg4"])
            P.op("dve", lambda e: e.tensor_scalar(out=ohg4, in0=ohg4, scalar1=-1.0, scalar2=BIG, op0=ALU.add, op1=ALU.mult),
                 r=["ohg4"], w=["ohg4"])
            P.op("dve", lambda e: e.tensor_tensor(out=lgm4.rearrange("p s (g k) -> p s g k", g=4),
                                                  in0=lg4[:, :, 4:36].rearrange("p s (g k) -> p s g k", g=4),
                                                  in1=ohg4.unsqueeze(3).to_broadcast([128, RG, 4, 8]), op=ALU.add),
                 r=["lg4", "ohg4"], w=["lgm4"])
            P.op("dve", lambda e: e.tensor_reduce(out=m1, in_=lgm4, axis=AX.X, op=ALU.max), r=["lgm4"], w=["q_m1"])
            P.op("dve", lambda e: e.tensor_tensor(out=oh1, in0=lgm4, in1=bcx(m1, 32), op=ALU.is_equal),
                 r=["lgm4", "q_m1"], w=K1)
            P.op("dve", lambda e: e.scalar_tensor_tensor(out=lgm24, in0=oh1, scalar=-BIG, in1=lgm4, op0=ALU.mult,
                                                         op1=ALU.add), r=K1 + ["lgm4"], w=["lgm24"])
            P.op("dve", lambda e: e.tensor_reduce(out=m2, in_=lgm24, axis=AX.X, op=ALU.max), r=["lgm24"], w=["q_m2"])
            P.op("dve", lambda e: e.tensor_tensor(out=oh2, in0=lgm24, in1=bcx(m2, 32), op=ALU.is_equal),
                 r=["lgm24", "q_m2"], w=K2)
            P.op("dve", lambda e: e.tensor_tensor(out=d21, in0=m2, in1=m1, op=ALU.subtract), r=["q_m1", "q_m2"], w=["q_d21"])
            P.op("act", lambda e: e.activation(out=p2, in_=d21, func=AF.Exp), r=["q_d21"], w=["q_p2"])
            P.op("dve", lambda e: e.tensor_scalar(out=den, in0=p2, scalar1=1.0, scalar2=None, op0=ALU.add),
                 r=["q_p2"], w=["q_den"])
            P.op("dve", lambda e: e.reciprocal(out=rden, in_=den), r=["q_den"], w=["q_rden"])
            P.op("dve", lambda e: e.tensor_tensor(out=gA, in0=grpw, in1=rden, op=ALU.mult), r=["q_grpw", "q_rden"], w=KA)
            P.op("dve", lambda e: e.tensor_tensor(out=gB, in0=gA, in1=p2, op=ALU.mult), r=KA + ["q_p2"], w=KB)
            P.op("pool", lambda e: e.tensor_tensor(out=OHall[:, st0:st0 + RG, :], in0=oh1, in1=oh2, op=ALU.add),
                 r=K1 + K2, w=KO)

        for g_ in range(NSTT // RG):
            router4(g_)

        pcnt, pcntk = rot.get()
        for st in range(NSTT):
            P.op("pe", lambda e, st=st: e.matmul(pcnt[:, 0:32], lhsT=onesb, rhs=OHall[:, st, :],
                                                 start=(st == 0), stop=(st == NSTT - 1)),
                 r=[f"OH{st}", "onesb"], w=[pcntk])
        P.op("act", lambda e: e.activation(out=cnt, in_=pcnt[:, 0:32], func=AF.Copy), r=[pcntk], w=["cnt"])
        big3 = big[:, 0:32 * 64].rearrange("p (e k) -> p e k", e=32)
        P.op("dve", lambda e: e.tensor_tensor(out=big3, in0=cnt.unsqueeze(2).to_broadcast([128, 32, 64]),
                                              in1=thr.unsqueeze(1).to_broadcast([128, 32, 64]), op=ALU.is_gt),
             r=["cnt", "cb"], w=["big"])
        P.op("dve", lambda e: e.tensor_reduce(out=nblk, in_=big3, axis=AX.X, op=ALU.add), r=["big"], w=["nblk"])
        big3b = big[:, 0:1024].rearrange("p (e f) -> p e f", e=32)
        P.op("dve", lambda e: e.tensor_tensor(out=big3b, in0=nblk.unsqueeze(1).to_broadcast([128, 32, 32]),
                                              in1=SLc.rearrange("p (e f) -> p e f", e=32), op=ALU.mult),
             r=["nblk", "cb", "big"], w=["big"])
        P.op("dve", lambda e: e.tensor_reduce(out=pstb, in_=big3b, axis=AX.X, op=ALU.add), r=["big"], w=["pstb"])
        P.op("dve", lambda e: e.tensor_tensor(out=pend, in0=pstb, in1=nblk, op=ALU.add), r=["pstb", "nblk"], w=["pend"])
        P.op("dve", lambda e: e.tensor_scalar(out=base, in0=pstb, scalar1=128.0, scalar2=None, op0=ALU.mult),
             r=["pstb"], w=["base"])
        big3c = big.rearrange("p (b e) -> p b e", b=NBLK)
        P.op("dve", lambda e: e.tensor_tensor(out=big3c, in0=pend.unsqueeze(1).to_broadcast([128, NBLK, 32]),
                                              in1=blkio.unsqueeze(2).to_broadcast([128, NBLK, 32]), op=ALU.is_le),
             r=["pend", "cb", "big", "pstb"], w=["big"])
        P.op("dve", lambda e: e.tensor_reduce(out=bexp, in_=big3c, axis=AX.X, op=ALU.add), r=["big"], w=["bexp"])
        P.op("dve", lambda e: e.tensor_scalar(out=bskip, in0=bexp, scalar1=float(NEXP) - 0.5, scalar2=None, op0=ALU.is_ge),
             r=["bexp"], w=["bskip"])
        P.op("dve", lambda e: e.tensor_tensor(out=bsame[:, NW:NBLK], in0=bexp[:, NW:NBLK], in1=bexp[:, 0:NBLK - NW],
                                              op=ALU.is_equal), r=["bexp"], w=["bsame"])
        P.op("dve", lambda e: e.tensor_tensor(out=bskip[:, NW:NBLK], in0=bskip[:, NW:NBLK], in1=bsame[:, NW:NBLK],
                                              op=ALU.max), r=["bskip", "bsame"], w=["bskip"])
        P.op("dve", lambda e: e.tensor_scalar(out=bexp, in0=bexp, scalar1=float(NEXP - 1), scalar2=128.0,
                                              op0=ALU.min, op1=ALU.mult), r=["bexp"], w=["bexp"])
        P.op("dve", lambda e: e.tensor_scalar(out=bexp, in0=bexp, scalar1=iotaP, scalar2=None, op0=ALU.add),
             r=["bexp", "cb"], w=["bexp"])
        P.op("dve", lambda e: e.scalar_tensor_tensor(out=bexp, in0=bskip, scalar=1.0e6, in1=bexp, op0=ALU.mult,
                                                     op1=ALU.add), r=["bexp", "bskip"], w=["bexp"])
        P.op("dve", lambda e: e.tensor_copy(out=IDXW, in_=bexp), r=["bexp"], w=["IDXW"])
        for st in range(NSTT):
            prk, prkk = rot.get()
            P.op("pe", lambda e, st=st, prk=prk: e.matmul(prk[:, 0:32], lhsT=SUb, rhs=OHall[:, st, :],
                                                          start=True, stop=(st == 0)), r=[f"OH{st}", "SUb"], w=[prkk])
            for s2 in range(st):
                P.op("pe", lambda e, s2=s2, st=st, prk=prk: e.matmul(prk[:, 0:32], lhsT=onesb, rhs=OHall[:, s2, :],
                                                                     start=False, stop=(s2 == st - 1)),
                     r=[f"OH{s2}", "onesb"], w=[prkk])
            P.op("dve", lambda e, prk=prk: e.tensor_tensor(out=posv, in0=prk[:, 0:32], in1=base, op=ALU.add),
                 r=[prkk, "base"], w=["posv"])
            for k, oha in ((0, oh1all), (1, oh2all)):
                P.op("dve", lambda e, st=st, oha=oha: e.tensor_tensor(out=ptmp, in0=posv, in1=oha[:, st, :], op=ALU.mult),
                     r=["posv", f"oh1_{st}", f"oh2_{st}"], w=["ptmp"])
                P.op("dve", lambda e, st=st, k=k: e.tensor_reduce(out=POSf[:, st, k:k + 1], in_=ptmp, axis=AX.X, op=ALU.add),
                     r=["ptmp"], w=["POSf"])
        P.op("dve", lambda e: e.tensor_copy(out=POSi, in_=POSf), r=["POSf"], w=["POSi"])

        if stage == "pbdbg":
            P.dma("sp", lambda e: e.dma_start(out=dbg_out["cnt"], in_=cnt), r=["cnt"], w=["dg1"])
            P.dma("sp", lambda e: e.dma_start(out=dbg_out["nblk"], in_=nblk), r=["nblk"], w=["dg2"])
            P.dma("sp", lambda e: e.dma_start(out=dbg_out["pstb"], in_=pstb), r=["pstb"], w=["dg3"])
            P.dma("sp", lambda e: e.dma_start(out=dbg_out["bexp"], in_=bexp), r=["bexp"], w=["dg4"])
            P.dma("sp", lambda e: e.dma_start(out=dbg_out["posf"], in_=POSf.rearrange("p a b -> p (a b)")), r=["POSf"], w=["dg5"])
            P.dma("sp", lambda e: e.dma_start(out=dbg_out["oh1"], in_=oh1all.rearrange("p a b -> p (a b)")),
                  r=[f"oh1_{st}" for st in range(NSTT)], w=["dg6"])
            P.dma("sp", lambda e: e.dma_start(out=dbg_out["oh2"], in_=oh2all.rearrange("p a b -> p (a b)")),
                  r=[f"oh2_{st}" for st in range(NSTT)], w=["dg7"])
            P.dma("sp", lambda e: e.dma_start(out=dbg_out["gab"], in_=gAB.rearrange("p a b -> p (a b)")),
                  r=[f"gA{st}" for st in range(NSTT)] + [f"gB{st}" for st in range(NSTT)], w=["dg8"])
            P.emit()
            return nc
        for st in range(NSTT):
            b = st // (NSTT // NB)
            xq = xb[st % 3]
            xk = f"xb{st % 3}"
            P.dma("sp", lambda e, st=st, xq=xq: e.dma_start(out=xq, in_=r_s[st * 128:(st + 1) * 128, :]), w=[xk])
            P.op("dve", lambda e, xq=xq, b=b: e.tensor_tensor(out=xq, in0=xq, in1=sc2r[:, b, :], op=ALU.mult),
                 r=[xk, "sc2r"], w=[xk])
            P.op("dve", lambda e, xq=xq, b=b: e.tensor_tensor(out=xq, in0=xq, in1=sh2r[:, b, :], op=ALU.add),
                 r=[xk, "sh2r"], w=[xk])
            for k in range(2):
                P.dma("pool", lambda e, st=st, k=k, xq=xq: e.indirect_dma_start(
                    out=xrows_s, out_offset=bass.IndirectOffsetOnAxis(ap=POSi[:, st, k:k + 1], axis=0),
                    in_=xq, in_offset=None), r=[xk, "POSi"], w=[f"xrows{st}_{k}"])
        if stage in ("pbs1", "pbs2"):
            P.emit()
            return nc

        pgu_banks = PsumRot(BANKS[0:2])
        NQ = 3
        wgate_rows = w_gate.rearrange("e (p k) n -> (e p) (k n)", k=8)
        wup_rows = w_up.rearrange("e (p k) n -> (e p) (k n)", k=8)
        wdn_rows = w_down.rearrange("e (p k) n -> (e p) (k n)", k=2)

        def blk_loadx(bi):
            q = bi % NQ
            P.dma("sp", lambda e: e.dma_start(out=xb[q], in_=xrows_s[bi * 128:(bi + 1) * 128, :]),
                  r=[f"xrows{st}_{k}" for st in range(NSTT) for k in range(2)], w=[f"xb{q}"])

        bnd = {}

        def bnd_reg(e):
            if "r" not in bnd:
                bnd["r"] = e.alloc_register("wbound")
                e.reg_mov(bnd["r"], NEXP * 128 - 1)
            return bnd["r"]

        def blk_load(bi):
            slot = bi % NW
            ioff = bass.IndirectOffsetOnAxis(ap=IDXW[:, bi:bi + 1], axis=0)
            P.dma("pool", lambda e: e.indirect_dma_start(out=Wg[slot], out_offset=None,
                                                         in_=wgate_rows, in_offset=ioff, bounds_check=bnd_reg(e), oob_is_err=False), r=["IDXW"], w=[f"wg{slot}"])
            P.dma("pool", lambda e: e.indirect_dma_start(out=Wu[slot], out_offset=None,
                                                         in_=wup_rows, in_offset=ioff, bounds_check=bnd_reg(e), oob_is_err=False), r=["IDXW"], w=[f"wu{slot}"])
            P.dma("pool", lambda e: e.indirect_dma_start(out=Wd[slot], out_offset=None,
                                                         in_=wdn_rows, in_offset=ioff, bounds_check=bnd_reg(e), oob_is_err=False), r=["IDXW"], w=[f"wd{slot}"])

        xt_banks = [BANKS[2], BANKS[3]]
        ht_banks = PsumRot(BANKS[6:8])

        def blk_xt(bi):
            q = bi % NQ
            xv = xb[q].rearrange("r (p k) -> r k p", k=8)
            xTf = xT[q].rearrange("p a b -> p (a b)")
            for hb_ in range(2):
                pt, ptk = xt_banks[hb_]
                for k4 in range(4):
                    kc = hb_ * 4 + k4
                    P.op("pe", lambda e, kc=kc, k4=k4, pt=pt: e.transpose(out=pt[:, k4 * 128:(k4 + 1) * 128],
                                                                          in_=xv[:, kc, :], identity=C["ident"]),
                         r=[f"xb{q}"], w=[ptk])
                if hb_ == 0:
                    P.op("act", lambda e, pt=pt: e.activation(out=xTf[:, 0:512], in_=pt, func=AF.Copy),
                         r=[ptk], w=[f"xTa{q}"])
                else:
                    P.op("dve", lambda e, pt=pt: e.tensor_copy(out=xTf[:, 512:1024], in_=pt), r=[ptk], w=[f"xTb{q}"])

        def blk_gu(bi):
            slot = bi % NW
            q = bi % NQ
            pgu, pguk = pgu_banks.get()
            for (Wm, wk, c0) in ((Wg, f"wg{slot}", 0), (Wu, f"wu{slot}", 256)):
                for kc in range(8):
                    P.op("pe", lambda e, kc=kc, Wm=Wm, c0=c0: e.matmul(
                        pgu[:, c0:c0 + 256], lhsT=xT[q][:, kc, :], rhs=Wm[slot][:, kc * 256:(kc + 1) * 256],
                        start=(kc == 0), stop=(kc == 7)), r=[f"xTa{q}", f"xTb{q}", wk], w=[pguk])
            P.op("act", lambda e: e.activation(out=sg[q], in_=pgu[:, 0:256], func=AF.Exp, scale=-1.0), r=[pguk], w=[f"sg{q}"])
            P.op("act", lambda e: e.activation(out=sg[q], in_=sg[q], func=AF.Ln, bias=1.0), r=[f"sg{q}"], w=[f"sg{q}"])
            P.op("act", lambda e: e.activation(out=sg[q], in_=sg[q], func=AF.Exp, scale=-1.0), r=[f"sg{q}"], w=[f"sg{q}"])
            P.op("dve", lambda e: e.tensor_tensor(out=sg[q], in0=pgu[:, 0:256], in1=sg[q], op=ALU.mult),
                 r=[pguk, f"sg{q}"], w=[f"sg{q}"])
            P.op("dve", lambda e: e.tensor_tensor(out=hid[q], in0=pgu[:, 256:512], in1=sg[q], op=ALU.mult),
                 r=[pguk, f"sg{q}"], w=[f"hid{q}"])

        def blk_tr(bi):
            q = bi % NQ
            pht, phtk = ht_banks.get()
            hv = hid[q].rearrange("r (p k) -> r k p", k=2)
            for k2 in range(2):
                P.op("pe", lambda e, k2=k2: e.transpose(out=pht[:, k2 * 128:(k2 + 1) * 128], in_=hv[:, k2, :],
                                                        identity=C["ident"]), r=[f"hid{q}"], w=[phtk])
            P.op("act", lambda e: e.activation(out=hidT[q], in_=pht[:, 0:256], func=AF.Copy), r=[phtk], w=[f"hidT{q}"])

        def blk_dn(bi):
            slot = bi % NW
            q = bi % NQ
            yq = bi % 2
            py, (k0, k1) = PS2[2], ("bank4", "bank5")
            for half in range(2):
                for k2 in range(2):
                    P.op("pe", lambda e, half=half, k2=k2: e.matmul(
                        py[:, half * 512:(half + 1) * 512], lhsT=hidT[q][:, k2 * 128:(k2 + 1) * 128],
                        rhs=Wd[slot][:, k2 * 1024 + half * 512:k2 * 1024 + (half + 1) * 512], start=(k2 == 0), stop=(k2 == 1)),
                        r=[f"hidT{q}", f"wd{slot}"], w=[(k0, k1)[half]])
            P.op("act", lambda e: e.activation(out=yb[yq][:, 0:512], in_=py[:, 0:512], func=AF.Copy), r=[k0], w=[f"yba{yq}"])
            P.op("dve", lambda e: e.tensor_copy(out=yb[yq][:, 512:1024], in_=py[:, 512:1024]), r=[k1], w=[f"ybb{yq}"])
            P.dma("sp", lambda e: e.dma_start(out=yrows_s[bi * 128:(bi + 1) * 128, :], in_=yb[yq]),
                  r=[f"yba{yq}", f"ybb{yq}"], w=[f"yrows{bi}"])

        nb_run = NBLK if stage != 'pbs3' else 6
        pendq = []
        blk_load(0)
        for b0_ in range(min(2, nb_run)):
            blk_loadx(b0_)
        for bi in range(nb_run):
            if bi + 2 < nb_run:
                blk_loadx(bi + 2)
            blk_xt(bi)
            blk_gu(bi)
            pendq.append(bi)
            if len(pendq) >= 2:
                blk_tr(pendq[-2])
            if len(pendq) >= 3:
                blk_dn(pendq.pop(0))
            if bi + 1 < nb_run:
                blk_load(bi + 1)
        if len(pendq) == 2:
            blk_dn(pendq.pop(0))
        while pendq:
            a = pendq.pop(0)
            blk_tr(a)
            blk_dn(a)
        YK = [f"yrows{bi}" for bi in range(nb_run)]
        if stage == "pbs3":
            P.emit()
            return nc

        def comb_fetch(st):
            q = st % 2
            P.dma("sp", lambda e: e.dma_start(out=rr[q], in_=r_s[st * 128:(st + 1) * 128, :]), w=[f"rr{q}"])
            P.dma("pool", lambda e: e.indirect_dma_start(
                out=y1[q], out_offset=None, in_=yrows_s,
                in_offset=bass.IndirectOffsetOnAxis(ap=POSi[:, st, 0:1], axis=0)), r=YK + ["POSi"], w=[f"y1_{q}"])
            P.dma("pool", lambda e: e.indirect_dma_start(
                out=y2[q], out_offset=None, in_=yrows_s,
                in_offset=bass.IndirectOffsetOnAxis(ap=POSi[:, st, 1:2], axis=0)), r=YK + ["POSi"], w=[f"y2_{q}"])

        def comb_compute(st):
            q = st % 2
            b = st // (NSTT // NB)
            P.op("act", lambda e: e.activation(out=y1[q], in_=y1[q], func=AF.Copy, scale=gAB[:, st, 0:1]),
                 r=[f"y1_{q}", f"gA{st}"], w=[f"y1_{q}"])
            P.op("dve", lambda e: e.scalar_tensor_tensor(out=y1[q], in0=y2[q], scalar=gAB[:, st, 1:2], in1=y1[q],
                                                         op0=ALU.mult, op1=ALU.add),
                 r=[f"y1_{q}", f"y2_{q}", f"gB{st}"], w=[f"y1_{q}"])
            P.op("dve", lambda e: e.tensor_tensor(out=y1[q], in0=y1[q], in1=gate2[:, b, :], op=ALU.mult),
                 r=[f"y1_{q}", "gate2"], w=[f"y1_{q}"])
            P.op("dve", lambda e: e.tensor_tensor(out=rr[q], in0=rr[q], in1=y1[q], op=ALU.add),
                 r=[f"rr{q}", f"y1_{q}"], w=[f"rr{q}"])
            for hf in range(2):
                P.op("dve", lambda e, hf=hf: e.bn_stats(out=bst2[:, hf, :], in_=rr[q][:, hf * 512:(hf + 1) * 512]),
                     r=[f"rr{q}"], w=["bst2"])
            P.op("dve", lambda e: e.bn_aggr(out=mv2[:, 0:2], in_=bst2.rearrange("p a b -> p (a b)")), r=["bst2"], w=["mv2"])
            P.op("act", lambda e: e.activation(out=mv2[:, 2:3], in_=mv2[:, 1:2], func=AF.Ln, bias=1e-5),
                 r=["mv2"], w=["mv2b"])
            P.op("act", lambda e: e.activation(out=mv2[:, 3:4], in_=mv2[:, 2:3], func=AF.Exp, scale=-0.5),
                 r=["mv2b"], w=["mv2c"])
            P.op("dve", lambda e: e.scalar_tensor_tensor(out=mv2[:, 2:3], in0=mv2[:, 0:1], scalar=-1.0, in1=mv2[:, 3:4],
                                                         op0=ALU.mult, op1=ALU.mult), r=["mv2", "mv2b", "mv2c"], w=["mv2b", "mv2d"])
            P.op("act", lambda e: e.activation(out=xh2, in_=rr[q], func=AF.Identity, scale=mv2[:, 3:4], bias=mv2[:, 2:3]),
                 r=[f"rr{q}", "mv2c", "mv2d"], w=["xh2"])
            oq = ob2[q]
            P.op("pool", lambda e: e.tensor_tensor(out=oq, in0=xh2, in1=g2, op=ALU.mult), r=["xh2", "g2"], w=[f"ob{q}"])
            P.op("pool", lambda e: e.tensor_tensor(out=oq, in0=oq, in1=b2, op=ALU.add), r=[f"ob{q}", "b2"], w=[f"ob{q}"])
            P.dma("sp", lambda e: e.dma_start(out=out[st * 128:(st + 1) * 128, :], in_=oq), r=[f"ob{q}"], w=["outd"])

        ob2 = [ob, yb[0]]
        comb_fetch(0)
        for st in range(NSTT):
            if st + 1 < NSTT:
                comb_fetch(st + 1)
            comb_compute(st)
        cnt_ = P.emit()
        print("phaseB op counts", cnt_)

    return nc


_NC_CACHE = {}


def _get_nc():
    if "nc" not in _NC_CACHE:
        _NC_CACHE["nc"] = build("full")
    return _NC_CACHE["nc"]


def make_in_maps(inputs):
    f = lambda a: np.ascontiguousarray(np.asarray(a, dtype=np.float32))
    shared = {
        "w_ada": f(inputs["w_ada"][0]), "b_ada": f(inputs["b_ada"]), "w_in": f(inputs["w_in"][0]),
        "conv_w": f(inputs["conv_w"][0]), "conv_norm_w": f(inputs["conv_norm_w"]),
        "dn_conv_w": f(inputs["dn_conv_w"][0]), "dn_A_log": f(inputs["dn_A_log"]),
        "dn_dt_bias": f(inputs["dn_dt_bias"]), "dn_norm_w": f(inputs["dn_norm_w"]),
        "w_out": f(inputs["w_out"][0]), "ln1_g": f(inputs["ln1_g"]), "ln1_b": f(inputs["ln1_b"]),
        "w_grp": f(inputs["w_grp"][0]), "b_grp": f(inputs["b_grp"]), "w_exp": f(inputs["w_exp"][0]),
        "b_exp": f(inputs["b_exp"]), "w_gate": f(inputs["w_gate"][0]), "w_up": f(inputs["w_up"][0]),
        "w_down": f(inputs["w_down"][0]), "ln2_g": f(inputs["ln2_g"]), "ln2_b": f(inputs["ln2_b"]),
        "cmat": CMAT, "mab": MAB, "cb": CB,
    }
    xs = f(inputs["x"]).reshape(8, TOK, D)
    cs = f(inputs["c"]).reshape(8, NB, D)
    return [dict(shared, x=xs[i], c=cs[i]) for i in range(8)]


def kernel(**inputs):
    nc = _get_nc()
    in_maps = make_in_maps(inputs)
    res = run_bass_kernel_spmd(nc, in_maps, core_ids=list(range(8)))
    outs = [np.asarray(r["out"], dtype=np.float32).reshape(NB, SEQ, D) for r in res.results]
    return np.concatenate(outs, axis=0)
```

```python
import contextlib
import numpy as np
import concourse.bass as bass
import concourse.mybir as mybir
from concourse.bass_utils import run_bass_kernel_spmd

F32 = mybir.dt.float32
BF16 = mybir.dt.bfloat16
AF = mybir.ActivationFunctionType
ALU = mybir.AluOpType
AX = mybir.AxisListType

ENGS = ("pe", "act", "dve", "pool", "sp")

D = 1024
SEQ = 2048
NB = 2
TOK = NB * SEQ
DIN = 3592
NEXP = 32
ALPHA = 2.0 ** 0.25
TT = 256
NT = TOK // TT
BIG = 30000.0


class Prog:
    NDMA = 24

    def __init__(self, nc, tag):
        self.nc = nc
        self.tag = tag
        self.ops = []
        self.chain_dma = False

    def op(self, eng, fn, r=(), w=()):
        self.ops.append(dict(eng=eng, fn=fn, r=tuple(r), w=tuple(w), dma=False))

    def dma(self, eng, fn, r=(), w=()):
        chain = (f"__q_{eng}",) if self.chain_dma else ()
        self.ops.append(dict(eng=eng, fn=fn, r=tuple(r), w=tuple(w) + chain, dma=True))

    def emit(self, final_wait_engine="sp"):
        nc = self.nc
        esem = {e: nc.alloc_semaphore(f"s_{e}_{self.tag}") for e in ENGS if e != "sp"}
        dsem = [nc.alloc_semaphore(f"d_{i}_{self.tag}") for i in range(self.NDMA)]
        ecount = {e: 0 for e in ENGS}
        dtotal = [0] * self.NDMA
        dnext = 0
        last_w = {}
        readers = {}
        waited = {e: {} for e in ENGS}
        per_eng = {e: [] for e in ENGS}
        tokens = []
        for i, o in enumerate(self.ops):
            E = o["eng"]
            deps = set()
            for k in o["r"]:
                if k in last_w:
                    deps.add(last_w[k])
            for k in o["w"]:
                if k in last_w:
                    deps.add(last_w[k])
                for rd in readers.get(k, ()):
                    deps.add(rd)
            waits = []
            for d in sorted(deps):
                od = self.ops[d]
                if (not od["dma"]) and od["eng"] == E and E == "pe" and not o["dma"]:
                    continue
                s, v = tokens[d]
                key = id(s)
                if waited[E].get(key, 0) >= v:
                    continue
                waited[E][key] = v
                waits.append((s, v))
            if o["dma"]:
                j = dnext
                dnext = (dnext + 1) % self.NDMA
                s = dsem[j]
                if dtotal[j] > 0 and waited[E].get(id(s), 0) < dtotal[j]:
                    waited[E][id(s)] = dtotal[j]
                    waits.append((s, dtotal[j]))
                dtotal[j] += 16
                tok = (s, dtotal[j])
                inc = 16
            else:
                ecount[E] += 1
                tok = (esem[E], ecount[E])
                inc = 1
            tokens.append(tok)
            per_eng[E].append((waits, o["fn"], tok[0], inc))
            for k in o["r"]:
                readers.setdefault(k, []).append(i)
            for k in o["w"]:
                last_w[k] = i
                readers[k] = []
        final_waits = [(esem[e], ecount[e]) for e in esem if ecount[e] > 0]
        final_waits += [(dsem[j], dtotal[j]) for j in range(self.NDMA) if dtotal[j] > 0]

        with nc.Block() as block:
            def mk(ename):
                def body(eng):
                    for waits, fn, s, inc in per_eng[ename]:
                        for (ws, wv) in waits:
                            eng.wait_ge(ws, wv)
                        ins = fn(eng)
                        ins.then_inc(s, inc)
                    if ename == final_wait_engine:
                        for (ws, wv) in final_waits:
                            eng.wait_ge(ws, wv)
                return body
            block.tensor(mk("pe"))
            block.scalar(mk("act"))
            block.vector(mk("dve"))
            block.gpsimd(mk("pool"))
            block.sync(mk("sp"))
        return dict(ecount)


class PsumRot:
    def __init__(self, items):
        self.items = list(items)
        self.i = 0

    def get(self):
        it = self.items[self.i]
        self.i = (self.i + 1) % len(self.items)
        return it


def make_consts():
    idx = np.arange(128)
    same = (idx[:, None] // 64) == (idx[None, :] // 64)
    c = {}
    c["ident"] = np.eye(128)
    c["ltb"] = (same & (idx[:, None] <= idx[None, :])) * 1.0
    c["bd"] = same * 1.0
    c["mnu"] = np.where(same & (idx[:, None] <= idx[None, :]), 0.0, -BIG)
    c["mnus"] = np.where(same & (idx[:, None] < idx[None, :]), 0.0, -BIG)
    c["mnls"] = np.where(same & (idx[:, None] > idx[None, :]), 0.0, -BIG)
    c["ones"] = np.ones((128, 128))
    c["gmat"] = same / 64.0
    c["omean"] = np.ones((128, 128)) / 128.0
    names = ["ident", "ltb", "bd", "mnu", "mnus", "mnls", "ones", "gmat", "omean"]
    cm = np.concatenate([c[n] for n in names], axis=1).astype(np.float32)
    mab = np.stack([(idx < 64) * 1.0, (idx >= 64) * 1.0], axis=1).astype(np.float32)
    return names, cm, mab


CNAMES, CMAT, MAB = make_consts()
NBLK = TOK * 2 // 128 + NEXP
NROWS = NBLK * 128


def make_consts_b():
    thr = np.broadcast_to(128.0 * np.arange(64)[None, :], (128, 64))
    e = np.arange(32)
    sl = np.broadcast_to((e[None, :] < e[:, None]).astype(np.float64).reshape(1, 1024), (128, 1024))
    blk = np.broadcast_to(np.arange(NBLK, dtype=np.float64)[None, :], (128, NBLK))
    iop = np.arange(128, dtype=np.float64)[:, None]
    idx = np.arange(128)
    su = (idx[:, None] < idx[None, :]) * 1.0
    return np.concatenate([thr, sl, blk, iop, su], axis=1).astype(np.float32)


CB = make_consts_b()


def build(stage="full", dbg=()):
    nc = bass.Bass("TRN2", target_bir_lowering=False)

    def din(name, shape, dt=F32):
        return nc.dram_tensor(name, list(shape), dt, kind="ExternalInput").ap()

    x = din("x", [TOK, D])
    c_in = din("c", [NB, D])
    w_ada = din("w_ada", [D, 6 * D])
    b_ada = din("b_ada", [1, 6 * D])
    w_in = din("w_in", [D, DIN])
    conv_w = din("conv_w", [3, 512])
    conv_norm_w = din("conv_norm_w", [1, 512])
    dn_conv_w = din("dn_conv_w", [4, 1536])
    dn_A_log = din("dn_A_log", [1, 4])
    dn_dt_bias = din("dn_dt_bias", [1, 4])
    dn_norm_w = din("dn_norm_w", [1, 128])
    w_out = din("w_out", [D, D])
    ln1_g = din("ln1_g", [1, D])
    ln1_b = din("ln1_b", [1, D])
    w_grp = din("w_grp", [D, 4])
    b_grp = din("b_grp", [1, 4])
    w_exp = din("w_exp", [D, 32])
    b_exp = din("b_exp", [1, 32])
    w_gate = din("w_gate", [NEXP, D, 256])
    w_up = din("w_up", [NEXP, D, 256])
    w_down = din("w_down", [NEXP, 256, D])
    ln2_g = din("ln2_g", [1, D])
    ln2_b = din("ln2_b", [1, D])
    cmat_d = din("cmat", list(CMAT.shape))
    mab_d = din("mab", [128, 2])
    cb_d = din("cb", list(CB.shape))
    out = nc.dram_tensor("out", [TOK, D], F32, kind="ExternalOutput").ap()
    r_s = nc.dram_tensor("r_scr", [TOK, D], F32, kind="Internal").ap()
    h2_s = nc.dram_tensor("h2_scr", [128, 8, TOK], BF16, kind="Internal").ap()
    g2_s = nc.dram_tensor("g2_scr", [128, 2 * D], F32, kind="Internal").ap()
    sh2_s = nc.dram_tensor("sh2_scr", [128, 2 * D], F32, kind="Internal").ap()
    sc2_s = nc.dram_tensor("sc2_scr", [128, 2 * D], F32, kind="Internal").ap()
    xrows_s = nc.dram_tensor("xrows_scr", [NROWS if stage != "pa1" else 128, D], F32, kind="Internal").ap()
    yrows_s = nc.dram_tensor("yrows_scr", [NROWS if stage != "pa1" else 128, D], F32, kind="Internal").ap()
    dbg_out = {}
    for (nm, shp) in dbg:
        dbg_out[nm] = nc.dram_tensor("dbg_" + nm, list(shp), F32, kind="ExternalOutput").ap()

    def sb(name, shape, dt=F32):
        return nc.alloc_sbuf_tensor("sb_" + name, list(shape), dt).ap()

    PS2 = [nc.alloc_psum_tensor(f"ps2_{i}", [128, 1024], F32).ap() for i in range(4)]
    BANKS = []
    for i in range(4):
        BANKS.append((PS2[i][:, 0:512], f"bank{2 * i}"))
        BANKS.append((PS2[i][:, 512:1024], f"bank{2 * i + 1}"))

    cm = sb("cm", [128, CMAT.shape[1]])
    C = {n: cm[:, i * 128:(i + 1) * 128] for i, n in enumerate(CNAMES)}
    mab = sb("mab", [128, 2])
    identb = sb("identb", [128, 128], BF16)
    modT = sb("modT", [128, 48, 2])
    s1p = sb("s1p", [128, 8, 2])
    A2 = sb("A2", [128, 8, 2])
    B2 = sb("B2", [128, 8, 2])
    gate_bc = {2: sb("gate1bc", [128, 2, D])}
    cw = sb("cw", [128, 4, 3])
    cnw = sb("cnw", [128, 4])
    dcw = sb("dcw", [128, 12, 4])
    dnw = sb("dnw", [128, 1])
    g1T = sb("g1T", [128, 8])
    b1T = sb("b1T", [128, 8])
    negA = sb("negA", [128, 4])
    dtb = sb("dtb", [128, 4])
    ag = sb("ag", [128, D])
    ab = sb("ab", [128, D])

    esA = contextlib.ExitStack()

    def tsbA(name, shape, dt=F32):
        return esA.enter_context(nc.sbuf_tensor("a_" + name, list(shape), dt)).ap()

    Win = tsbA("win", [128, 8, DIN], BF16)
    Wout = tsbA("wout", [128, 8, D], BF16)

    P = Prog(nc, "p0")
    P.dma("sp", lambda e: e.dma_start(out=cm, in_=cmat_d), w=["cm"])
    P.dma("sp", lambda e: e.dma_start(out=mab, in_=mab_d), w=["mab"])
    P.op("dve", lambda e: e.tensor_copy(out=identb, in_=C["ident"]), r=["cm"], w=["identb"])

    with nc.sbuf_tensor("t_cT", [128, 8, 2], F32) as cT_h, \
            nc.sbuf_tensor("t_cact", [128, 8, 2], BF16) as cact_h, \
            nc.sbuf_tensor("t_cbc", [128, 8, 2, 128], BF16) as cbc_h, \
            nc.sbuf_tensor("t_brow", [1, 6 * D], BF16) as brow_h, \
            nc.sbuf_tensor("t_onesr", [1, 128], BF16) as onesr_h, \
            nc.sbuf_tensor("t_wa0", [128, 8, D], BF16) as wa0_h, \
            nc.sbuf_tensor("t_wa1", [128, 8, D], BF16) as wa1_h, \
            nc.sbuf_tensor("t_ws0", [128, 8, 512], F32) as ws0_h, \
            nc.sbuf_tensor("t_ws1", [128, 8, 512], F32) as ws1_h, \
            nc.sbuf_tensor("t_sp2", [128, 8, 2], F32) as sp2_h, \
            nc.sbuf_tensor("t_g2bc", [128, 2, D], F32) as g2bc_h, \
            nc.sbuf_tensor("t_sh2bc", [128, 2, D], F32) as sh2bc_h, \
            nc.sbuf_tensor("t_sc2bc", [128, 2, D], F32) as sc2bc_h:
        gate_bc[5] = g2bc_h.ap()
        gate_bc[3] = sh2bc_h.ap()
        gate_bc[4] = sc2bc_h.ap()
        cT, cact, cbc, brow, onesr, sp2 = (t.ap() for t in (cT_h, cact_h, cbc_h, brow_h, onesr_h, sp2_h))
        wa = [wa0_h.ap(), wa1_h.ap()]
        wstg = [ws0_h.ap(), ws1_h.ap()]
        for b in range(NB):
            P.dma("sp", lambda e, b=b: e.dma_start(
                out=cT[:, :, b], in_=c_in[b, :].rearrange("(kc p) -> p kc", p=128),
                allow_slow_non_contiguous=True), w=["cT"])
        P.op("act", lambda e: e.activation(out=cact, in_=cT, func=AF.Silu), r=["cT"], w=["cact"])
        P.op("dve", lambda e: e.tensor_copy(out=cbc, in_=cact.unsqueeze(3).to_broadcast([128, 8, 2, 128])),
             r=["cact"], w=["cbc"])
        P.dma("pool", lambda e: e.dma_start(out=brow, in_=b_ada), w=["brow"])
        for kc in range(8):
            for (c0, c1) in ((0, 2048), (2048, DIN)):
                P.dma("pool", lambda e, kc=kc, c0=c0, c1=c1: e.dma_start(
                    out=Win[:, kc, c0:c1], in_=w_in[kc * 128:(kc + 1) * 128, c0:c1]), w=[f"Win{kc}_{c0}"])
            P.dma("pool", lambda e, kc=kc: e.dma_start(
                out=Wout[:, kc, :], in_=w_out[kc * 128:(kc + 1) * 128, :]), w=[f"Wout{kc}"])
        P.op("dve", lambda e: e.memset(onesr, 1.0), w=["onesr"])
        for k in range(3):
            P.dma("sp", lambda e, k=k: e.dma_start(out=cw[:, :, k],
                                                   in_=conv_w[k, :].rearrange("(j p) -> p j", p=128),
                                                   allow_slow_non_contiguous=True), w=["cw"])
        P.dma("sp", lambda e: e.dma_start(out=cnw, in_=conv_norm_w[0, :].rearrange("(j p) -> p j", p=128),
                                          allow_slow_non_contiguous=True), w=["cnw"])
        for k in range(4):
            P.dma("sp", lambda e, k=k: e.dma_start(out=dcw[:, :, k],
                                                   in_=dn_conv_w[k, :].rearrange("(j p) -> p j", p=128),
                                                   allow_slow_non_contiguous=True), w=["dcw"])
        P.dma("sp", lambda e: e.dma_start(out=dnw, in_=dn_norm_w.rearrange("o p -> p o"),
                                          allow_slow_non_contiguous=True), w=["dnw"])
        P.dma("sp", lambda e: e.dma_start(out=g1T, in_=ln1_g[0, :].rearrange("(j p) -> p j", p=128),
                                          allow_slow_non_contiguous=True), w=["g1T"])
        P.dma("sp", lambda e: e.dma_start(out=b1T, in_=ln1_b[0, :].rearrange("(j p) -> p j", p=128),
                                          allow_slow_non_contiguous=True), w=["b1T"])
        P.dma("sp", lambda e: e.dma_start(out=negA, in_=dn_A_log[0, :].partition_broadcast(128)), w=["negA"])
        P.dma("sp", lambda e: e.dma_start(out=dtb, in_=dn_dt_bias[0, :].partition_broadcast(128)), w=["dtb"])
        P.op("act", lambda e: e.activation(out=negA, in_=negA, func=AF.Exp), r=["negA"], w=["negA"])
        P.op("dve", lambda e: e.tensor_scalar(out=negA, in0=negA, scalar1=-1.0, scalar2=None, op0=ALU.mult),
             r=["negA"], w=["negA"])

        ps0 = PsumRot(BANKS[0:1])
        psr = PsumRot(BANKS[1:3])
        modps, modk = ps0.get()
        for j in range(6):
            wj = wa[j % 2]
            wk = f"wa{j % 2}"
            for hf in range(2):
                si = (2 * j + hf) % 2
                stg = wstg[si]
                P.dma("sp", lambda e, j=j, hf=hf, stg=stg: e.dma_start(
                    out=stg, in_=w_ada[:, j * D + hf * 512:j * D + (hf + 1) * 512].rearrange("(kc p) n -> p kc n", p=128)),
                    w=[f"wstg{si}"])
                eng_ = "dve" if hf == 0 else "act"
                if eng_ == "dve":
                    P.op("dve", lambda e, wj=wj, hf=hf, stg=stg: e.tensor_copy(out=wj[:, :, hf * 512:(hf + 1) * 512], in_=stg),
                         r=[f"wstg{si}"], w=[wk + f"_{hf}"])
                else:
                    P.op("act", lambda e, wj=wj, hf=hf, stg=stg: e.activation(out=wj[:, :, hf * 512:(hf + 1) * 512], in_=stg,
                                                                              func=AF.Copy), r=[f"wstg{si}"], w=[wk + f"_{hf}"])
            for cc in range(8):
                col = (j * 8 + cc) * 2
                for kc in range(8):
                    P.op("pe", lambda e, wj=wj, kc=kc, cc=cc, col=col: e.matmul(
                        modps[:, col:col + 2], lhsT=wj[:, kc, cc * 128:(cc + 1) * 128], rhs=cact[:, kc, :],
                        start=(kc == 0), stop=False), r=[wk + "_0", wk + "_1", "cact"], w=[modk])
                P.op("pe", lambda e, j=j, cc=cc, col=col: e.matmul(
                    modps[:, col:col + 2], lhsT=brow[0:1, j * D + cc * 128:j * D + (cc + 1) * 128],
                    rhs=onesr[0:1, 0:2], start=False, stop=True), r=["brow", "onesr"], w=[modk])
            if j in (2, 3, 4, 5):
                for b in range(NB):
                    for half in range(2):
                        pt, pk = psr.get()
                        for kc in range(8):
                            P.op("pe", lambda e, wj=wj, kc=kc, b=b, half=half, pt=pt: e.matmul(
                                pt, lhsT=cbc[:, kc, b, :], rhs=wj[:, kc, half * 512:(half + 1) * 512],
                                start=(kc == 0), stop=False), r=[wk + "_0", wk + "_1", "cbc"], w=[pk])
                        P.op("pe", lambda e, j=j, half=half, pt=pt: e.matmul(
                            pt, lhsT=onesr[0:1, :], rhs=brow[0:1, j * D + half * 512:j * D + (half + 1) * 512],
                            start=False, stop=True), r=["brow", "onesr"], w=[pk])
                        P.op("act", lambda e, j=j, b=b, half=half, pt=pt: e.activation(
                            out=gate_bc[j][:, b, half * 512:(half + 1) * 512], in_=pt, func=AF.Copy),
                            r=[pk], w=[f"gbc{j}"])
        P.op("dve", lambda e: e.tensor_copy(out=modT.rearrange("p a b -> p (a b)"), in_=modps[:, 0:96]),
             r=[modk], w=["modT"])
        P.op("dve", lambda e: e.tensor_scalar(out=s1p, in0=modT[:, 8:16, :], scalar1=1.0, scalar2=None, op0=ALU.add),
             r=["modT"], w=["s1p"])
        P.op("dve", lambda e: e.tensor_scalar(out=sp2, in0=modT[:, 32:40, :], scalar1=1.0, scalar2=None, op0=ALU.add),
             r=["modT"], w=["sp2"])
        P.op("dve", lambda e: e.tensor_tensor(out=A2, in0=sp2, in1=g1T.unsqueeze(2).to_broadcast([128, 8, 2]),
                                              op=ALU.mult), r=["sp2", "g1T"], w=["A2"])
        P.op("dve", lambda e: e.tensor_tensor(out=B2, in0=sp2, in1=b1T.unsqueeze(2).to_broadcast([128, 8, 2]),
                                              op=ALU.mult), r=["sp2", "b1T"], w=["B2"])
        P.op("dve", lambda e: e.tensor_tensor(out=B2, in0=B2, in1=modT[:, 24:32, :], op=ALU.add),
             r=["B2", "modT"], w=["B2"])
        P.dma("sp", lambda e: e.dma_start(out=ag, in_=ln1_g[0, :].partition_broadcast(128)), w=["ag"])
        P.dma("sp", lambda e: e.dma_start(out=ab, in_=ln1_b[0, :].partition_broadcast(128)), w=["ab"])
        P.op("dve", lambda e: e.tensor_scalar(out=ag, in0=ag, scalar1=ALPHA, scalar2=None, op0=ALU.mult),
             r=["ag"], w=["ag"])
        P.op("dve", lambda e: e.tensor_scalar(out=ab, in0=ab, scalar1=ALPHA, scalar2=None, op0=ALU.mult),
             r=["ab"], w=["ab"])
        P.dma("sp", lambda e: e.dma_start(out=g2_s, in_=gate_bc[5].rearrange("p a b -> p (a b)")),
              r=["gbc5"], w=["g2s"])
        P.dma("sp", lambda e: e.dma_start(out=sh2_s, in_=gate_bc[3].rearrange("p a b -> p (a b)")),
              r=["gbc3"], w=["sh2s"])
        P.dma("sp", lambda e: e.dma_start(out=sc2_s, in_=gate_bc[4].rearrange("p a b -> p (a b)")),
              r=["gbc4"], w=["sc2s"])
        if stage == "p0":
            P.dma("sp", lambda e: e.dma_start(out=dbg_out["modT"], in_=modT.rearrange("p a b -> p (a b)")),
                  r=["modT"], w=["dbgo"])
            P.dma("sp", lambda e: e.dma_start(out=dbg_out["g1bc"], in_=gate_bc[2].rearrange("p a b -> p (a b)")),
                  r=["gbc2"], w=["dbgo2"])
        P.emit()

    if stage == "p0":
        return nc
    with esA as es:
        tsb = tsbA
        P = Prog(nc, "pa")

        xt = tsb("xt", [128, 2, D])
        hT = tsb("hT", [128, 8, TT], BF16)
        cutail = tsb("cutail", [128, 4, 2])
        qtail = tsb("qtail", [128, 12, 3])
        qkvc2 = [tsb(f"qkvc{i}", [128, 12, TT]) for i in range(2)]
        zs2 = [tsb(f"zs{i}", [128, 4, TT]) for i in range(2)]
        mixT2 = [tsb(f"mixT{i}", [128, 8, TT], BF16) for i in range(3)]
        blsb2 = [tsb(f"blsb{i}", [128, 16]) for i in range(2)]
        S = tsb("S", [128, 4, 128])
        csb = tsb("csb", [128, TT])
        cuf2 = [tsb(f"cuf{i}", [128, TT + 2]) for i in range(2)]
        acc2 = [tsb(f"acc{i}", [128, TT]) for i in range(2)]
        ybuf2 = [tsb(f"ybuf{i}", [128, TT]) for i in range(2)]
        sqb2 = [tsb(f"sqb{i}", [128, TT]) for i in range(2)]
        sgt2 = [tsb(f"sgt{i}", [128, TT]) for i in range(2)]
        halo = [tsb(f"halo{i}", [128, TT + 3]) for i in range(2)]
        sm2 = [tsb(f"sm{i}", [128, 2, 64]) for i in range(2)]
        T = [tsb(f"T{i}", [128, 512]) for i in range(13)]
        r0 = tsb("r0", [128, D])
        xh = tsb("xh", [128, D])
        h2t = tsb("h2t", [128, 8, TT], BF16)
        bst = tsb("bst", [128, 2, 6])
        mv = tsb("mv", [128, 4])
        rotA = PsumRot(BANKS[0:4])
        rotB = PsumRot(BANKS[4:8])
        rot2A = PsumRot([(PS2[i], (f"bank{2 * i}", f"bank{2 * i + 1}")) for i in (0, 1)])

        def v3(ap):
            return ap.rearrange("p (h j) -> p h j", h=4)

        def bc_h(ap128):
            return ap128.unsqueeze(1).to_broadcast([128, 4, 128])

        def bc_j(ap4):
            return ap4.unsqueeze(2).to_broadcast([128, 4, 128])

        EPS_RMS = 1e-6

        def stage1(ti):
            pp = ti % 2
            b = ti // (NT // NB)
            first = (ti % (NT // NB) == 0)
            qkvc, zs, mixT, blsb = qkvc2[pp], zs2[pp], mixT2[ti % 3], blsb2[pp]
            mp = ti % 3
            rot = rotA
            P.dma("sp", lambda e: e.dma_start(
                out=xt, in_=x[ti * TT:(ti + 1) * TT, :].rearrange("(s p) f -> p s f", p=128)), w=["xt"])
            if first:
                P.op("pool", lambda e: e.memset(cutail, 0.0), w=["cutail"])
                P.op("pool", lambda e: e.memset(qtail, 0.0), w=["qtail"])
            for kc in range(8):
                pt, pk = rot.get()
                for s_ in range(2):
                    P.op("pe", lambda e, pt=pt, s_=s_, kc=kc: e.transpose(
                        out=pt[:, s_ * 128:(s_ + 1) * 128], in_=xt[:, s_, kc * 128:(kc + 1) * 128],
                        identity=C["ident"]), r=["xt"], w=[pk])
                P.op("act", lambda e, pt=pt, kc=kc: e.activation(
                    out=hT[:, kc, :], in_=pt[:, 0:TT], func=AF.Identity,
                    scale=s1p[:, kc, b:b + 1], bias=modT[:, kc, b:b + 1]), r=[pk], w=[f"hT{kc}"])

            def proj(oc):
                pt, pk = rot.get()
                for kc in range(8):
                    P.op("pe", lambda e, pt=pt, kc=kc: e.matmul(
                        pt[:, 0:TT], lhsT=Win[:, kc, oc * 128:(oc + 1) * 128], rhs=hT[:, kc, :],
                        start=(kc == 0), stop=(kc == 7)), r=[f"hT{kc}", "Win"], w=[pk])
                return pt[:, 0:TT], pk

            deferred = []

            def flush(keep=0):
                while len(deferred) > keep:
                    deferred.pop(0)()

            def rstd_part2(srcbuf, srck, lhs, q):
                sqb = sqb2[q]
                pm, pmk = rot.get()
                P.op("pe", lambda e: e.matmul(pm[:, 0:TT], lhsT=lhs, rhs=sqb, start=True, stop=True),
                     r=[f"sqb{q}"], w=[pmk])
                P.op("act", lambda e: e.activation(out=sqb, in_=pm[:, 0:TT], func=AF.Ln, bias=EPS_RMS),
                     r=[pmk], w=[f"sqb{q}"])
                P.op("act", lambda e: e.activation(out=sqb, in_=sqb, func=AF.Exp, scale=-0.5),
                     r=[f"sqb{q}"], w=[f"sqb{q}"])

            for j in range(4):
                q = j % 2
                cuf, acc, ybuf, sqb = cuf2[q], acc2[q], ybuf2[q], sqb2[q]
                pb, pbk = proj(j)
                pc, pck = proj(4 + j)
                pu, puk = proj(8 + j)
                P.op("act", lambda e, pc=pc: e.activation(out=csb, in_=pc, func=AF.Copy), r=[pck], w=["csb"])
                P.op("dve", lambda e, pu=pu, cuf=cuf: e.tensor_tensor(out=cuf[:, 2:TT + 2], in0=pu, in1=csb, op=ALU.mult),
                     r=[puk, "csb"], w=[f"cuf{q}"])
                P.op("pool", lambda e, j=j, cuf=cuf: e.tensor_copy(out=cuf[:, 0:2], in_=cutail[:, j, :]),
                     r=["cutail"], w=[f"cufh{q}"])
                P.op("act", lambda e, j=j, cuf=cuf, acc=acc: e.activation(out=acc, in_=cuf[:, 2:TT + 2], func=AF.Copy,
                                                                          scale=cw[:, j, 2:3]), r=[f"cuf{q}"], w=[f"acc{q}"])
                P.op("dve", lambda e, j=j, cuf=cuf, acc=acc: e.scalar_tensor_tensor(
                    out=acc, in0=cuf[:, 1:TT + 1], scalar=cw[:, j, 1:2], in1=acc, op0=ALU.mult, op1=ALU.add),
                    r=[f"cuf{q}", f"cufh{q}", f"acc{q}"], w=[f"acc{q}"])
                P.op("dve", lambda e, j=j, cuf=cuf, acc=acc: e.scalar_tensor_tensor(
                    out=acc, in0=cuf[:, 0:TT], scalar=cw[:, j, 0:1], in1=acc, op0=ALU.mult, op1=ALU.add),
                    r=[f"cuf{q}", f"cufh{q}", f"acc{q}"], w=[f"acc{q}"])
                P.op("pool", lambda e, j=j, cuf=cuf: e.tensor_copy(out=cutail[:, j, :], in_=cuf[:, TT:TT + 2]),
                     r=[f"cuf{q}"], w=["cutail"])
                P.op("dve", lambda e, pb=pb, acc=acc, ybuf=ybuf: e.tensor_tensor(out=ybuf, in0=pb, in1=acc, op=ALU.mult),
                     r=[pbk, f"acc{q}"], w=[f"ybuf{q}"])
                P.op("act", lambda e, ybuf=ybuf, sqb=sqb: e.activation(out=sqb, in_=ybuf, func=AF.Square),
                     r=[f"ybuf{q}"], w=[f"sqb{q}"])

                def part2(j=j, q=q, ybuf=ybuf, sqb=sqb):
                    rstd_part2(None, None, C["gmat"], q)
                    P.op("dve", lambda e: e.scalar_tensor_tensor(
                        out=mixT[:, j, :], in0=ybuf, scalar=cnw[:, j:j + 1], in1=sqb, op0=ALU.mult, op1=ALU.mult),
                        r=[f"ybuf{q}", f"sqb{q}"], w=[f"mixT{mp}_{j}"])
                flush(0)
                deferred.append(part2)

            for j in range(12):
                q = j % 2
                sqb = sqb2[q]
                pq, pqk = proj(12 + j)
                hb = halo[q]
                hk = f"halo{q}"
                qk_ = f"qk{pp}_{j}"
                P.op("act", lambda e, pq=pq, hb=hb: e.activation(out=hb[:, 3:TT + 3], in_=pq, func=AF.Copy),
                     r=[pqk], w=[hk])
                P.op("pool", lambda e, j=j, hb=hb: e.tensor_copy(out=hb[:, 0:3], in_=qtail[:, j, :]),
                     r=["qtail"], w=[hk + "h"])
                P.op("pool", lambda e, j=j, hb=hb: e.tensor_scalar(
                    out=qkvc[:, j, :], in0=hb[:, 3:TT + 3], scalar1=dcw[:, j, 3:4], scalar2=0.0, op0=ALU.mult,
                    op1=ALU.add), r=[hk], w=[qk_])
                for k in (2, 1, 0):
                    P.op("dve", lambda e, j=j, hb=hb, k=k: e.scalar_tensor_tensor(
                        out=qkvc[:, j, :], in0=hb[:, k:TT + k], scalar=dcw[:, j, k:k + 1], in1=qkvc[:, j, :],
                        op0=ALU.mult, op1=ALU.add), r=[hk, hk + "h", qk_], w=[qk_])
                P.op("pool", lambda e, j=j, hb=hb: e.tensor_copy(out=qtail[:, j, :], in_=hb[:, TT:TT + 3]),
                     r=[hk], w=["qtail"])
                sgt = sgt2[q]
                P.op("act", lambda e, j=j, sgt=sgt: e.activation(out=sgt, in_=qkvc[:, j, :], func=AF.Exp, scale=-1.0),
                     r=[qk_], w=[f"sgt{q}"])
                P.op("act", lambda e, sgt=sgt: e.activation(out=sgt, in_=sgt, func=AF.Ln, bias=1.0),
                     r=[f"sgt{q}"], w=[f"sgt{q}"])
                P.op("act", lambda e, sgt=sgt: e.activation(out=sgt, in_=sgt, func=AF.Exp, scale=-1.0),
                     r=[f"sgt{q}"], w=[f"sgt{q}"])
                P.op("pool", lambda e, j=j, sgt=sgt: e.tensor_tensor(out=qkvc[:, j, :], in0=qkvc[:, j, :], in1=sgt,
                                                                     op=ALU.mult), r=[qk_, f"sgt{q}"], w=[qk_])
                if j < 8:
                    P.op("act", lambda e, j=j, sqb=sqb: e.activation(out=sqb, in_=qkvc[:, j, :], func=AF.Square),
                         r=[qk_], w=[f"sqb{q}"])

                    def part2(j=j, q=q, sqb=sqb, qk_=qk_):
                        rstd_part2(None, None, C["ones"], q)
                        sc = (128.0 ** -0.5) if j < 4 else 1.0
                        P.op("dve", lambda e: e.scalar_tensor_tensor(
                            out=qkvc[:, j, :], in0=qkvc[:, j, :], scalar=sc, in1=sqb, op0=ALU.mult, op1=ALU.mult),
                            r=[qk_, f"sqb{q}"], w=[qk_])
                    flush(0)
                    deferred.append(part2)
                else:
                    flush(0)
            flush(0)
            for j in range(4):
                pz, pzk = proj(24 + j)
                sgt = sgt2[j % 2]
                sk = f"sgt{j % 2}"
                P.op("act", lambda e, pz=pz, sgt=sgt: e.activation(out=sgt, in_=pz, func=AF.Exp, scale=-1.0),
                     r=[pzk], w=[sk])
                P.op("act", lambda e, sgt=sgt: e.activation(out=sgt, in_=sgt, func=AF.Ln, bias=1.0), r=[sk], w=[sk])
                P.op("act", lambda e, sgt=sgt: e.activation(out=sgt, in_=sgt, func=AF.Exp, scale=-1.0), r=[sk], w=[sk])
                P.op("dve", lambda e, pz=pz, j=j, sgt=sgt: e.tensor_tensor(out=zs[:, j, :], in0=pz, in1=sgt, op=ALU.mult),
                     r=[pzk, sk], w=[f"zs{pp}_{j}"])
            p8, p8k = rot.get()
            for s_ in range(2):
                for kc in range(8):
                    P.op("pe", lambda e, s_=s_, kc=kc: e.matmul(
                        p8[:, s_ * 8:(s_ + 1) * 8], lhsT=hT[:, kc, s_ * 128:(s_ + 1) * 128],
                        rhs=Win[:, kc, 3584:3592], start=(kc == 0), stop=(kc == 7)),
                        r=[f"hT{kc}", "Win"], w=[p8k])
            P.op("act", lambda e: e.activation(out=blsb, in_=p8[:, 0:16], func=AF.Copy), r=[p8k], w=[f"blsb{pp}"])
            for s_ in range(2):
                smx = sm2[pp][:, s_, :]
                beta, xa, g, _g, egc, bge, dl, eL, sA, sB = [smx[:, i * 4:(i + 1) * 4] for i in range(10)]
                gcs = smx[:, 40:48]
                gcum = gcs[:, 0:4]
                glast = gcs[:, 4:8]
                SK = f"smk{pp}_{s_}"
                bl = blsb[:, s_ * 8:(s_ + 1) * 8]
                blk = f"blsb{pp}"
                P.op("act", lambda e, beta=beta, bl=bl: e.activation(out=beta, in_=bl[:, 0:4], func=AF.Exp, scale=-1.0),
                     r=[blk], w=[SK])
                P.op("dve", lambda e, beta=beta: e.tensor_scalar(out=beta, in0=beta, scalar1=1.0, scalar2=None, op0=ALU.add),
                     r=[SK], w=[SK])
                P.op("dve", lambda e, beta=beta: e.reciprocal(out=beta, in_=beta), r=[SK], w=[SK])
                P.op("dve", lambda e, xa=xa, bl=bl: e.tensor_tensor(out=xa, in0=bl[:, 4:8], in1=dtb, op=ALU.add),
                     r=[blk, SK], w=[SK])
                P.op("act", lambda e, xa=xa: e.activation(out=xa, in_=xa, func=AF.Exp), r=[SK], w=[SK])
                P.op("act", lambda e, xa=xa: e.activation(out=xa, in_=xa, func=AF.Ln, bias=1.0), r=[SK], w=[SK])
                P.op("dve", lambda e, g=g, xa=xa: e.tensor_tensor(out=g, in0=xa, in1=negA, op=ALU.mult), r=[SK], w=[SK])
                pc_, pck = rot.get()
                P.op("pe", lambda e, pc_=pc_, g=g: e.matmul(pc_[:, 0:4], lhsT=C["ltb"], rhs=g, start=True, stop=True),
                     r=[SK], w=[pck])
                P.op("pe", lambda e, pc_=pc_, g=g: e.matmul(pc_[:, 4:8], lhsT=C["bd"], rhs=g, start=True, stop=True),
                     r=[SK], w=[pck])
                P.op("act", lambda e, pc_=pc_, gcs=gcs: e.activation(out=gcs, in_=pc_[:, 0:8], func=AF.Copy), r=[pck], w=[SK])
                P.op("act", lambda e, egc=egc, gcum=gcum: e.activation(out=egc, in_=gcum, func=AF.Exp), r=[SK], w=[SK])
                P.op("dve", lambda e, bge=bge, beta=beta, egc=egc: e.tensor_tensor(out=bge, in0=beta, in1=egc, op=ALU.mult),
                     r=[SK], w=[SK])
                P.op("dve", lambda e, dl=dl, glast=glast, gcum=gcum: e.tensor_tensor(out=dl, in0=glast, in1=gcum,
                                                                                     op=ALU.subtract), r=[SK], w=[SK])
                P.op("act", lambda e, eL=eL, dl=dl: e.activation(out=eL, in_=dl, func=AF.Exp), r=[SK], w=[SK])
                P.op("dve", lambda e, sA=sA, eL=eL: e.tensor_scalar(out=sA, in0=eL, scalar1=mab[:, 0:1], scalar2=None,
                                                                    op0=ALU.mult), r=[SK], w=[SK])
                P.op("dve", lambda e, sB=sB, eL=eL: e.tensor_scalar(out=sB, in0=eL, scalar1=mab[:, 1:2], scalar2=None,
                                                                    op0=ALU.mult), r=[SK], w=[SK])

        def stage2(ti):
            first = (ti % (NT // NB) == 0)
            if first:
                P.op("pool", lambda e: e.memset(S, 0.0), w=["S0", "S1", "S2", "S3"])
            for s_ in range(2):
                gdn_sub(ti, s_)

        def stage3(ti):
            b = ti // (NT // NB)
            for s_ in range(2):
                ln1_sub(ti, s_, b)
            P.dma("sp", lambda e: e.dma_start(out=h2_s[:, :, ti * TT:(ti + 1) * TT], in_=h2t),
                  r=["h2t"], w=["h2s"])

        def gdn_sub(ti, s_):
            pp = ti % 2
            rot = rotB
            qkvc, zs, mixT, blsb = qkvc2[pp], zs2[pp], mixT2[ti % 3], blsb2[pp]
            mp = ti % 3
            bl = blsb[:, s_ * 8:(s_ + 1) * 8]
            blk = f"blsb{pp}"
            cs = slice(s_ * 128, (s_ + 1) * 128)
            R1, R2, tU, tL, egr, U, L, QKm, Xa, Xb, Pb, PTb, bv = T
            kR1, kR2, ktU, ktL, kegr, kU, kL, kQKm, kXa, kXb, kPb, kPTb, kbv = [f"T{i}" for i in range(13)]
            keA, kkeA, keB, kkeB = R2, kR2, tL, ktL
            u, ku, wT, kwT, qdT, kqdT, delta, kdelta = U, kU, L, kL, Pb, kPb, PTb, kPTb
            smx = sm2[pp][:, s_, :]
            beta, xa, g, _g, egc, bge, dl, eL, sA, sB = [smx[:, i * 4:(i + 1) * 4] for i in range(10)]
            gcs = smx[:, 40:48]
            gcum = gcs[:, 0:4]
            glast = gcs[:, 4:8]
            SK = f"smk{pp}_{s_}"
            qk = lambda j: f"qk{pp}_{j}"
            P.op("dve", lambda e: e.tensor_tensor(out=v3(R1), in0=bc_h(C["ltb"]), in1=bc_j(g), op=ALU.mult),
                 r=[SK], w=[kR1])
            P.op("dve", lambda e: e.tensor_tensor(out=v3(R2), in0=bc_h(C["ident"]), in1=bc_j(beta), op=ALU.mult),
                 r=[SK], w=[kR2])
            pgr, pgrk = rot.get()
            P.op("pe", lambda e: e.matmul(pgr, lhsT=C["ones"], rhs=R1, start=True, stop=True), r=[kR1], w=[pgrk])
            pbr, pbrk = rot.get()
            P.op("pe", lambda e: e.matmul(pbr, lhsT=C["ones"], rhs=R2, start=True, stop=True), r=[kR2], w=[pbrk])
            P.op("dve", lambda e: e.tensor_tensor(out=v3(tU), in0=v3(pgr), in1=bc_j(gcum), op=ALU.subtract),
                 r=[pgrk, SK], w=[ktU])
            P.op("dve", lambda e: e.tensor_tensor(out=v3(R1), in0=v3(tU), in1=bc_h(C["mnu"]), op=ALU.add),
                 r=[ktU], w=[kR1])
            P.op("act", lambda e: e.activation(out=R1, in_=R1, func=AF.Exp), r=[kR1], w=[kR1])
            P.op("dve", lambda e: e.tensor_tensor(out=v3(R2), in0=v3(tU), in1=bc_h(C["mnus"]), op=ALU.add),
                 r=[ktU], w=[kR2])
            P.op("act", lambda e: e.activation(out=R2, in_=R2, func=AF.Exp), r=[kR2], w=[kR2])
            P.op("dve", lambda e: e.tensor_tensor(out=R2, in0=R2, in1=pbr, op=ALU.mult), r=[kR2, pbrk], w=[kR2])
            P.op("dve", lambda e: e.scalar_tensor_tensor(out=v3(tL), in0=v3(pgr), scalar=-1.0, in1=bc_j(gcum),
                                                         op0=ALU.mult, op1=ALU.add), r=[pgrk, SK], w=[ktL])
            P.op("dve", lambda e: e.tensor_tensor(out=v3(tL), in0=v3(tL), in1=bc_h(C["mnls"]), op=ALU.add),
                 r=[ktL], w=[ktL])
            P.op("act", lambda e: e.activation(out=tL, in_=tL, func=AF.Exp), r=[ktL], w=[ktL])
            P.op("dve", lambda e: e.tensor_tensor(out=v3(tL), in0=v3(tL), in1=bc_j(beta), op=ALU.mult),
                 r=[ktL, SK], w=[ktL])
            P.op("act", lambda e: e.activation(out=egr, in_=pgr, func=AF.Exp), r=[pgrk], w=[kegr])
            pkk, pkkk = rot.get()
            for h in range(4):
                P.op("pe", lambda e, h=h: e.matmul(pkk[:, h * 128:(h + 1) * 128], lhsT=qkvc[:, 4 + h, cs],
                                                   rhs=qkvc[:, 4 + h, cs], start=True, stop=True),
                     r=[qk(4 + h)], w=[pkkk])
            P.op("dve", lambda e: e.tensor_tensor(out=U, in0=pkk, in1=R2, op=ALU.mult), r=[pkkk, kR2], w=[kU])
            P.op("dve", lambda e: e.tensor_tensor(out=L, in0=pkk, in1=tL, op=ALU.mult), r=[pkkk, ktL], w=[kL])
            pqk_, pqkk = rot.get()
            for h in range(4):
                P.op("pe", lambda e, h=h: e.matmul(pqk_[:, h * 128:(h + 1) * 128], lhsT=qkvc[:, 4 + h, cs],
                                                   rhs=qkvc[:, h, cs], start=True, stop=True),
                     r=[qk(4 + h), qk(h)], w=[pqkk])
            P.op("dve", lambda e: e.tensor_tensor(out=QKm, in0=pqk_, in1=R1, op=ALU.mult), r=[pqkk, kR1], w=[kQKm])
            P.op("dve", lambda e: e.tensor_tensor(out=v3(Xa), in0=bc_h(C["ident"]), in1=v3(U), op=ALU.subtract),
                 r=[kU], w=[kXa])
            pkt, pktk = rot.get()
            for h in range(4):
                P.op("pe", lambda e, h=h: e.transpose(out=pkt[:, h * 128:(h + 1) * 128], in_=qkvc[:, 4 + h, cs],
                                                      identity=C["ident"]), r=[qk(4 + h)], w=[pktk])
            kbg, kkbg = tU, ktU
            P.op("dve", lambda e: e.tensor_tensor(out=v3(kbg), in0=v3(pkt), in1=bc_j(bge), op=ALU.mult),
                 r=[pktk, SK], w=[kkbg])
            P.op("dve", lambda e: e.tensor_tensor(out=v3(keA), in0=v3(pkt), in1=bc_j(sA), op=ALU.mult),
                 r=[pktk, SK, kU], w=[kkeA])
            P.op("dve", lambda e: e.tensor_tensor(out=v3(keB), in0=v3(pkt), in1=bc_j(sB), op=ALU.mult),
                 r=[pktk, SK, kL], w=[kkeB])
            pvt, pvtk = rot.get()
            for h in range(4):
                P.op("pe", lambda e, h=h: e.transpose(out=pvt[:, h * 128:(h + 1) * 128], in_=qkvc[:, 8 + h, cs],
                                                      identity=C["ident"]), r=[qk(8 + h)], w=[pvtk])
            P.op("dve", lambda e: e.tensor_tensor(out=v3(bv), in0=v3(pvt), in1=bc_j(beta), op=ALU.mult),
                 r=[pvtk, SK], w=[kbv])
            Pc, PTc, kPc, kPTc = U, L, kU, kL
            Pn, PTn, kPn, kPTn = Pb, PTb, kPb, kPTb
            Xc, Xn, kXc, kXn = Xa, Xb, kXa, kXb
            for k in range(1, 6):
                if k < 5:
                    pp_, ppk = rot.get()
                    for h in range(4):
                        hs = slice(h * 128, (h + 1) * 128)
                        P.op("pe", lambda e, hs=hs, PTc=PTc, Pc=Pc, pp_=pp_: e.matmul(
                            pp_[:, hs], lhsT=PTc[:, hs], rhs=Pc[:, hs], start=True, stop=True),
                            r=[kPc, kPTc], w=[ppk])
                ppt, pptk = rot.get()
                for h in range(4):
                    hs = slice(h * 128, (h + 1) * 128)
                    P.op("pe", lambda e, hs=hs, PTc=PTc, Pc=Pc, ppt=ppt: e.matmul(
                        ppt[:, hs], lhsT=Pc[:, hs], rhs=PTc[:, hs], start=True, stop=True),
                        r=[kPc, kPTc], w=[pptk])
                if k < 5:
                    P.op("act", lambda e, Pn=Pn, pp_=pp_: e.activation(out=Pn, in_=pp_, func=AF.Copy), r=[ppk], w=[kPn])
                P.op("dve", lambda e, PTn=PTn, ppt=ppt: e.tensor_copy(out=PTn, in_=ppt), r=[pptk], w=[kPTn])
                px, pxk = rot.get()
                for h in range(4):
                    hs = slice(h * 128, (h + 1) * 128)
                    P.op("pe", lambda e, hs=hs, PTn=PTn, Xc=Xc, px=px: e.matmul(
                        px[:, hs], lhsT=PTn[:, hs], rhs=Xc[:, hs], start=True, stop=True),
                        r=[kPTn, kXc], w=[pxk])
                P.op("dve", lambda e, Xn=Xn, Xc=Xc, px=px: e.tensor_tensor(out=Xn, in0=px, in1=Xc, op=ALU.add),
                     r=[pxk, kXc], w=[kXn])
                Pc, Pn, kPc, kPn = Pn, Pc, kPn, kPc
                PTc, PTn, kPTc, kPTn = PTn, PTc, kPTn, kPTc
                Xc, Xn, kXc, kXn = Xn, Xc, kXn, kXc
            TTm, kTT = Xc, kXc
            assert TTm is Xb
            pu_, puk = rot.get()
            pw_, pwk = rot.get()
            for h in range(4):
                hs = slice(h * 128, (h + 1) * 128)
                P.op("pe", lambda e, hs=hs: e.matmul(pu_[:, hs], lhsT=TTm[:, hs], rhs=bv[:, hs], start=True, stop=True),
                     r=[kTT, kbv], w=[puk])
            for h in range(4):
                hs = slice(h * 128, (h + 1) * 128)
                P.op("pe", lambda e, hs=hs: e.matmul(pw_[:, hs], lhsT=kbg[:, hs], rhs=TTm[:, hs], start=True, stop=True),
                     r=[kTT, kkbg], w=[pwk])
            P.op("act", lambda e: e.activation(out=u, in_=pu_, func=AF.Copy), r=[puk], w=[ku])
            P.op("act", lambda e: e.activation(out=wT, in_=pw_, func=AF.Copy), r=[pwk], w=[kwT])
            P.op("dve", lambda e: e.tensor_tensor(out=v3(qdT), in0=qkvc[:, 0:4, cs], in1=v3(egr), op=ALU.mult),
                 r=[qk(h) for h in range(4)] + [kegr], w=[kqdT])
            po, pok = rot.get()
            others = [it_ for it_ in rotB.items if it_[1] != pok]
            oi = 0
            for ch in range(2):
                rows = slice(ch * 64, ch * 64 + 64)
                keX, kkeX = (keA, kkeA) if ch == 0 else (keB, kkeB)
                pws, pwsk = others[oi % 3]
                oi += 1
                for h in range(4):
                    hs = slice(h * 128, (h + 1) * 128)
                    P.op("pe", lambda e, hs=hs, h=h, pws=pws: e.matmul(pws[:, hs], lhsT=wT[:, hs], rhs=S[:, h, :],
                                                                      start=True, stop=True),
                         r=[kwT, f"S{h}"], w=[pwsk])
                P.op("dve", lambda e, rows=rows, pws=pws: e.tensor_tensor(out=delta[rows, :], in0=u[rows, :],
                                                                          in1=pws[rows, :], op=ALU.subtract),
                     r=[ku, pwsk], w=[kdelta])
                for h in range(4):
                    hs = slice(h * 128, (h + 1) * 128)
                    oc_ = slice(h * 128 + ch * 64, h * 128 + ch * 64 + 64)
                    P.op("pe", lambda e, h=h, oc_=oc_: e.matmul(po[:, oc_], lhsT=S[:, h, :], rhs=qdT[:, oc_],
                                                                start=True, stop=False),
                         r=[f"S{h}", kqdT], w=[pok])
                    P.op("pe", lambda e, hs=hs, oc_=oc_: e.matmul(po[:, oc_], lhsT=delta[:, hs], rhs=QKm[:, oc_],
                                                                  start=False, stop=True),
                         r=[kdelta, kQKm], w=[pok])
                pss, pssk = others[oi % 3]
                oi += 1
                for h in range(4):
                    hs = slice(h * 128, (h + 1) * 128)
                    P.op("pe", lambda e, hs=hs, keX=keX, pss=pss: e.matmul(pss[:, hs], lhsT=keX[:, hs], rhs=delta[:, hs],
                                                                          start=True, stop=True),
                         r=[kkeX, kdelta], w=[pssk])
                for h in range(4):
                    hs = slice(h * 128, (h + 1) * 128)
                    dcol = h * 128 + ch * 64 + 63
                    P.op("dve", lambda e, h=h, hs=hs, dcol=dcol, pss=pss: e.scalar_tensor_tensor(
                        out=S[:, h, :], in0=S[:, h, :], scalar=egr[:, dcol:dcol + 1], in1=pss[:, hs],
                        op0=ALU.mult, op1=ALU.add), r=[f"S{h}", kegr, pssk], w=[f"S{h}"])
            osb, kosb = bv, kbv
            sq2, ksq2 = R1, kR1
            P.op("act", lambda e: e.activation(out=osb, in_=po, func=AF.Copy), r=[pok], w=[kosb])
            P.op("act", lambda e: e.activation(out=sq2, in_=po, func=AF.Square), r=[pok], w=[ksq2])
            pm, pmk = others[oi % 3]
            P.op("pe", lambda e: e.matmul(pm, lhsT=C["omean"], rhs=sq2, start=True, stop=True), r=[ksq2], w=[pmk])
            P.op("act", lambda e: e.activation(out=sq2, in_=pm, func=AF.Ln, bias=EPS_RMS), r=[pmk], w=[ksq2])
            P.op("act", lambda e: e.activation(out=sq2, in_=sq2, func=AF.Exp, scale=-0.5), r=[ksq2], w=[ksq2])
            P.op("dve", lambda e: e.scalar_tensor_tensor(out=osb, in0=osb, scalar=dnw[:, 0:1], in1=sq2,
                                                         op0=ALU.mult, op1=ALU.mult), r=[kosb, ksq2], w=[kosb])
            P.op("pool", lambda e: e.tensor_tensor(out=mixT[:, 4:8, cs], in0=v3(osb), in1=zs[:, :, cs], op=ALU.mult),
                 r=[kosb] + [f"zs{pp}_{j}" for j in range(4)], w=[f"mixT{mp}_{4 + j}" for j in range(4)])

        def ln1_sub(ti, s_, b):
            mp = ti % 3
            mixT = mixT2[mp]
            rot = rotA
            cs = slice(s_ * 128, (s_ + 1) * 128)
            tok0 = ti * TT + s_ * 128
            P.dma("sp", lambda e: e.dma_start(out=xh, in_=x[tok0:tok0 + 128, :]), w=["xh"])
            pm2, (k0, k1) = rot2A.get()
            for half in range(2):
                for kc in range(8):
                    P.op("pe", lambda e, half=half, kc=kc: e.matmul(
                        pm2[:, half * 512:(half + 1) * 512], lhsT=mixT[:, kc, cs],
                        rhs=Wout[:, kc, half * 512:(half + 1) * 512], start=(kc == 0), stop=(kc == 7)),
                        r=[f"mixT{mp}_{kc}", "Wout"], w=[(k0, k1)[half]])
            P.op("dve", lambda e: e.tensor_tensor(out=r0, in0=pm2, in1=gate_bc[2][:, b, :], op=ALU.mult),
                 r=[k0, k1], w=["r0"])
            P.op("dve", lambda e: e.scalar_tensor_tensor(out=r0, in0=xh, scalar=ALPHA, in1=r0,
                                                         op0=ALU.mult, op1=ALU.add), r=["xh", "r0"], w=["r0"])
            for hf in range(2):
                P.op("dve", lambda e, hf=hf: e.bn_stats(out=bst[:, hf, :], in_=r0[:, hf * 512:(hf + 1) * 512]),
                     r=["r0"], w=["bst"])
            P.op("dve", lambda e: e.bn_aggr(out=mv[:, 0:2], in_=bst.rearrange("p a b -> p (a b)")), r=["bst"], w=["mv"])
            P.op("act", lambda e: e.activation(out=mv[:, 2:3], in_=mv[:, 1:2], func=AF.Ln, bias=1e-5),
                 r=["mv"], w=["mv2"])
            P.op("act", lambda e: e.activation(out=mv[:, 3:4], in_=mv[:, 2:3], func=AF.Exp, scale=-0.5),
                 r=["mv2"], w=["mv3"])
            P.op("dve", lambda e: e.tensor_scalar(out=xh, in0=r0, scalar1=mv[:, 0:1], scalar2=mv[:, 3:4],
                                                  op0=ALU.subtract, op1=ALU.mult), r=["r0", "mv", "mv3"], w=["xh"])
            P.op("pool", lambda e: e.tensor_tensor(out=r0, in0=xh, in1=ag, op=ALU.mult), r=["xh"], w=["r0"])
            P.op("pool", lambda e: e.tensor_tensor(out=r0, in0=r0, in1=ab, op=ALU.add), r=["r0"], w=["r0"])
            P.dma("sp", lambda e: e.dma_start(out=r_s[tok0:tok0 + 128, :], in_=r0), r=["r0"], w=["rs"])
            for kc in range(8):
                pt, pk = rot.get()
                P.op("pe", lambda e, pt=pt, kc=kc: e.transpose(out=pt[:, 0:128], in_=xh[:, kc * 128:(kc + 1) * 128],
                                                               identity=C["ident"]), r=["xh"], w=[pk])
                P.op("act", lambda e, pt=pt, kc=kc: e.activation(
                    out=h2t[:, kc, cs], in_=pt[:, 0:128], func=AF.Identity,
                    scale=A2[:, kc, b:b + 1], bias=B2[:, kc, b:b + 1]), r=[pk], w=["h2t"])

        def capture(fn, *a):
            old = P.ops
            P.ops = []
            fn(*a)
            got = P.ops
            P.ops = old
            return got

        def merge(a, b_):
            out_, i, j = [], 0, 0
            na, nb = max(len(a), 1), max(len(b_), 1)
            while i < len(a) or j < len(b_):
                if j >= len(b_) or (i < len(a) and i * nb <= j * na):
                    out_.append(a[i]); i += 1
                else:
                    out_.append(b_[j]); j += 1
            return out_

        ntiles = NT if stage != "pa1" else 2

        def threadA(ti):
            if ti + 1 < ntiles:
                stage1(ti + 1)
            if ti - 1 >= 0:
                stage3(ti - 1)

        P.ops += capture(stage1, 0)
        for ti in range(ntiles):
            a = capture(threadA, ti)
            b_ = capture(stage2, ti)
            P.ops += merge(a, b_)
        P.ops += capture(stage3, ntiles - 1)
        if stage == "pa1":
            P.dma("pool", lambda e: e.dma_start(out=dbg_out["mixT"].rearrange("p (a b) -> p a b", a=8), in_=mixT2[1]),
                  r=[f"mixT1_{j}" for j in range(8)], w=["dbg1"])
            P.dma("sp", lambda e: e.dma_start(out=dbg_out["r"], in_=r_s[0:512, :]), r=["rs"], w=["dbg2"])
            P.dma("pool", lambda e: e.dma_start(out=dbg_out["h2"].rearrange("p (a b) -> p a b", a=8),
                                                in_=h2_s[:, :, 0:512]), r=["h2s"], w=["dbg3"])
            P.dma("sp", lambda e: e.dma_start(out=dbg_out["qkvc"].rearrange("p (a b) -> p a b", a=12), in_=qkvc2[1]),
                  r=[f"qk1_{j}" for j in range(12)], w=["dbg4"])
        cnt = P.emit()
        print("phaseA op counts", cnt)

    if stage in ("pa", "pa1"):
        return nc
    I32 = mybir.dt.int32
    with contextlib.ExitStack() as es:
        def tsb(name, shape, dt=F32):
            return es.enter_context(nc.sbuf_tensor("b_" + name, list(shape), dt)).ap()

        P = Prog(nc, "pb")
        NSTT = TOK // 128
        NW = 3
        cb = tsb("cb", list(CB.shape))
        thr = cb[:, 0:64]
        SLc = cb[:, 64:64 + 1024]
        blkio = cb[:, 1088:1088 + NBLK]
        iotaP = cb[:, 1088 + NBLK:1089 + NBLK]
        SUf = cb[:, 1089 + NBLK:1089 + NBLK + 128]
        SUb = tsb("sub", [128, 128], BF16)
        onesb = tsb("onesb", [128, 128], BF16)
        Wr = tsb("wr", [128, 8, 36], BF16)
        b36 = tsb("b36", [128, 36])
        g2 = tsb("ln2g", [128, D])
        b2 = tsb("ln2b", [128, D])
        gate2 = tsb("gate2", [128, 2, D])
        h2c = [tsb(f"h2c{i}", [128, 8, 512], BF16) for i in range(2)]
        OHall = tsb("ohall", [128, NSTT, 32], BF16)
        oh1all = tsb("oh1all", [128, NSTT, 32])
        oh2all = tsb("oh2all", [128, NSTT, 32])
        gAB = tsb("gab", [128, NSTT, 2])
        POSf = tsb("posf", [128, NSTT, 2])
        POSi = tsb("posi", [128, NSTT, 2], I32)
        cnt = tsb("cnt", [128, 32])
        nblk = tsb("nblk", [128, 32])
        pstb = tsb("pstb", [128, 32])
        pend = tsb("pend", [128, 32])
        base = tsb("base", [128, 32])
        big = tsb("big", [128, NBLK * 32])
        bexp = tsb("bexp", [128, NBLK])
        bskip = tsb("bskip", [128, NBLK])
        bsame = tsb("bsame", [128, NBLK])
        IDXW = tsb("idxw", [128, NBLK], I32)
        posv = tsb("posv", [128, 32])
        ptmp = tsb("ptmp", [128, 32])
        lg = tsb("lg", [128, 36])
        lgm = tsb("lgm", [128, 32])
        lgm2 = tsb("lgm2", [128, 32])
        rs_ = tsb("rsm", [128, 32])
        Wg = [tsb(f"wg{i}", [128, 2048], BF16) for i in range(NW)]
        Wu = [tsb(f"wu{i}", [128, 2048], BF16) for i in range(NW)]
        Wd = [tsb(f"wd{i}", [128, 2048], BF16) for i in range(NW)]
        xb = [tsb(f"xb{i}", [128, D]) for i in range(3)]
        sh2r = tsb("sh2r", [128, 2, D])
        sc2r = tsb("sc2r", [128, 2, D])
        xT = [tsb(f"xT{i}", [128, 8, 128], BF16) for i in range(3)]
        sg = [tsb(f"sg{i}", [128, 256]) for i in range(3)]
        hid = [tsb(f"hid{i}", [128, 256]) for i in range(3)]
        hidT = [tsb(f"hidT{i}", [128, 256], BF16) for i in range(3)]
        yb = [tsb(f"yb{i}", [128, D]) for i in range(2)]
        y1 = [tsb(f"y1_{i}", [128, D]) for i in range(2)]
        y2 = [tsb(f"y2_{i}", [128, D]) for i in range(2)]
        rr = [tsb(f"rr{i}", [128, D]) for i in range(2)]
        xh2 = tsb("xh2", [128, D])
        ob = tsb("ob", [128, D])
        bst2 = tsb("bst2", [128, 2, 6])
        mv2 = tsb("mv2", [128, 4])
        rot = PsumRot(BANKS[0:2])
        rot2 = PsumRot([(PS2[i], (f"bank{2 * i}", f"bank{2 * i + 1}")) for i in (2, 3)])

        P.dma("sp", lambda e: e.dma_start(out=cb, in_=cb_d), w=["cb"])
        P.op("dve", lambda e: e.tensor_copy(out=SUb, in_=SUf), r=["cb"], w=["SUb"])
        P.op("dve", lambda e: e.memset(onesb, 1.0), w=["onesb"])
        for kc in range(8):
            P.dma("pool", lambda e, kc=kc: e.dma_start(out=Wr[:, kc, 0:4], in_=w_grp[kc * 128:(kc + 1) * 128, :]),
                  w=["Wr"])
            P.dma("pool", lambda e, kc=kc: e.dma_start(out=Wr[:, kc, 4:36], in_=w_exp[kc * 128:(kc + 1) * 128, :]),
                  w=["Wr"])
        P.dma("sp", lambda e: e.dma_start(out=b36[:, 0:4], in_=b_grp[0, :].partition_broadcast(128)), w=["b36"])
        P.dma("sp", lambda e: e.dma_start(out=b36[:, 4:36], in_=b_exp[0, :].partition_broadcast(128)), w=["b36"])
        P.dma("sp", lambda e: e.dma_start(out=gate2.rearrange("p a b -> p (a b)"), in_=g2_s), w=["gate2"])
        P.dma("sp", lambda e: e.dma_start(out=sh2r.rearrange("p a b -> p (a b)"), in_=sh2_s), w=["sh2r"])
        P.dma("sp", lambda e: e.dma_start(out=sc2r.rearrange("p a b -> p (a b)"), in_=sc2_s), w=["sc2r"])
        P.op("pool", lambda e: e.tensor_scalar(out=sc2r, in0=sc2r, scalar1=1.0, scalar2=1.0 / ALPHA, op0=ALU.add,
                                               op1=ALU.mult), r=["sc2r"], w=["sc2r"])
        P.dma("sp", lambda e: e.dma_start(out=g2, in_=ln2_g[0, :].partition_broadcast(128)), w=["g2"])
        P.dma("sp", lambda e: e.dma_start(out=b2, in_=ln2_b[0, :].partition_broadcast(128)), w=["b2"])

        RG = 4
        lg4 = tsb("lg4", [128, RG, 36])
        lgm4 = tsb("lgm4", [128, RG, 32])
        lgm24 = tsb("lgm24", [128, RG, 32])
        eg4 = tsb("eg4", [128, RG, 4])
        ohg4 = tsb("ohg4", [128, RG, 4])
        rsc = tsb("rsc", [128, 12, RG])

        def router4(g_):
            st0 = g_ * RG
            ci = g_ % 2
            P.dma("sp", lambda e: e.dma_start(out=h2c[ci], in_=h2_s[:, :, st0 * 128:st0 * 128 + 512]), w=[f"h2c{ci}"])
            plg, plgk = rot.get()
            for i in range(RG):
                cs = slice(i * 128, (i + 1) * 128)
                for kc in range(8):
                    P.op("pe", lambda e, kc=kc, i=i, cs=cs: e.matmul(plg[:, i * 36:(i + 1) * 36], lhsT=h2c[ci][:, kc, cs],
                                                                      rhs=Wr[:, kc, :], start=(kc == 0), stop=(kc == 7)),
                         r=[f"h2c{ci}", "Wr"], w=[plgk])
            gmax, sume, grpw, m1, m2, d21, p2, den, rden = [rsc[:, i, :] for i in range(9)]
            sts = range(st0, st0 + RG)
            K1 = [f"oh1_{st}" for st in sts]
            K2 = [f"oh2_{st}" for st in sts]
            KA = [f"gA{st}" for st in sts]
            KB = [f"gB{st}" for st in sts]
            KO = [f"OH{st}" for st in sts]
            oh1 = oh1all[:, st0:st0 + RG, :]
            oh2 = oh2all[:, st0:st0 + RG, :]
            gA = gAB[:, st0:st0 + RG, 0]
            gB = gAB[:, st0:st0 + RG, 1]
            bcx = lambda ap, n: ap.unsqueeze(2).to_broadcast([128, RG, n])
            P.op("dve", lambda e: e.tensor_tensor(out=lg4, in0=plg[:, 0:RG * 36].rearrange("p (s n) -> p s n", s=RG),
                                                  in1=b36.unsqueeze(1).to_broadcast([128, RG, 36]), op=ALU.add),
                 r=[plgk, "b36"], w=["lg4"])
            P.op("dve", lambda e: e.tensor_reduce(out=gmax, in_=lg4[:, :, 0:4], axis=AX.X, op=ALU.max), r=["lg4"], w=["q_gmax"])
            P.op("dve", lambda e: e.tensor_tensor(out=eg4, in0=lg4[:, :, 0:4], in1=bcx(gmax, 4), op=ALU.subtract),
                 r=["lg4", "q_gmax"], w=["eg4"])
            P.op("act", lambda e: e.activation(out=eg4, in_=eg4, func=AF.Exp), r=["eg4"], w=["eg4"])
            P.op("dve", lambda e: e.tensor_reduce(out=sume, in_=eg4, axis=AX.X, op=ALU.add), r=["eg4"], w=["q_sume"])
            P.op("dve", lambda e: e.reciprocal(out=grpw, in_=sume), r=["q_sume"], w=["q_grpw"])
            P.op("dve", lambda e: e.tensor_tensor(out=ohg4, in0=lg4[:, :, 0:4], in1=bcx(gmax, 4), op=ALU.is_equal),
                 r=["lg4", "q_gmax"], w=["ohg4"])
            P.op("dve", lambda e: e.tensor_scalar(out=ohg4, in0=ohg4, scalar1=-1.0, scalar2=BIG, op0=ALU.add, op1=ALU.mult),
                 r=["ohg4"], w=["ohg4"])
            P.op("dve", lambda e: e.tensor_tensor(out=lgm4.rearrange("p s (g k) -> p s g k", g=4),
                                                  in0=lg4[:, :, 4:36].rearrange("p s (g k) -> p s g k", g=4),
                                                  in1=ohg4.unsqueeze(3).to_broadcast([128, RG, 4, 8]), op=ALU.add),
                 r=["lg4", "ohg4"], w=["lgm4"])
            P.op("dve", lambda e: e.tensor_reduce(out=m1, in_=lgm4, axis=AX.X, op=ALU.max), r=["lgm4"], w=["q_m1"])
            P.op("dve", lambda e: e.tensor_tensor(out=oh1, in0=lgm4, in1=bcx(m1, 32), op=ALU.is_equal),
                 r=["lgm4", "q_m1"], w=K1)
            P.op("dve", lambda e: e.scalar_tensor_tensor(out=lgm24, in0=oh1, scalar=-BIG, in1=lgm4, op0=ALU.mult,
                                                         op1=ALU.add), r=K1 + ["lgm4"], w=["lgm24"])
            P.op("dve", lambda e: e.tensor_reduce(out=m2, in_=lgm24, axis=AX.X, op=ALU.max), r=["lgm24"], w=["q_m2"])
            P.op("dve", lambda e: e.tensor_tensor(out=oh2, in0=lgm24, in1=bcx(m2, 32), op=ALU.is_equal),
                 r=["lgm24", "q_m2"], w=K2)
            P.op("dve", lambda e: e.tensor_tensor(out=d21, in0=m2, in1=m1, op=ALU.subtract), r=["q_m1", "q_m2"], w=["q_d21"])
            P.op("act", lambda e: e.activation(out=p2, in_=d21, func=AF.Exp), r=["q_d21"], w=["q_p2"])
            P.op("dve", lambda e: e.tensor_scalar(out=den, in0=p2, scalar1=1.0, scalar2=None, op0=ALU.add),
                 r=["q_p2"], w=["q_den"])
            P.op("dve", lambda e: e.reciprocal(out=rden, in_=den), r=["q_den"], w=["q_rden"])
            P.op("dve", lambda e: e.tensor_tensor(out=gA, in0=grpw, in1=rden, op=ALU.mult), r=["q_grpw", "q_rden"], w=KA)
            P.op("dve", lambda e: e.tensor_tensor(out=gB, in0=gA, in1=p2, op=ALU.mult), r=KA + ["q_p2"], w=KB)
            P.op("pool", lambda e: e.tensor_tensor(out=OHall[:, st0:st0 + RG, :], in0=oh1, in1=oh2, op=ALU.add),
                 r=K1 + K2, w=KO)

        for g_ in range(NSTT // RG):
            router4(g_)

        pcnt, pcntk = rot.get()
        for st in range(NSTT):
            P.op("pe", lambda e, st=st: e.matmul(pcnt[:, 0:32], lhsT=onesb, rhs=OHall[:, st, :],
                                                 start=(st == 0), stop=(st == NSTT - 1)),
                 r=[f"OH{st}", "onesb"], w=[pcntk])
        P.op("act", lambda e: e.activation(out=cnt, in_=pcnt[:, 0:32], func=AF.Copy), r=[pcntk], w=["cnt"])
        big3 = big[:, 0:32 * 64].rearrange("p (e k) -> p e k", e=32)
        P.op("dve", lambda e: e.tensor_tensor(out=big3, in0=cnt.unsqueeze(2).to_broadcast([128, 32, 64]),
                                              in1=thr.unsqueeze(1).to_broadcast([128, 32, 64]), op=ALU.is_gt),
             r=["cnt", "cb"], w=["big"])
        P.op("dve", lambda e: e.tensor_reduce(out=nblk, in_=big3, axis=AX.X, op=ALU.add), r=["big"], w=["nblk"])
        big3b = big[:, 0:1024].rearrange("p (e f) -> p e f", e=32)
        P.op("dve", lambda e: e.tensor_tensor(out=big3b, in0=nblk.unsqueeze(1).to_broadcast([128, 32, 32]),
                                              in1=SLc.rearrange("p (e f) -> p e f", e=32), op=ALU.mult),
             r=["nblk", "cb", "big"], w=["big"])
        P.op("dve", lambda e: e.tensor_reduce(out=pstb, in_=big3b, axis=AX.X, op=ALU.add), r=["big"], w=["pstb"])
        P.op("dve", lambda e: e.tensor_tensor(out=pend, in0=pstb, in1=nblk, op=ALU.add), r=["pstb", "nblk"], w=["pend"])
        P.op("dve", lambda e: e.tensor_scalar(out=base, in0=pstb, scalar1=128.0, scalar2=None, op0=ALU.mult),
             r=["pstb"], w=["base"])
        big3c = big.rearrange("p (b e) -> p b e", b=NBLK)
        P.op("dve", lambda e: e.tensor_tensor(out=big3c, in0=pend.unsqueeze(1).to_broadcast([128, NBLK, 32]),
                                              in1=blkio.unsqueeze(2).to_broadcast([128, NBLK, 32]), op=ALU.is_le),
             r=["pend", "cb", "big", "pstb"], w=["big"])
        P.op("dve", lambda e: e.tensor_reduce(out=bexp, in_=big3c, axis=AX.X, op=ALU.add), r=["big"], w=["bexp"])
        P.op("dve", lambda e: e.tensor_scalar(out=bskip, in0=bexp, scalar1=float(NEXP) - 0.5, scalar2=None, op0=ALU.is_ge),
             r=["bexp"], w=["bskip"])
        P.op("dve", lambda e: e.tensor_tensor(out=bsame[:, NW:NBLK], in0=bexp[:, NW:NBLK], in1=bexp[:, 0:NBLK - NW],
                                              op=ALU.is_equal), r=["bexp"], w=["bsame"])
        P.op("dve", lambda e: e.tensor_tensor(out=bskip[:, NW:NBLK], in0=bskip[:, NW:NBLK], in1=bsame[:, NW:NBLK],
                                              op=ALU.max), r=["bskip", "bsame"], w=["bskip"])
        P.op("dve", lambda e: e.tensor_scalar(out=bexp, in0=bexp, scalar1=float(NEXP - 1), scalar2=128.0,
                                              op0=ALU.min, op1=ALU.mult), r=["bexp"], w=["bexp"])
        P.op("dve", lambda e: e.tensor_scalar(out=bexp, in0=bexp, scalar1=iotaP, scalar2=None, op0=ALU.add),
             r=["bexp", "cb"], w=["bexp"])
        P.op("dve", lambda e: e.scalar_tensor_tensor(out=bexp, in0=bskip, scalar=1.0e6, in1=bexp, op0=ALU.mult,
                                                     op1=ALU.add), r=["bexp", "bskip"], w=["bexp"])
        P.op("dve", lambda e: e.tensor_copy(out=IDXW, in_=bexp), r=["bexp"], w=["IDXW"])
        posv4 = tsb("posv4", [128, RG, 32])
        ptmp4 = tsb("ptmp4", [128, RG, 32])
        for g_ in range(NSTT // RG):
            st0 = g_ * RG
            prk, prkk = rot.get()
            for i in range(RG):
                st = st0 + i
                osl = slice(i * 32, (i + 1) * 32)
                P.op("pe", lambda e, st=st, osl=osl, prk=prk: e.matmul(prk[:, osl], lhsT=SUb, rhs=OHall[:, st, :],
                                                                      start=True, stop=(st == 0)),
                     r=[f"OH{st}", "SUb"], w=[prkk])
                for s2 in range(st):
                    P.op("pe", lambda e, s2=s2, st=st, osl=osl, prk=prk: e.matmul(
                        prk[:, osl], lhsT=onesb, rhs=OHall[:, s2, :], start=False, stop=(s2 == st - 1)),
                        r=[f"OH{s2}", "onesb"], w=[prkk])
            P.op("dve", lambda e, prk=prk: e.tensor_tensor(
                out=posv4, in0=prk[:, 0:RG * 32].rearrange("p (s n) -> p s n", s=RG),
                in1=base.unsqueeze(1).to_broadcast([128, RG, 32]), op=ALU.add), r=[prkk, "base"], w=["posv4"])
            KK = [f"oh1_{st}" for st in range(st0, st0 + RG)] + [f"oh2_{st}" for st in range(st0, st0 + RG)]
            for k, oha in ((0, oh1all), (1, oh2all)):
                P.op("dve", lambda e, oha=oha, st0=st0: e.tensor_tensor(out=ptmp4, in0=posv4, in1=oha[:, st0:st0 + RG, :],
                                                                        op=ALU.mult), r=["posv4"] + KK, w=["ptmp4"])
                P.op("dve", lambda e, k=k, st0=st0: e.tensor_reduce(out=POSf[:, st0:st0 + RG, k], in_=ptmp4, axis=AX.X,
                                                                    op=ALU.add), r=["ptmp4"], w=["POSf"])
        P.op("dve", lambda e: e.tensor_copy(out=POSi, in_=POSf), r=["POSf"], w=["POSi"])

        if stage == "pbdbg":
            P.dma("sp", lambda e: e.dma_start(out=dbg_out["cnt"], in_=cnt), r=["cnt"], w=["dg1"])
            P.dma("sp", lambda e: e.dma_start(out=dbg_out["nblk"], in_=nblk), r=["nblk"], w=["dg2"])
            P.dma("sp", lambda e: e.dma_start(out=dbg_out["pstb"], in_=pstb), r=["pstb"], w=["dg3"])
            P.dma("sp", lambda e: e.dma_start(out=dbg_out["bexp"], in_=bexp), r=["bexp"], w=["dg4"])
            P.dma("sp", lambda e: e.dma_start(out=dbg_out["posf"], in_=POSf.rearrange("p a b -> p (a b)")), r=["POSf"], w=["dg5"])
            P.dma("sp", lambda e: e.dma_start(out=dbg_out["oh1"], in_=oh1all.rearrange("p a b -> p (a b)")),
                  r=[f"oh1_{st}" for st in range(NSTT)], w=["dg6"])
            P.dma("sp", lambda e: e.dma_start(out=dbg_out["oh2"], in_=oh2all.rearrange("p a b -> p (a b)")),
                  r=[f"oh2_{st}" for st in range(NSTT)], w=["dg7"])
            P.dma("sp", lambda e: e.dma_start(out=dbg_out["gab"], in_=gAB.rearrange("p a b -> p (a b)")),
                  r=[f"gA{st}" for st in range(NSTT)] + [f"gB{st}" for st in range(NSTT)], w=["dg8"])
            P.emit()
            return nc
        for st in range(NSTT):
            b = st // (NSTT // NB)
            xq = xb[st % 3]
            xk = f"xb{st % 3}"
            P.dma("sp", lambda e, st=st, xq=xq: e.dma_start(out=xq, in_=r_s[st * 128:(st + 1) * 128, :]), w=[xk])
            P.op("dve", lambda e, xq=xq, b=b: e.tensor_tensor(out=xq, in0=xq, in1=sc2r[:, b, :], op=ALU.mult),
                 r=[xk, "sc2r"], w=[xk])
            P.op("dve", lambda e, xq=xq, b=b: e.tensor_tensor(out=xq, in0=xq, in1=sh2r[:, b, :], op=ALU.add),
                 r=[xk, "sh2r"], w=[xk])
            for k in range(2):
                P.dma("pool", lambda e, st=st, k=k, xq=xq: e.indirect_dma_start(
                    out=xrows_s, out_offset=bass.IndirectOffsetOnAxis(ap=POSi[:, st, k:k + 1], axis=0),
                    in_=xq, in_offset=None), r=[xk, "POSi"], w=[f"xrows{st}_{k}"])
        if stage in ("pbs1", "pbs2"):
            P.emit()
            return nc

        pgu_banks = PsumRot(BANKS[0:2])
        NQ = 3
        wgate_rows = w_gate.rearrange("e (p k) n -> (e p) (k n)", k=8)
        wup_rows = w_up.rearrange("e (p k) n -> (e p) (k n)", k=8)
        wdn_rows = w_down.rearrange("e (p k) n -> (e p) (k n)", k=2)

        def blk_loadx(bi):
            q = bi % NQ
            P.dma("sp", lambda e: e.dma_start(out=xb[q], in_=xrows_s[bi * 128:(bi + 1) * 128, :]),
                  r=[f"xrows{st}_{k}" for st in range(NSTT) for k in range(2)], w=[f"xb{q}"])

        bnd = {}

        def bnd_reg(e):
            if "r" not in bnd:
                bnd["r"] = e.alloc_register("wbound")
                e.reg_mov(bnd["r"], NEXP * 128 - 1)
            return bnd["r"]

        def blk_load(bi):
            slot = bi % NW
            ioff = bass.IndirectOffsetOnAxis(ap=IDXW[:, bi:bi + 1], axis=0)
            P.dma("pool", lambda e: e.indirect_dma_start(out=Wg[slot], out_offset=None,
                                                         in_=wgate_rows, in_offset=ioff, bounds_check=bnd_reg(e), oob_is_err=False), r=["IDXW"], w=[f"wg{slot}"])
            P.dma("pool", lambda e: e.indirect_dma_start(out=Wu[slot], out_offset=None,
                                                         in_=wup_rows, in_offset=ioff, bounds_check=bnd_reg(e), oob_is_err=False), r=["IDXW"], w=[f"wu{slot}"])
            P.dma("pool", lambda e: e.indirect_dma_start(out=Wd[slot], out_offset=None,
                                                         in_=wdn_rows, in_offset=ioff, bounds_check=bnd_reg(e), oob_is_err=False), r=["IDXW"], w=[f"wd{slot}"])

        xt_banks = [BANKS[2], BANKS[3]]
        ht_banks = PsumRot(BANKS[6:8])

        def blk_xt(bi):
            q = bi % NQ
            xv = xb[q].rearrange("r (p k) -> r k p", k=8)
            xTf = xT[q].rearrange("p a b -> p (a b)")
            for hb_ in range(2):
                pt, ptk = xt_banks[hb_]
                for k4 in range(4):
                    kc = hb_ * 4 + k4
                    P.op("pe", lambda e, kc=kc, k4=k4, pt=pt: e.transpose(out=pt[:, k4 * 128:(k4 + 1) * 128],
                                                                          in_=xv[:, kc, :], identity=C["ident"]),
                         r=[f"xb{q}"], w=[ptk])
                if hb_ == 0:
                    P.op("act", lambda e, pt=pt: e.activation(out=xTf[:, 0:512], in_=pt, func=AF.Copy),
                         r=[ptk], w=[f"xTa{q}"])
                else:
                    P.op("dve", lambda e, pt=pt: e.tensor_copy(out=xTf[:, 512:1024], in_=pt), r=[ptk], w=[f"xTb{q}"])

        def blk_gu(bi):
            slot = bi % NW
            q = bi % NQ
            pgu, pguk = pgu_banks.get()
            for (Wm, wk, c0) in ((Wg, f"wg{slot}", 0), (Wu, f"wu{slot}", 256)):
                for kc in range(8):
                    P.op("pe", lambda e, kc=kc, Wm=Wm, c0=c0: e.matmul(
                        pgu[:, c0:c0 + 256], lhsT=xT[q][:, kc, :], rhs=Wm[slot][:, kc * 256:(kc + 1) * 256],
                        start=(kc == 0), stop=(kc == 7)), r=[f"xTa{q}", f"xTb{q}", wk], w=[pguk])
            P.op("act", lambda e: e.activation(out=sg[q], in_=pgu[:, 0:256], func=AF.Exp, scale=-1.0), r=[pguk], w=[f"sg{q}"])
            P.op("act", lambda e: e.activation(out=sg[q], in_=sg[q], func=AF.Ln, bias=1.0), r=[f"sg{q}"], w=[f"sg{q}"])
            P.op("act", lambda e: e.activation(out=sg[q], in_=sg[q], func=AF.Exp, scale=-1.0), r=[f"sg{q}"], w=[f"sg{q}"])
            P.op("dve", lambda e: e.tensor_tensor(out=sg[q], in0=pgu[:, 0:256], in1=sg[q], op=ALU.mult),
                 r=[pguk, f"sg{q}"], w=[f"sg{q}"])
            P.op("dve", lambda e: e.tensor_tensor(out=hid[q], in0=pgu[:, 256:512], in1=sg[q], op=ALU.mult),
                 r=[pguk, f"sg{q}"], w=[f"hid{q}"])

        def blk_tr(bi):
            q = bi % NQ
            pht, phtk = ht_banks.get()
            hv = hid[q].rearrange("r (p k) -> r k p", k=2)
            for k2 in range(2):
                P.op("pe", lambda e, k2=k2: e.transpose(out=pht[:, k2 * 128:(k2 + 1) * 128], in_=hv[:, k2, :],
                                                        identity=C["ident"]), r=[f"hid{q}"], w=[phtk])
            P.op("act", lambda e: e.activation(out=hidT[q], in_=pht[:, 0:256], func=AF.Copy), r=[phtk], w=[f"hidT{q}"])

        def blk_dn(bi):
            slot = bi % NW
            q = bi % NQ
            yq = bi % 2
            py, (k0, k1) = PS2[2], ("bank4", "bank5")
            for half in range(2):
                for k2 in range(2):
                    P.op("pe", lambda e, half=half, k2=k2: e.matmul(
                        py[:, half * 512:(half + 1) * 512], lhsT=hidT[q][:, k2 * 128:(k2 + 1) * 128],
                        rhs=Wd[slot][:, k2 * 1024 + half * 512:k2 * 1024 + (half + 1) * 512], start=(k2 == 0), stop=(k2 == 1)),
                        r=[f"hidT{q}", f"wd{slot}"], w=[(k0, k1)[half]])
            P.op("act", lambda e: e.activation(out=yb[yq][:, 0:512], in_=py[:, 0:512], func=AF.Copy), r=[k0], w=[f"yba{yq}"])
            P.op("dve", lambda e: e.tensor_copy(out=yb[yq][:, 512:1024], in_=py[:, 512:1024]), r=[k1], w=[f"ybb{yq}"])
            P.dma("sp", lambda e: e.dma_start(out=yrows_s[bi * 128:(bi + 1) * 128, :], in_=yb[yq]),
                  r=[f"yba{yq}", f"ybb{yq}"], w=[f"yrows{bi}"])

        nb_run = NBLK if stage != 'pbs3' else 6
        pendq = []
        blk_load(0)
        for b0_ in range(min(2, nb_run)):
            blk_loadx(b0_)
        for bi in range(nb_run):
            if bi + 2 < nb_run:
                blk_loadx(bi + 2)
            blk_xt(bi)
            blk_gu(bi)
            pendq.append(bi)
            if len(pendq) >= 2:
                blk_tr(pendq[-2])
            if len(pendq) >= 3:
                blk_dn(pendq.pop(0))
            if bi + 1 < nb_run:
                blk_load(bi + 1)
        if len(pendq) == 2:
            blk_dn(pendq.pop(0))
        while pendq:
            a = pendq.pop(0)
            blk_tr(a)
            blk_dn(a)
        YK = [f"yrows{bi}" for bi in range(nb_run)]
        if stage == "pbs3":
            P.emit()
            return nc

        def comb_fetch(st):
            q = st % 2
            P.dma("sp", lambda e: e.dma_start(out=rr[q], in_=r_s[st * 128:(st + 1) * 128, :]), w=[f"rr{q}"])
            P.dma("pool", lambda e: e.indirect_dma_start(
                out=y1[q], out_offset=None, in_=yrows_s,
                in_offset=bass.IndirectOffsetOnAxis(ap=POSi[:, st, 0:1], axis=0)), r=YK + ["POSi"], w=[f"y1_{q}"])
            P.dma("pool", lambda e: e.indirect_dma_start(
                out=y2[q], out_offset=None, in_=yrows_s,
                in_offset=bass.IndirectOffsetOnAxis(ap=POSi[:, st, 1:2], axis=0)), r=YK + ["POSi"], w=[f"y2_{q}"])

        def comb_compute(st):
            q = st % 2
            b = st // (NSTT // NB)
            bst2q = bst2p[q]
            mv2q = mv2p[q]
            P.op("act", lambda e: e.activation(out=y1[q], in_=y1[q], func=AF.Copy, scale=gAB[:, st, 0:1]),
                 r=[f"y1_{q}", f"gA{st}"], w=[f"y1_{q}"])
            P.op("dve", lambda e: e.scalar_tensor_tensor(out=y1[q], in0=y2[q], scalar=gAB[:, st, 1:2], in1=y1[q],
                                                         op0=ALU.mult, op1=ALU.add),
                 r=[f"y1_{q}", f"y2_{q}", f"gB{st}"], w=[f"y1_{q}"])
            P.op("dve", lambda e: e.tensor_tensor(out=y1[q], in0=y1[q], in1=gate2[:, b, :], op=ALU.mult),
                 r=[f"y1_{q}", "gate2"], w=[f"y1_{q}"])
            P.op("dve", lambda e: e.tensor_tensor(out=rr[q], in0=rr[q], in1=y1[q], op=ALU.add),
                 r=[f"rr{q}", f"y1_{q}"], w=[f"rr{q}"])
            for hf in range(2):
                P.op("dve", lambda e, hf=hf: e.bn_stats(out=bst2q[:, hf, :], in_=rr[q][:, hf * 512:(hf + 1) * 512]),
                     r=[f"rr{q}"], w=[f"bst2{q}"])
            P.op("dve", lambda e: e.bn_aggr(out=mv2q[:, 0:2], in_=bst2q.rearrange("p a b -> p (a b)")), r=[f"bst2{q}"], w=[f"mv2{q}"])
            P.op("act", lambda e: e.activation(out=mv2q[:, 2:3], in_=mv2q[:, 1:2], func=AF.Ln, bias=1e-5),
                 r=[f"mv2{q}"], w=[f"mv2b{q}"])
            P.op("act", lambda e: e.activation(out=mv2q[:, 3:4], in_=mv2q[:, 2:3], func=AF.Exp, scale=-0.5),
                 r=[f"mv2b{q}"], w=[f"mv2c{q}"])
            P.op("dve", lambda e: e.scalar_tensor_tensor(out=mv2q[:, 2:3], in0=mv2q[:, 0:1], scalar=-1.0, in1=mv2q[:, 3:4],
                                                         op0=ALU.mult, op1=ALU.mult), r=[f"mv2{q}", f"mv2b{q}", f"mv2c{q}"], w=[f"mv2b{q}", f"mv2d{q}"])
            P.op("act", lambda e: e.activation(out=y2[q], in_=rr[q], func=AF.Identity, scale=mv2q[:, 3:4], bias=mv2q[:, 2:3]),
                 r=[f"rr{q}", f"mv2c{q}", f"mv2d{q}", f"y2_{q}"], w=[f"y2_{q}"])
            oq = ob2[q]
            P.op("pool", lambda e: e.tensor_tensor(out=oq, in0=y2[q], in1=g2, op=ALU.mult), r=[f"y2_{q}", "g2"], w=[f"ob{q}"])
            P.op("pool", lambda e: e.tensor_tensor(out=oq, in0=oq, in1=b2, op=ALU.add), r=[f"ob{q}", "b2"], w=[f"ob{q}"])
            P.dma("sp", lambda e: e.dma_start(out=out[st * 128:(st + 1) * 128, :], in_=oq), r=[f"ob{q}"], w=["outd"])

        ob2 = [ob, yb[0]]
        bst2p = [bst2, tsb("bst2b", [128, 2, 6])]
        mv2p = [mv2, tsb("mv2b_", [128, 4])]
        comb_fetch(0)
        for st in range(NSTT):
            if st + 1 < NSTT:
                comb_fetch(st + 1)
            comb_compute(st)
        cnt_ = P.emit()
        print("phaseB op counts", cnt_)

    return nc


_NC_CACHE = {}


def _get_nc():
    if "nc" not in _NC_CACHE:
        _NC_CACHE["nc"] = build("full")
    return _NC_CACHE["nc"]


def make_in_maps(inputs):
    f = lambda a: np.ascontiguousarray(np.asarray(a, dtype=np.float32))
    shared = {
        "w_ada": f(inputs["w_ada"][0]), "b_ada": f(inputs["b_ada"]), "w_in": f(inputs["w_in"][0]),
        "conv_w": f(inputs["conv_w"][0]), "conv_norm_w": f(inputs["conv_norm_w"]),
        "dn_conv_w": f(inputs["dn_conv_w"][0]), "dn_A_log": f(inputs["dn_A_log"]),
        "dn_dt_bias": f(inputs["dn_dt_bias"]), "dn_norm_w": f(inputs["dn_norm_w"]),
        "w_out": f(inputs["w_out"][0]), "ln1_g": f(inputs["ln1_g"]), "ln1_b": f(inputs["ln1_b"]),
        "w_grp": f(inputs["w_grp"][0]), "b_grp": f(inputs["b_grp"]), "w_exp": f(inputs["w_exp"][0]),
        "b_exp": f(inputs["b_exp"]), "w_gate": f(inputs["w_gate"][0]), "w_up": f(inputs["w_up"][0]),
        "w_down": f(inputs["w_down"][0]), "ln2_g": f(inputs["ln2_g"]), "ln2_b": f(inputs["ln2_b"]),
        "cmat": CMAT, "mab": MAB, "cb": CB,
    }
    xs = f(inputs["x"]).reshape(8, TOK, D)
    cs = f(inputs["c"]).reshape(8, NB, D)
    return [dict(shared, x=xs[i], c=cs[i]) for i in range(8)]


def kernel(**inputs):
    nc = _get_nc()
    in_maps = make_in_maps(inputs)
    res = run_bass_kernel_spmd(nc, in_maps, core_ids=list(range(8)))
    outs = [np.asarray(r["out"], dtype=np.float32).reshape(NB, SEQ, D) for r in res.results]
    return np.concatenate(outs, axis=0)
```
